# Optimizing a Trainium2 kernel written in Bass

```python
import jax, jax.numpy as jnp
from jax import lax
import numpy as np

D_MODEL = 2048
BATCH = 4
SEQ = 4096
DEPTH = 1

GRID_W = 64
CTX_LEN = 256

POOL_WINDOWS = (2, 4, 8, 16)
POOL_GROUPS = 4
POOL_GROUP = 256
D_POOL = POOL_GROUPS * POOL_GROUP

HEAD_SIZE = 64
D_ATT = D_MODEL
N_HEADS = D_ATT // HEAD_SIZE
D_DECAY_LORA = 96
D_ICLR_LORA = 96
D_GATE_LORA = 256
GN_EPS = 64e-5
D_SHIFT = 3 * D_ATT + 2 * D_DECAY_LORA + 2 * D_ICLR_LORA + D_GATE_LORA
SHIFT_SPLITS = (D_ATT, 2 * D_ATT, 3 * D_ATT,
                3 * D_ATT + D_DECAY_LORA,
                3 * D_ATT + 2 * D_DECAY_LORA,
                3 * D_ATT + 2 * D_DECAY_LORA + D_ICLR_LORA,
                3 * D_ATT + 2 * D_DECAY_LORA + 2 * D_ICLR_LORA)

D_IN = D_POOL + D_SHIFT + 2 * D_MODEL
IN_SPLITS = (D_POOL, D_POOL + D_SHIFT)

N_EXPERTS = 64
TOP_K = 8
N_GROUPS = 8
TOPK_GROUPS = 4
D_EXPERT = 512
D_SHARED = 512
ROUTED_SCALE = 2.5
MOE_BLOCK = 128

NORM_EPS = 1e-6

kernel_name = "hybrid_pool_rwkv7_moe_prefix_dit_layer"


def rms_norm(x, g):
    xf = x.astype(jnp.float32)
    y = xf * lax.rsqrt(jnp.mean(xf * xf, axis=-1, keepdims=True) + NORM_EPS)
    return (y * g.astype(jnp.float32)).astype(x.dtype)


def grid_shift(u, rows):
    B, T, C = u.shape
    g = u.reshape(B, rows, GRID_W, C // 4, 4)
    left = jnp.pad(g[:, :, :-1, :, 0], ((0, 0), (0, 0), (1, 0), (0, 0)))
    right = jnp.pad(g[:, :, 1:, :, 1], ((0, 0), (0, 0), (0, 1), (0, 0)))
    up = jnp.pad(g[:, :-1, :, :, 2], ((0, 0), (1, 0), (0, 0), (0, 0)))
    down = jnp.pad(g[:, 1:, :, :, 3], ((0, 0), (0, 1), (0, 0), (0, 0)))
    return jnp.stack([left, right, up, down], axis=-1).reshape(B, T, C)


def seq_shift(u):
    B, T, C = u.shape
    g = u.reshape(B, T, C // 2, 2)
    prev = jnp.pad(g[:, :-1, :, 0], ((0, 0), (1, 0), (0, 0)))
    nxt = jnp.pad(g[:, 1:, :, 1], ((0, 0), (0, 1), (0, 0)))
    return jnp.stack([prev, nxt], axis=-1).reshape(B, T, C)


def multiscale_pool(u):
    L = u.shape[-2]
    uf = u.astype(jnp.float32)
    cs = jnp.cumsum(uf, axis=-2)
    cs = jnp.concatenate([jnp.zeros_like(cs[..., :1, :]), cs], axis=-2)
    t = jnp.arange(L)
    outs = []
    for gi, w in enumerate(POOL_WINDOWS):
        sl = slice(gi * POOL_GROUP, (gi + 1) * POOL_GROUP)
        cg = cs[..., sl]
        lo = jnp.clip(t - w // 2, 0, L)
        hi = jnp.clip(t + (w - w // 2), 0, L)
        win_sum = jnp.take(cg, hi, axis=-2) - jnp.take(cg, lo, axis=-2)
        mean = win_sum / (hi - lo).astype(jnp.float32)[:, None]
        outs.append(mean - uf[..., sl])
    return jnp.concatenate(outs, axis=-1).astype(u.dtype)


def pool_branch(u, pool_w, pool_scale, w_pool_out):
    d = multiscale_pool(u)
    d = d.reshape(d.shape[:-1] + (POOL_GROUPS, POOL_GROUP))
    y = jnp.einsum('...gc,gcd->...gd', d, pool_w)
    y = y.reshape(y.shape[:-2] + (D_POOL,)) * pool_scale
    return y @ w_pool_out


def rwkv_prepare(s, w0, w2, a0, a2, k_k, k_a):
    B, T, _ = s.shape
    f32 = jnp.float32
    r, k, v, wd_f, wd_b, ad_f, ad_b, gd = jnp.split(s, SHIFT_SPLITS, axis=-1)

    def heads(a):
        return a.astype(f32).reshape(B, T, N_HEADS, HEAD_SIZE)

    def decay(wd, w0_d, w2_d):
        w = -jax.nn.softplus(-(w0_d + jnp.tanh(wd) @ w2_d).astype(f32)) - 0.5
        return heads(jnp.exp(-jnp.exp(w)))

    def iclr(ad, a0_d, a2_d):
        return heads(jax.nn.sigmoid((a0_d + ad @ a2_d).astype(f32)))

    kh = heads(k)
    kk = kh * k_k.astype(f32).reshape(N_HEADS, HEAD_SIZE)
    kk = kk * lax.rsqrt(jnp.sum(kk * kk, axis=-1, keepdims=True) + 1e-12)
    ka = k_a.astype(f32).reshape(N_HEADS, HEAD_SIZE)
    a_f = iclr(ad_f, a0[0], a2[0])
    a_b = iclr(ad_b, a0[1], a2[1])
    k_f = kh * (1.0 + (a_f - 1.0) * ka)
    k_b = kh * (1.0 + (a_b - 1.0) * ka)
    return (heads(r), heads(v), kk,
            decay(wd_f, w0[0], w2[0]), decay(wd_b, w0[1], w2[1]),
            k_f, k_b, kk * a_f, kk * a_b, gd)


def wkv_scan(s0, r, w, k, v, kk, b, reverse, emit):
    def tm(a):
        return jnp.moveaxis(a, 1, 0)

    def step(S, inp):
        r_t, w_t, k_t, v_t, kk_t, b_t = inp
        S = (S * w_t[:, :, None, :]
             - jnp.einsum('bhij,bhj->bhi', S, kk_t)[..., None] * b_t[:, :, None, :]
             + v_t[..., None] * k_t[:, :, None, :])
        y = jnp.einsum('bhij,bhj->bhi', S, r_t) if emit else None
        return S, y

    S, ys = lax.scan(step, s0, (tm(r), tm(w), tm(k), tm(v), tm(kk), tm(b)), reverse=reverse)
    return S, (jnp.moveaxis(ys, 0, 1) if emit else None)


def rwkv_readout(y, r, v, k_f, k_b, gd, g2, r_k, lnx_w, lnx_b, w_rwkv_out):
    B, T = y.shape[:2]
    f32 = jnp.float32
    mean = jnp.mean(y, axis=-1, keepdims=True)
    var = jnp.mean(jnp.square(y - mean), axis=-1, keepdims=True)
    yn = ((y - mean) * lax.rsqrt(var + GN_EPS)).reshape(B, T, D_ATT)
    yn = yn * lnx_w.astype(f32) + lnx_b.astype(f32)
    bonus = jnp.sum(r * (k_f + k_b) * r_k.astype(f32), axis=-1, keepdims=True) * v
    g = (jax.nn.sigmoid(gd) @ g2).astype(f32)
    out = (yn + bonus.reshape(B, T, D_ATT)) * g
    return out.astype(gd.dtype) @ w_rwkv_out


def merge_branches(y_pool, y_rwkv, gates, w_out_l):
    g_pool, g_rwkv = jnp.split(gates, 2, axis=-1)
    return (jax.nn.sigmoid(g_pool) * y_pool + jax.nn.sigmoid(g_rwkv) * y_rwkv) @ w_out_l


def moe_ffn(h, router_w, router_bias, w_gate, w_up, w_down, sh_gate, sh_up, sh_down):
    shp = h.shape
    f32 = jnp.float32
    ht = h.reshape(-1, D_MODEL)
    n = ht.shape[0]
    scores = jax.nn.sigmoid((ht @ router_w).astype(f32))
    sel = scores + router_bias.astype(f32)
    grp = sel.reshape(n, N_GROUPS, N_EXPERTS // N_GROUPS)
    grp_score = jnp.sum(lax.top_k(grp, 2)[0], axis=-1)
    _, gidx = lax.top_k(grp_score, TOPK_GROUPS)
    gmask = jnp.sum(jax.nn.one_hot(gidx, N_GROUPS, dtype=f32), axis=-2)
    emask = jnp.repeat(gmask, N_EXPERTS // N_GROUPS, axis=-1)
    _, eidx = lax.top_k(jnp.where(emask > 0, sel, -jnp.inf), TOP_K)
    wsel = jnp.take_along_axis(scores, eidx, axis=-1)
    wsel = wsel / jnp.sum(wsel, axis=-1, keepdims=True) * ROUTED_SCALE

    nk = n * TOP_K
    cap = nk + N_EXPERTS * MOE_BLOCK
    n_blocks = cap // MOE_BLOCK
    e_flat = eidx.reshape(-1)
    tok_flat = jnp.repeat(jnp.arange(n, dtype=jnp.int32), TOP_K)
    oh = jax.nn.one_hot(e_flat, N_EXPERTS, dtype=jnp.int32)
    csum = jnp.cumsum(oh, axis=0)
    rank = jnp.take_along_axis(csum, e_flat[:, None], axis=-1)[:, 0] - 1
    counts = csum[-1]
    padded = (counts + MOE_BLOCK - 1) // MOE_BLOCK * MOE_BLOCK
    pstarts = jnp.cumsum(padded) - padded
    dest = pstarts[e_flat] + rank
    tok_buf = jnp.zeros((cap,), jnp.int32).at[dest].set(tok_flat)
    w_buf = jnp.zeros((cap,), f32).at[dest].set(wsel.reshape(-1))
    block_start = jnp.arange(n_blocks, dtype=jnp.int32) * MOE_BLOCK
    block_expert = jnp.minimum(
        jnp.sum(block_start[:, None] >= (pstarts + padded)[None, :], axis=-1), N_EXPERTS - 1)

    def expert_block(args):
        tok, wgt, e = args
        hb = ht[tok]
        act = jax.nn.silu(hb @ w_gate[e]) * (hb @ w_up[e])
        return (act @ w_down[e]) * wgt[:, None].astype(hb.dtype)

    y = lax.map(expert_block, (tok_buf.reshape(n_blocks, MOE_BLOCK),
                               w_buf.reshape(n_blocks, MOE_BLOCK), block_expert))
    routed = jnp.zeros_like(ht).at[tok_buf].add(y.reshape(cap, D_MODEL))
    shared = (jax.nn.silu(ht @ sh_gate) * (ht @ sh_up)) @ sh_down
    return (routed + shared).reshape(shp)


def setup_inputs(seed: int = 0) -> dict:
    key = jax.random.key(seed)
    keys = jax.random.split(key, 40)
    counter = iter(range(40))

    def nrm(shape, scale):
        return jax.random.normal(keys[next(counter)], shape, jnp.float32) * scale

    def uni(shape, lo, hi):
        return jax.random.uniform(keys[next(counter)], shape, jnp.float32, lo, hi)

    L, D = DEPTH, D_MODEL
    return {
        "x": nrm((BATCH, SEQ, D), 1.0),
        "c": nrm((BATCH, D), 1.0),
        "ctx": nrm((BATCH, CTX_LEN, D), 1.0),
        "c_ctx": nrm((D,), 1.0),
        "norm1_g": 1.0 + nrm((L, D), 0.05),
        "norm2_g": 1.0 + nrm((L, D), 0.05),
        "ada_w": nrm((L, D, 6 * D), 0.5 * D ** -0.5),
        "ada_b": nrm((L, 6 * D), 0.02),
        "w_in": nrm((L, D, D_IN), D ** -0.5),
        "shift_mu": uni((L, D_SHIFT), 0.0, 1.0),
        "pool_w": nrm((L, POOL_GROUPS, POOL_GROUP, POOL_GROUP), POOL_GROUP ** -0.5),
        "pool_scale": 1.0 + nrm((L, D_POOL), 0.1),
        "w_pool_out": nrm((L, D_POOL, D), D_POOL ** -0.5),
        "decay_w0": uni((L, 2, D_ATT), -6.0, 0.0),
        "decay_w2": nrm((L, 2, D_DECAY_LORA, D_ATT), 0.5 * D_DECAY_LORA ** -0.5),
        "iclr_a0": nrm((L, 2, D_ATT), 0.5),
        "iclr_a2": nrm((L, 2, D_ICLR_LORA, D_ATT), 0.5 * D_ICLR_LORA ** -0.5),
        "gate_g2": nrm((L, D_GATE_LORA, D_ATT), D_GATE_LORA ** -0.5),
        "k_k": 0.85 + nrm((L, D_ATT), 0.1),
        "k_a": 1.0 + nrm((L, D_ATT), 0.1),
        "r_k": nrm((L, N_HEADS, HEAD_SIZE), 0.1),
        "lnx_w": 1.0 + nrm((L, D_ATT), 0.05),
        "lnx_b": nrm((L, D_ATT), 0.02),
        "w_rwkv_out": nrm((L, D_ATT, D), D_ATT ** -0.5),
        "w_out": nrm((L, D, D), D ** -0.5),
        "router_w": nrm((L, D, N_EXPERTS), D ** -0.5),
        "router_bias": nrm((L, N_EXPERTS), 0.01),
        "exp_w_gate": nrm((L, N_EXPERTS, D, D_EXPERT), D ** -0.5),
        "exp_w_up": nrm((L, N_EXPERTS, D, D_EXPERT), D ** -0.5),
        "exp_w_down": nrm((L, N_EXPERTS, D_EXPERT, D), D_EXPERT ** -0.5),
        "shared_w_gate": nrm((L, D, D_SHARED), D ** -0.5),
        "shared_w_up": nrm((L, D, D_SHARED), D ** -0.5),
        "shared_w_down": nrm((L, D_SHARED, D), D_SHARED ** -0.5),
        "final_g": 1.0 + nrm((D,), 0.05),
    }


def reference(x, c, ctx, c_ctx, norm1_g, norm2_g, ada_w, ada_b, w_in, shift_mu, pool_w,
              pool_scale, w_pool_out, decay_w0, decay_w2, iclr_a0, iclr_a2, gate_g2, k_k, k_a,
              r_k, lnx_w, lnx_b, w_rwkv_out, w_out, router_w, router_bias, exp_w_gate, exp_w_up,
              exp_w_down, shared_w_gate, shared_w_up, shared_w_down, final_g):
    B, T, _ = x.shape
    rows = T // GRID_W
    s0 = jnp.zeros((B, N_HEADS, HEAD_SIZE, HEAD_SIZE), jnp.float32)
    xc = ctx
    for l in range(DEPTH):
        last = l == DEPTH - 1
        rw = (decay_w0[l], decay_w2[l], iclr_a0[l], iclr_a2[l], k_k[l], k_a[l])
        ro = (gate_g2[l], r_k[l], lnx_w[l], lnx_b[l], w_rwkv_out[l])
        po = (pool_w[l], pool_scale[l], w_pool_out[l])
        moe = (router_w[l], router_bias[l], exp_w_gate[l], exp_w_up[l], exp_w_down[l],
               shared_w_gate[l], shared_w_up[l], shared_w_down[l])

        mod = jax.nn.silu(c) @ ada_w[l] + ada_b[l]
        sh1, sc1, gt1, sh2, sc2, gt2 = [m[:, None, :] for m in jnp.split(mod, 6, axis=-1)]
        modc = jax.nn.silu(c_ctx) @ ada_w[l] + ada_b[l]
        sh1c, sc1c, gt1c, sh2c, sc2c, gt2c = jnp.split(modc, 6, axis=-1)

        hc = rms_norm(xc, norm1_g[l]) * (1.0 + sc1c) + sh1c
        if last:
            slab_c = hc @ w_in[l][:, D_POOL:D_POOL + D_SHIFT]
        else:
            uc, slab_c, gates_c = jnp.split(hc @ w_in[l], IN_SPLITS, axis=-1)
        slab_c = slab_c + (seq_shift(slab_c) - slab_c) * shift_mu[l]
        rc, vc, kkc, wfc, wbc, kfc, kbc, bfc, bbc, gdc = rwkv_prepare(slab_c, *rw)
        st_f, yc_f = wkv_scan(s0, rc, wfc, kfc, vc, kkc, bfc, False, not last)
        st_b, yc_b = wkv_scan(s0, rc, wbc, kbc, vc, kkc, bbc, True, not last)

        h = rms_norm(x, norm1_g[l]) * (1.0 + sc1) + sh1
        u, slab, gates = jnp.split(h @ w_in[l], IN_SPLITS, axis=-1)
        slab = slab + (grid_shift(slab, rows) - slab) * shift_mu[l]
        r, v, kk, wf, wb, kf, kb, bf, bb, gd = rwkv_prepare(slab, *rw)
        _, y_f = wkv_scan(st_f, r, wf, kf, v, kk, bf, False, True)
        _, y_b = wkv_scan(st_b, r, wb, kb, v, kk, bb, True, True)
        y_rwkv = rwkv_readout(y_f + y_b, r, v, kf, kb, gd, *ro)
        y_pool = pool_branch(u.reshape(B, rows, GRID_W, D_POOL), *po).reshape(B, T, D_MODEL)
        x = x + gt1 * merge_branches(y_pool, y_rwkv, gates, w_out[l])

        x = x + gt2 * moe_ffn(rms_norm(x, norm2_g[l]) * (1.0 + sc2) + sh2, *moe)

        if not last:
            yc_rwkv = rwkv_readout(yc_f + yc_b, rc, vc, kfc, kbc, gdc, *ro)
            yc_pool = pool_branch(uc, *po)
            xc = xc + gt1c * merge_branches(yc_pool, yc_rwkv, gates_c, w_out[l])
            xc = xc + gt2c * moe_ffn(rms_norm(xc, norm2_g[l]) * (1.0 + sc2c) + sh2c, *moe)
    return rms_norm(x, final_g)
```

```python
import numpy as np
import concourse.bass as bass
import concourse.mybir as mybir
from contextlib import ExitStack

F32 = mybir.dt.float32
BF16 = mybir.dt.bfloat16
I32 = mybir.dt.int32
U32 = mybir.dt.uint32
ALU = mybir.AluOpType
AF = mybir.ActivationFunctionType
AX = mybir.AxisListType

ENGS = ("pe", "act", "dve", "pool", "sp")
ARENA_BASE = 16640
ARENA_TOP = 229344
DTSIZE = {F32: 4, BF16: 2, I32: 4, U32: 4}
NDSEM = 6


class _Op:
    __slots__ = ("eng", "fn", "deps", "signal", "sigval", "is_dma", "dsem", "dval", "dprev", "idx")

    def __init__(self, eng, fn, is_dma):
        self.eng = eng
        self.fn = fn
        self.deps = []
        self.signal = False
        self.sigval = 0
        self.is_dma = is_dma
        self.dsem = None
        self.dval = 0
        self.dprev = None


class Prog:
    def __init__(self, nc):
        self.nc = nc
        self.ops = []
        self.track = {}
        self.ndma = {e: 0 for e in ENGS}
        self.dma_ops = {e: [] for e in ENGS}
        self.es = ExitStack()
        self.out_dmas = []
        self.sp = ARENA_BASE
        self.sp_max = ARENA_BASE

    def sb(self, name, shape, dtype):
        nbytes = int(np.prod(shape[1:])) * DTSIZE[dtype]
        nbytes = (nbytes + 31) // 32 * 32
        off = self.sp
        self.sp += nbytes
        self.sp_max = max(self.sp_max, self.sp)
        assert self.sp <= ARENA_TOP, "SBUF arena overflow: %s needs %d at %d" % (name, nbytes, off)
        return self.nc.alloc_sbuf_tensor_at(name, list(shape), dtype, offset=off)

    def getreg(self, e, val):
        if not hasattr(self, "_regs"):
            self._regs = {}
        key = (id(e), val)
        if key not in self._regs:
            r = e.alloc_register("creg%d" % len(self._regs))
            e.reg_mov(r, val)
            self._regs[key] = r
        return self._regs[key]

    def mark(self):
        return self.sp

    def release(self, m):
        self.sp = m

    def ps(self, name, shape, dtype=F32):
        return self.es.enter_context(self.nc.psum_tensor(name, list(shape), dtype))

    @staticmethod
    def _key(k):
        if isinstance(k, tuple):
            return k[0], k[1]
        if isinstance(k, str):
            return k, None
        t = getattr(k, "tensor", k)
        return t.name, None

    def _conf(self, name, sub):
        d = self.track.setdefault(name, {})
        if sub is None:
            return list(d.keys())
        return [s for s in (sub, None) if s in d]

    def _record(self, op, reads, writes):
        r2, w2 = [], list(writes)
        for k_ in reads:
            nm = self._key(k_)[0]
            if nm.startswith("psb") or nm.startswith("pbt"):
                w2.append(k_)
            else:
                r2.append(k_)
        reads, writes = r2, w2
        deps = set()
        for k in reads:
            name, sub = self._key(k)
            d = self.track.setdefault(name, {})
            for s in self._conf(name, sub):
                w = d[s][0]
                if w is not None:
                    deps.add(w)
        for k in writes:
            name, sub = self._key(k)
            d = self.track.setdefault(name, {})
            for s in self._conf(name, sub):
                w, rs = d[s]
                if w is not None:
                    deps.add(w)
                for r in rs:
                    deps.add(r)
        for k in reads:
            name, sub = self._key(k)
            d = self.track[name]
            d.setdefault(sub, [None, []])[1].append(op)
        for k in writes:
            name, sub = self._key(k)
            d = self.track[name]
            if sub is None:
                d.clear()
            d[sub] = [op, []]
        deps.discard(op)
        for y in deps:
            if y.eng == "pe" and op.eng == "pe" and not y.is_dma:
                continue
            op.deps.append(y)
            if not y.is_dma:
                y.signal = True

    def op(self, eng, fn, reads=(), writes=(), after=()):
        o = _Op(eng, fn, False)
        o.idx = len(self.ops)
        self.ops.append(o)
        self._record(o, reads, writes)
        for y in after:
            o.deps.append(y)
            if not y.is_dma:
                y.signal = True
        return o

    def dma(self, eng, out, in_, reads=None, writes=None, is_out=False, **kw):
        if reads is None:
            reads = [in_]
        if writes is None:
            writes = [out]

        def fn(e, out=out, in_=in_, kw=kw):
            return e.dma_start(out=out, in_=in_, **kw)
        o = _Op(eng, fn, True)
        o.idx = len(self.ops)
        self.ops.append(o)
        self._record(o, reads, writes)
        n = self.ndma[eng]
        self.ndma[eng] += 1
        o.dsem = (eng, n % NDSEM)
        o.dval = 16 * (n // NDSEM + 1)
        if n >= NDSEM:
            o.dprev = self.dma_ops[eng][n - NDSEM]
        self.dma_ops[eng].append(o)
        if is_out:
            self.out_dmas.append(o)
        return o

    def dma_fn(self, eng, fn, reads, writes, is_out=False):
        o = _Op(eng, fn, True)
        o.idx = len(self.ops)
        self.ops.append(o)
        self._record(o, reads, writes)
        n = self.ndma[eng]
        self.ndma[eng] += 1
        o.dsem = (eng, n % NDSEM)
        o.dval = 16 * (n // NDSEM + 1)
        if n >= NDSEM:
            o.dprev = self.dma_ops[eng][n - NDSEM]
        self.dma_ops[eng].append(o)
        if is_out:
            self.out_dmas.append(o)
        return o

    def barrier(self):
        last = {}
        for o in self.ops:
            if not o.is_dma:
                last[o.eng] = o
        pend = [o for e in ENGS for o in self.dma_ops[e][-NDSEM:]]
        bops = []
        for e in ENGS:
            after = [o for o in last.values()] + pend
            bops.append((e, after))
        res = []
        for e, after in bops:
            res.append(self.op(e, lambda eng: eng.nop(), after=after))
        for o in res:
            o.signal = True
        self._barrier_ops = res
        self.track.clear()
        return res

    def emit(self):
        nc = self.nc
        if self.out_dmas:
            self.op("sp", lambda eng: eng.nop(), after=list(self.out_dmas))
        cnt = {e: 0 for e in ENGS}
        for o in self.ops:
            if o.is_dma:
                continue
            if o.signal:
                cnt[o.eng] += 1
                o.sigval = cnt[o.eng]
        es = self.es
        csem = {e: es.enter_context(nc.semaphore("c_" + e)) for e in ENGS}
        dsem = {}
        for e in ENGS:
            if self.ndma[e]:
                for i in range(min(NDSEM, self.ndma[e])):
                    dsem[(e, i)] = es.enter_context(nc.semaphore("d_%s%d" % (e, i)))
        per = {e: [o for o in self.ops if o.eng == e] for e in ENGS}
        block = es.enter_context(nc.Block())

        def run(eng_name, eng):
            waited = {}

            def wait(sem_key, sem, val):
                if waited.get(sem_key, 0) >= val:
                    return
                waited[sem_key] = val
                eng.wait_ge(sem, val)

            for o in per[eng_name]:
                for y in o.deps:
                    if y.is_dma:
                        wait(y.dsem, dsem[y.dsem], y.dval)
                    else:
                        wait(("c", y.eng), csem[y.eng], y.sigval)
                if o.is_dma:
                    if o.dprev is not None:
                        wait(o.dsem, dsem[o.dsem], o.dprev.dval)
                    ins = o.fn(eng)
                    ins.then_inc(dsem[o.dsem], 16)
                else:
                    ins = o.fn(eng)
                    if o.signal:
                        ins.then_inc(csem[eng_name], 1)

        @block.tensor
        def _(e):
            run("pe", e)

        @block.scalar
        def _(e):
            run("act", e)

        @block.vector
        def _(e):
            run("dve", e)

        @block.gpsimd
        def _(e):
            run("pool", e)

        @block.sync
        def _(e):
            run("sp", e)

    def close(self):
        self.es.close()


import numpy as np

D = 2048
NT = 4096
NOWN = 2048
NCTX = 256
KC = 16
NCH = 94
SLAB0 = 8
NSL = 54
DINP = NCH * 128
EPS = 1e-6
SQD = float(np.sqrt(2048.0))


class K:
    pass


def build(stage=99, dbg=(), ntiles=None, cut=None, skipA=False, nchunks=None, ccut=None, ecut=None, nexp=None):
    nc = bass.Bass("TRN2", target_bir_lowering=False)
    p = Prog(nc)
    k = K()
    k.nc, k.p = nc, p
    k.dbg = {}
    k.ntiles = ntiles
    k.cut = cut
    k.nchunks = nchunks
    k.ccut = ccut
    k.ecut = ecut
    k.nexp = nexp

    def din(name, shape, dt=F32):
        return nc.dram_tensor(name, list(shape), dt, kind="ExternalInput").ap()

    def dscr(name, shape, dt):
        if skipA and name in ("slabL", "slabC", "uT", "sgT"):
            return nc.dram_tensor(name, list(shape), dt, kind="ExternalInput").ap()
        if name in dbg:
            a = nc.dram_tensor(name, list(shape), dt, kind="ExternalOutput").ap()
            k.dbg[name] = a
            return a
        return nc.dram_tensor(name, list(shape), dt).ap()

    k.din, k.dscr = din, dscr
    if not skipA:
        k.xT = din("xT", [D, NT])
        k.cxT = din("cxT", [D, NCTX])
        k.cT2 = din("cT2", [128, KC, 2])
        k.adaw = din("adaw", [D, 6 * D])
        k.adab = din("adab", [128, 96])
        k.g1 = din("g1", [128, KC])
        k.win = din("win", [D, DINP])
    k.slabL = dscr("slabL", [NSL * 128, NT], BF16)
    k.slabC = dscr("slabC", [NSL * 128, NCTX], BF16)
    k.uT = dscr("uT", [1024, NOWN], BF16)
    k.sgT = dscr("sgT", [4096, NOWN], BF16)
    k.modd = dscr("modd", [128, 96, 2], F32)

    k.ones_bf = p.sb("ones_bf", [128, 128], BF16)
    p.op("pool", lambda e: e.memset(k.ones_bf[:], 1.0), writes=[k.ones_bf])
    k.eps_t = p.sb("eps_t", [128, 2], F32)
    p.op("pool", lambda e: e.memset(k.eps_t[:, 0:1], EPS), writes=[k.eps_t])
    p.op("pool", lambda e: e.memset(k.eps_t[:, 1:2], 1e-12), writes=[k.eps_t])
    k.mod = p.sb("mod", [128, 96, 2], F32)
    k.A1 = p.sb("A1", [128, KC, 2], F32)
    k.g1s = p.sb("g1s", [128, KC], F32)
    k.ident = p.sb("ident_bf", [128, 128], BF16)
    k.psb = [p.ps("psb%d" % i, [128, 512], F32) for i in range(6)]
    k.pbt = [p.ps("pbt%d" % i, [128, 1024], BF16) for i in range(2)]

    m0 = p.mark()
    if skipA:
        modin = din("modin", [128, 96, 2])
        p.dma("sp", k.mod[:], modin)
    if not skipA:
        phase0(k)
        p.barrier()
        p.release(m0)
        if stage >= 1:
            phaseA(k)
            p.release(m0)
    mB = p.mark()
    if stage >= 2:
        phaseB(k)
    if stage >= 3:
        mC = p.mark()
        phaseC(k)
        p.release(mC)
    if stage >= 4:
        phaseD(k)
    if stage >= 5:
        phaseE(k)
    p.emit()
    p.close()
    return nc, k


def phase0(k):
    p, nc = k.p, k.nc
    c32 = p.sb("c32", [128, KC, 2], F32)
    cs = p.sb("c_silu", [128, KC, 2], BF16)
    adab = p.sb("adab_sb", [128, 96], F32)
    p.dma("sp", c32[:], k.cT2)
    p.dma("sp", adab[:], k.adab)
    p.dma("sp", k.g1s[:], k.g1)
    p.op("act", lambda e: e.activation(out=cs[:], in_=c32[:], func=AF.Silu), reads=[c32], writes=[cs])
    NB = 16
    CW = 768
    wb = [p.sb("adaw_bf%d" % i, [128, KC, CW], BF16) for i in range(2)]
    src = k.adaw.rearrange("(kc p) n -> p kc n", p=128)
    ps = k.psb[0]
    for blk in range(NB):
        w = wb[blk % 2]
        for h in range(2):
            p.dma("pool", w[:, h * 8:(h + 1) * 8, :], src[:, h * 8:(h + 1) * 8, blk * CW:(blk + 1) * CW],
                  reads=[], writes=[(w.name, h)])
        for oc in range(6):
            g = blk * 6 + oc
            for kc in range(KC):
                p.op("pe", lambda e, w=w, oc=oc, kc=kc, g=g: e.matmul(
                    ps[:, 2 * g:2 * g + 2], lhsT=w[:, kc, oc * 128:(oc + 1) * 128], rhs=cs[:, kc, :],
                    start=(kc == 0), stop=(kc == KC - 1)),
                    reads=[(w.name, kc // 8), cs], writes=[ps])
    p.op("dve", lambda e: e.tensor_tensor(
        out=k.mod[:], in0=ps[:, 0:192].rearrange("p (g t) -> p g t", t=2),
        in1=adab[:].unsqueeze(2).to_broadcast([128, 96, 2]), op=ALU.add),
        reads=[ps, adab], writes=[k.mod])
    p.op("dve", lambda e: e.tensor_scalar(out=k.A1[:], in0=k.mod[:, 16:32, :], scalar1=1.0, scalar2=None,
                                          op0=ALU.add), reads=[k.mod], writes=[k.A1])
    p.op("dve", lambda e: e.tensor_tensor(out=k.A1[:], in0=k.A1[:],
                                          in1=k.g1s[:].unsqueeze(2).to_broadcast([128, KC, 2]), op=ALU.mult),
         reads=[k.A1, k.g1s], writes=[k.A1])
    if "modd" in k.dbg:
        p.dma("sp", k.modd, k.mod[:], is_out=True)


def phaseA(k):
    p, nc = k.p, k.nc
    hT = p.sb("hT", [128, KC, NOWN], BF16)
    xin = [p.sb("xin%d" % i, [128, KC, 256], F32) for i in range(2)]
    sq = p.sb("sq", [128, KC, 256], BF16)
    rstd = p.sb("rstd", [128, 256], F32)
    rms = p.sb("rms", [128, 256], F32)
    xn = p.sb("xn", [128, KC, 256], F32)
    wblk = [p.sb("wblk%d" % i, [128, KC, 256], BF16) for i in range(3)]
    stg = [p.sb("stgA%d" % i, [128, 512], BF16) for i in range(4)]
    winv = k.win.rearrange("(kc p) n -> p kc n", p=128)
    st = {"x": 0, "w": 0, "s": 0, "ps": 0}

    def norm_group(srcT, t0, ntok, which):
        v = srcT.rearrange("(kc p) t -> p kc t", p=128)
        for s in range(ntok // 256):
            xb = xin[st["x"] % 2]
            st["x"] += 1
            for h in range(2):
                p.dma("sp", xb[:, h * 8:(h + 1) * 8, :], v[:, h * 8:(h + 1) * 8, t0 + s * 256:t0 + (s + 1) * 256],
                      reads=[], writes=[(xb.name, h)])
            p.op("act", lambda e, xb=xb: e.activation(out=sq[:], in_=xb[:], func=AF.Square), reads=[xb], writes=[sq])
            ps = k.psb[5]
            for kc in range(KC):
                p.op("pe", lambda e, kc=kc: e.matmul(ps[:, 0:256], lhsT=k.ones_bf[:], rhs=sq[:, kc, :],
                                                      start=(kc == 0), stop=(kc == KC - 1)),
                     reads=[k.ones_bf, sq], writes=[ps])
            p.op("act", lambda e: e.activation(out=rms[:], in_=ps[:, 0:256], func=AF.Sqrt, scale=1.0 / D, bias=k.eps_t[:, 0:1]),
                 reads=[ps, k.eps_t], writes=[rms])
            p.op("dve", lambda e: e.reciprocal(out=rstd[:], in_=rms[:]), reads=[rms], writes=[rstd])
            p.op("dve", lambda e, xb=xb: e.tensor_tensor(out=xn[:], in0=xb[:],
                                                         in1=rstd[:].unsqueeze(1).to_broadcast([128, KC, 256]), op=ALU.mult),
                 reads=[xb, rstd], writes=[xn])
            for kc in range(KC):
                p.op("act", lambda e, kc=kc, s=s: e.activation(
                    out=hT[:, kc, s * 256:(s + 1) * 256], in_=xn[:, kc, :], func=AF.Identity,
                    scale=k.A1[:, kc, which:which + 1], bias=k.mod[:, kc, which:which + 1]),
                    reads=[xn, k.A1, k.mod], writes=[(hT.name, s)])

    def proj_group(ntok, chunks, sink):
        nsub = max(1, ntok // 512)
        w = min(512, ntok)
        for b0 in range(0, len(chunks), 2):
            wb = wblk[st["w"] % 3]
            st["w"] += 1
            c0 = chunks[b0]
            for h in range(2):
                p.dma("pool", wb[:, h * 8:(h + 1) * 8, :], winv[:, h * 8:(h + 1) * 8, c0 * 128:(c0 + 2) * 128],
                      reads=[], writes=[(wb.name, h)])
            for s in range(nsub):
                for j in range(2):
                    ch = chunks[b0 + j]
                    ps = k.psb[st["ps"] % 5]
                    st["ps"] += 1
                    for kc in range(KC):
                        p.op("pe", lambda e, wb=wb, j=j, kc=kc, s=s, ps=ps: e.matmul(
                            ps[:, 0:w], lhsT=wb[:, kc, j * 128:(j + 1) * 128], rhs=hT[:, kc, s * 512:s * 512 + w],
                            start=(kc == 0), stop=(kc == KC - 1)),
                            reads=[(wb.name, kc // 8), (hT.name, (s * 512) // 256), (hT.name, (s * 512 + w - 1) // 256)],
                            writes=[ps])
                    sink(ch, s, w, ps)

    def mk_sink(slab_dst, tok0):
        def sink(ch, s, w, ps):
            sg = stg[st["s"] % 4]
            st["s"] += 1
            eng = "act" if (st["s"] % 2) else "dve"
            if ch >= 62:
                p.op("act", lambda e, sg=sg, ps=ps: e.activation(out=sg[:, 0:w], in_=ps[:, 0:w], func=AF.Sigmoid),
                     reads=[ps], writes=[sg])
                dst = k.sgT[(ch - 62) * 128:(ch - 61) * 128, s * 512:s * 512 + w]
            else:
                if eng == "act":
                    p.op("act", lambda e, sg=sg, ps=ps: e.activation(out=sg[:, 0:w], in_=ps[:, 0:w], func=AF.Copy),
                         reads=[ps], writes=[sg])
                else:
                    p.op("dve", lambda e, sg=sg, ps=ps: e.tensor_copy(out=sg[:, 0:w], in_=ps[:, 0:w]),
                         reads=[ps], writes=[sg])
                if ch < 8:
                    dst = k.uT[ch * 128:(ch + 1) * 128, s * 512:s * 512 + w]
                else:
                    cc = ch - SLAB0
                    dst = slab_dst[cc * 128:(cc + 1) * 128, tok0 + s * 512:tok0 + s * 512 + w]
            p.dma("sp", dst, sg[:, 0:w], reads=[sg], writes=[])
        return sink

    slab_chunks = list(range(SLAB0, SLAB0 + NSL))
    norm_group(k.xT, 0, NOWN, 0)
    proj_group(NOWN, list(range(NCH)), mk_sink(k.slabL, 0))
    norm_group(k.xT, NOWN, NOWN, 0)
    proj_group(NOWN, slab_chunks, mk_sink(k.slabL, NOWN))
    norm_group(k.cxT, 0, NCTX, 1)
    proj_group(NCTX, slab_chunks, mk_sink(k.slabC, 0))
    p.barrier()
    for name in ("slabL", "slabC", "uT", "sgT"):
        if name in k.dbg:
            pass


import numpy as np

C0 = float(np.exp(-0.5))
NCHK = 68
TT = 256


class Cut(Exception):
    pass


def phaseB(k):
    try:
        _phaseB(k)
    except Cut:
        k.p.barrier()


def _phaseB(k):
    def cut(n):
        if getattr(k, "cut", None) == n:
            raise Cut()
    p, nc = k.p, k.nc
    din, dscr = k.din, k.dscr
    k.mixc_d = din("mixc", [128, 7, 54])
    k.vecs_d = din("vecs", [128, 7, 16])
    k.lw2_d = din("lw2", [96, 4, 2048])
    k.bones_d = din("bones", [128, 128])
    k.hsel_d = din("hsel", [128, 2])
    k.Q = [dscr("QA", [2048, NCHK * 256], BF16), dscr("QB", [2048, NCHK * 256], BF16)]
    k.Vtok = dscr("Vtok", [NCHK * 64, 2048], BF16)
    k.KpT = [dscr("KpTA", [NCHK * 64, 2048], BF16), dscr("KpTB", [NCHK * 64, 2048], BF16)]
    k.BpT = [dscr("BpTA", [NCHK * 64, 2048], BF16), dscr("BpTB", [NCHK * 64, 2048], BF16)]
    k.bcoef = dscr("bcoef", [2048, 32], F32)
    k.sgdT = dscr("sgdT", [256, 2048], BF16)
    k.wtot = [p.sb("wtotA", [128, 16, NCHK], F32), p.sb("wtotB", [128, 16, NCHK], F32)]

    markB = p.mark()
    mixc = p.sb("mixc_sb", [128, 7, 54], F32)
    omu = p.sb("omu", [128, 54], F32)
    vecs = p.sb("vecs_sb", [128, 7, 16], F32)
    oka = p.sb("oka", [128, 16], F32)
    lw2 = p.sb("lw2_sb", [96, 4, 2048], BF16)
    bones = p.sb("bones_sb", [128, 128], BF16)
    hsel = p.sb("hsel_sb", [128, 2], BF16)
    rmask = p.sb("rmask", [128, TT], F32)
    ident = k.ident
    p.dma("sp", mixc[:], k.mixc_d)
    p.dma("sp", vecs[:], k.vecs_d)
    for i in range(4):
        p.dma("pool", lw2[:, i, :], k.lw2_d[:, i, :], writes=[(lw2.name, i)])
    p.dma("pool", bones[:], k.bones_d)
    p.dma("pool", hsel[:], k.hsel_d)
    p.op("dve", lambda e: e.tensor_scalar(out=omu[:], in0=mixc[:, 0, :], scalar1=-1.0, scalar2=1.0, op0=ALU.mult, op1=ALU.add),
         reads=[mixc], writes=[omu])
    p.op("dve", lambda e: e.tensor_scalar(out=oka[:], in0=vecs[:, 5, :], scalar1=-1.0, scalar2=1.0, op0=ALU.mult, op1=ALU.add),
         reads=[vecs], writes=[oka])
    p.op("dve", lambda e: e.memset(rmask[:], 1.0), writes=[rmask])
    p.op("dve", lambda e: e.memset(rmask[:].rearrange("p (r c) -> p r c", c=64)[:, :, 0:1], 0.0), reads=[rmask], writes=[rmask])
    p.op("pool", lambda e: e.memset(ident[:], 1.0), writes=[ident])
    p.op("pool", lambda e: e.affine_select(out=ident[:], in_=ident[:], pattern=[[-1, 128]], compare_op=ALU.is_equal,
                                           fill=0.0, base=0, channel_multiplier=1), reads=[ident], writes=[ident])

    cut(1)
    NB = 2
    raw3 = [p.sb("raw3_%d" % i, [128, 3, 384], BF16) for i in range(NB)]
    rawl = [p.sb("rawl_%d" % i, [128, 384], BF16) for i in range(NB)]
    colsv = p.sb("colsv", [128, 4], BF16)
    M3 = [p.sb("M3_%d" % i, [128, 3, TT], F32) for i in range(NB)]
    ML = p.sb("ML", [128, TT], F32)
    tw = p.sb("tw", [128, 4, TT], BF16)
    sgd = p.sb("sgd", [128, 2, TT], BF16)
    f = {}
    for nm in ("lw", "aa", "cL", "X2", "X3", "X4", "e1", "e2", "e3", "e4", "kd", "bd", "ck"):
        f[nm] = [p.sb("f_%s%d" % (nm, i), [128, TT], F32) for i in range(2)]
    kkr = p.sb("kkr", [128, TT], F32)
    sqk = p.sb("sqk", [128, TT], BF16)
    rn = p.sb("rn", [128, TT], F32)
    kk = p.sb("kk", [128, TT], F32)
    vbf = p.sb("vbf", [128, TT], BF16)
    kpb = [p.sb("kpb%d" % i, [128, TT], BF16) for i in range(2)]
    bpb = [p.sb("bpb%d" % i, [128, TT], BF16) for i in range(2)]
    ksum = p.sb("ksum", [128, TT], F32)
    prod = p.sb("prodb", [128, TT], BF16)
    Qst = [[p.sb("Qst%d_%d" % (d, i), [128, 4, 4, 64], BF16) for i in range(2)] for d in range(2)]
    TM = {nm: p.sb("TM_" + nm, [128, 2, 2048], BF16) for nm in ("v", "kA", "bA", "kB", "bB")}
    bco = p.sb("bco", [128, 2, 32], F32)
    psL = [[k.psb[0], k.psb[1]], [k.psb[0], k.psb[1]]]
    psK = k.psb[2]
    psT = [(k.pbt[0], k.pbt[1]), (k.pbt[0], k.pbt[1])]
    psBon = k.psb[3]
    cnt = {"e": 0}

    def ew():
        cnt["e"] += 1
        return "pool" if cnt["e"] % 2 == 0 else "dve"

    def mix(eng, out, buf, cc, latent, key, okey):
        if latent:
            ctr = buf[:, 64:320]
            views = [(buf[:, 0:256], 3), (buf[:, 128:384], 4)]
        else:
            ctr = buf[:, 1:257]
            views = [(buf[:, 0:256], 5), (buf[:, 2:258], 6)]
        p.op(eng, lambda e: e.tensor_scalar(out=out, in0=ctr, scalar1=omu[:, cc:cc + 1], scalar2=None, op0=ALU.mult),
             reads=[key, omu], writes=[okey])
        for v, ci in views:
            p.op(eng, lambda e, v=v, ci=ci: e.scalar_tensor_tensor(out=out, in0=v, scalar=mixc[:, ci, cc:cc + 1], in1=out,
                                                                     op0=ALU.mult, op1=ALU.add),
                 reads=[key, mixc, okey], writes=[okey])
        if latent:
            c63 = buf[:, 63:256:64]
            c0 = buf[:, 128:321:64]
            p.op(eng, lambda e: e.tensor_copy(out=colsv[:], in_=c63), reads=[key], writes=[colsv])
            p.op(eng, lambda e: e.memset(c63, 0.0), reads=[key], writes=[key])
            p.op(eng, lambda e: e.scalar_tensor_tensor(out=out, in0=buf[:, 63:319], scalar=mixc[:, 1, cc:cc + 1], in1=out,
                                                       op0=ALU.mult, op1=ALU.add), reads=[key, mixc, okey], writes=[okey])
            p.op(eng, lambda e: e.tensor_copy(out=c63, in_=colsv[:]), reads=[colsv, key], writes=[key])
            p.op(eng, lambda e: e.memset(c0, 0.0), reads=[key], writes=[key])
            p.op(eng, lambda e: e.scalar_tensor_tensor(out=out, in0=buf[:, 65:321], scalar=mixc[:, 2, cc:cc + 1], in1=out,
                                                       op0=ALU.mult, op1=ALU.add), reads=[key, mixc, okey], writes=[okey])

    def load_raw(dst, src_rows, ti, latent, name):
        if latent:
            r0 = 4 * ti - 1
            lo, hi = max(r0, 0), min(r0 + 6, 64)
            if r0 < 0:
                p.op("pool", lambda e: e.memset(dst[:, :, 0:64], 0.0), writes=[name])
            if r0 + 6 > 64:
                p.op("pool", lambda e: e.memset(dst[:, :, 320:384], 0.0), writes=[name])
            p.dma("sp", dst[:, :, (lo - r0) * 64:(hi - r0) * 64], src_rows[:, :, lo * 64:hi * 64], reads=[], writes=[name])
        else:
            p.op("pool", lambda e: e.memset(dst[:, :, 0:1], 0.0), writes=[name])
            p.op("pool", lambda e: e.memset(dst[:, :, 257:258], 0.0), writes=[name])
            p.dma("sp", dst[:, :, 1:257], src_rows, reads=[], writes=[name])

    slabLv = k.slabL.rearrange("(cc p) t -> p cc t", p=128)
    slabCv = k.slabC.rearrange("(cc p) t -> p cc t", p=128)
    tiles = [("c", 0)] + [("l", ti) for ti in range(16)]
    if getattr(k, "ntiles", None):
        tiles = tiles[:k.ntiles]
    def do_tile(kind, ti, itbase):
        latent = kind == "l"
        own = latent and ti < 8
        src = slabLv if latent else slabCv
        chunk0 = 4 + 4 * ti if latent else 0
        tok0 = chunk0 * 64
        for j, cc in enumerate(range(48, 54)):
            if cc >= 52 and not own:
                continue
            rb = rawl[j % NB]
            load_raw(rb[:].unsqueeze(1), src[:, cc:cc + 1, :], ti, latent, rb.name)
            mix("dve", ML[:], rb[:], cc, latent, rb.name, ML.name)
            if j < 2:
                p.op("act", lambda e, j=j: e.activation(out=tw[:, j, :], in_=ML[:], func=AF.Tanh), reads=[ML], writes=[(tw.name, j)])
            elif j < 4:
                p.op("act", lambda e, j=j: e.activation(out=tw[:, j, :], in_=ML[:], func=AF.Copy), reads=[ML], writes=[(tw.name, j)])
            else:
                p.op("act", lambda e, j=j: e.activation(out=sgd[:, j - 4, :], in_=ML[:], func=AF.Sigmoid), reads=[ML], writes=[sgd])
                p.dma("sp", k.sgdT[(j - 4) * 128:(j - 3) * 128, ti * TT:(ti + 1) * TT], sgd[:, j - 4, :], reads=[sgd], writes=[])
        cut(2)
        def hp_body(hp, it):
            rb = raw3[it % NB]
            m3 = M3[it % NB]
            load_raw(rb[:], src[:, hp:hp + 33:16, :], ti, latent, rb.name)
            for j in range(3):
                mix("dve", m3[:, j, :], rb[:, j, :], hp + 16 * j, latent, (rb.name, j), (m3.name, j))
            cut(3)
            rM, kM, vM = m3[:, 0, :], m3[:, 1, :], m3[:, 2, :]
            ch = slice(hp * 128, (hp + 1) * 128)
            pl = psL[it % 2]
            for d in range(2):
                p.op("pe", lambda e, d=d, pl=pl: e.matmul(pl[0][:, d * TT:(d + 1) * TT], lhsT=lw2[:, d, ch], rhs=tw[0:96, d, :],
                                                          start=True, stop=True),
                     reads=[(lw2.name, d), (tw.name, d)], writes=[pl[0]])
            for d in range(2):
                p.op("pe", lambda e, d=d, pl=pl: e.matmul(pl[1][:, d * TT:(d + 1) * TT], lhsT=lw2[:, 2 + d, ch], rhs=tw[0:96, 2 + d, :],
                                                          start=True, stop=True),
                     reads=[(lw2.name, 2 + d), (tw.name, 2 + d)], writes=[pl[1]])
            for d in range(2):
                p.op("act", lambda e, d=d, pl=pl: e.activation(out=f["lw"][d][:], in_=pl[0][:, d * TT:(d + 1) * TT], func=AF.Sigmoid,
                                                               bias=vecs[:, d, hp:hp + 1]), reads=[pl[0], vecs], writes=[f["lw"][d]])
                p.op("act", lambda e, d=d, pl=pl: e.activation(out=f["aa"][d][:], in_=pl[1][:, d * TT:(d + 1) * TT], func=AF.Sigmoid,
                                                               bias=vecs[:, 2 + d, hp:hp + 1]), reads=[pl[1], vecs], writes=[f["aa"][d]])
            p.op("dve", lambda e: e.tensor_scalar(out=kkr[:], in0=kM, scalar1=vecs[:, 4, hp:hp + 1], scalar2=None, op0=ALU.mult),
                 reads=[(m3.name, 1), vecs], writes=[kkr])
            p.op("act", lambda e: e.activation(out=sqk[:], in_=kkr[:], func=AF.Square), reads=[kkr], writes=[sqk])
            p.op("pe", lambda e: e.matmul(psK[:, 0:TT], lhsT=bones[:], rhs=sqk[:], start=True, stop=True), reads=[bones, sqk], writes=[psK])
            p.op("act", lambda e: e.activation(out=rn[:], in_=psK[:, 0:TT], func=AF.Sqrt, bias=k.eps_t[:, 1:2]), reads=[psK, k.eps_t], writes=[rn])
            p.op("dve", lambda e: e.reciprocal(out=rn[:], in_=rn[:]), reads=[rn], writes=[rn])
            p.op(ew(), lambda e: e.tensor_tensor(out=kk[:], in0=kkr[:], in1=rn[:], op=ALU.mult), reads=[kkr, rn], writes=[kk])
            cut(4)
            p.op("act", lambda e: e.activation(out=vbf[:], in_=vM, func=AF.Copy), reads=[(m3.name, 2)], writes=[vbf])
            pt, pt2 = psT[it % 2]
            ptb = pt[:]
            ptb2 = pt2[:]
            for s in range(2):
                p.op("pe", lambda e, s=s, ptb=ptb: e.transpose(ptb[:, s * 128:(s + 1) * 128], vbf[:, s * 128:(s + 1) * 128], ident[:]),
                     reads=[vbf, ident], writes=[pt])
            cut(5)
            def d_body(d):
                lw, aa = f["lw"][d], f["aa"][d]
                cL, X2, X3, X4 = f["cL"][d], f["X2"][d], f["X3"][d], f["X4"][d]
                e1, e2, e3, e4 = f["e1"][d], f["e2"][d], f["e3"][d], f["e4"][d]
                kd, bd, ck = f["kd"][d], f["bd"][d], f["ck"][d]
                Q = Qst[d][it % 2]
                p.op("dve", lambda e, cL=cL, lw=lw: e.tensor_tensor_scan(out=cL[:], data0=rmask[:], data1=lw[:], initial=0.0,
                                                                          op0=ALU.mult, op1=ALU.add), reads=[rmask, lw], writes=[cL])
                tot = cL[:].rearrange("p (r c) -> p r c", c=64)[:, :, 63:64]
                p.op(ew(), lambda e, X2=X2, cL=cL, lw=lw: e.tensor_tensor(out=X2[:], in0=cL[:], in1=lw[:], op=ALU.subtract),
                     reads=[cL, lw], writes=[X2])
                p.op(ew(), lambda e, X3=X3, cL=cL, tot=tot: e.tensor_tensor(
                    out=X3[:].rearrange("p (r c) -> p r c", c=64), in0=tot.to_broadcast([128, 4, 64]),
                    in1=cL[:].rearrange("p (r c) -> p r c", c=64), op=ALU.subtract), reads=[cL], writes=[X3])
                p.op("act", lambda e, d=d, tot=tot: e.activation(out=k.wtot[d][:, hp, chunk0:chunk0 + 4].unsqueeze(2), in_=tot,
                                                                func=AF.Exp, scale=-C0), reads=[cL], writes=[(k.wtot[d].name, it)])
                if d == 0:
                    srcs = [(cL, -C0), (X2, -C0), (cL, C0), (X3, -C0)]
                else:
                    p.op(ew(), lambda e, X4=X4, X3=X3, lw=lw: e.tensor_tensor(out=X4[:], in0=X3[:], in1=lw[:], op=ALU.add),
                         reads=[X3, lw], writes=[X4])
                    srcs = [(X4, -C0), (X3, -C0), (X4, C0), (X2, -C0)]
                for (sx, sc), eo in zip(srcs, (e1, e2, e3, e4)):
                    p.op("act", lambda e, sx=sx, sc=sc, eo=eo: e.activation(out=eo[:], in_=sx[:], func=AF.Exp, scale=sc),
                         reads=[sx], writes=[eo])
                p.op("dve", lambda e, ck=ck, aa=aa: e.tensor_scalar(out=ck[:], in0=aa[:], scalar1=vecs[:, 5, hp:hp + 1], scalar2=oka[:, hp:hp + 1],
                                                                   op0=ALU.mult, op1=ALU.add), reads=[aa, vecs, oka], writes=[ck])
                p.op(ew(), lambda e, kd=kd, ck=ck: e.tensor_tensor(out=kd[:], in0=kM, in1=ck[:], op=ALU.mult), reads=[(m3.name, 1), ck], writes=[kd])
                p.op(ew(), lambda e, bd=bd, aa=aa: e.tensor_tensor(out=bd[:], in0=kk[:], in1=aa[:], op=ALU.mult), reads=[kk, aa], writes=[bd])
                Qv = lambda a: Q[:, :, a, :]
                r3 = lambda t: t.rearrange("p (r c) -> p r c", c=64)
                p.op(ew(), lambda e, e1=e1: e.tensor_tensor(out=Qv(3), in0=r3(rM), in1=r3(e1[:]), op=ALU.mult), reads=[(m3.name, 0), e1], writes=[Q])
                p.op(ew(), lambda e, e2=e2: e.tensor_tensor(out=Qv(2), in0=r3(kk[:]), in1=r3(e2[:]), op=ALU.mult), reads=[kk, e2], writes=[Q])
                p.op(ew(), lambda e, e3=e3, kd=kd: e.tensor_tensor(out=Qv(1), in0=r3(kd[:]), in1=r3(e3[:]), op=ALU.mult), reads=[kd, e3], writes=[Q])
                p.op(ew(), lambda e, e3=e3, bd=bd: e.tensor_tensor(out=Qv(0), in0=r3(bd[:]), in1=r3(e3[:]), op=ALU.mult), reads=[bd, e3], writes=[Q])
                p.dma("sp", k.Q[d][ch, chunk0 * 256:(chunk0 + 4) * 256], Q[:].rearrange("p a b c -> p (a b c)"), reads=[Q], writes=[])
                p.op(ew(), lambda e, e4=e4, kd=kd, d=d: e.tensor_tensor(out=kpb[d][:], in0=kd[:], in1=e4[:], op=ALU.mult), reads=[kd, e4], writes=[kpb[d]])
                p.op(ew(), lambda e, e4=e4, bd=bd, d=d: e.tensor_tensor(out=bpb[d][:], in0=bd[:], in1=e4[:], op=ALU.mult), reads=[bd, e4], writes=[bpb[d]])
                for s in range(2):
                    p.op("pe", lambda e, s=s, d=d, ptb=ptb: e.transpose(ptb[:, (2 + 4 * d + s) * 128:(3 + 4 * d + s) * 128],
                                                                        kpb[d][:, s * 128:(s + 1) * 128], ident[:]),
                         reads=[kpb[d], ident], writes=[pt])
                    if d == 0:
                        p.op("pe", lambda e, s=s, d=d, ptb=ptb: e.transpose(ptb[:, (4 + s) * 128:(5 + s) * 128],
                                                                            bpb[d][:, s * 128:(s + 1) * 128], ident[:]),
                             reads=[bpb[d], ident], writes=[pt])
                    else:
                        p.op("pe", lambda e, s=s, d=d, ptb2=ptb2: e.transpose(ptb2[:, s * 128:(s + 1) * 128],
                                                                              bpb[d][:, s * 128:(s + 1) * 128], ident[:]),
                             reads=[bpb[d], ident], writes=[pt2])
            for d_ in range(2):
                d_body(d_)
            cut(6)
            for bi, nm in enumerate(("v", "kA", "bA", "kB", "bB")):
                for s2 in range(2):
                    if bi < 4:
                        src_ps = ptb[:, (bi * 2 + s2) * 128:(bi * 2 + s2 + 1) * 128]
                        pkey = pt
                    else:
                        src_ps = ptb2[:, s2 * 128:(s2 + 1) * 128]
                        pkey = pt2
                    eng = "act" if (bi + s2) % 2 else "dve"
                    if getattr(k, "evac_dve", False):
                        eng = "dve"
                    if eng == "act":
                        p.op("act", lambda e, nm=nm, src_ps=src_ps, s2=s2: e.activation(out=TM[nm][:, s2, ch], in_=src_ps, func=AF.Copy),
                             reads=[pkey], writes=[(TM[nm].name, hp)])
                    else:
                        p.op("dve", lambda e, nm=nm, src_ps=src_ps, s2=s2: e.tensor_copy(out=TM[nm][:, s2, ch], in_=src_ps),
                             reads=[pkey], writes=[(TM[nm].name, hp)])
            cut(7)
            if own:
                p.op(ew(), lambda e: e.tensor_tensor(out=ksum[:], in0=f["kd"][0][:], in1=f["kd"][1][:], op=ALU.add),
                     reads=[f["kd"][0], f["kd"][1]], writes=[ksum])
                p.op("dve", lambda e: e.scalar_tensor_tensor(out=prod[:], in0=ksum[:], scalar=vecs[:, 6, hp:hp + 1], in1=rM,
                                                            op0=ALU.mult, op1=ALU.mult), reads=[ksum, vecs, (m3.name, 0)], writes=[prod])
                for s in range(2):
                    p.op("pe", lambda e, s=s: e.matmul(psBon[:, s * 32 + 2 * hp:s * 32 + 2 * hp + 2], lhsT=prod[:, s * 128:(s + 1) * 128],
                                                       rhs=hsel[:], start=True, stop=True), reads=[prod, hsel], writes=[psBon])
        for hp_ in range(16):
            hp_body(hp_, itbase + hp_ + 1)
        for nm, dst in (("v", k.Vtok), ("kA", k.KpT[0]), ("bA", k.BpT[0]), ("kB", k.KpT[1]), ("bB", k.BpT[1])):
            for s in range(2):
                p.dma("sp", dst[tok0 + s * 128:tok0 + (s + 1) * 128, :], TM[nm][:, s, :], reads=[TM[nm]], writes=[])
        if own:
            p.op("dve", lambda e: e.tensor_copy(out=bco[:], in_=psBon[:, 0:64].rearrange("p (s h) -> p s h", h=32)), reads=[psBon], writes=[bco])
            for s in range(2):
                p.dma("sp", k.bcoef[ti * TT + s * 128:ti * TT + (s + 1) * 128, :], bco[:, s, :], reads=[bco], writes=[])
    for tix, (kind_, ti_) in enumerate(tiles):
        do_tile(kind_, ti_, tix * 16)
    p.barrier()
    p.release(markB)


import numpy as np

NCHK = 68


def phaseC(k):
    p, nc = k.p, k.nc
    din, dscr = k.din, k.dscr
    k.cmask_d = din("cmask", [128, 2, 128])
    k.nmask_d = din("nmask", [64, 3, 64])
    k.Yscr = [dscr("YA", [2048, 2048], F32), dscr("YB", [2048, 2048], F32)]
    k.Sdbg = dscr("Sdbg", [2, 128, 16 * 64], F32)

    cmask = p.sb("cmask_sb", [128, 2, 128], F32)
    nmask = p.sb("nmask_sb", [64, 3, 64], F32)
    p.dma("sp", cmask[:], k.cmask_d)
    p.dma("sp", nmask[:], k.nmask_d)
    FM = [p.sb("FM%d" % i, [128, 16, 256], BF16) for i in range(2)]
    UV = [p.sb("UV%d" % i, [128, 2048], BF16) for i in range(2)]
    VZ = [p.sb("VZ%d" % i, [128, 2048], BF16) for i in range(2)]
    FMm = [p.sb("FMm%d" % i, [128, 16, 2, 128], BF16) for i in range(2)]
    for i in range(2):
        p.op("pool", lambda e, i=i: e.memset(VZ[i][:], 0.0), writes=[VZ[i]])
        p.op("pool", lambda e, i=i: e.memset(FMm[i][:], 0.0), writes=[FMm[i]])
    KB = [p.sb("KB%d" % i, [128, 2048], BF16) for i in range(2)]
    AT = p.sb("AT_sb", [128, 32, 128], BF16)
    Pm = [p.sb("Pm%d" % g, [128, 8, 64], BF16) for g in range(4)]
    PT = [p.sb("PTm%d" % g, [128, 8, 64], BF16) for g in range(4)]
    Gm = [p.sb("Gm%d" % g, [128, 8, 64], BF16) for g in range(4)]
    Qm = [p.sb("Qm%d" % g, [128, 8, 64], BF16) for g in range(4)]
    nrhs = [p.sb("nrhs%d" % g, [128, 8, 64], BF16) for g in range(4)]
    for g in range(4):
        for t_ in (Pm, PT, Gm, Qm, nrhs):
            p.op("pool", lambda e, t_=t_, g=g: e.memset(t_[g][:], 0.0), writes=[t_[g]])
    Ysb = [p.sb("Ysb%d" % i, [64, 2048], F32) for i in range(2)]
    S32 = p.sb("S32", [128, 16, 64], F32)
    Sbf = p.sb("Sbf", [128, 16, 64], BF16)
    Stmp = p.sb("Stmp", [128, 4, 64], F32)
    psAT = [k.psb[0], k.psb[1]]
    psP, psPT, psQ, psY = k.psb[2], k.psb[3], k.psb[4], k.psb[5]
    ident = nmask[:, 2, :]
    Qv = [k.Q[d].rearrange("(hp p) x -> p hp x", p=128) for d in range(2)]
    cnt = {"e": 0, "ld": 0}

    def evac_eng():
        cnt["e"] += 1
        return "act" if cnt["e"] % 2 else "dve"

    def copy_evac(out, in_, reads, writes, scale=None):
        eng = evac_eng()
        if eng == "act":
            if scale is None:
                p.op("act", lambda e: e.activation(out=out, in_=in_, func=AF.Copy), reads=reads, writes=writes)
            else:
                p.op("act", lambda e: e.activation(out=out, in_=in_, func=AF.Copy, scale=scale), reads=reads, writes=writes)
        else:
            if scale is None:
                p.op("dve", lambda e: e.tensor_copy(out=out, in_=in_), reads=reads, writes=writes)
            else:
                p.op("dve", lambda e: e.tensor_scalar(out=out, in0=in_, scalar1=scale, scalar2=None, op0=ALU.mult), reads=reads, writes=writes)

    def load_chunk(d, c, buf):
        fm, uv, kb = FM[buf], UV[buf], KB[buf]
        vz, fmm = VZ[buf], FMm[buf]
        p.dma("sp", fm[:], Qv[d][:, :, c * 256:(c + 1) * 256], reads=[], writes=[fm])
        p.dma("sp", fmm[0:64, :, 0, :], Qv[d][0:64, :, c * 256 + 128:(c + 1) * 256], reads=[], writes=[(fmm.name, 0)])
        p.dma("sp", fmm[64:128, :, 1, :], Qv[d][64:128, :, c * 256 + 128:(c + 1) * 256], reads=[], writes=[(fmm.name, 1)])
        p.dma("sp", uv[64:128, :], k.Vtok[c * 64:(c + 1) * 64, :], reads=[], writes=[(uv.name, "V")])
        p.dma("sp", vz[64:128, :], k.Vtok[c * 64:(c + 1) * 64, :], reads=[], writes=[(vz.name, "V")])
        p.dma("sp", kb[0:64, :], k.BpT[d][c * 64:(c + 1) * 64, :], reads=[], writes=[(kb.name, 0)])
        p.dma("sp", kb[64:128, :], k.KpT[d][c * 64:(c + 1) * 64, :], reads=[], writes=[(kb.name, 1)])

    def do_chunk(d, c, buf, emit, ytok0):
        fm, uv, kb = FM[buf], UV[buf], KB[buf]
        vz, fmm = VZ[buf], FMm[buf]
        ysb = Ysb[buf]

        def fmv(h, a0, a1):
            return fm[:, h // 2, a0 * 64:a1 * 64]

        def fmk(h, a0, a1):
            return fmm[:, h // 2, h % 2, a0 * 64:a1 * 64]

        def sv(h):
            return Sbf[:, h // 2, :]

        def stage1(g):
            for hl in range(8):
                h = g * 8 + hl
                pa = psAT[hl // 4]
                p.op("pe", lambda e, h=h, hl=hl, pa=pa: e.matmul(pa[:, (hl % 4) * 128:(hl % 4 + 1) * 128], lhsT=fmv(h, 0, 2), rhs=fmk(h, 0, 2),
                                                               start=True, stop=True), reads=[fm, fmm], writes=[pa])
            for hl in range(8):
                h = g * 8 + hl
                p.op("pe", lambda e, h=h, hl=hl: e.matmul(psP[0:64, hl * 64:(hl + 1) * 64], lhsT=fmk(h, 0, 1), rhs=fmv(h, 0, 1),
                                                         start=True, stop=True), reads=[fm, fmm], writes=[psP])
            for half in range(2):
                pa = psAT[half]
                p.op("dve", lambda e, half=half, pa=pa: e.tensor_tensor(
                    out=AT[:, g * 8 + half * 4:g * 8 + half * 4 + 4, :], in0=pa[:].rearrange("p (h c) -> p h c", c=128),
                    in1=cmask[:, d, :].unsqueeze(1).to_broadcast([128, 4, 128]), op=ALU.mult), reads=[pa, cmask], writes=[(AT.name, g)])
            p.op("dve", lambda e: e.tensor_tensor(out=Pm[g][0:64, :, :], in0=psP[0:64, :].rearrange("p (h c) -> p h c", c=64),
                                                  in1=nmask[:, d, :].unsqueeze(1).to_broadcast([64, 8, 64]), op=ALU.mult),
                 reads=[psP, nmask], writes=[Pm[g]])
            p.op("dve", lambda e: e.tensor_tensor(out=Qm[g][0:64, :, :], in0=ident.unsqueeze(1).to_broadcast([64, 8, 64]),
                                                  in1=AT[0:64, g * 8:(g + 1) * 8, 0:64], op=ALU.subtract),
                 reads=[(AT.name, g), nmask], writes=[Qm[g]])

        def ptv(g, kk_, hl):
            if kk_ == 0:
                return AT[:, g * 8 + hl, 0:64]
            return PT[g][:, hl, :]

        def stage2(g, kk_):
            for hl in range(8):
                p.op("pe", lambda e, hl=hl: e.matmul(psP[0:64, hl * 64:(hl + 1) * 64], lhsT=ptv(g, kk_ - 1, hl), rhs=Pm[g][:, hl, :],
                                                    start=True, stop=True),
                     reads=[Pm[g], PT[g], (AT.name, g)], writes=[psP])
            if kk_ < 5:
                for hl in range(8):
                    p.op("pe", lambda e, hl=hl: e.matmul(psPT[0:64, hl * 64:(hl + 1) * 64], lhsT=Pm[g][:, hl, :], rhs=ptv(g, kk_ - 1, hl),
                                                        start=True, stop=True),
                         reads=[Pm[g], PT[g], (AT.name, g)], writes=[psPT])
            p3 = psP[0:64, :].rearrange("p (h c) -> p h c", c=64)
            p.op("dve", lambda e: e.tensor_tensor(out=Gm[g][0:64, :, :], in0=p3, in1=ident.unsqueeze(1).to_broadcast([64, 8, 64]), op=ALU.add),
                 reads=[psP, nmask], writes=[Gm[g]])
            if kk_ < 5:
                p.op("act", lambda e: e.activation(out=Pm[g][0:64, :, :].rearrange("p h c -> p (h c)"), in_=psP[0:64, :], func=AF.Copy), reads=[psP], writes=[Pm[g]])
                copy_evac(PT[g][0:64, :, :].rearrange("p h c -> p (h c)"), psPT[0:64, :], [psPT], [PT[g]])
            for hl in range(8):
                p.op("pe", lambda e, hl=hl: e.matmul(psQ[0:64, hl * 64:(hl + 1) * 64], lhsT=Gm[g][:, hl, :], rhs=Qm[g][:, hl, :],
                                                    start=True, stop=True), reads=[Gm[g], Qm[g]], writes=[psQ])
            copy_evac(Qm[g][0:64, :, :].rearrange("p h c -> p (h c)"), psQ[0:64, :], [psQ], [Qm[g]])

        def stage3(g):
            for hl in range(8):
                h = g * 8 + hl
                p.op("pe", lambda e, h=h, hl=hl: e.matmul(psQ[0:64, hl * 64:(hl + 1) * 64], lhsT=fmk(h, 0, 1), rhs=sv(h),
                                                         start=True, stop=False), reads=[fmm, (Sbf.name, h // 8)], writes=[psQ])
                p.op("pe", lambda e, h=h, hl=hl: e.matmul(psQ[0:64, hl * 64:(hl + 1) * 64], lhsT=AT[:, h, 0:64],
                                                         rhs=vz[:, h * 64:(h + 1) * 64], start=False, stop=True),
                     reads=[(AT.name, g), vz], writes=[psQ])
            copy_evac(nrhs[g][0:64, :, :].rearrange("p h c -> p (h c)"), psQ[0:64, :], [psQ], [nrhs[g]], scale=-1.0)
            for hl in range(8):
                p.op("pe", lambda e, hl=hl: e.matmul(psPT[0:64, hl * 64:(hl + 1) * 64], lhsT=Qm[g][:, hl, :], rhs=nrhs[g][:, hl, :],
                                                    start=True, stop=True), reads=[Qm[g], nrhs[g]], writes=[psPT])
            copy_evac(uv[0:64, g * 512:(g + 1) * 512], psPT[0:64, :], [psPT], [(uv.name, ("U", g))])

        def stage5(g):
            for hl in range(8):
                h = g * 8 + hl
                p.op("pe", lambda e, h=h, hl=hl: e.matmul(psY[0:64, hl * 64:(hl + 1) * 64], lhsT=fmk(h, 1, 2), rhs=sv(h),
                                                         start=True, stop=False), reads=[fmm, (Sbf.name, h // 8)], writes=[psY])
                p.op("pe", lambda e, h=h, hl=hl: e.matmul(psY[0:64, hl * 64:(hl + 1) * 64], lhsT=AT[:, h, 64:128],
                                                         rhs=uv[:, h * 64:(h + 1) * 64], start=False, stop=True),
                     reads=[(AT.name, g), (uv.name, "V"), (uv.name, ("U", g))], writes=[psY])
            copy_evac(ysb[:, g * 512:(g + 1) * 512], psY[0:64, :], [psY], [(ysb.name, g)])

        def stage6(g):
            ps4 = psY[:].rearrange("p (a b c) -> p a b c", a=4, b=2)
            for hpl in range(4):
                hp = g * 4 + hpl
                h0, h1 = 2 * hp, 2 * hp + 1
                p.op("pe", lambda e, hpl=hpl, h0=h0: e.matmul(ps4[0:64, hpl, 0, :], lhsT=kb[:, h0 * 64:(h0 + 1) * 64], rhs=uv[:, h0 * 64:(h0 + 1) * 64],
                                                             start=True, stop=True),
                     reads=[kb, (uv.name, "V"), (uv.name, ("U", g))], writes=[psY])
                p.op("pe", lambda e, hpl=hpl, h0=h0, h1=h1: e.matmul(ps4[:, hpl, 1, :], lhsT=kb[:, h0 * 64:(h1 + 1) * 64], rhs=uv[:, h1 * 64:(h1 + 1) * 64],
                                                                    start=True, stop=True),
                     reads=[kb, (uv.name, "V"), (uv.name, ("U", g))], writes=[psY])
            for hh in range(2):
                rows = slice(hh * 64, (hh + 1) * 64)
                p.op("dve", lambda e, rows=rows: e.tensor_tensor(
                    out=Stmp[rows, :, :], in0=S32[rows, g * 4:(g + 1) * 4, :],
                    in1=k.wtot[d][rows, g * 4:(g + 1) * 4, c:c + 1].to_broadcast([64, 4, 64]), op=ALU.mult),
                    reads=[(S32.name, g), k.wtot[d]], writes=[(Stmp.name, hh)])
                p.op("dve", lambda e, rows=rows, hh=hh: e.tensor_tensor(
                    out=S32[rows, g * 4:(g + 1) * 4, :], in0=Stmp[rows, :, :], in1=ps4[rows, :, hh, :], op=ALU.add),
                    reads=[(Stmp.name, hh), psY], writes=[(S32.name, g)])
            p.op("act", lambda e: e.activation(out=Sbf[:].rearrange("p a b -> p (a b)")[:, g * 256:(g + 1) * 256],
                                               in_=S32[:].rearrange("p a b -> p (a b)")[:, g * 256:(g + 1) * 256], func=AF.Copy),
                 reads=[(S32.name, g)], writes=[(Sbf.name, g)])

        cc_ = getattr(k, "ccut", None) or 99
        for g in range(4):
            stage1(g)
        if cc_ <= 1:
            return
        for kk_ in range(1, 6):
            for g in range(4):
                stage2(g, kk_)
        if cc_ <= 2:
            return
        for g in range(4):
            stage3(g)
        if cc_ <= 3:
            return
        if emit:
            for g in range(4):
                stage5(g)
            p.dma("sp", k.Yscr[d][ytok0:ytok0 + 64, :], ysb[:], reads=[ysb], writes=[])
        if cc_ <= 5:
            return
        for g in range(4):
            stage6(g)

    ncap = getattr(k, "nchunks", None)
    for d in range(2):
        if d == 0:
            seq = [(c, False) for c in range(4)] + [(c, True) for c in range(4, 36)]
        else:
            seq = [(c, False) for c in range(3, -1, -1)] + [(c, False) for c in range(67, 35, -1)] + [(c, True) for c in range(35, 3, -1)]
        if ncap:
            seq = seq[:ncap]
        p.op("dve", lambda e: e.memset(S32[:], 0.0), writes=[S32])
        p.op("pool", lambda e: e.memset(Sbf[:], 0.0), writes=[Sbf])
        load_chunk(d, seq[0][0], cnt["ld"] % 2)
        for i, (c, emit) in enumerate(seq):
            buf = cnt["ld"] % 2
            cnt["ld"] += 1
            if i + 1 < len(seq):
                load_chunk(d, seq[i + 1][0], cnt["ld"] % 2)
            do_chunk(d, c, buf, emit, (c - 4) * 64)
        if "Sdbg" in k.dbg:
            p.dma("sp", k.Sdbg[d], S32[:].rearrange("p a b -> p (a b)"), reads=[S32], writes=[])
    p.barrier()


import numpy as np

GT = 256
NTS = 2
NR = 4
GN_EPS = 64e-5
NORM_EPS = 1e-6
D = 2048


def phaseD(k):
    p, nc = k.p, k.nc
    din, dscr = k.din, k.dscr
    k.xown_d = din("xown", [2048, 2048])
    k.rows_d = din("rows", [3, 2048])
    k.g2n_d = din("g2n", [128, 16])
    k.g2w_d = din("g2w", [256, 2048])
    k.poolw_d = din("poolw", [4, 256, 256])
    k.pools_d = din("pools", [128, 8])
    k.wpo_d = din("wpo", [1024, 2048])
    k.wro_d = din("wro", [2048, 2048])
    k.wout_d = din("wout", [2048, 2048])
    k.poolc_d = din("poolc", [1, 4 * 64 + 2])
    k.rowbuf = dscr("rowbuf", [4, 2048], F32)
    k.x1scr = dscr("x1scr", [2048, 2048], F32)
    k.h2T = dscr("h2Tscr", [2048, 2048], BF16)
    k.h2tok = dscr("h2tok", [2048, 2048], BF16)

    k.rowt = {}
    mD = p.mark()
    for i, nm in enumerate(("lnxw", "lnxb")):
        k.rowt[nm] = p.sb("row_" + nm, [128, 2048], F32)
        p.dma("sp", k.rowt[nm][:], k.rows_d[i:i + 1, :].partition_broadcast(128))
    g2n = p.sb("g2n_sb", [128, 16], F32)
    A2 = p.sb("A2_fm", [128, 16], F32)
    p.dma("sp", g2n[:], k.g2n_d)
    p.op("dve", lambda e: e.tensor_scalar(out=A2[:], in0=k.mod[:, 64:80, 0], scalar1=1.0, scalar2=None, op0=ALU.add), reads=[k.mod], writes=[A2])
    p.op("dve", lambda e: e.tensor_tensor(out=A2[:], in0=A2[:], in1=g2n[:], op=ALU.mult), reads=[A2, g2n], writes=[A2])
    rb = k.rowbuf.rearrange("r (oc q) -> r q oc", q=128)
    o1 = p.dma("sp", rb[0], k.mod[:, 32:48, 0], reads=[k.mod], writes=["rowbuf"], allow_slow_non_contiguous=True)
    o2 = p.dma("sp", rb[1], A2[:], reads=[A2], writes=["rowbuf"], allow_slow_non_contiguous=True)
    o3 = p.dma("sp", rb[2], k.mod[:, 48:64, 0], reads=[k.mod], writes=["rowbuf"], allow_slow_non_contiguous=True)
    o4 = p.dma("sp", rb[3], k.mod[:, 80:96, 0], reads=[k.mod], writes=["rowbuf"], allow_slow_non_contiguous=True)
    for i, nm in enumerate(("gt1", "A2", "sh2")):
        k.rowt[nm] = p.sb("row_" + nm, [128, 2048], F32)
        p.dma("sp", k.rowt[nm][:], k.rowbuf[i:i + 1, :].partition_broadcast(128), reads=["rowbuf"], writes=[k.rowt[nm]])

    g2w = p.sb("g2w_sb", [128, 2, 2048], BF16)
    poolw = p.sb("poolw_sb", [128, 4, 2, 256], BF16)
    pools = p.sb("pools_sb", [128, 8], F32)
    poolc = p.sb("poolc_sb", [128, 258], F32)
    gneps = p.sb("gneps", [128, 1], F32)
    p.dma("pool", g2w[:], k.g2w_d.rearrange("(kc q) n -> q kc n", q=128))
    p.dma("pool", poolw[:], k.poolw_d.rearrange("g (kc q) d -> q g kc d", q=128))
    p.dma("sp", pools[:], k.pools_d)
    p.dma("sp", poolc[:], k.poolc_d.partition_broadcast(128))
    p.op("pool", lambda e: e.memset(gneps[:], GN_EPS), writes=[gneps])
    ident = k.ident

    ya = p.sb("ya", [128, 2048], F32)
    yb = p.sb("yb", [128, 2048], F32)
    sq = p.sb("ysq", [128, 2048], F32)
    vt = p.sb("vt", [128, 2048], BF16)
    st1 = p.sb("st1", [128, 32], F32)
    st2 = p.sb("st2", [128, 32], F32)
    st3 = p.sb("st3", [128, 32], F32)
    bc = p.sb("bc", [128, 32], F32)
    sgdt = p.sb("sgdt", [128, 2, 128], BF16)
    prebf = p.sb("prebf", [128, 2048], BF16)
    preT = p.sb("preT", [128, 16, GT], BF16)
    Upad = p.sb("Upad", [128, NR, 96], F32)
    Sa = p.sb("Sa", [128, NR, 96], F32)
    Sb_ = p.sb("Sb_", [128, NR, 96], F32)
    dtmp = p.sb("dtmp", [128, NR, 64], F32)
    ubf = p.sb("ubf", [128, GT], BF16)
    dT = p.sb("dT", [128, 8, GT], BF16)
    zT = p.sb("zT", [128, 8, GT], BF16)
    sgt = [p.sb("sgt%d" % i, [128, GT], BF16) for i in range(2)]
    mrg = p.sb("mrg", [128, GT], F32)
    mT = p.sb("mT", [128, 16, GT], BF16)
    wblk = [p.sb("wblkD%d" % i, [128, 16, 512], BF16) for i in range(2)]
    x1g = p.sb("x1g", [128, NTS, 2048], BF16)
    x1 = p.sb("x1t", [128, 2048], F32)
    h2 = p.sb("h2t", [128, 2048], BF16)
    h2Ts = p.sb("h2Ts", [128, 16, 128], BF16)
    ss = p.sb("ssD", [128, 2], F32)
    psb = k.psb
    pbt = k.pbt
    cnt = {"w": 0}
    p.op("dve", lambda e: e.memset(Upad[:], 0.0), writes=[Upad])

    def wload(src_ap, nk):
        w = wblk[cnt["w"] % 2]
        cnt["w"] += 1
        for h in range(0, nk, 8):
            p.dma("pool", w[:, h:h + 8, :], src_ap[:, h:h + 8, :], reads=[], writes=[(w.name, h // 8)])
        return w

    wpo_v = k.wpo_d.rearrange("(kc q) n -> q kc n", q=128)
    wro_v = k.wro_d.rearrange("(kc q) n -> q kc n", q=128)
    wout_v = k.wout_d.rearrange("(kc q) n -> q kc n", q=128)

    def readout_tile(tg, ts):
        ti = tg * NTS + ts
        t0 = ti * 128
        p.dma("sp", ya[:], k.Yscr[0][t0:t0 + 128, :], reads=[], writes=[ya])
        p.dma("sp", yb[:], k.Yscr[1][t0:t0 + 128, :], reads=[], writes=[yb])
        p.dma("sp", vt[:], k.Vtok[256 + t0:256 + t0 + 128, :], reads=[], writes=[vt])
        p.dma("sp", bc[:], k.bcoef[t0:t0 + 128, :], reads=[], writes=[bc])
        p.dma("sp", sgdt[:], k.sgdT.rearrange("(kc q) t -> q kc t", q=128)[:, :, t0:t0 + 128], reads=[], writes=[sgdt])
        y3 = ya[:].rearrange("p (h c) -> p h c", c=64)
        p.op("dve", lambda e: e.tensor_tensor(out=ya[:], in0=ya[:], in1=yb[:], op=ALU.add), reads=[ya, yb], writes=[ya])
        p.op("dve", lambda e: e.tensor_reduce(out=st1[:], in_=y3, axis=AX.X, op=ALU.add), reads=[ya], writes=[st1])
        p.op("act", lambda e: e.activation(out=sq[:], in_=ya[:], func=AF.Square), reads=[ya], writes=[sq])
        p.op("dve", lambda e: e.tensor_reduce(out=st2[:], in_=sq[:].rearrange("p (h c) -> p h c", c=64), axis=AX.X, op=ALU.add),
             reads=[sq], writes=[st2])
        p.op("dve", lambda e: e.tensor_scalar(out=st1[:], in0=st1[:], scalar1=1.0 / 64, scalar2=None, op0=ALU.mult), reads=[st1], writes=[st1])
        p.op("dve", lambda e: e.tensor_tensor(out=st3[:], in0=st1[:], in1=st1[:], op=ALU.mult), reads=[st1], writes=[st3])
        p.op("dve", lambda e: e.scalar_tensor_tensor(out=st2[:], in0=st2[:], scalar=1.0 / 64, in1=st3[:], op0=ALU.mult, op1=ALU.subtract),
             reads=[st2, st3], writes=[st2])
        p.op("act", lambda e: e.activation(out=st2[:], in_=st2[:], func=AF.Sqrt, bias=gneps[:, 0:1]), reads=[st2, gneps], writes=[st2])
        p.op("dve", lambda e: e.reciprocal(out=st2[:], in_=st2[:]), reads=[st2], writes=[st2])
        p.op("dve", lambda e: e.tensor_tensor(out=y3, in0=y3, in1=st1[:].unsqueeze(2).to_broadcast([128, 32, 64]), op=ALU.subtract),
             reads=[ya, st1], writes=[ya])
        p.op("dve", lambda e: e.tensor_tensor(out=y3, in0=y3, in1=st2[:].unsqueeze(2).to_broadcast([128, 32, 64]), op=ALU.mult),
             reads=[ya, st2], writes=[ya])
        p.op("dve", lambda e: e.tensor_tensor(out=ya[:], in0=ya[:], in1=k.rowt["lnxw"][:], op=ALU.mult), reads=[ya, k.rowt["lnxw"]], writes=[ya])
        p.op("dve", lambda e: e.tensor_tensor(out=ya[:], in0=ya[:], in1=k.rowt["lnxb"][:], op=ALU.add), reads=[ya, k.rowt["lnxb"]], writes=[ya])
        p.op("dve", lambda e: e.tensor_tensor(out=yb[:].rearrange("p (h c) -> p h c", c=64), in0=vt[:].rearrange("p (h c) -> p h c", c=64),
                                              in1=bc[:].unsqueeze(2).to_broadcast([128, 32, 64]), op=ALU.mult), reads=[vt, bc], writes=[yb])
        p.op("dve", lambda e: e.tensor_tensor(out=ya[:], in0=ya[:], in1=yb[:], op=ALU.add), reads=[ya, yb], writes=[ya])
        for cb in range(4):
            ps = psb[cb % 2]
            for kc in range(2):
                p.op("pe", lambda e, cb=cb, kc=kc, ps=ps: e.matmul(ps[:, :], lhsT=sgdt[:, kc, :], rhs=g2w[:, kc, cb * 512:(cb + 1) * 512],
                                                                  start=(kc == 0), stop=(kc == 1)), reads=[sgdt, g2w], writes=[ps])
            p.op("dve", lambda e, cb=cb, ps=ps: e.tensor_tensor(out=prebf[:, cb * 512:(cb + 1) * 512], in0=ya[:, cb * 512:(cb + 1) * 512],
                                                                 in1=ps[:, :], op=ALU.mult), reads=[ya, ps], writes=[(prebf.name, cb)])
        for half in range(2):
            pt = pbt[half]
            for j in range(8):
                kc = half * 8 + j
                p.op("pe", lambda e, kc=kc, j=j, pt=pt: e.transpose(pt[:, j * 128:(j + 1) * 128], prebf[:, kc * 128:(kc + 1) * 128], ident[:]),
                     reads=[prebf, ident], writes=[pt])
            if half:
                p.op("act", lambda e, half=half, pt=pt: e.activation(out=preT[:, half * 8:(half + 1) * 8, ts * 128:(ts + 1) * 128],
                                                                    in_=pt[:].rearrange("p (a b) -> p a b", b=128), func=AF.Copy),
                     reads=[pt], writes=[(preT.name, ts)])
            else:
                p.op("dve", lambda e, half=half, pt=pt: e.tensor_copy(out=preT[:, half * 8:(half + 1) * 8, ts * 128:(ts + 1) * 128],
                                                                     in_=pt[:].rearrange("p (a b) -> p a b", b=128)),
                     reads=[pt], writes=[(preT.name, ts)])

    def pool_group(tg):
        t0 = tg * GT
        for cc in range(8):
            gi = cc // 2
            p.dma("sp", ubf[:], k.uT[cc * 128:(cc + 1) * 128, t0:t0 + GT], reads=[], writes=[ubf])
            p.op("act", lambda e: e.activation(out=Upad[:, :, 16:80], in_=ubf[:].rearrange("p (r c) -> p r c", c=64), func=AF.Copy),
                 reads=[ubf], writes=[Upad])
            src, lo, hi = Upad, 2, 94
            p.op("dve", lambda e: e.tensor_tensor(out=Sa[:, :, 2:94], in0=Upad[:, :, 1:93], in1=Upad[:, :, 2:94], op=ALU.add),
                 reads=[Upad], writes=[Sa])
            cur, oth = Sa, Sb_
            sh = 1
            lo, hi = 2, 94
            for step in range(gi):
                nlo, nhi = lo + sh, hi - sh
                p.op("dve", lambda e, cur=cur, oth=oth, nlo=nlo, nhi=nhi, sh=sh: e.tensor_tensor(
                    out=oth[:, :, nlo:nhi], in0=cur[:, :, nlo - sh:nhi - sh], in1=cur[:, :, nlo + sh:nhi + sh], op=ALU.add),
                    reads=[cur], writes=[oth])
                cur, oth = oth, cur
                lo, hi = nlo, nhi
                sh *= 2
            p.op("dve", lambda e, cur=cur: e.tensor_scalar(out=dtmp[:], in0=cur[:, :, 16:80], scalar1=poolc[:, 256:257], scalar2=None, op0=ALU.mult),
                 reads=[cur, poolc], writes=[dtmp])
            p.op("dve", lambda e, cur=cur: e.scalar_tensor_tensor(out=dtmp[:], in0=cur[:, :, 17:81], scalar=poolc[:, 257:258], in1=dtmp[:],
                                                                  op0=ALU.mult, op1=ALU.add), reads=[cur, poolc, dtmp], writes=[dtmp])
            p.op("dve", lambda e, gi=gi: e.tensor_tensor(out=dtmp[:], in0=dtmp[:],
                                                        in1=poolc[:, gi * 64:(gi + 1) * 64].unsqueeze(1).to_broadcast([128, NR, 64]), op=ALU.mult),
                 reads=[dtmp, poolc], writes=[dtmp])
            p.op("dve", lambda e, cc=cc: e.tensor_tensor(out=dT[:, cc, :].rearrange("p (r c) -> p r c", c=64), in0=dtmp[:], in1=Upad[:, :, 16:80],
                                                        op=ALU.subtract), reads=[dtmp, Upad], writes=[(dT.name, cc)])
        for gi in range(4):
            for dc in range(2):
                ps = psb[(gi * 2 + dc) % 2]
                for kc in range(2):
                    p.op("pe", lambda e, gi=gi, dc=dc, kc=kc, ps=ps: e.matmul(ps[:, 0:GT], lhsT=poolw[:, gi, kc, dc * 128:(dc + 1) * 128],
                                                                            rhs=dT[:, gi * 2 + kc, :], start=(kc == 0), stop=(kc == 1)),
                         reads=[poolw, (dT.name, gi * 2 + kc)], writes=[ps])
                oc = gi * 2 + dc
                p.op("act", lambda e, oc=oc, ps=ps: e.activation(out=zT[:, oc, :], in_=ps[:, 0:GT], func=AF.Copy, scale=pools[:, oc:oc + 1]),
                     reads=[ps, pools], writes=[(zT.name, oc)])

    def merge_group(tg):
        t0 = tg * GT
        for blk in range(4):
            wp = wload(wpo_v[:, :, blk * 512:(blk + 1) * 512], 8)
            wr = wload(wro_v[:, :, blk * 512:(blk + 1) * 512], 16)
            for j in range(4):
                fc = blk * 4 + j
                p.dma("sp", sgt[0][:], k.sgT[fc * 128:(fc + 1) * 128, t0:t0 + GT], reads=[], writes=[sgt[0]])
                p.dma("sp", sgt[1][:], k.sgT[2048 + fc * 128:2048 + (fc + 1) * 128, t0:t0 + GT], reads=[], writes=[sgt[1]])
                ps = psb[2]
                for kc in range(8):
                    p.op("pe", lambda e, kc=kc, j=j, wp=wp, ps=ps: e.matmul(ps[:, 0:GT], lhsT=wp[:, kc, j * 128:(j + 1) * 128], rhs=zT[:, kc, :],
                                                                          start=(kc == 0), stop=(kc == 7)), reads=[(wp.name, 0), zT], writes=[ps])
                p.op("dve", lambda e, ps=ps: e.tensor_tensor(out=mrg[:], in0=sgt[0][:], in1=ps[:, 0:GT], op=ALU.mult), reads=[sgt[0], ps], writes=[mrg])
                ps2 = psb[3]
                for kc in range(16):
                    p.op("pe", lambda e, kc=kc, j=j, wr=wr, ps2=ps2: e.matmul(ps2[:, 0:GT], lhsT=wr[:, kc, j * 128:(j + 1) * 128], rhs=preT[:, kc, :],
                                                                            start=(kc == 0), stop=(kc == 15)),
                         reads=[(wr.name, kc // 8), preT], writes=[ps2])
                p.op("dve", lambda e, ps2=ps2: e.tensor_tensor(out=sgt[1][:], in0=sgt[1][:], in1=ps2[:, 0:GT], op=ALU.mult), reads=[sgt[1], ps2], writes=[sgt[1]])
                p.op("dve", lambda e, fc=fc: e.tensor_tensor(out=mT[:, fc, :], in0=mrg[:], in1=sgt[1][:], op=ALU.add), reads=[mrg, sgt[1]],
                     writes=[(mT.name, fc)])

    def outproj_group(tg):
        for cb in range(4):
            w = wload(wout_v[:, :, cb * 512:(cb + 1) * 512], 16)
            cs = slice(cb * 512, (cb + 1) * 512)
            for ts in range(NTS):
                ps = psb[4 + ts % 2]
                for kc in range(16):
                    p.op("pe", lambda e, kc=kc, w=w, ps=ps, ts=ts: e.matmul(ps[:, :], lhsT=mT[:, kc, ts * 128:(ts + 1) * 128], rhs=w[:, kc, :],
                                                                          start=(kc == 0), stop=(kc == 15)), reads=[(w.name, kc // 8), mT], writes=[ps])
                p.op("dve", lambda e, cs=cs, ps=ps, ts=ts: e.tensor_tensor(out=x1g[:, ts, cs], in0=ps[:, :], in1=k.rowt["gt1"][:, cs], op=ALU.mult),
                     reads=[ps, k.rowt["gt1"]], writes=[(x1g.name, ts)])
        for ts in range(NTS):
            outproj_tile(tg, ts)

    def outproj_tile(tg, ts):
            ti = tg * NTS + ts
            t0 = ti * 128
            p.dma("sp", yb[:], k.xown_d[t0:t0 + 128, :], reads=[], writes=[yb])
            p.op("dve", lambda e: e.tensor_tensor(out=x1[:], in0=x1g[:, ts, :], in1=yb[:], op=ALU.add), reads=[(x1g.name, ts), yb], writes=[x1])
            p.dma("sp", k.x1scr[t0:t0 + 128, :], x1[:], reads=[x1], writes=[])
            p.op("act", lambda e: e.activation(out=sq[:], in_=x1[:], func=AF.Square), reads=[x1], writes=[sq])
            p.op("dve", lambda e: e.tensor_reduce(out=ss[:, 0:1], in_=sq[:], axis=AX.X, op=ALU.add), reads=[sq], writes=[ss])
            p.op("act", lambda e: e.activation(out=ss[:, 1:2], in_=ss[:, 0:1], func=AF.Sqrt, scale=1.0 / D, bias=k.eps_t[:, 0:1]),
                 reads=[ss, k.eps_t], writes=[ss])
            p.op("dve", lambda e: e.reciprocal(out=ss[:, 1:2], in_=ss[:, 1:2]), reads=[ss], writes=[ss])
            p.op("dve", lambda e: e.scalar_tensor_tensor(out=sq[:], in0=x1[:], scalar=ss[:, 1:2], in1=k.rowt["A2"][:], op0=ALU.mult, op1=ALU.mult),
                 reads=[x1, ss, k.rowt["A2"]], writes=[sq])
            p.op("dve", lambda e: e.tensor_tensor(out=h2[:], in0=sq[:], in1=k.rowt["sh2"][:], op=ALU.add), reads=[sq, k.rowt["sh2"]], writes=[h2])
            p.dma("sp", k.h2tok[t0:t0 + 128, :], h2[:], reads=[h2], writes=[])
            for half in range(2):
                pt = pbt[half]
                for j in range(8):
                    kc = half * 8 + j
                    p.op("pe", lambda e, kc=kc, j=j, pt=pt: e.transpose(pt[:, j * 128:(j + 1) * 128], h2[:, kc * 128:(kc + 1) * 128], ident[:]),
                         reads=[h2, ident], writes=[pt])
                if half:
                    p.op("act", lambda e, pt=pt: e.activation(out=h2Ts[:, 8:16, :].rearrange("p a b -> p (a b)"), in_=pt[:], func=AF.Copy),
                         reads=[pt], writes=[(h2Ts.name, 1)])
                else:
                    p.op("dve", lambda e, pt=pt: e.tensor_copy(out=h2Ts[:, 0:8, :].rearrange("p a b -> p (a b)"), in_=pt[:]),
                         reads=[pt], writes=[(h2Ts.name, 0)])
            p.dma("sp", k.h2T.rearrange("(kc q) t -> q kc t", q=128)[:, :, t0:t0 + 128], h2Ts[:], reads=[h2Ts], writes=[])

    ng = getattr(k, "ngroupsD", None) or (2048 // GT)
    for tg in range(ng):
        for ts in range(NTS):
            readout_tile(tg, ts)
        pool_group(tg)
        merge_group(tg)
        outproj_group(tg)
    p.barrier()
    p.release(mD)


import numpy as np

CAP = 1024
NSUB = CAP // 512
NSLOT = 64 * CAP
D = 2048


def phaseE(k):
    p, nc = k.p, k.nc
    din, dscr = k.din, k.dscr
    k.routw_d = din("routw", [2048, 64])
    k.routb_d = din("routb", [1, 128])
    k.tri_d = din("tri", [128, 128])
    k.exg_d = din("exg", [64, 2048, 512])
    k.exu_d = din("exu", [64, 2048, 512])
    k.exd_d = din("exd", [64, 512, 2048])
    k.shg_d = din("shg", [2048, 512])
    k.shu_d = din("shu", [2048, 512])
    k.shd_d = din("shd", [512, 2048])
    k.out_d = nc.dram_tensor("out", [2048, 2048], F32, kind="ExternalOutput").ap()
    k.Xg = dscr("Xg", [NSLOT, 2048], BF16)
    k.Yg = dscr("Yg", [NSLOT, 2048], BF16)
    k.shscr = dscr("shscr", [2048, 2048], F32)
    k.routdbg = dscr("routdbg", [2048, 16], F32)
    psb, pbt = k.psb, k.pbt
    ident = k.ident

    slots_all = p.sb("slots_all", [128, 128], I32)
    wk_all = p.sb("wk_all", [128, 16, 8], F32)
    mE = p.mark()

    routw = p.sb("routw_sb", [128, 16, 64], BF16)
    routb = p.sb("routb_sb", [128, 128], F32)
    tri = p.sb("tri_sb", [128, 128], BF16)
    base = p.sb("base_cnt", [128, 64], F32)
    shg = p.sb("shg_sb", [128, 16, 512], BF16)
    shu = p.sb("shu_sb", [128, 16, 512], BF16)
    shd = p.sb("shd_sb", [128, 4, 2048], BF16)
    p.dma("pool", routw[:], k.routw_d.rearrange("(kc q) n -> q kc n", q=128))
    p.dma("sp", routb[:], k.routb_d.partition_broadcast(128))
    p.dma("pool", tri[:], k.tri_d)
    for h in range(2):
        p.dma("pool", shg[:, h * 8:(h + 1) * 8, :], k.shg_d.rearrange("(kc q) n -> q kc n", q=128)[:, h * 8:(h + 1) * 8, :], writes=[(shg.name, h)])
        p.dma("pool", shu[:, h * 8:(h + 1) * 8, :], k.shu_d.rearrange("(kc q) n -> q kc n", q=128)[:, h * 8:(h + 1) * 8, :], writes=[(shu.name, h)])
    p.dma("pool", shd[:], k.shd_d.rearrange("(kc q) n -> q kc n", q=128))
    p.op("dve", lambda e: e.memset(base[:], 0.0), writes=[base])
    h2Tg = p.sb("h2Tg", [128, 16, 512], BF16)
    h2r = p.sb("h2r", [128, 2048], BF16)
    sg = p.sb("sgE", [128, 512], F32)
    actT = p.sb("actT", [128, 4, 512], BF16)
    sho = p.sb("sho", [128, 2048], F32)
    R = {}
    for nm, w in (("sc", 64), ("sel", 64), ("tmp", 64), ("msel", 64), ("emask", 64), ("wd", 64), ("key", 64), ("oh", 64), ("rk", 64),
                  ("m1", 8), ("m2", 8), ("gs", 8), ("g8", 8), ("gm", 8), ("pen", 8), ("e8", 8), ("k8", 8), ("den", 2)):
        R[nm] = p.sb("R_" + nm, [128, w], F32)
    emb = p.sb("emask_bf", [128, 64], BF16)
    h2Tv = k.h2T.rearrange("(kc q) t -> q kc t", q=128)

    def shared_group(tg):
        t0 = tg * 512
        for h in range(2):
            p.dma("sp", h2Tg[:, h * 8:(h + 1) * 8, :], h2Tv[:, h * 8:(h + 1) * 8, t0:t0 + 512], reads=[], writes=[(h2Tg.name, h)])
        for dc in range(4):
            for kc in range(16):
                p.op("pe", lambda e, dc=dc, kc=kc: e.matmul(psb[0][:, :], lhsT=shg[:, kc, dc * 128:(dc + 1) * 128], rhs=h2Tg[:, kc, :],
                                                           start=(kc == 0), stop=(kc == 15)), reads=[shg, h2Tg], writes=[psb[0]])
            for kc in range(16):
                p.op("pe", lambda e, dc=dc, kc=kc: e.matmul(psb[1][:, :], lhsT=shu[:, kc, dc * 128:(dc + 1) * 128], rhs=h2Tg[:, kc, :],
                                                           start=(kc == 0), stop=(kc == 15)), reads=[shu, h2Tg], writes=[psb[1]])
            p.op("act", lambda e: e.activation(out=sg[:], in_=psb[0][:, :], func=AF.Silu), reads=[psb[0]], writes=[sg])
            p.op("dve", lambda e, dc=dc: e.tensor_tensor(out=actT[:, dc, :], in0=sg[:], in1=psb[1][:, :], op=ALU.mult), reads=[sg, psb[1]],
                 writes=[(actT.name, dc)])
        for ts in range(4):
            for cb in range(4):
                ps = psb[2 + cb % 2]
                for dc in range(4):
                    p.op("pe", lambda e, ts=ts, cb=cb, dc=dc, ps=ps: e.matmul(ps[:, :], lhsT=actT[:, dc, ts * 128:(ts + 1) * 128],
                                                                            rhs=shd[:, dc, cb * 512:(cb + 1) * 512], start=(dc == 0), stop=(dc == 3)),
                         reads=[actT, shd], writes=[ps])
                p.op("act" if cb % 2 else "dve",
                     (lambda e, cb=cb, ps=ps: e.activation(out=sho[:, cb * 512:(cb + 1) * 512], in_=ps[:, :], func=AF.Copy)) if cb % 2 else
                     (lambda e, cb=cb, ps=ps: e.tensor_copy(out=sho[:, cb * 512:(cb + 1) * 512], in_=ps[:, :])),
                     reads=[ps], writes=[(sho.name, cb)])
            p.dma("sp", k.shscr[t0 + ts * 128:t0 + (ts + 1) * 128, :], sho[:], reads=[sho], writes=[])
            route_tile(tg * 4 + ts, ts)

    def route_tile(ti, ts):
        t0 = ti * 128
        ps = psb[4]
        for kc in range(16):
            p.op("pe", lambda e, kc=kc: e.matmul(ps[:, 0:64], lhsT=h2Tg[:, kc, ts * 128:(ts + 1) * 128], rhs=routw[:, kc, :],
                                                start=(kc == 0), stop=(kc == 15)), reads=[h2Tg, routw], writes=[ps])
        sc, sel, tmp, msel, emask, wd, key, oh, rk = (R[n] for n in ("sc", "sel", "tmp", "msel", "emask", "wd", "key", "oh", "rk"))
        m1, m2, gs, g8, gm, pen, e8, k8, den = (R[n] for n in ("m1", "m2", "gs", "g8", "gm", "pen", "e8", "k8", "den"))
        v3 = lambda t: t[:].rearrange("p (g e) -> p g e", e=8)
        b3 = lambda t: t[:].unsqueeze(2).to_broadcast([128, 8, 8])
        p.op("act", lambda e: e.activation(out=sc[:], in_=ps[:, 0:64], func=AF.Sigmoid), reads=[ps], writes=[sc])
        p.op("dve", lambda e: e.tensor_tensor(out=sel[:], in0=sc[:], in1=routb[:, 0:64], op=ALU.add), reads=[sc, routb], writes=[sel])
        p.op("dve", lambda e: e.tensor_reduce(out=m1[:], in_=v3(sel), axis=AX.X, op=ALU.max), reads=[sel], writes=[m1])
        p.op("dve", lambda e: e.tensor_tensor(out=v3(tmp), in0=v3(sel), in1=b3(m1), op=ALU.is_equal), reads=[sel, m1], writes=[tmp])
        p.op("dve", lambda e: e.scalar_tensor_tensor(out=tmp[:], in0=tmp[:], scalar=-1e9, in1=sel[:], op0=ALU.mult, op1=ALU.add),
             reads=[tmp, sel], writes=[tmp])
        p.op("dve", lambda e: e.tensor_reduce(out=m2[:], in_=v3(tmp), axis=AX.X, op=ALU.max), reads=[tmp], writes=[m2])
        p.op("dve", lambda e: e.tensor_tensor(out=gs[:], in0=m1[:], in1=m2[:], op=ALU.add), reads=[m1, m2], writes=[gs])
        p.op("dve", lambda e: e.max(out=g8[:], in_=gs[:]), reads=[gs], writes=[g8])
        p.op("dve", lambda e: e.tensor_scalar(out=gm[:], in0=gs[:], scalar1=g8[:, 3:4], scalar2=None, op0=ALU.is_ge), reads=[gs, g8], writes=[gm])
        p.op("dve", lambda e: e.tensor_scalar(out=pen[:], in0=gm[:], scalar1=1e9, scalar2=-1e9, op0=ALU.mult, op1=ALU.add), reads=[gm], writes=[pen])
        p.op("dve", lambda e: e.tensor_tensor(out=v3(msel), in0=v3(sel), in1=b3(gm), op=ALU.mult), reads=[sel, gm], writes=[msel])
        p.op("dve", lambda e: e.tensor_tensor(out=v3(msel), in0=v3(msel), in1=b3(pen), op=ALU.add), reads=[msel, pen], writes=[msel])
        p.op("dve", lambda e: e.max(out=e8[:], in_=msel[:]), reads=[msel], writes=[e8])
        p.op("dve", lambda e: e.tensor_scalar(out=emask[:], in0=msel[:], scalar1=e8[:, 7:8], scalar2=None, op0=ALU.is_ge), reads=[msel, e8], writes=[emask])
        p.op("dve", lambda e: e.tensor_tensor(out=wd[:], in0=sc[:], in1=emask[:], op=ALU.mult), reads=[sc, emask], writes=[wd])
        p.op("dve", lambda e: e.tensor_reduce(out=den[:, 0:1], in_=wd[:], axis=AX.X, op=ALU.add), reads=[wd], writes=[den])
        p.op("dve", lambda e: e.reciprocal(out=den[:, 1:2], in_=den[:, 0:1]), reads=[den], writes=[den])
        p.op("dve", lambda e: e.tensor_scalar(out=wd[:], in0=wd[:], scalar1=den[:, 1:2], scalar2=2.5, op0=ALU.mult, op1=ALU.mult), reads=[wd, den], writes=[wd])
        p.op("act", lambda e: e.activation(out=emb[:], in_=emask[:], func=AF.Copy), reads=[emask], writes=[emb])
        ps2 = psb[5]
        p.op("pe", lambda e: e.matmul(ps2[:, 0:64], lhsT=tri[:], rhs=emb[:], start=True, stop=True), reads=[tri, emb], writes=[ps2])
        p.op("dve", lambda e: e.tensor_tensor(out=rk[:], in0=ps2[:, 0:64], in1=base[:], op=ALU.add), reads=[ps2, base], writes=[rk])
        p.op("dve", lambda e: e.tensor_scalar(out=oh[:], in0=rk[:], scalar1=float(CAP) - 0.5, scalar2=None, op0=ALU.is_lt), reads=[rk], writes=[oh])
        p.op("dve", lambda e: e.tensor_tensor(out=oh[:], in0=oh[:], in1=emask[:], op=ALU.mult), reads=[oh, emask], writes=[oh])
        p.op("dve", lambda e: e.tensor_tensor(out=key[:], in0=rk[:], in1=routb[:, 64:128], op=ALU.add), reads=[rk, routb], writes=[key])
        p.op("dve", lambda e: e.scalar_tensor_tensor(out=key[:], in0=key[:], scalar=1.0, in1=oh[:], op0=ALU.add, op1=ALU.mult), reads=[key, oh], writes=[key])
        p.op("dve", lambda e: e.tensor_scalar(out=key[:], in0=key[:], scalar1=-1.0, scalar2=None, op0=ALU.add), reads=[key], writes=[key])
        p.op("dve", lambda e: e.max(out=k8[:], in_=key[:]), reads=[key], writes=[k8])
        p.op("dve", lambda e: e.tensor_copy(out=slots_all[:, ti * 8:(ti + 1) * 8], in_=k8[:]), reads=[k8], writes=[(slots_all.name, ti)])
        for j in range(8):
            p.op("dve", lambda e, j=j: e.tensor_scalar(out=oh[:], in0=key[:], scalar1=k8[:, j:j + 1], scalar2=None, op0=ALU.is_equal),
                 reads=[key, k8], writes=[oh])
            p.op("dve", lambda e: e.tensor_tensor(out=oh[:], in0=oh[:], in1=wd[:], op=ALU.mult), reads=[oh, wd], writes=[oh])
            p.op("dve", lambda e, j=j: e.tensor_reduce(out=wk_all[:, ti, j:j + 1], in_=oh[:], axis=AX.X, op=ALU.add), reads=[oh],
                 writes=[(wk_all.name, ti)])
        p.op("pe", lambda e: e.matmul(ps2[:, 64:128], lhsT=k.ones_bf[:], rhs=emb[:], start=True, stop=True), reads=[k.ones_bf, emb], writes=[ps2])
        p.op("dve", lambda e: e.tensor_tensor(out=base[:], in0=base[:], in1=ps2[:, 64:128], op=ALU.add), reads=[base, ps2], writes=[base])
        p.dma("sp", h2r[:], k.h2tok[t0:t0 + 128, :], reads=[], writes=[h2r])
        for j in range(8):
            p.dma_fn("pool", lambda e, j=j: e.indirect_dma_start(
                out=k.Xg, out_offset=bass.IndirectOffsetOnAxis(ap=slots_all[:, ti * 8 + j:ti * 8 + j + 1], axis=0), in_=h2r[:], in_offset=None,
                bounds_check=p.getreg(e, NSLOT - 1), oob_is_err=False), reads=[h2r, (slots_all.name, ti)], writes=[])
        if "routdbg" in k.dbg:
            p.dma("sp", k.routdbg[t0:t0 + 128, 0:8], k8[:], reads=[k8], writes=[])
            p.dma("sp", k.routdbg[t0:t0 + 128, 8:16], wk_all[:, ti, :], reads=[(wk_all.name, ti)], writes=[])

    ngE = getattr(k, "ngroupsE", None) or 4
    for tg in range(ngE):
        shared_group(tg)
    p.barrier()
    p.release(mE)
    if getattr(k, "ecut", None) == 1:
        return

    NEXP = getattr(k, "nexp", None) or 64
    Wg = [p.sb("Wg%d" % i, [128, 16, 512], BF16) for i in range(2)]
    Wu = [p.sb("Wu%d" % i, [128, 16, 512], BF16) for i in range(2)]
    Wd = [p.sb("Wd%d" % i, [128, 4, 2048], BF16) for i in range(2)]
    Xs = p.sb("Xs", [128, 4, 2048], BF16)
    XT = p.sb("XT", [128, 16, 512], BF16)
    sg2 = p.sb("sg2", [128, 512], F32)
    act2 = p.sb("act2", [128, 4, 512], BF16)
    yst = [p.sb("yst%d" % i, [128, 2048], BF16) for i in range(2)]
    exg_v = k.exg_d.rearrange("e (kc q) n -> e q kc n", q=128)
    exu_v = k.exu_d.rearrange("e (kc q) n -> e q kc n", q=128)
    exd_v = k.exd_d.rearrange("e (kc q) n -> e q kc n", q=128)
    cnt = {"y": 0}

    def load_w(e_):
        b = e_ % 2
        for h in range(2):
            p.dma("pool", Wg[b][:, h * 8:(h + 1) * 8, :], exg_v[e_][:, h * 8:(h + 1) * 8, :], reads=[], writes=[(Wg[b].name, h)])
            p.dma("pool", Wu[b][:, h * 8:(h + 1) * 8, :], exu_v[e_][:, h * 8:(h + 1) * 8, :], reads=[], writes=[(Wu[b].name, h)])
        for h in range(2):
            p.dma("pool", Wd[b][:, h * 2:(h + 1) * 2, :], exd_v[e_][:, h * 2:(h + 1) * 2, :], reads=[], writes=[(Wd[b].name, h)])

    def expert(e_):
        for sub in range(NSUB):
            expert_sub(e_, sub)

    def expert_sub(e_, sub):
        b = e_ % 2
        r0 = e_ * CAP + sub * 512
        p.dma("sp", Xs[:], k.Xg[r0:r0 + 512, :].rearrange("(st q) f -> q st f", q=128), reads=[], writes=[Xs])
        for st in range(4):
            for half in range(2):
                pt = pbt[half]
                for j in range(8):
                    kc = half * 8 + j
                    p.op("pe", lambda e, st=st, kc=kc, j=j, pt=pt: e.transpose(pt[:, j * 128:(j + 1) * 128], Xs[:, st, kc * 128:(kc + 1) * 128], ident[:]),
                         reads=[Xs, ident], writes=[pt])
                if half:
                    p.op("act", lambda e, st=st, pt=pt: e.activation(out=XT[:, 8:16, st * 128:(st + 1) * 128],
                                                                    in_=pt[:].rearrange("p (a b) -> p a b", b=128), func=AF.Copy),
                         reads=[pt], writes=[(XT.name, st)])
                else:
                    p.op("dve", lambda e, st=st, pt=pt: e.tensor_copy(out=XT[:, 0:8, st * 128:(st + 1) * 128],
                                                                     in_=pt[:].rearrange("p (a b) -> p a b", b=128)),
                         reads=[pt], writes=[(XT.name, st)])
        for dc in range(4):
            for kc in range(16):
                p.op("pe", lambda e, dc=dc, kc=kc: e.matmul(psb[0][:, :], lhsT=Wg[b][:, kc, dc * 128:(dc + 1) * 128], rhs=XT[:, kc, :],
                                                           start=(kc == 0), stop=(kc == 15)), reads=[(Wg[b].name, kc // 8), XT], writes=[psb[0]])
            for kc in range(16):
                p.op("pe", lambda e, dc=dc, kc=kc: e.matmul(psb[1][:, :], lhsT=Wu[b][:, kc, dc * 128:(dc + 1) * 128], rhs=XT[:, kc, :],
                                                           start=(kc == 0), stop=(kc == 15)), reads=[(Wu[b].name, kc // 8), XT], writes=[psb[1]])
            p.op("act", lambda e: e.activation(out=sg2[:], in_=psb[0][:, :], func=AF.Silu), reads=[psb[0]], writes=[sg2])
            p.op("dve", lambda e, dc=dc: e.tensor_tensor(out=act2[:, dc, :], in0=sg2[:], in1=psb[1][:, :], op=ALU.mult), reads=[sg2, psb[1]],
                 writes=[(act2.name, dc)])
        for st in range(4):
            ys = yst[cnt["y"] % 2]
            cnt["y"] += 1
            for cb in range(4):
                ps = psb[2 + cb]
                for dc in range(4):
                    p.op("pe", lambda e, st=st, cb=cb, dc=dc, ps=ps: e.matmul(ps[:, :], lhsT=act2[:, dc, st * 128:(st + 1) * 128],
                                                                            rhs=Wd[b][:, dc, cb * 512:(cb + 1) * 512], start=(dc == 0), stop=(dc == 3)),
                         reads=[act2, (Wd[b].name, dc // 2)], writes=[ps])
                if cb % 2:
                    p.op("act", lambda e, cb=cb, ps=ps, ys=ys: e.activation(out=ys[:, cb * 512:(cb + 1) * 512], in_=ps[:, :], func=AF.Copy),
                         reads=[ps], writes=[(ys.name, cb)])
                else:
                    p.op("dve", lambda e, cb=cb, ps=ps, ys=ys: e.tensor_copy(out=ys[:, cb * 512:(cb + 1) * 512], in_=ps[:, :]),
                         reads=[ps], writes=[(ys.name, cb)])
            p.dma("sp", k.Yg[r0 + st * 128:r0 + (st + 1) * 128, :], ys[:], reads=[ys], writes=[])

    load_w(0)
    for e_ in range(NEXP):
        if e_ + 1 < NEXP:
            load_w(e_ + 1)
        expert(e_)
    p.barrier()
    p.release(mE)
    if getattr(k, "ecut", None) == 2:
        return

    rows = {}
    for i, nm in ((3, "gt2"),):
        rows[nm] = p.sb("rowE_" + nm, [128, 2048], F32)
        p.dma("sp", rows[nm][:], k.rowbuf[i:i + 1, :].partition_broadcast(128), reads=[], writes=[rows[nm]])
    rows["fing"] = p.sb("rowE_fing", [128, 2048], F32)
    p.dma("sp", rows["fing"][:], k.rows_d[2:3, :].partition_broadcast(128), reads=[], writes=[rows["fing"]])
    acc = p.sb("accE", [128, 2048], F32)
    yk = [p.sb("yk%d" % i, [128, 2048], BF16) for i in range(2)]
    x1t = p.sb("x1E", [128, 2048], F32)
    sqE = p.sb("sqE", [128, 2048], F32)
    ssE = p.sb("ssE", [128, 2], F32)

    def combine_tile(ti):
        t0 = ti * 128
        p.dma("sp", acc[:], k.shscr[t0:t0 + 128, :], reads=[], writes=[acc])
        p.dma("sp", x1t[:], k.x1scr[t0:t0 + 128, :], reads=[], writes=[x1t])
        for j in range(8):
            y = yk[j % 2]
            p.op("pool", lambda e, y=y: e.memset(y[:], 0.0), writes=[y])
            p.dma_fn("pool", lambda e, j=j, y=y: e.indirect_dma_start(
                out=y[:], out_offset=None, in_=k.Yg, in_offset=bass.IndirectOffsetOnAxis(ap=slots_all[:, ti * 8 + j:ti * 8 + j + 1], axis=0),
                bounds_check=p.getreg(e, NSLOT - 1), oob_is_err=False), reads=[y, slots_all], writes=[y])
            p.op("dve", lambda e, j=j, y=y: e.scalar_tensor_tensor(out=acc[:], in0=y[:], scalar=wk_all[:, ti, j:j + 1], in1=acc[:],
                                                                    op0=ALU.mult, op1=ALU.add), reads=[y, wk_all, acc], writes=[acc])
        p.op("dve", lambda e: e.tensor_tensor(out=acc[:], in0=acc[:], in1=rows["gt2"][:], op=ALU.mult), reads=[acc, rows["gt2"]], writes=[acc])
        p.op("dve", lambda e: e.tensor_tensor(out=x1t[:], in0=x1t[:], in1=acc[:], op=ALU.add), reads=[x1t, acc], writes=[x1t])
        p.op("act", lambda e: e.activation(out=sqE[:], in_=x1t[:], func=AF.Square), reads=[x1t], writes=[sqE])
        p.op("dve", lambda e: e.tensor_reduce(out=ssE[:, 0:1], in_=sqE[:], axis=AX.X, op=ALU.add), reads=[sqE], writes=[ssE])
        p.op("act", lambda e: e.activation(out=ssE[:, 1:2], in_=ssE[:, 0:1], func=AF.Sqrt, scale=1.0 / D, bias=k.eps_t[:, 0:1]),
             reads=[ssE, k.eps_t], writes=[ssE])
        p.op("dve", lambda e: e.reciprocal(out=ssE[:, 1:2], in_=ssE[:, 1:2]), reads=[ssE], writes=[ssE])
        p.op("dve", lambda e: e.scalar_tensor_tensor(out=sqE[:], in0=x1t[:], scalar=ssE[:, 1:2], in1=rows["fing"][:], op0=ALU.mult, op1=ALU.mult),
             reads=[x1t, ssE, rows["fing"]], writes=[sqE])
        p.dma("sp", k.out_d[t0:t0 + 128, :], sqE[:], reads=[sqE], writes=[], is_out=True)

    for ti in range(4 * ngE):
        combine_tile(ti)
    p.barrier()


import numpy as np

D = 2048


def fm(v, kc=16):
    return np.ascontiguousarray(v.reshape(kc, 128).T)


def prep_shared(inp):
    out = {}
    w_in = inp["w_in"][0]
    for half in (0, 1):
        u = w_in[:, 0:1024]
        slab = w_in[:, 1024:1024 + 6784]
        gates = w_in[:, 1024 + 6784:]
        r, kk_, v = slab[:, 0:2048], slab[:, 2048:4096], slab[:, 4096:6144]
        wd = [slab[:, 6144:6240], slab[:, 6240:6336]]
        ad = [slab[:, 6336:6432], slab[:, 6432:6528]]
        gd = slab[:, 6528:6784]
        dA, dB = (0, 1) if half == 0 else (1, 0)
        z32 = np.zeros((D, 32), np.float32)
        win = np.concatenate([u, r, kk_, v, wd[dA], z32, wd[dB], z32, ad[dA], z32, ad[dB], z32, gd, gates], axis=1)
        out[half] = {"win": np.ascontiguousarray(win)}
    return out


def prep_core(inp, shared, b, half):
    x = inp["x"][b]
    ctx = inp["ctx"][b]
    if half == 1:
        x = x[::-1]
        ctx = ctx[::-1]
    m = {}
    m["xT"] = np.ascontiguousarray(x.T)
    m["cxT"] = np.ascontiguousarray(ctx.T)
    c2 = np.stack([inp["c"][b], inp["c_ctx"]], axis=1)
    m["cT2"] = np.ascontiguousarray(c2.reshape(16, 128, 2).transpose(1, 0, 2))
    m["adaw"] = inp["ada_w"][0]
    m["adab"] = fm(inp["ada_b"][0], 96)
    m["g1"] = fm(inp["norm1_g"][0])
    m["win"] = shared[half]["win"]
    return m


def prep_core_b(inp, b, half, m):
    dA, dB = (0, 1) if half == 0 else (1, 0)
    mu = inp["shift_mu"][0]
    z32 = np.zeros(32, np.float32)
    secs = [mu[6144:6240], mu[6240:6336]]
    seca = [mu[6336:6432], mu[6432:6528]]
    mup = np.concatenate([mu[0:6144], secs[dA], z32, secs[dB], z32, seca[dA], z32, seca[dB], z32, mu[6528:6784]])
    cls = np.arange(6912) % 4
    valid = np.ones(6912, bool)
    for s0 in (6144, 6272, 6400, 6528):
        valid[s0 + 96:s0 + 128] = False
    def sel(mask):
        return np.where(mask & valid, mup, np.float32(0)).astype(np.float32)
    if half == 0:
        cm1, cp1, cm64, cp64 = sel(cls == 0), sel(cls == 1), sel(cls == 2), sel(cls == 3)
        ccm1, ccp1 = sel(cls % 2 == 0), sel(cls % 2 == 1)
    else:
        cm1, cp1, cm64, cp64 = sel(cls == 1), sel(cls == 0), sel(cls == 3), sel(cls == 2)
        ccm1, ccp1 = sel(cls % 2 == 1), sel(cls % 2 == 0)
    mupv = np.where(valid, mup, np.float32(0)).astype(np.float32)
    arrs = [mupv, cm1, cp1, cm64, cp64, ccm1, ccp1]
    m["mixc"] = np.ascontiguousarray(np.stack([a.reshape(54, 128).T for a in arrs], axis=1))
    w0, a0 = inp["decay_w0"][0], inp["iclr_a0"][0]
    vs = [w0[dA], w0[dB], a0[dA], a0[dB], inp["k_k"][0], inp["k_a"][0], inp["r_k"][0].reshape(-1)]
    m["vecs"] = np.ascontiguousarray(np.stack([fm(v) for v in vs], axis=1))
    w2, a2 = inp["decay_w2"][0], inp["iclr_a2"][0]
    m["lw2"] = np.ascontiguousarray(np.stack([w2[dA], w2[dB], a2[dA], a2[dB]], axis=1))
    pidx = np.arange(128)
    m["bones"] = (pidx[:, None] // 64 == pidx[None, :] // 64).astype(np.float32)
    m["hsel"] = (pidx[:, None] // 64 == np.arange(2)[None, :]).astype(np.float32)
    return m


def prep_core_c(m):
    t = np.arange(64)
    lt = (t[:, None] < t[None, :]).astype(np.float32)
    le = (t[:, None] <= t[None, :]).astype(np.float32)
    gt = (t[:, None] > t[None, :]).astype(np.float32)
    ge = (t[:, None] >= t[None, :]).astype(np.float32)
    mA = np.block([[lt, le], [lt, le]])
    mB = np.block([[gt, ge], [gt, ge]])
    m["cmask"] = np.ascontiguousarray(np.stack([mA, mB], axis=1).astype(np.float32))
    nA = lt.T.copy()
    nB = gt.T.copy()
    m["nmask"] = np.ascontiguousarray(np.stack([nA, nB, np.eye(64, dtype=np.float32)], axis=1).astype(np.float32))
    return m


def prep_core_de(inp, b, half, m, with_experts=True):
    x = inp["x"][b]
    if half == 1:
        x = x[::-1]
    m["xown"] = np.ascontiguousarray(x[:2048])
    m["rows"] = np.ascontiguousarray(np.stack([inp["lnx_w"][0], inp["lnx_b"][0], inp["final_g"]], axis=0))
    m["g2n"] = fm(inp["norm2_g"][0])
    m["g2w"] = inp["gate_g2"][0]
    m["poolw"] = inp["pool_w"][0]
    m["pools"] = fm(inp["pool_scale"][0], 8)
    m["wpo"] = inp["w_pool_out"][0]
    m["wro"] = inp["w_rwkv_out"][0]
    m["wout"] = inp["w_out"][0]
    t = np.arange(64)
    cnts = []
    for w in (2, 4, 8, 16):
        lo = np.clip(t - w // 2, 0, 64)
        hi = np.clip(t + (w - w // 2), 0, 64)
        c = (hi - lo).astype(np.float32)
        if half == 1:
            c = c[::-1]
        cnts.append(np.float32(1.0) / c)
    flags = np.array([1.0, 0.0] if half == 0 else [0.0, 1.0], np.float32)
    m["poolc"] = np.ascontiguousarray(np.concatenate(cnts + [flags]).astype(np.float32)[None, :])
    m["routw"] = inp["router_w"][0]
    ecap = (np.arange(64) * 1024).astype(np.float32)
    m["routb"] = np.ascontiguousarray(np.concatenate([inp["router_bias"][0], ecap]).astype(np.float32)[None, :])
    tt = np.arange(128)
    m["tri"] = (tt[:, None] < tt[None, :]).astype(np.float32)
    if with_experts:
        m["exg"] = inp["exp_w_gate"][0]
        m["exu"] = inp["exp_w_up"][0]
        m["exd"] = inp["exp_w_down"][0]
    m["shg"] = inp["shared_w_gate"][0]
    m["shu"] = inp["shared_w_up"][0]
    m["shd"] = inp["shared_w_down"][0]
    return m


from concourse.bass_utils import run_bass_kernel_spmd


def kernel(**inputs):
    inp = {k_: np.asarray(v) for k_, v in inputs.items()}
    shared = prep_shared(inp)
    maps = []
    for core in range(8):
        b, half = core // 2, core % 2
        m = prep_core(inp, shared, b, half)
        m = prep_core_b(inp, b, half, m)
        m = prep_core_c(m)
        m = prep_core_de(inp, b, half, m)
        maps.append(m)
    nc, k = build(stage=5)
    res = run_bass_kernel_spmd(nc, maps, core_ids=list(range(8)))
    out = np.empty((4, 4096, 2048), np.float32)
    for core in range(8):
        b, half = core // 2, core % 2
        o = np.asarray(res.results[core]["out"])
        if half == 0:
            out[b, :2048] = o
        else:
            out[b, 2048:] = o[::-1]
    return out
```

```python
import numpy as np
import concourse.bass as bass
import concourse.mybir as mybir
from contextlib import ExitStack

F32 = mybir.dt.float32
BF16 = mybir.dt.bfloat16
I32 = mybir.dt.int32
U32 = mybir.dt.uint32
ALU = mybir.AluOpType
AF = mybir.ActivationFunctionType
AX = mybir.AxisListType

ENGS = ("pe", "act", "dve", "pool", "sp")
ARENA_BASE = 16640
ARENA_TOP = 229344
DTSIZE = {F32: 4, BF16: 2, I32: 4, U32: 4}
NDSEM = 6


class _Op:
    __slots__ = ("eng", "fn", "deps", "signal", "sigval", "is_dma", "dsem", "dval", "dprev", "idx")

    def __init__(self, eng, fn, is_dma):
        self.eng = eng
        self.fn = fn
        self.deps = []
        self.signal = False
        self.sigval = 0
        self.is_dma = is_dma
        self.dsem = None
        self.dval = 0
        self.dprev = None


class Prog:
    def __init__(self, nc):
        self.nc = nc
        self.ops = []
        self.track = {}
        self.ndma = {e: 0 for e in ENGS}
        self.dma_ops = {e: [] for e in ENGS}
        self.es = ExitStack()
        self.out_dmas = []
        self.sp = ARENA_BASE
        self.sp_max = ARENA_BASE

    def sb(self, name, shape, dtype):
        nbytes = int(np.prod(shape[1:])) * DTSIZE[dtype]
        nbytes = (nbytes + 31) // 32 * 32
        off = self.sp
        self.sp += nbytes
        self.sp_max = max(self.sp_max, self.sp)
        assert self.sp <= ARENA_TOP, "SBUF arena overflow: %s needs %d at %d" % (name, nbytes, off)
        return self.nc.alloc_sbuf_tensor_at(name, list(shape), dtype, offset=off)

    def getreg(self, e, val):
        if not hasattr(self, "_regs"):
            self._regs = {}
        key = (id(e), val)
        if key not in self._regs:
            r = e.alloc_register("creg%d" % len(self._regs))
            e.reg_mov(r, val)
            self._regs[key] = r
        return self._regs[key]

    def mark(self):
        return self.sp

    def release(self, m):
        self.sp = m

    def ps(self, name, shape, dtype=F32):
        return self.es.enter_context(self.nc.psum_tensor(name, list(shape), dtype))

    @staticmethod
    def _key(k):
        if isinstance(k, tuple):
            return k[0], k[1]
        if isinstance(k, str):
            return k, None
        t = getattr(k, "tensor", k)
        return t.name, None

    def _conf(self, name, sub):
        d = self.track.setdefault(name, {})
        if sub is None:
            return list(d.keys())
        return [s for s in (sub, None) if s in d]

    def _record(self, op, reads, writes):
        r2, w2 = [], list(writes)
        for k_ in reads:
            nm = self._key(k_)[0]
            if nm.startswith("psb") or nm.startswith("pbt"):
                w2.append(k_)
            else:
                r2.append(k_)
        reads, writes = r2, w2
        deps = set()
        for k in reads:
            name, sub = self._key(k)
            d = self.track.setdefault(name, {})
            for s in self._conf(name, sub):
                w = d[s][0]
                if w is not None:
                    deps.add(w)
        for k in writes:
            name, sub = self._key(k)
            d = self.track.setdefault(name, {})
            for s in self._conf(name, sub):
                w, rs = d[s]
                if w is not None:
                    deps.add(w)
                for r in rs:
                    deps.add(r)
        for k in reads:
            name, sub = self._key(k)
            d = self.track[name]
            d.setdefault(sub, [None, []])[1].append(op)
        for k in writes:
            name, sub = self._key(k)
            d = self.track[name]
            if sub is None:
                d.clear()
            d[sub] = [op, []]
        deps.discard(op)
        for y in deps:
            if y.eng == "pe" and op.eng == "pe" and not y.is_dma:
                continue
            op.deps.append(y)
            if not y.is_dma:
                y.signal = True

    def op(self, eng, fn, reads=(), writes=(), after=()):
        o = _Op(eng, fn, False)
        o.idx = len(self.ops)
        self.ops.append(o)
        self._record(o, reads, writes)
        for y in after:
            o.deps.append(y)
            if not y.is_dma:
                y.signal = True
        return o

    def dma(self, eng, out, in_, reads=None, writes=None, is_out=False, **kw):
        if reads is None:
            reads = [in_]
        if writes is None:
            writes = [out]

        def fn(e, out=out, in_=in_, kw=kw):
            return e.dma_start(out=out, in_=in_, **kw)
        o = _Op(eng, fn, True)
        o.idx = len(self.ops)
        self.ops.append(o)
        self._record(o, reads, writes)
        n = self.ndma[eng]
        self.ndma[eng] += 1
        o.dsem = (eng, n % NDSEM)
        o.dval = 16 * (n // NDSEM + 1)
        if n >= NDSEM:
            o.dprev = self.dma_ops[eng][n - NDSEM]
        self.dma_ops[eng].append(o)
        if is_out:
            self.out_dmas.append(o)
        return o

    def dma_fn(self, eng, fn, reads, writes, is_out=False):
        o = _Op(eng, fn, True)
        o.idx = len(self.ops)
        self.ops.append(o)
        self._record(o, reads, writes)
        n = self.ndma[eng]
        self.ndma[eng] += 1
        o.dsem = (eng, n % NDSEM)
        o.dval = 16 * (n // NDSEM + 1)
        if n >= NDSEM:
            o.dprev = self.dma_ops[eng][n - NDSEM]
        self.dma_ops[eng].append(o)
        if is_out:
            self.out_dmas.append(o)
        return o

    def barrier(self):
        last = {}
        for o in self.ops:
            if not o.is_dma:
                last[o.eng] = o
        pend = [o for e in ENGS for o in self.dma_ops[e][-NDSEM:]]
        bops = []
        for e in ENGS:
            after = [o for o in last.values()] + pend
            bops.append((e, after))
        res = []
        for e, after in bops:
            res.append(self.op(e, lambda eng: eng.nop(), after=after))
        for o in res:
            o.signal = True
        self._barrier_ops = res
        self.track.clear()
        return res

    def emit(self):
        nc = self.nc
        if self.out_dmas:
            self.op("sp", lambda eng: eng.nop(), after=list(self.out_dmas))
        cnt = {e: 0 for e in ENGS}
        for o in self.ops:
            if o.is_dma:
                continue
            if o.signal:
                cnt[o.eng] += 1
                o.sigval = cnt[o.eng]
        es = self.es
        csem = {e: es.enter_context(nc.semaphore("c_" + e)) for e in ENGS}
        dsem = {}
        for e in ENGS:
            if self.ndma[e]:
                for i in range(min(NDSEM, self.ndma[e])):
                    dsem[(e, i)] = es.enter_context(nc.semaphore("d_%s%d" % (e, i)))
        per = {e: [o for o in self.ops if o.eng == e] for e in ENGS}
        block = es.enter_context(nc.Block())

        def run(eng_name, eng):
            waited = {}

            def wait(sem_key, sem, val):
                if waited.get(sem_key, 0) >= val:
                    return
                waited[sem_key] = val
                eng.wait_ge(sem, val)

            for o in per[eng_name]:
                for y in o.deps:
                    if y.is_dma:
                        wait(y.dsem, dsem[y.dsem], y.dval)
                    else:
                        wait(("c", y.eng), csem[y.eng], y.sigval)
                if o.is_dma:
                    if o.dprev is not None:
                        wait(o.dsem, dsem[o.dsem], o.dprev.dval)
                    ins = o.fn(eng)
                    ins.then_inc(dsem[o.dsem], 16)
                else:
                    ins = o.fn(eng)
                    if o.signal:
                        ins.then_inc(csem[eng_name], 1)

        @block.tensor
        def _(e):
            run("pe", e)

        @block.scalar
        def _(e):
            run("act", e)

        @block.vector
        def _(e):
            run("dve", e)

        @block.gpsimd
        def _(e):
            run("pool", e)

        @block.sync
        def _(e):
            run("sp", e)

    def close(self):
        self.es.close()


import numpy as np

D = 2048
NT = 4096
NOWN = 2048
NCTX = 256
KC = 16
NCH = 94
SLAB0 = 8
NSL = 54
DINP = NCH * 128
EPS = 1e-6
SQD = float(np.sqrt(2048.0))


class K:
    pass


def build(stage=99, dbg=(), ntiles=None, cut=None, skipA=False, nchunks=None, ccut=None, ecut=None, nexp=None):
    nc = bass.Bass("TRN2", target_bir_lowering=False)
    p = Prog(nc)
    k = K()
    k.nc, k.p = nc, p
    k.dbg = {}
    k.ntiles = ntiles
    k.cut = cut
    k.nchunks = nchunks
    k.ccut = ccut
    k.ecut = ecut
    k.nexp = nexp

    def din(name, shape, dt=F32):
        return nc.dram_tensor(name, list(shape), dt, kind="ExternalInput").ap()

    def dscr(name, shape, dt):
        if skipA and name in ("slabL", "slabC", "uT", "sgT"):
            return nc.dram_tensor(name, list(shape), dt, kind="ExternalInput").ap()
        if name in dbg:
            a = nc.dram_tensor(name, list(shape), dt, kind="ExternalOutput").ap()
            k.dbg[name] = a
            return a
        return nc.dram_tensor(name, list(shape), dt).ap()

    k.din, k.dscr = din, dscr
    if not skipA:
        k.xT = din("xT", [D, NT])
        k.cxT = din("cxT", [D, NCTX])
        k.cT2 = din("cT2", [128, KC, 2])
        k.adaw = din("adaw", [D, 6 * D])
        k.adab = din("adab", [128, 96])
        k.g1 = din("g1", [128, KC])
        k.win = din("win", [D, DINP])
    k.slabL = dscr("slabL", [NSL * 128, NT], BF16)
    k.slabC = dscr("slabC", [NSL * 128, NCTX], BF16)
    k.uT = dscr("uT", [1024, NOWN], BF16)
    k.sgT = dscr("sgT", [4096, NOWN], BF16)
    k.modd = dscr("modd", [128, 96, 2], F32)

    k.ones_bf = p.sb("ones_bf", [128, 128], BF16)
    p.op("pool", lambda e: e.memset(k.ones_bf[:], 1.0), writes=[k.ones_bf])
    k.eps_t = p.sb("eps_t", [128, 2], F32)
    p.op("pool", lambda e: e.memset(k.eps_t[:, 0:1], EPS), writes=[k.eps_t])
    p.op("pool", lambda e: e.memset(k.eps_t[:, 1:2], 1e-12), writes=[k.eps_t])
    k.mod = p.sb("mod", [128, 96, 2], F32)
    k.A1 = p.sb("A1", [128, KC, 2], F32)
    k.g1s = p.sb("g1s", [128, KC], F32)
    k.ident = p.sb("ident_bf", [128, 128], BF16)
    k.psb = [p.ps("psb%d" % i, [128, 512], F32) for i in range(6)]
    k.pbt = [p.ps("pbt%d" % i, [128, 1024], BF16) for i in range(2)]

    m0 = p.mark()
    if skipA:
        modin = din("modin", [128, 96, 2])
        p.dma("sp", k.mod[:], modin)
    if not skipA:
        phase0(k)
        p.barrier()
        p.release(m0)
        if stage >= 1:
            phaseA(k)
            p.release(m0)
    mB = p.mark()
    if stage >= 2:
        phaseB(k)
    if stage >= 3:
        mC = p.mark()
        phaseC(k)
        p.release(mC)
    if stage >= 4:
        phaseD(k)
    if stage >= 5:
        phaseE(k)
    p.emit()
    p.close()
    return nc, k


def phase0(k):
    p, nc = k.p, k.nc
    c32 = p.sb("c32", [128, KC, 2], F32)
    cs = p.sb("c_silu", [128, KC, 2], BF16)
    adab = p.sb("adab_sb", [128, 96], F32)
    p.dma("sp", c32[:], k.cT2)
    p.dma("sp", adab[:], k.adab)
    p.dma("sp", k.g1s[:], k.g1)
    p.op("act", lambda e: e.activation(out=cs[:], in_=c32[:], func=AF.Silu), reads=[c32], writes=[cs])
    NB = 16
    CW = 768
    wb = [p.sb("adaw_bf%d" % i, [128, KC, CW], BF16) for i in range(2)]
    src = k.adaw.rearrange("(kc p) n -> p kc n", p=128)
    ps = k.psb[0]
    for blk in range(NB):
        w = wb[blk % 2]
        for h in range(2):
            p.dma("pool", w[:, h * 8:(h + 1) * 8, :], src[:, h * 8:(h + 1) * 8, blk * CW:(blk + 1) * CW],
                  reads=[], writes=[(w.name, h)])
        for oc in range(6):
            g = blk * 6 + oc
            for kc in range(KC):
                p.op("pe", lambda e, w=w, oc=oc, kc=kc, g=g: e.matmul(
                    ps[:, 2 * g:2 * g + 2], lhsT=w[:, kc, oc * 128:(oc + 1) * 128], rhs=cs[:, kc, :],
                    start=(kc == 0), stop=(kc == KC - 1)),
                    reads=[(w.name, kc // 8), cs], writes=[ps])
    p.op("dve", lambda e: e.tensor_tensor(
        out=k.mod[:], in0=ps[:, 0:192].rearrange("p (g t) -> p g t", t=2),
        in1=adab[:].unsqueeze(2).to_broadcast([128, 96, 2]), op=ALU.add),
        reads=[ps, adab], writes=[k.mod])
    p.op("dve", lambda e: e.tensor_scalar(out=k.A1[:], in0=k.mod[:, 16:32, :], scalar1=1.0, scalar2=None,
                                          op0=ALU.add), reads=[k.mod], writes=[k.A1])
    p.op("dve", lambda e: e.tensor_tensor(out=k.A1[:], in0=k.A1[:],
                                          in1=k.g1s[:].unsqueeze(2).to_broadcast([128, KC, 2]), op=ALU.mult),
         reads=[k.A1, k.g1s], writes=[k.A1])
    if "modd" in k.dbg:
        p.dma("sp", k.modd, k.mod[:], is_out=True)


def phaseA(k):
    p, nc = k.p, k.nc
    hT = p.sb("hT", [128, KC, NOWN], BF16)
    xin = [p.sb("xin%d" % i, [128, KC, 256], F32) for i in range(2)]
    sq = p.sb("sq", [128, KC, 256], BF16)
    rstd = p.sb("rstd", [128, 256], F32)
    rms = p.sb("rms", [128, 256], F32)
    xn = p.sb("xn", [128, KC, 256], F32)
    wblk = [p.sb("wblk%d" % i, [128, KC, 256], BF16) for i in range(3)]
    stg = [p.sb("stgA%d" % i, [128, 512], BF16) for i in range(4)]
    winv = k.win.rearrange("(kc p) n -> p kc n", p=128)
    st = {"x": 0, "w": 0, "s": 0, "ps": 0}

    def norm_group(srcT, t0, ntok, which):
        v = srcT.rearrange("(kc p) t -> p kc t", p=128)
        for s in range(ntok // 256):
            xb = xin[st["x"] % 2]
            st["x"] += 1
            for h in range(2):
                p.dma("sp", xb[:, h * 8:(h + 1) * 8, :], v[:, h * 8:(h + 1) * 8, t0 + s * 256:t0 + (s + 1) * 256],
                      reads=[], writes=[(xb.name, h)])
            p.op("act", lambda e, xb=xb: e.activation(out=sq[:], in_=xb[:], func=AF.Square), reads=[xb], writes=[sq])
            ps = k.psb[5]
            for kc in range(KC):
                p.op("pe", lambda e, kc=kc: e.matmul(ps[:, 0:256], lhsT=k.ones_bf[:], rhs=sq[:, kc, :],
                                                      start=(kc == 0), stop=(kc == KC - 1)),
                     reads=[k.ones_bf, sq], writes=[ps])
            p.op("act", lambda e: e.activation(out=rms[:], in_=ps[:, 0:256], func=AF.Sqrt, scale=1.0 / D, bias=k.eps_t[:, 0:1]),
                 reads=[ps, k.eps_t], writes=[rms])
            p.op("dve", lambda e: e.reciprocal(out=rstd[:], in_=rms[:]), reads=[rms], writes=[rstd])
            p.op("dve", lambda e, xb=xb: e.tensor_tensor(out=xn[:], in0=xb[:],
                                                         in1=rstd[:].unsqueeze(1).to_broadcast([128, KC, 256]), op=ALU.mult),
                 reads=[xb, rstd], writes=[xn])
            for kc in range(KC):
                p.op("act", lambda e, kc=kc, s=s: e.activation(
                    out=hT[:, kc, s * 256:(s + 1) * 256], in_=xn[:, kc, :], func=AF.Identity,
                    scale=k.A1[:, kc, which:which + 1], bias=k.mod[:, kc, which:which + 1]),
                    reads=[xn, k.A1, k.mod], writes=[(hT.name, s)])

    def proj_group(ntok, chunks, sink):
        nsub = max(1, ntok // 512)
        w = min(512, ntok)
        for b0 in range(0, len(chunks), 2):
            wb = wblk[st["w"] % 3]
            st["w"] += 1
            c0 = chunks[b0]
            for h in range(2):
                p.dma("pool", wb[:, h * 8:(h + 1) * 8, :], winv[:, h * 8:(h + 1) * 8, c0 * 128:(c0 + 2) * 128],
                      reads=[], writes=[(wb.name, h)])
            for s in range(nsub):
                for j in range(2):
                    ch = chunks[b0 + j]
                    ps = k.psb[st["ps"] % 5]
                    st["ps"] += 1
                    for kc in range(KC):
                        p.op("pe", lambda e, wb=wb, j=j, kc=kc, s=s, ps=ps: e.matmul(
                            ps[:, 0:w], lhsT=wb[:, kc, j * 128:(j + 1) * 128], rhs=hT[:, kc, s * 512:s * 512 + w],
                            start=(kc == 0), stop=(kc == KC - 1)),
                            reads=[(wb.name, kc // 8), (hT.name, (s * 512) // 256), (hT.name, (s * 512 + w - 1) // 256)],
                            writes=[ps])
                    sink(ch, s, w, ps)

    def mk_sink(slab_dst, tok0):
        def sink(ch, s, w, ps):
            sg = stg[st["s"] % 4]
            st["s"] += 1
            eng = "act" if (st["s"] % 2) else "dve"
            if ch >= 62:
                p.op("act", lambda e, sg=sg, ps=ps: e.activation(out=sg[:, 0:w], in_=ps[:, 0:w], func=AF.Sigmoid),
                     reads=[ps], writes=[sg])
                dst = k.sgT[(ch - 62) * 128:(ch - 61) * 128, s * 512:s * 512 + w]
            else:
                if eng == "act":
                    p.op("act", lambda e, sg=sg, ps=ps: e.activation(out=sg[:, 0:w], in_=ps[:, 0:w], func=AF.Copy),
                         reads=[ps], writes=[sg])
                else:
                    p.op("dve", lambda e, sg=sg, ps=ps: e.tensor_copy(out=sg[:, 0:w], in_=ps[:, 0:w]),
                         reads=[ps], writes=[sg])
                if ch < 8:
                    dst = k.uT[ch * 128:(ch + 1) * 128, s * 512:s * 512 + w]
                else:
                    cc = ch - SLAB0
                    dst = slab_dst[cc * 128:(cc + 1) * 128, tok0 + s * 512:tok0 + s * 512 + w]
            p.dma("sp", dst, sg[:, 0:w], reads=[sg], writes=[])
        return sink

    slab_chunks = list(range(SLAB0, SLAB0 + NSL))
    norm_group(k.xT, 0, NOWN, 0)
    proj_group(NOWN, list(range(NCH)), mk_sink(k.slabL, 0))
    norm_group(k.xT, NOWN, NOWN, 0)
    proj_group(NOWN, slab_chunks, mk_sink(k.slabL, NOWN))
    norm_group(k.cxT, 0, NCTX, 1)
    proj_group(NCTX, slab_chunks, mk_sink(k.slabC, 0))
    p.barrier()
    for name in ("slabL", "slabC", "uT", "sgT"):
        if name in k.dbg:
            pass


import numpy as np

C0 = float(np.exp(-0.5))
NCHK = 68
TT = 256


class Cut(Exception):
    pass


def phaseB(k):
    try:
        _phaseB(k)
    except Cut:
        k.p.barrier()


def _phaseB(k):
    def cut(n):
        if getattr(k, "cut", None) == n:
            raise Cut()
    p, nc = k.p, k.nc
    din, dscr = k.din, k.dscr
    k.mixc_d = din("mixc", [128, 7, 54])
    k.vecs_d = din("vecs", [128, 7, 16])
    k.lw2_d = din("lw2", [96, 4, 2048])
    k.bones_d = din("bones", [128, 128])
    k.hsel_d = din("hsel", [128, 2])
    k.Q = [dscr("QA", [2048, NCHK * 256], BF16), dscr("QB", [2048, NCHK * 256], BF16)]
    k.Vtok = dscr("Vtok", [NCHK * 64, 2048], BF16)
    k.KpT = [dscr("KpTA", [NCHK * 64, 2048], BF16), dscr("KpTB", [NCHK * 64, 2048], BF16)]
    k.BpT = [dscr("BpTA", [NCHK * 64, 2048], BF16), dscr("BpTB", [NCHK * 64, 2048], BF16)]
    k.bcoef = dscr("bcoef", [2048, 32], F32)
    k.sgdT = dscr("sgdT", [256, 2048], BF16)
    k.wtot = [p.sb("wtotA", [128, 16, NCHK], F32), p.sb("wtotB", [128, 16, NCHK], F32)]

    markB = p.mark()
    mixc = p.sb("mixc_sb", [128, 7, 54], F32)
    omu = p.sb("omu", [128, 54], F32)
    vecs = p.sb("vecs_sb", [128, 7, 16], F32)
    oka = p.sb("oka", [128, 16], F32)
    lw2 = p.sb("lw2_sb", [96, 4, 2048], BF16)
    bones = p.sb("bones_sb", [128, 128], BF16)
    hsel = p.sb("hsel_sb", [128, 2], BF16)
    rmask = p.sb("rmask", [128, TT], F32)
    ident = k.ident
    p.dma("sp", mixc[:], k.mixc_d)
    p.dma("sp", vecs[:], k.vecs_d)
    for i in range(4):
        p.dma("pool", lw2[:, i, :], k.lw2_d[:, i, :], writes=[(lw2.name, i)])
    p.dma("pool", bones[:], k.bones_d)
    p.dma("pool", hsel[:], k.hsel_d)
    p.op("dve", lambda e: e.tensor_scalar(out=omu[:], in0=mixc[:, 0, :], scalar1=-1.0, scalar2=1.0, op0=ALU.mult, op1=ALU.add),
         reads=[mixc], writes=[omu])
    p.op("dve", lambda e: e.tensor_scalar(out=oka[:], in0=vecs[:, 5, :], scalar1=-1.0, scalar2=1.0, op0=ALU.mult, op1=ALU.add),
         reads=[vecs], writes=[oka])
    p.op("dve", lambda e: e.memset(rmask[:], 1.0), writes=[rmask])
    p.op("dve", lambda e: e.memset(rmask[:].rearrange("p (r c) -> p r c", c=64)[:, :, 0:1], 0.0), reads=[rmask], writes=[rmask])
    p.op("pool", lambda e: e.memset(ident[:], 1.0), writes=[ident])
    p.op("pool", lambda e: e.affine_select(out=ident[:], in_=ident[:], pattern=[[-1, 128]], compare_op=ALU.is_equal,
                                           fill=0.0, base=0, channel_multiplier=1), reads=[ident], writes=[ident])

    cut(1)
    NB = 2
    raw3 = [p.sb("raw3_%d" % i, [128, 3, 384], BF16) for i in range(NB)]
    rawl = [p.sb("rawl_%d" % i, [128, 384], BF16) for i in range(NB)]
    colsv = p.sb("colsv", [128, 4], BF16)
    M3 = [p.sb("M3_%d" % i, [128, 3, TT], F32) for i in range(NB)]
    ML = p.sb("ML", [128, TT], F32)
    tw = p.sb("tw", [128, 4, TT], BF16)
    sgd = p.sb("sgd", [128, 2, TT], BF16)
    f = {}
    for nm in ("lw", "aa", "cL", "X2", "X3", "X4", "e1", "e2", "e3", "e4", "kd", "bd", "ck"):
        f[nm] = [p.sb("f_%s%d" % (nm, i), [128, TT], F32) for i in range(2)]
    kkr = p.sb("kkr", [128, TT], F32)
    sqk = p.sb("sqk", [128, TT], BF16)
    rn = p.sb("rn", [128, TT], F32)
    kk = p.sb("kk", [128, TT], F32)
    vbf = p.sb("vbf", [128, TT], BF16)
    kpb = [p.sb("kpb%d" % i, [128, TT], BF16) for i in range(2)]
    bpb = [p.sb("bpb%d" % i, [128, TT], BF16) for i in range(2)]
    ksum = p.sb("ksum", [128, TT], F32)
    prod = p.sb("prodb", [128, TT], BF16)
    Qst = [[p.sb("Qst%d_%d" % (d, i), [128, 4, 4, 64], BF16) for i in range(2)] for d in range(2)]
    TM = {nm: p.sb("TM_" + nm, [128, 2, 2048], BF16) for nm in ("v", "kA", "bA", "kB", "bB")}
    bco = p.sb("bco", [128, 2, 32], F32)
    psL = [[k.psb[0], k.psb[1]], [k.psb[0], k.psb[1]]]
    psK = k.psb[2]
    psT = [(k.pbt[0], k.pbt[1]), (k.pbt[0], k.pbt[1])]
    psBon = k.psb[3]
    cnt = {"e": 0}

    def ew():
        cnt["e"] += 1
        return "pool" if cnt["e"] % 2 == 0 else "dve"

    def mix(eng, out, buf, cc, latent, key, okey):
        if latent:
            ctr = buf[:, 64:320]
            views = [(buf[:, 0:256], 3), (buf[:, 128:384], 4)]
        else:
            ctr = buf[:, 1:257]
            views = [(buf[:, 0:256], 5), (buf[:, 2:258], 6)]
        p.op(eng, lambda e: e.tensor_scalar(out=out, in0=ctr, scalar1=omu[:, cc:cc + 1], scalar2=None, op0=ALU.mult),
             reads=[key, omu], writes=[okey])
        for v, ci in views:
            p.op(eng, lambda e, v=v, ci=ci: e.scalar_tensor_tensor(out=out, in0=v, scalar=mixc[:, ci, cc:cc + 1], in1=out,
                                                                     op0=ALU.mult, op1=ALU.add),
                 reads=[key, mixc, okey], writes=[okey])
        if latent:
            c63 = buf[:, 63:256:64]
            c0 = buf[:, 128:321:64]
            p.op(eng, lambda e: e.tensor_copy(out=colsv[:], in_=c63), reads=[key], writes=[colsv])
            p.op(eng, lambda e: e.memset(c63, 0.0), reads=[key], writes=[key])
            p.op(eng, lambda e: e.scalar_tensor_tensor(out=out, in0=buf[:, 63:319], scalar=mixc[:, 1, cc:cc + 1], in1=out,
                                                       op0=ALU.mult, op1=ALU.add), reads=[key, mixc, okey], writes=[okey])
            p.op(eng, lambda e: e.tensor_copy(out=c63, in_=colsv[:]), reads=[colsv, key], writes=[key])
            p.op(eng, lambda e: e.memset(c0, 0.0), reads=[key], writes=[key])
            p.op(eng, lambda e: e.scalar_tensor_tensor(out=out, in0=buf[:, 65:321], scalar=mixc[:, 2, cc:cc + 1], in1=out,
                                                       op0=ALU.mult, op1=ALU.add), reads=[key, mixc, okey], writes=[okey])

    def load_raw(dst, src_rows, ti, latent, name):
        if latent:
            r0 = 4 * ti - 1
            lo, hi = max(r0, 0), min(r0 + 6, 64)
            if r0 < 0:
                p.op("pool", lambda e: e.memset(dst[:, :, 0:64], 0.0), writes=[name])
            if r0 + 6 > 64:
                p.op("pool", lambda e: e.memset(dst[:, :, 320:384], 0.0), writes=[name])
            p.dma("sp", dst[:, :, (lo - r0) * 64:(hi - r0) * 64], src_rows[:, :, lo * 64:hi * 64], reads=[], writes=[name])
        else:
            p.op("pool", lambda e: e.memset(dst[:, :, 0:1], 0.0), writes=[name])
            p.op("pool", lambda e: e.memset(dst[:, :, 257:258], 0.0), writes=[name])
            p.dma("sp", dst[:, :, 1:257], src_rows, reads=[], writes=[name])

    slabLv = k.slabL.rearrange("(cc p) t -> p cc t", p=128)
    slabCv = k.slabC.rearrange("(cc p) t -> p cc t", p=128)
    tiles = [("c", 0)] + [("l", ti) for ti in range(16)]
    if getattr(k, "ntiles", None):
        tiles = tiles[:k.ntiles]
    def do_tile(kind, ti, itbase):
        latent = kind == "l"
        own = latent and ti < 8
        src = slabLv if latent else slabCv
        chunk0 = 4 + 4 * ti if latent else 0
        tok0 = chunk0 * 64
        for j, cc in enumerate(range(48, 54)):
            if cc >= 52 and not own:
                continue
            rb = rawl[j % NB]
            load_raw(rb[:].unsqueeze(1), src[:, cc:cc + 1, :], ti, latent, rb.name)
            mix("dve", ML[:], rb[:], cc, latent, rb.name, ML.name)
            if j < 2:
                p.op("act", lambda e, j=j: e.activation(out=tw[:, j, :], in_=ML[:], func=AF.Tanh), reads=[ML], writes=[(tw.name, j)])
            elif j < 4:
                p.op("act", lambda e, j=j: e.activation(out=tw[:, j, :], in_=ML[:], func=AF.Copy), reads=[ML], writes=[(tw.name, j)])
            else:
                p.op("act", lambda e, j=j: e.activation(out=sgd[:, j - 4, :], in_=ML[:], func=AF.Sigmoid), reads=[ML], writes=[sgd])
                p.dma("sp", k.sgdT[(j - 4) * 128:(j - 3) * 128, ti * TT:(ti + 1) * TT], sgd[:, j - 4, :], reads=[sgd], writes=[])
        cut(2)
        def hp_body(hp, it):
            rb = raw3[it % NB]
            m3 = M3[it % NB]
            load_raw(rb[:], src[:, hp:hp + 33:16, :], ti, latent, rb.name)
            for j in range(3):
                mix("dve", m3[:, j, :], rb[:, j, :], hp + 16 * j, latent, (rb.name, j), (m3.name, j))
            cut(3)
            rM, kM, vM = m3[:, 0, :], m3[:, 1, :], m3[:, 2, :]
            ch = slice(hp * 128, (hp + 1) * 128)
            pl = psL[it % 2]
            for d in range(2):
                p.op("pe", lambda e, d=d, pl=pl: e.matmul(pl[0][:, d * TT:(d + 1) * TT], lhsT=lw2[:, d, ch], rhs=tw[0:96, d, :],
                                                          start=True, stop=True),
                     reads=[(lw2.name, d), (tw.name, d)], writes=[pl[0]])
            for d in range(2):
                p.op("pe", lambda e, d=d, pl=pl: e.matmul(pl[1][:, d * TT:(d + 1) * TT], lhsT=lw2[:, 2 + d, ch], rhs=tw[0:96, 2 + d, :],
                                                          start=True, stop=True),
                     reads=[(lw2.name, 2 + d), (tw.name, 2 + d)], writes=[pl[1]])
            for d in range(2):
                p.op("act", lambda e, d=d, pl=pl: e.activation(out=f["lw"][d][:], in_=pl[0][:, d * TT:(d + 1) * TT], func=AF.Sigmoid,
                                                               bias=vecs[:, d, hp:hp + 1]), reads=[pl[0], vecs], writes=[f["lw"][d]])
                p.op("act", lambda e, d=d, pl=pl: e.activation(out=f["aa"][d][:], in_=pl[1][:, d * TT:(d + 1) * TT], func=AF.Sigmoid,
                                                               bias=vecs[:, 2 + d, hp:hp + 1]), reads=[pl[1], vecs], writes=[f["aa"][d]])
            p.op("dve", lambda e: e.tensor_scalar(out=kkr[:], in0=kM, scalar1=vecs[:, 4, hp:hp + 1], scalar2=None, op0=ALU.mult),
                 reads=[(m3.name, 1), vecs], writes=[kkr])
            p.op("act", lambda e: e.activation(out=sqk[:], in_=kkr[:], func=AF.Square), reads=[kkr], writes=[sqk])
            p.op("pe", lambda e: e.matmul(psK[:, 0:TT], lhsT=bones[:], rhs=sqk[:], start=True, stop=True), reads=[bones, sqk], writes=[psK])
            p.op("act", lambda e: e.activation(out=rn[:], in_=psK[:, 0:TT], func=AF.Sqrt, bias=k.eps_t[:, 1:2]), reads=[psK, k.eps_t], writes=[rn])
            p.op("dve", lambda e: e.reciprocal(out=rn[:], in_=rn[:]), reads=[rn], writes=[rn])
            p.op(ew(), lambda e: e.tensor_tensor(out=kk[:], in0=kkr[:], in1=rn[:], op=ALU.mult), reads=[kkr, rn], writes=[kk])
            cut(4)
            p.op("act", lambda e: e.activation(out=vbf[:], in_=vM, func=AF.Copy), reads=[(m3.name, 2)], writes=[vbf])
            pt, pt2 = psT[it % 2]
            ptb = pt[:]
            ptb2 = pt2[:]
            for s in range(2):
                p.op("pe", lambda e, s=s, ptb=ptb: e.transpose(ptb[:, s * 128:(s + 1) * 128], vbf[:, s * 128:(s + 1) * 128], ident[:]),
                     reads=[vbf, ident], writes=[pt])
            cut(5)
            def d_body(d):
                lw, aa = f["lw"][d], f["aa"][d]
                cL, X2, X3, X4 = f["cL"][d], f["X2"][d], f["X3"][d], f["X4"][d]
                e1, e2, e3, e4 = f["e1"][d], f["e2"][d], f["e3"][d], f["e4"][d]
                kd, bd, ck = f["kd"][d], f["bd"][d], f["ck"][d]
                Q = Qst[d][it % 2]
                p.op("dve", lambda e, cL=cL, lw=lw: e.tensor_tensor_scan(out=cL[:], data0=rmask[:], data1=lw[:], initial=0.0,
                                                                          op0=ALU.mult, op1=ALU.add), reads=[rmask, lw], writes=[cL])
                tot = cL[:].rearrange("p (r c) -> p r c", c=64)[:, :, 63:64]
                p.op(ew(), lambda e, X2=X2, cL=cL, lw=lw: e.tensor_tensor(out=X2[:], in0=cL[:], in1=lw[:], op=ALU.subtract),
                     reads=[cL, lw], writes=[X2])
                p.op(ew(), lambda e, X3=X3, cL=cL, tot=tot: e.tensor_tensor(
                    out=X3[:].rearrange("p (r c) -> p r c", c=64), in0=tot.to_broadcast([128, 4, 64]),
                    in1=cL[:].rearrange("p (r c) -> p r c", c=64), op=ALU.subtract), reads=[cL], writes=[X3])
                p.op("act", lambda e, d=d, tot=tot: e.activation(out=k.wtot[d][:, hp, chunk0:chunk0 + 4].unsqueeze(2), in_=tot,
                                                                func=AF.Exp, scale=-C0), reads=[cL], writes=[(k.wtot[d].name, it)])
                if d == 0:
                    srcs = [(cL, -C0), (X2, -C0), (cL, C0), (X3, -C0)]
                else:
                    p.op(ew(), lambda e, X4=X4, X3=X3, lw=lw: e.tensor_tensor(out=X4[:], in0=X3[:], in1=lw[:], op=ALU.add),
                         reads=[X3, lw], writes=[X4])
                    srcs = [(X4, -C0), (X3, -C0), (X4, C0), (X2, -C0)]
                for (sx, sc), eo in zip(srcs, (e1, e2, e3, e4)):
                    p.op("act", lambda e, sx=sx, sc=sc, eo=eo: e.activation(out=eo[:], in_=sx[:], func=AF.Exp, scale=sc),
                         reads=[sx], writes=[eo])
                p.op("dve", lambda e, ck=ck, aa=aa: e.tensor_scalar(out=ck[:], in0=aa[:], scalar1=vecs[:, 5, hp:hp + 1], scalar2=oka[:, hp:hp + 1],
                                                                   op0=ALU.mult, op1=ALU.add), reads=[aa, vecs, oka], writes=[ck])
                p.op(ew(), lambda e, kd=kd, ck=ck: e.tensor_tensor(out=kd[:], in0=kM, in1=ck[:], op=ALU.mult), reads=[(m3.name, 1), ck], writes=[kd])
                p.op(ew(), lambda e, bd=bd, aa=aa: e.tensor_tensor(out=bd[:], in0=kk[:], in1=aa[:], op=ALU.mult), reads=[kk, aa], writes=[bd])
                Qv = lambda a: Q[:, :, a, :]
                r3 = lambda t: t.rearrange("p (r c) -> p r c", c=64)
                p.op(ew(), lambda e, e1=e1: e.tensor_tensor(out=Qv(3), in0=r3(rM), in1=r3(e1[:]), op=ALU.mult), reads=[(m3.name, 0), e1], writes=[Q])
                p.op(ew(), lambda e, e2=e2: e.tensor_tensor(out=Qv(2), in0=r3(kk[:]), in1=r3(e2[:]), op=ALU.mult), reads=[kk, e2], writes=[Q])
                p.op(ew(), lambda e, e3=e3, kd=kd: e.tensor_tensor(out=Qv(1), in0=r3(kd[:]), in1=r3(e3[:]), op=ALU.mult), reads=[kd, e3], writes=[Q])
                p.op(ew(), lambda e, e3=e3, bd=bd: e.tensor_tensor(out=Qv(0), in0=r3(bd[:]), in1=r3(e3[:]), op=ALU.mult), reads=[bd, e3], writes=[Q])
                p.dma("sp", k.Q[d][ch, chunk0 * 256:(chunk0 + 4) * 256], Q[:].rearrange("p a b c -> p (a b c)"), reads=[Q], writes=[])
                p.op(ew(), lambda e, e4=e4, kd=kd, d=d: e.tensor_tensor(out=kpb[d][:], in0=kd[:], in1=e4[:], op=ALU.mult), reads=[kd, e4], writes=[kpb[d]])
                p.op(ew(), lambda e, e4=e4, bd=bd, d=d: e.tensor_tensor(out=bpb[d][:], in0=bd[:], in1=e4[:], op=ALU.mult), reads=[bd, e4], writes=[bpb[d]])
                for s in range(2):
                    p.op("pe", lambda e, s=s, d=d, ptb=ptb: e.transpose(ptb[:, (2 + 4 * d + s) * 128:(3 + 4 * d + s) * 128],
                                                                        kpb[d][:, s * 128:(s + 1) * 128], ident[:]),
                         reads=[kpb[d], ident], writes=[pt])
                    if d == 0:
                        p.op("pe", lambda e, s=s, d=d, ptb=ptb: e.transpose(ptb[:, (4 + s) * 128:(5 + s) * 128],
                                                                            bpb[d][:, s * 128:(s + 1) * 128], ident[:]),
                             reads=[bpb[d], ident], writes=[pt])
                    else:
                        p.op("pe", lambda e, s=s, d=d, ptb2=ptb2: e.transpose(ptb2[:, s * 128:(s + 1) * 128],
                                                                              bpb[d][:, s * 128:(s + 1) * 128], ident[:]),
                             reads=[bpb[d], ident], writes=[pt2])
            for d_ in range(2):
                d_body(d_)
            cut(6)
            for bi, nm in enumerate(("v", "kA", "bA", "kB", "bB")):
                for s2 in range(2):
                    if bi < 4:
                        src_ps = ptb[:, (bi * 2 + s2) * 128:(bi * 2 + s2 + 1) * 128]
                        pkey = pt
                    else:
                        src_ps = ptb2[:, s2 * 128:(s2 + 1) * 128]
                        pkey = pt2
                    eng = "act" if (bi + s2) % 2 else "dve"
                    if getattr(k, "evac_dve", False):
                        eng = "dve"
                    if eng == "act":
                        p.op("act", lambda e, nm=nm, src_ps=src_ps, s2=s2: e.activation(out=TM[nm][:, s2, ch], in_=src_ps, func=AF.Copy),
                             reads=[pkey], writes=[(TM[nm].name, hp)])
                    else:
                        p.op("dve", lambda e, nm=nm, src_ps=src_ps, s2=s2: e.tensor_copy(out=TM[nm][:, s2, ch], in_=src_ps),
                             reads=[pkey], writes=[(TM[nm].name, hp)])
            cut(7)
            if own:
                p.op(ew(), lambda e: e.tensor_tensor(out=ksum[:], in0=f["kd"][0][:], in1=f["kd"][1][:], op=ALU.add),
                     reads=[f["kd"][0], f["kd"][1]], writes=[ksum])
                p.op("dve", lambda e: e.scalar_tensor_tensor(out=prod[:], in0=ksum[:], scalar=vecs[:, 6, hp:hp + 1], in1=rM,
                                                            op0=ALU.mult, op1=ALU.mult), reads=[ksum, vecs, (m3.name, 0)], writes=[prod])
                for s in range(2):
                    p.op("pe", lambda e, s=s: e.matmul(psBon[:, s * 32 + 2 * hp:s * 32 + 2 * hp + 2], lhsT=prod[:, s * 128:(s + 1) * 128],
                                                       rhs=hsel[:], start=True, stop=True), reads=[prod, hsel], writes=[psBon])
        for hp_ in range(16):
            hp_body(hp_, itbase + hp_ + 1)
        for nm, dst in (("v", k.Vtok), ("kA", k.KpT[0]), ("bA", k.BpT[0]), ("kB", k.KpT[1]), ("bB", k.BpT[1])):
            for s in range(2):
                p.dma("sp", dst[tok0 + s * 128:tok0 + (s + 1) * 128, :], TM[nm][:, s, :], reads=[TM[nm]], writes=[])
        if own:
            p.op("dve", lambda e: e.tensor_copy(out=bco[:], in_=psBon[:, 0:64].rearrange("p (s h) -> p s h", h=32)), reads=[psBon], writes=[bco])
            for s in range(2):
                p.dma("sp", k.bcoef[ti * TT + s * 128:ti * TT + (s + 1) * 128, :], bco[:, s, :], reads=[bco], writes=[])
    for tix, (kind_, ti_) in enumerate(tiles):
        do_tile(kind_, ti_, tix * 16)
    p.barrier()
    p.release(markB)


import numpy as np

NCHK = 68


def phaseC(k):
    p, nc = k.p, k.nc
    din, dscr = k.din, k.dscr
    k.cmask_d = din("cmask", [128, 2, 128])
    k.nmask_d = din("nmask", [64, 3, 64])
    k.Yscr = [dscr("YA", [2048, 2048], F32), dscr("YB", [2048, 2048], F32)]
    k.Sdbg = dscr("Sdbg", [2, 128, 16 * 64], F32)

    cmask = p.sb("cmask_sb", [128, 2, 128], F32)
    nmask = p.sb("nmask_sb", [64, 3, 64], F32)
    p.dma("sp", cmask[:], k.cmask_d)
    p.dma("sp", nmask[:], k.nmask_d)
    FM = [p.sb("FM%d" % i, [128, 16, 256], BF16) for i in range(2)]
    UV = [p.sb("UV%d" % i, [128, 2048], BF16) for i in range(2)]
    VZ = [p.sb("VZ%d" % i, [128, 2048], BF16) for i in range(2)]
    FMm = [p.sb("FMm%d" % i, [128, 16, 2, 128], BF16) for i in range(2)]
    for i in range(2):
        p.op("pool", lambda e, i=i: e.memset(VZ[i][:], 0.0), writes=[VZ[i]])
        p.op("pool", lambda e, i=i: e.memset(FMm[i][:], 0.0), writes=[FMm[i]])
    KB = [p.sb("KB%d" % i, [128, 2048], BF16) for i in range(2)]
    AT = p.sb("AT_sb", [128, 32, 128], BF16)
    Pm = [p.sb("Pm%d" % g, [128, 8, 64], BF16) for g in range(4)]
    PT = [p.sb("PTm%d" % g, [128, 8, 64], BF16) for g in range(4)]
    Gm = [p.sb("Gm%d" % g, [128, 8, 64], BF16) for g in range(4)]
    Qm = [p.sb("Qm%d" % g, [128, 8, 64], BF16) for g in range(4)]
    nrhs = [p.sb("nrhs%d" % g, [128, 8, 64], BF16) for g in range(4)]
    for g in range(4):
        for t_ in (Pm, PT, Gm, Qm, nrhs):
            p.op("pool", lambda e, t_=t_, g=g: e.memset(t_[g][:], 0.0), writes=[t_[g]])
    Ysb = [p.sb("Ysb%d" % i, [64, 2048], F32) for i in range(2)]
    S32 = p.sb("S32", [128, 16, 64], F32)
    Sbf = p.sb("Sbf", [128, 16, 64], BF16)
    Stmp = p.sb("Stmp", [128, 4, 64], F32)
    psAT = [k.psb[0], k.psb[1]]
    psP, psPT, psQ, psY = k.psb[2], k.psb[3], k.psb[4], k.psb[5]
    psP0, psPT0, psQ0 = psP, psPT, psQ
    ident = nmask[:, 2, :]
    Qv = [k.Q[d].rearrange("(hp p) x -> p hp x", p=128) for d in range(2)]
    cnt = {"e": 0, "ld": 0}

    def evac_eng():
        cnt["e"] += 1
        return "act" if cnt["e"] % 2 else "dve"

    def copy_evac(out, in_, reads, writes, scale=None):
        eng = evac_eng()
        if eng == "act":
            if scale is None:
                p.op("act", lambda e: e.activation(out=out, in_=in_, func=AF.Copy), reads=reads, writes=writes)
            else:
                p.op("act", lambda e: e.activation(out=out, in_=in_, func=AF.Copy, scale=scale), reads=reads, writes=writes)
        else:
            if scale is None:
                p.op("dve", lambda e: e.tensor_copy(out=out, in_=in_), reads=reads, writes=writes)
            else:
                p.op("dve", lambda e: e.tensor_scalar(out=out, in0=in_, scalar1=scale, scalar2=None, op0=ALU.mult), reads=reads, writes=writes)

    def load_chunk(d, c, buf):
        fm, uv, kb = FM[buf], UV[buf], KB[buf]
        vz, fmm = VZ[buf], FMm[buf]
        p.dma("sp", fm[:], Qv[d][:, :, c * 256:(c + 1) * 256], reads=[], writes=[fm])
        p.dma("sp", fmm[0:64, :, 0, :], Qv[d][0:64, :, c * 256 + 128:(c + 1) * 256], reads=[], writes=[(fmm.name, 0)])
        p.dma("sp", fmm[64:128, :, 1, :], Qv[d][64:128, :, c * 256 + 128:(c + 1) * 256], reads=[], writes=[(fmm.name, 1)])
        p.dma("sp", uv[64:128, :], k.Vtok[c * 64:(c + 1) * 64, :], reads=[], writes=[(uv.name, "V")])
        p.dma("sp", vz[64:128, :], k.Vtok[c * 64:(c + 1) * 64, :], reads=[], writes=[(vz.name, "V")])
        p.dma("sp", kb[0:64, :], k.BpT[d][c * 64:(c + 1) * 64, :], reads=[], writes=[(kb.name, 0)])
        p.dma("sp", kb[64:128, :], k.KpT[d][c * 64:(c + 1) * 64, :], reads=[], writes=[(kb.name, 1)])

    def do_chunk(d, c, buf, emit, ytok0):
        fm, uv, kb = FM[buf], UV[buf], KB[buf]
        vz, fmm = VZ[buf], FMm[buf]
        ysb = Ysb[buf]

        def fmv(h, a0, a1):
            return fm[:, h // 2, a0 * 64:a1 * 64]

        def fmk(h, a0, a1):
            return fmm[:, h // 2, h % 2, a0 * 64:a1 * 64]

        def sv(h):
            return Sbf[:, h // 2, :]

        def stage1(g):
            for hl in range(8):
                h = g * 8 + hl
                pa = psAT[hl // 4]
                p.op("pe", lambda e, h=h, hl=hl, pa=pa: e.matmul(pa[:, (hl % 4) * 128:(hl % 4 + 1) * 128], lhsT=fmv(h, 0, 2), rhs=fmk(h, 0, 2),
                                                               start=True, stop=True), reads=[fm, fmm], writes=[pa])
            for hl in range(8):
                h = g * 8 + hl
                p.op("pe", lambda e, h=h, hl=hl: e.matmul(psP[0:64, hl * 64:(hl + 1) * 64], lhsT=fmk(h, 0, 1), rhs=fmv(h, 0, 1),
                                                         start=True, stop=True), reads=[fm, fmm], writes=[psP])
            for half in range(2):
                pa = psAT[half]
                p.op("dve", lambda e, half=half, pa=pa: e.tensor_tensor(
                    out=AT[:, g * 8 + half * 4:g * 8 + half * 4 + 4, :], in0=pa[:].rearrange("p (h c) -> p h c", c=128),
                    in1=cmask[:, d, :].unsqueeze(1).to_broadcast([128, 4, 128]), op=ALU.mult), reads=[pa, cmask], writes=[(AT.name, g)])
            p.op("dve", lambda e: e.tensor_tensor(out=Pm[g][0:64, :, :], in0=psP[0:64, :].rearrange("p (h c) -> p h c", c=64),
                                                  in1=nmask[:, d, :].unsqueeze(1).to_broadcast([64, 8, 64]), op=ALU.mult),
                 reads=[psP, nmask], writes=[Pm[g]])
            p.op("dve", lambda e: e.tensor_tensor(out=Qm[g][0:64, :, :], in0=ident.unsqueeze(1).to_broadcast([64, 8, 64]),
                                                  in1=AT[0:64, g * 8:(g + 1) * 8, 0:64], op=ALU.subtract),
                 reads=[(AT.name, g), nmask], writes=[Qm[g]])

        def ptv(g, kk_, hl):
            if kk_ == 0:
                return AT[:, g * 8 + hl, 0:64]
            return PT[g][:, hl, :]

        def stage2(g, kk_):
            psP, psPT, psQ = (psP0, psPT0, psQ0) if g % 2 == 0 else (psAT[0], psAT[1], psY)
            for hl in range(8):
                p.op("pe", lambda e, hl=hl: e.matmul(psP[0:64, hl * 64:(hl + 1) * 64], lhsT=ptv(g, kk_ - 1, hl), rhs=Pm[g][:, hl, :],
                                                    start=True, stop=True),
                     reads=[Pm[g], PT[g], (AT.name, g)], writes=[psP])
            if kk_ < 5:
                for hl in range(8):
                    p.op("pe", lambda e, hl=hl: e.matmul(psPT[0:64, hl * 64:(hl + 1) * 64], lhsT=Pm[g][:, hl, :], rhs=ptv(g, kk_ - 1, hl),
                                                        start=True, stop=True),
                         reads=[Pm[g], PT[g], (AT.name, g)], writes=[psPT])
            p3 = psP[0:64, :].rearrange("p (h c) -> p h c", c=64)
            p.op("dve", lambda e: e.tensor_tensor(out=Gm[g][0:64, :, :], in0=p3, in1=ident.unsqueeze(1).to_broadcast([64, 8, 64]), op=ALU.add),
                 reads=[psP, nmask], writes=[Gm[g]])
            if kk_ < 5:
                p.op("act", lambda e: e.activation(out=Pm[g][0:64, :, :].rearrange("p h c -> p (h c)"), in_=psP[0:64, :], func=AF.Copy), reads=[psP], writes=[Pm[g]])
                copy_evac(PT[g][0:64, :, :].rearrange("p h c -> p (h c)"), psPT[0:64, :], [psPT], [PT[g]])
            for hl in range(8):
                p.op("pe", lambda e, hl=hl: e.matmul(psQ[0:64, hl * 64:(hl + 1) * 64], lhsT=Gm[g][:, hl, :], rhs=Qm[g][:, hl, :],
                                                    start=True, stop=True), reads=[Gm[g], Qm[g]], writes=[psQ])
            copy_evac(Qm[g][0:64, :, :].rearrange("p h c -> p (h c)"), psQ[0:64, :], [psQ], [Qm[g]])

        def stage3(g):
            for hl in range(8):
                h = g * 8 + hl
                p.op("pe", lambda e, h=h, hl=hl: e.matmul(psQ[0:64, hl * 64:(hl + 1) * 64], lhsT=fmk(h, 0, 1), rhs=sv(h),
                                                         start=True, stop=False), reads=[fmm, (Sbf.name, h // 8)], writes=[psQ])
                p.op("pe", lambda e, h=h, hl=hl: e.matmul(psQ[0:64, hl * 64:(hl + 1) * 64], lhsT=AT[:, h, 0:64],
                                                         rhs=vz[:, h * 64:(h + 1) * 64], start=False, stop=True),
                     reads=[(AT.name, g), vz], writes=[psQ])
            copy_evac(nrhs[g][0:64, :, :].rearrange("p h c -> p (h c)"), psQ[0:64, :], [psQ], [nrhs[g]], scale=-1.0)
            for hl in range(8):
                p.op("pe", lambda e, hl=hl: e.matmul(psPT[0:64, hl * 64:(hl + 1) * 64], lhsT=Qm[g][:, hl, :], rhs=nrhs[g][:, hl, :],
                                                    start=True, stop=True), reads=[Qm[g], nrhs[g]], writes=[psPT])
            copy_evac(uv[0:64, g * 512:(g + 1) * 512], psPT[0:64, :], [psPT], [(uv.name, ("U", g))])

        def stage5(g):
            for hl in range(8):
                h = g * 8 + hl
                p.op("pe", lambda e, h=h, hl=hl: e.matmul(psY[0:64, hl * 64:(hl + 1) * 64], lhsT=fmk(h, 1, 2), rhs=sv(h),
                                                         start=True, stop=False), reads=[fmm, (Sbf.name, h // 8)], writes=[psY])
                p.op("pe", lambda e, h=h, hl=hl: e.matmul(psY[0:64, hl * 64:(hl + 1) * 64], lhsT=AT[:, h, 64:128],
                                                         rhs=uv[:, h * 64:(h + 1) * 64], start=False, stop=True),
                     reads=[(AT.name, g), (uv.name, "V"), (uv.name, ("U", g))], writes=[psY])
            copy_evac(ysb[:, g * 512:(g + 1) * 512], psY[0:64, :], [psY], [(ysb.name, g)])

        def stage6(g):
            ps4 = psY[:].rearrange("p (a b c) -> p a b c", a=4, b=2)
            for hpl in range(4):
                hp = g * 4 + hpl
                h0, h1 = 2 * hp, 2 * hp + 1
                p.op("pe", lambda e, hpl=hpl, h0=h0: e.matmul(ps4[0:64, hpl, 0, :], lhsT=kb[:, h0 * 64:(h0 + 1) * 64], rhs=uv[:, h0 * 64:(h0 + 1) * 64],
                                                             start=True, stop=True),
                     reads=[kb, (uv.name, "V"), (uv.name, ("U", g))], writes=[psY])
                p.op("pe", lambda e, hpl=hpl, h0=h0, h1=h1: e.matmul(ps4[:, hpl, 1, :], lhsT=kb[:, h0 * 64:(h1 + 1) * 64], rhs=uv[:, h1 * 64:(h1 + 1) * 64],
                                                                    start=True, stop=True),
                     reads=[kb, (uv.name, "V"), (uv.name, ("U", g))], writes=[psY])
            for hh in range(2):
                rows = slice(hh * 64, (hh + 1) * 64)
                p.op("dve", lambda e, rows=rows: e.tensor_tensor(
                    out=Stmp[rows, :, :], in0=S32[rows, g * 4:(g + 1) * 4, :],
                    in1=k.wtot[d][rows, g * 4:(g + 1) * 4, c:c + 1].to_broadcast([64, 4, 64]), op=ALU.mult),
                    reads=[(S32.name, g), k.wtot[d]], writes=[(Stmp.name, hh)])
                p.op("dve", lambda e, rows=rows, hh=hh: e.tensor_tensor(
                    out=S32[rows, g * 4:(g + 1) * 4, :], in0=Stmp[rows, :, :], in1=ps4[rows, :, hh, :], op=ALU.add),
                    reads=[(Stmp.name, hh), psY], writes=[(S32.name, g)])
            p.op("act", lambda e: e.activation(out=Sbf[:].rearrange("p a b -> p (a b)")[:, g * 256:(g + 1) * 256],
                                               in_=S32[:].rearrange("p a b -> p (a b)")[:, g * 256:(g + 1) * 256], func=AF.Copy),
                 reads=[(S32.name, g)], writes=[(Sbf.name, g)])

        cc_ = getattr(k, "ccut", None) or 99
        for g in range(4):
            stage1(g)
        if cc_ <= 1:
            return
        for kk_ in range(1, 6):
            for g in range(4):
                stage2(g, kk_)
        if cc_ <= 2:
            return
        for g in range(4):
            stage3(g)
        if cc_ <= 3:
            return
        if emit:
            for g in range(4):
                stage5(g)
            p.dma("sp", k.Yscr[d][ytok0:ytok0 + 64, :], ysb[:], reads=[ysb], writes=[])
        if cc_ <= 5:
            return
        for g in range(4):
            stage6(g)

    ncap = getattr(k, "nchunks", None)
    for d in range(2):
        if d == 0:
            seq = [(c, False) for c in range(4)] + [(c, True) for c in range(4, 36)]
        else:
            seq = [(c, False) for c in range(3, -1, -1)] + [(c, False) for c in range(67, 35, -1)] + [(c, True) for c in range(35, 3, -1)]
        if ncap:
            seq = seq[:ncap]
        p.op("dve", lambda e: e.memset(S32[:], 0.0), writes=[S32])
        p.op("pool", lambda e: e.memset(Sbf[:], 0.0), writes=[Sbf])
        load_chunk(d, seq[0][0], cnt["ld"] % 2)
        for i, (c, emit) in enumerate(seq):
            buf = cnt["ld"] % 2
            cnt["ld"] += 1
            if i + 1 < len(seq):
                load_chunk(d, seq[i + 1][0], cnt["ld"] % 2)
            do_chunk(d, c, buf, emit, (c - 4) * 64)
        if "Sdbg" in k.dbg:
            p.dma("sp", k.Sdbg[d], S32[:].rearrange("p a b -> p (a b)"), reads=[S32], writes=[])
    p.barrier()


import numpy as np

GT = 256
NTS = 2
NR = 4
GN_EPS = 64e-5
NORM_EPS = 1e-6
D = 2048


def phaseD(k):
    p, nc = k.p, k.nc
    din, dscr = k.din, k.dscr
    k.xown_d = din("xown", [2048, 2048])
    k.rows_d = din("rows", [3, 2048])
    k.g2n_d = din("g2n", [128, 16])
    k.g2w_d = din("g2w", [256, 2048])
    k.poolw_d = din("poolw", [4, 256, 256])
    k.pools_d = din("pools", [128, 8])
    k.wpo_d = din("wpo", [1024, 2048])
    k.wro_d = din("wro", [2048, 2048])
    k.wout_d = din("wout", [2048, 2048])
    k.poolc_d = din("poolc", [1, 4 * 64 + 2])
    k.rowbuf = dscr("rowbuf", [4, 2048], F32)
    k.x1scr = dscr("x1scr", [2048, 2048], F32)
    k.h2T = dscr("h2Tscr", [2048, 2048], BF16)
    k.h2tok = dscr("h2tok", [2048, 2048], BF16)

    k.rowt = {}
    mD = p.mark()
    for i, nm in enumerate(("lnxw", "lnxb")):
        k.rowt[nm] = p.sb("row_" + nm, [128, 2048], F32)
        p.dma("sp", k.rowt[nm][:], k.rows_d[i:i + 1, :].partition_broadcast(128))
    g2n = p.sb("g2n_sb", [128, 16], F32)
    A2 = p.sb("A2_fm", [128, 16], F32)
    p.dma("sp", g2n[:], k.g2n_d)
    p.op("dve", lambda e: e.tensor_scalar(out=A2[:], in0=k.mod[:, 64:80, 0], scalar1=1.0, scalar2=None, op0=ALU.add), reads=[k.mod], writes=[A2])
    p.op("dve", lambda e: e.tensor_tensor(out=A2[:], in0=A2[:], in1=g2n[:], op=ALU.mult), reads=[A2, g2n], writes=[A2])
    rb = k.rowbuf.rearrange("r (oc q) -> r q oc", q=128)
    o1 = p.dma("sp", rb[0], k.mod[:, 32:48, 0], reads=[k.mod], writes=["rowbuf"], allow_slow_non_contiguous=True)
    o2 = p.dma("sp", rb[1], A2[:], reads=[A2], writes=["rowbuf"], allow_slow_non_contiguous=True)
    o3 = p.dma("sp", rb[2], k.mod[:, 48:64, 0], reads=[k.mod], writes=["rowbuf"], allow_slow_non_contiguous=True)
    o4 = p.dma("sp", rb[3], k.mod[:, 80:96, 0], reads=[k.mod], writes=["rowbuf"], allow_slow_non_contiguous=True)
    for i, nm in enumerate(("gt1", "A2", "sh2")):
        k.rowt[nm] = p.sb("row_" + nm, [128, 2048], F32)
        p.dma("sp", k.rowt[nm][:], k.rowbuf[i:i + 1, :].partition_broadcast(128), reads=["rowbuf"], writes=[k.rowt[nm]])

    g2w = p.sb("g2w_sb", [128, 2, 2048], BF16)
    poolw = p.sb("poolw_sb", [128, 4, 2, 256], BF16)
    pools = p.sb("pools_sb", [128, 8], F32)
    poolc = p.sb("poolc_sb", [128, 258], F32)
    gneps = p.sb("gneps", [128, 1], F32)
    p.dma("pool", g2w[:], k.g2w_d.rearrange("(kc q) n -> q kc n", q=128))
    p.dma("pool", poolw[:], k.poolw_d.rearrange("g (kc q) d -> q g kc d", q=128))
    p.dma("sp", pools[:], k.pools_d)
    p.dma("sp", poolc[:], k.poolc_d.partition_broadcast(128))
    p.op("pool", lambda e: e.memset(gneps[:], GN_EPS), writes=[gneps])
    ident = k.ident

    ya = p.sb("ya", [128, 2048], F32)
    yb = p.sb("yb", [128, 2048], F32)
    sq = p.sb("ysq", [128, 2048], F32)
    vt = p.sb("vt", [128, 2048], BF16)
    st1 = p.sb("st1", [128, 32], F32)
    st2 = p.sb("st2", [128, 32], F32)
    st3 = p.sb("st3", [128, 32], F32)
    bc = p.sb("bc", [128, 32], F32)
    sgdt = p.sb("sgdt", [128, 2, 128], BF16)
    prebf = p.sb("prebf", [128, 2048], BF16)
    preT = p.sb("preT", [128, 16, GT], BF16)
    Upad = p.sb("Upad", [128, NR, 96], F32)
    Sa = p.sb("Sa", [128, NR, 96], F32)
    Sb_ = p.sb("Sb_", [128, NR, 96], F32)
    dtmp = p.sb("dtmp", [128, NR, 64], F32)
    ubf = p.sb("ubf", [128, GT], BF16)
    dT = p.sb("dT", [128, 8, GT], BF16)
    zT = p.sb("zT", [128, 8, GT], BF16)
    sgt = [p.sb("sgt%d" % i, [128, GT], BF16) for i in range(2)]
    mrg = p.sb("mrg", [128, GT], F32)
    mT = p.sb("mT", [128, 16, GT], BF16)
    wblk = [p.sb("wblkD%d" % i, [128, 16, 512], BF16) for i in range(2)]
    x1g = p.sb("x1g", [128, NTS, 2048], BF16)
    x1 = p.sb("x1t", [128, 2048], F32)
    h2 = p.sb("h2t", [128, 2048], BF16)
    h2Ts = p.sb("h2Ts", [128, 16, 128], BF16)
    ss = p.sb("ssD", [128, 2], F32)
    psb = k.psb
    pbt = k.pbt
    cnt = {"w": 0}
    p.op("dve", lambda e: e.memset(Upad[:], 0.0), writes=[Upad])

    def wload(src_ap, nk):
        w = wblk[cnt["w"] % 2]
        cnt["w"] += 1
        for h in range(0, nk, 8):
            p.dma("pool", w[:, h:h + 8, :], src_ap[:, h:h + 8, :], reads=[], writes=[(w.name, h // 8)])
        return w

    wpo_v = k.wpo_d.rearrange("(kc q) n -> q kc n", q=128)
    wro_v = k.wro_d.rearrange("(kc q) n -> q kc n", q=128)
    wout_v = k.wout_d.rearrange("(kc q) n -> q kc n", q=128)

    def readout_tile(tg, ts):
        ti = tg * NTS + ts
        t0 = ti * 128
        p.dma("sp", ya[:], k.Yscr[0][t0:t0 + 128, :], reads=[], writes=[ya])
        p.dma("sp", yb[:], k.Yscr[1][t0:t0 + 128, :], reads=[], writes=[yb])
        p.dma("sp", vt[:], k.Vtok[256 + t0:256 + t0 + 128, :], reads=[], writes=[vt])
        p.dma("sp", bc[:], k.bcoef[t0:t0 + 128, :], reads=[], writes=[bc])
        p.dma("sp", sgdt[:], k.sgdT.rearrange("(kc q) t -> q kc t", q=128)[:, :, t0:t0 + 128], reads=[], writes=[sgdt])
        y3 = ya[:].rearrange("p (h c) -> p h c", c=64)
        p.op("dve", lambda e: e.tensor_tensor(out=ya[:], in0=ya[:], in1=yb[:], op=ALU.add), reads=[ya, yb], writes=[ya])
        p.op("dve", lambda e: e.tensor_reduce(out=st1[:], in_=y3, axis=AX.X, op=ALU.add), reads=[ya], writes=[st1])
        p.op("act", lambda e: e.activation(out=sq[:], in_=ya[:], func=AF.Square), reads=[ya], writes=[sq])
        p.op("dve", lambda e: e.tensor_reduce(out=st2[:], in_=sq[:].rearrange("p (h c) -> p h c", c=64), axis=AX.X, op=ALU.add),
             reads=[sq], writes=[st2])
        p.op("dve", lambda e: e.tensor_scalar(out=st1[:], in0=st1[:], scalar1=1.0 / 64, scalar2=None, op0=ALU.mult), reads=[st1], writes=[st1])
        p.op("dve", lambda e: e.tensor_tensor(out=st3[:], in0=st1[:], in1=st1[:], op=ALU.mult), reads=[st1], writes=[st3])
        p.op("dve", lambda e: e.scalar_tensor_tensor(out=st2[:], in0=st2[:], scalar=1.0 / 64, in1=st3[:], op0=ALU.mult, op1=ALU.subtract),
             reads=[st2, st3], writes=[st2])
        p.op("act", lambda e: e.activation(out=st2[:], in_=st2[:], func=AF.Sqrt, bias=gneps[:, 0:1]), reads=[st2, gneps], writes=[st2])
        p.op("dve", lambda e: e.reciprocal(out=st2[:], in_=st2[:]), reads=[st2], writes=[st2])
        p.op("dve", lambda e: e.tensor_tensor(out=y3, in0=y3, in1=st1[:].unsqueeze(2).to_broadcast([128, 32, 64]), op=ALU.subtract),
             reads=[ya, st1], writes=[ya])
        p.op("dve", lambda e: e.tensor_tensor(out=y3, in0=y3, in1=st2[:].unsqueeze(2).to_broadcast([128, 32, 64]), op=ALU.mult),
             reads=[ya, st2], writes=[ya])
        p.op("dve", lambda e: e.tensor_tensor(out=ya[:], in0=ya[:], in1=k.rowt["lnxw"][:], op=ALU.mult), reads=[ya, k.rowt["lnxw"]], writes=[ya])
        p.op("dve", lambda e: e.tensor_tensor(out=ya[:], in0=ya[:], in1=k.rowt["lnxb"][:], op=ALU.add), reads=[ya, k.rowt["lnxb"]], writes=[ya])
        p.op("dve", lambda e: e.tensor_tensor(out=yb[:].rearrange("p (h c) -> p h c", c=64), in0=vt[:].rearrange("p (h c) -> p h c", c=64),
                                              in1=bc[:].unsqueeze(2).to_broadcast([128, 32, 64]), op=ALU.mult), reads=[vt, bc], writes=[yb])
        p.op("dve", lambda e: e.tensor_tensor(out=ya[:], in0=ya[:], in1=yb[:], op=ALU.add), reads=[ya, yb], writes=[ya])
        for cb in range(4):
            ps = psb[cb % 2]
            for kc in range(2):
                p.op("pe", lambda e, cb=cb, kc=kc, ps=ps: e.matmul(ps[:, :], lhsT=sgdt[:, kc, :], rhs=g2w[:, kc, cb * 512:(cb + 1) * 512],
                                                                  start=(kc == 0), stop=(kc == 1)), reads=[sgdt, g2w], writes=[ps])
            p.op("dve", lambda e, cb=cb, ps=ps: e.tensor_tensor(out=prebf[:, cb * 512:(cb + 1) * 512], in0=ya[:, cb * 512:(cb + 1) * 512],
                                                                 in1=ps[:, :], op=ALU.mult), reads=[ya, ps], writes=[(prebf.name, cb)])
        for half in range(2):
            pt = pbt[half]
            for j in range(8):
                kc = half * 8 + j
                p.op("pe", lambda e, kc=kc, j=j, pt=pt: e.transpose(pt[:, j * 128:(j + 1) * 128], prebf[:, kc * 128:(kc + 1) * 128], ident[:]),
                     reads=[prebf, ident], writes=[pt])
            if half:
                p.op("act", lambda e, half=half, pt=pt: e.activation(out=preT[:, half * 8:(half + 1) * 8, ts * 128:(ts + 1) * 128],
                                                                    in_=pt[:].rearrange("p (a b) -> p a b", b=128), func=AF.Copy),
                     reads=[pt], writes=[(preT.name, ts)])
            else:
                p.op("dve", lambda e, half=half, pt=pt: e.tensor_copy(out=preT[:, half * 8:(half + 1) * 8, ts * 128:(ts + 1) * 128],
                                                                     in_=pt[:].rearrange("p (a b) -> p a b", b=128)),
                     reads=[pt], writes=[(preT.name, ts)])

    def pool_group(tg):
        t0 = tg * GT
        for cc in range(8):
            gi = cc // 2
            p.dma("sp", ubf[:], k.uT[cc * 128:(cc + 1) * 128, t0:t0 + GT], reads=[], writes=[ubf])
            p.op("act", lambda e: e.activation(out=Upad[:, :, 16:80], in_=ubf[:].rearrange("p (r c) -> p r c", c=64), func=AF.Copy),
                 reads=[ubf], writes=[Upad])
            src, lo, hi = Upad, 2, 94
            p.op("dve", lambda e: e.tensor_tensor(out=Sa[:, :, 2:94], in0=Upad[:, :, 1:93], in1=Upad[:, :, 2:94], op=ALU.add),
                 reads=[Upad], writes=[Sa])
            cur, oth = Sa, Sb_
            sh = 1
            lo, hi = 2, 94
            for step in range(gi):
                nlo, nhi = lo + sh, hi - sh
                p.op("dve", lambda e, cur=cur, oth=oth, nlo=nlo, nhi=nhi, sh=sh: e.tensor_tensor(
                    out=oth[:, :, nlo:nhi], in0=cur[:, :, nlo - sh:nhi - sh], in1=cur[:, :, nlo + sh:nhi + sh], op=ALU.add),
                    reads=[cur], writes=[oth])
                cur, oth = oth, cur
                lo, hi = nlo, nhi
                sh *= 2
            p.op("dve", lambda e, cur=cur: e.tensor_scalar(out=dtmp[:], in0=cur[:, :, 16:80], scalar1=poolc[:, 256:257], scalar2=None, op0=ALU.mult),
                 reads=[cur, poolc], writes=[dtmp])
            p.op("dve", lambda e, cur=cur: e.scalar_tensor_tensor(out=dtmp[:], in0=cur[:, :, 17:81], scalar=poolc[:, 257:258], in1=dtmp[:],
                                                                  op0=ALU.mult, op1=ALU.add), reads=[cur, poolc, dtmp], writes=[dtmp])
            p.op("dve", lambda e, gi=gi: e.tensor_tensor(out=dtmp[:], in0=dtmp[:],
                                                        in1=poolc[:, gi * 64:(gi + 1) * 64].unsqueeze(1).to_broadcast([128, NR, 64]), op=ALU.mult),
                 reads=[dtmp, poolc], writes=[dtmp])
            p.op("dve", lambda e, cc=cc: e.tensor_tensor(out=dT[:, cc, :].rearrange("p (r c) -> p r c", c=64), in0=dtmp[:], in1=Upad[:, :, 16:80],
                                                        op=ALU.subtract), reads=[dtmp, Upad], writes=[(dT.name, cc)])
        for gi in range(4):
            for dc in range(2):
                ps = psb[(gi * 2 + dc) % 2]
                for kc in range(2):
                    p.op("pe", lambda e, gi=gi, dc=dc, kc=kc, ps=ps: e.matmul(ps[:, 0:GT], lhsT=poolw[:, gi, kc, dc * 128:(dc + 1) * 128],
                                                                            rhs=dT[:, gi * 2 + kc, :], start=(kc == 0), stop=(kc == 1)),
                         reads=[poolw, (dT.name, gi * 2 + kc)], writes=[ps])
                oc = gi * 2 + dc
                p.op("act", lambda e, oc=oc, ps=ps: e.activation(out=zT[:, oc, :], in_=ps[:, 0:GT], func=AF.Copy, scale=pools[:, oc:oc + 1]),
                     reads=[ps, pools], writes=[(zT.name, oc)])

    def merge_group(tg):
        t0 = tg * GT
        for blk in range(4):
            wp = wload(wpo_v[:, :, blk * 512:(blk + 1) * 512], 8)
            wr = wload(wro_v[:, :, blk * 512:(blk + 1) * 512], 16)
            for j in range(4):
                fc = blk * 4 + j
                p.dma("sp", sgt[0][:], k.sgT[fc * 128:(fc + 1) * 128, t0:t0 + GT], reads=[], writes=[sgt[0]])
                p.dma("sp", sgt[1][:], k.sgT[2048 + fc * 128:2048 + (fc + 1) * 128, t0:t0 + GT], reads=[], writes=[sgt[1]])
                ps = psb[2]
                for kc in range(8):
                    p.op("pe", lambda e, kc=kc, j=j, wp=wp, ps=ps: e.matmul(ps[:, 0:GT], lhsT=wp[:, kc, j * 128:(j + 1) * 128], rhs=zT[:, kc, :],
                                                                          start=(kc == 0), stop=(kc == 7)), reads=[(wp.name, 0), zT], writes=[ps])
                p.op("dve", lambda e, ps=ps: e.tensor_tensor(out=mrg[:], in0=sgt[0][:], in1=ps[:, 0:GT], op=ALU.mult), reads=[sgt[0], ps], writes=[mrg])
                ps2 = psb[3]
                for kc in range(16):
                    p.op("pe", lambda e, kc=kc, j=j, wr=wr, ps2=ps2: e.matmul(ps2[:, 0:GT], lhsT=wr[:, kc, j * 128:(j + 1) * 128], rhs=preT[:, kc, :],
                                                                            start=(kc == 0), stop=(kc == 15)),
                         reads=[(wr.name, kc // 8), preT], writes=[ps2])
                p.op("dve", lambda e, ps2=ps2: e.tensor_tensor(out=sgt[1][:], in0=sgt[1][:], in1=ps2[:, 0:GT], op=ALU.mult), reads=[sgt[1], ps2], writes=[sgt[1]])
                p.op("dve", lambda e, fc=fc: e.tensor_tensor(out=mT[:, fc, :], in0=mrg[:], in1=sgt[1][:], op=ALU.add), reads=[mrg, sgt[1]],
                     writes=[(mT.name, fc)])

    def outproj_group(tg):
        for cb in range(4):
            w = wload(wout_v[:, :, cb * 512:(cb + 1) * 512], 16)
            cs = slice(cb * 512, (cb + 1) * 512)
            for ts in range(NTS):
                ps = psb[4 + ts % 2]
                for kc in range(16):
                    p.op("pe", lambda e, kc=kc, w=w, ps=ps, ts=ts: e.matmul(ps[:, :], lhsT=mT[:, kc, ts * 128:(ts + 1) * 128], rhs=w[:, kc, :],
                                                                          start=(kc == 0), stop=(kc == 15)), reads=[(w.name, kc // 8), mT], writes=[ps])
                p.op("dve", lambda e, cs=cs, ps=ps, ts=ts: e.tensor_tensor(out=x1g[:, ts, cs], in0=ps[:, :], in1=k.rowt["gt1"][:, cs], op=ALU.mult),
                     reads=[ps, k.rowt["gt1"]], writes=[(x1g.name, ts)])
        for ts in range(NTS):
            outproj_tile(tg, ts)

    def outproj_tile(tg, ts):
            ti = tg * NTS + ts
            t0 = ti * 128
            p.dma("sp", yb[:], k.xown_d[t0:t0 + 128, :], reads=[], writes=[yb])
            p.op("dve", lambda e: e.tensor_tensor(out=x1[:], in0=x1g[:, ts, :], in1=yb[:], op=ALU.add), reads=[(x1g.name, ts), yb], writes=[x1])
            p.dma("sp", k.x1scr[t0:t0 + 128, :], x1[:], reads=[x1], writes=[])
            p.op("act", lambda e: e.activation(out=sq[:], in_=x1[:], func=AF.Square), reads=[x1], writes=[sq])
            p.op("dve", lambda e: e.tensor_reduce(out=ss[:, 0:1], in_=sq[:], axis=AX.X, op=ALU.add), reads=[sq], writes=[ss])
            p.op("act", lambda e: e.activation(out=ss[:, 1:2], in_=ss[:, 0:1], func=AF.Sqrt, scale=1.0 / D, bias=k.eps_t[:, 0:1]),
                 reads=[ss, k.eps_t], writes=[ss])
            p.op("dve", lambda e: e.reciprocal(out=ss[:, 1:2], in_=ss[:, 1:2]), reads=[ss], writes=[ss])
            p.op("dve", lambda e: e.scalar_tensor_tensor(out=sq[:], in0=x1[:], scalar=ss[:, 1:2], in1=k.rowt["A2"][:], op0=ALU.mult, op1=ALU.mult),
                 reads=[x1, ss, k.rowt["A2"]], writes=[sq])
            p.op("dve", lambda e: e.tensor_tensor(out=h2[:], in0=sq[:], in1=k.rowt["sh2"][:], op=ALU.add), reads=[sq, k.rowt["sh2"]], writes=[h2])
            p.dma("sp", k.h2tok[t0:t0 + 128, :], h2[:], reads=[h2], writes=[])
            for half in range(2):
                pt = pbt[half]
                for j in range(8):
                    kc = half * 8 + j
                    p.op("pe", lambda e, kc=kc, j=j, pt=pt: e.transpose(pt[:, j * 128:(j + 1) * 128], h2[:, kc * 128:(kc + 1) * 128], ident[:]),
                         reads=[h2, ident], writes=[pt])
                if half:
                    p.op("act", lambda e, pt=pt: e.activation(out=h2Ts[:, 8:16, :].rearrange("p a b -> p (a b)"), in_=pt[:], func=AF.Copy),
                         reads=[pt], writes=[(h2Ts.name, 1)])
                else:
                    p.op("dve", lambda e, pt=pt: e.tensor_copy(out=h2Ts[:, 0:8, :].rearrange("p a b -> p (a b)"), in_=pt[:]),
                         reads=[pt], writes=[(h2Ts.name, 0)])
            p.dma("sp", k.h2T.rearrange("(kc q) t -> q kc t", q=128)[:, :, t0:t0 + 128], h2Ts[:], reads=[h2Ts], writes=[])

    ng = getattr(k, "ngroupsD", None) or (2048 // GT)
    for tg in range(ng):
        for ts in range(NTS):
            readout_tile(tg, ts)
        pool_group(tg)
        merge_group(tg)
        outproj_group(tg)
    p.barrier()
    p.release(mD)


import numpy as np

CAP = 768
SUBS = ((0, 512), (512, 256))
NSLOT = 64 * CAP
D = 2048


def phaseE(k):
    p, nc = k.p, k.nc
    din, dscr = k.din, k.dscr
    k.routw_d = din("routw", [2048, 64])
    k.routb_d = din("routb", [1, 128])
    k.tri_d = din("tri", [128, 128])
    k.exg_d = din("exg", [64, 2048, 512])
    k.exu_d = din("exu", [64, 2048, 512])
    k.exd_d = din("exd", [64, 512, 2048])
    k.shg_d = din("shg", [2048, 512])
    k.shu_d = din("shu", [2048, 512])
    k.shd_d = din("shd", [512, 2048])
    k.out_d = nc.dram_tensor("out", [2048, 2048], F32, kind="ExternalOutput").ap()
    k.Xg = dscr("Xg", [NSLOT, 2048], BF16)
    k.Yg = dscr("Yg", [NSLOT, 2048], BF16)
    k.shscr = dscr("shscr", [2048, 2048], F32)
    k.routdbg = dscr("routdbg", [2048, 16], F32)
    psb, pbt = k.psb, k.pbt
    ident = k.ident

    slots_all = p.sb("slots_all", [128, 128], I32)
    wk_all = p.sb("wk_all", [128, 16, 8], F32)
    mE = p.mark()

    routw = p.sb("routw_sb", [128, 16, 64], BF16)
    routb = p.sb("routb_sb", [128, 128], F32)
    tri = p.sb("tri_sb", [128, 128], BF16)
    base = p.sb("base_cnt", [128, 64], F32)
    shg = p.sb("shg_sb", [128, 16, 512], BF16)
    shu = p.sb("shu_sb", [128, 16, 512], BF16)
    shd = p.sb("shd_sb", [128, 4, 2048], BF16)
    p.dma("pool", routw[:], k.routw_d.rearrange("(kc q) n -> q kc n", q=128))
    p.dma("sp", routb[:], k.routb_d.partition_broadcast(128))
    p.dma("pool", tri[:], k.tri_d)
    for h in range(2):
        p.dma("pool", shg[:, h * 8:(h + 1) * 8, :], k.shg_d.rearrange("(kc q) n -> q kc n", q=128)[:, h * 8:(h + 1) * 8, :], writes=[(shg.name, h)])
        p.dma("pool", shu[:, h * 8:(h + 1) * 8, :], k.shu_d.rearrange("(kc q) n -> q kc n", q=128)[:, h * 8:(h + 1) * 8, :], writes=[(shu.name, h)])
    p.dma("pool", shd[:], k.shd_d.rearrange("(kc q) n -> q kc n", q=128))
    p.op("dve", lambda e: e.memset(base[:], 0.0), writes=[base])
    h2Tg = p.sb("h2Tg", [128, 16, 512], BF16)
    h2r = p.sb("h2r", [128, 2048], BF16)
    sg = p.sb("sgE", [128, 512], F32)
    actT = p.sb("actT", [128, 4, 512], BF16)
    sho = p.sb("sho", [128, 2048], F32)
    R = {}
    for nm, w in (("sc", 64), ("sel", 64), ("tmp", 64), ("msel", 64), ("emask", 64), ("wd", 64), ("key", 64), ("oh", 64), ("rk", 64),
                  ("m1", 8), ("m2", 8), ("gs", 8), ("g8", 8), ("gm", 8), ("pen", 8), ("e8", 8), ("k8", 8), ("den", 2)):
        R[nm] = p.sb("R_" + nm, [128, w], F32)
    emb = p.sb("emask_bf", [128, 64], BF16)
    h2Tv = k.h2T.rearrange("(kc q) t -> q kc t", q=128)

    def shared_group(tg):
        t0 = tg * 512
        for h in range(2):
            p.dma("sp", h2Tg[:, h * 8:(h + 1) * 8, :], h2Tv[:, h * 8:(h + 1) * 8, t0:t0 + 512], reads=[], writes=[(h2Tg.name, h)])
        for dc in range(4):
            for kc in range(16):
                p.op("pe", lambda e, dc=dc, kc=kc: e.matmul(psb[0][:, :], lhsT=shg[:, kc, dc * 128:(dc + 1) * 128], rhs=h2Tg[:, kc, :],
                                                           start=(kc == 0), stop=(kc == 15)), reads=[shg, h2Tg], writes=[psb[0]])
            for kc in range(16):
                p.op("pe", lambda e, dc=dc, kc=kc: e.matmul(psb[1][:, :], lhsT=shu[:, kc, dc * 128:(dc + 1) * 128], rhs=h2Tg[:, kc, :],
                                                           start=(kc == 0), stop=(kc == 15)), reads=[shu, h2Tg], writes=[psb[1]])
            p.op("act", lambda e: e.activation(out=sg[:], in_=psb[0][:, :], func=AF.Silu), reads=[psb[0]], writes=[sg])
            p.op("dve", lambda e, dc=dc: e.tensor_tensor(out=actT[:, dc, :], in0=sg[:], in1=psb[1][:, :], op=ALU.mult), reads=[sg, psb[1]],
                 writes=[(actT.name, dc)])
        for ts in range(4):
            for cb in range(4):
                ps = psb[2 + cb % 2]
                for dc in range(4):
                    p.op("pe", lambda e, ts=ts, cb=cb, dc=dc, ps=ps: e.matmul(ps[:, :], lhsT=actT[:, dc, ts * 128:(ts + 1) * 128],
                                                                            rhs=shd[:, dc, cb * 512:(cb + 1) * 512], start=(dc == 0), stop=(dc == 3)),
                         reads=[actT, shd], writes=[ps])
                p.op("act" if cb % 2 else "dve",
                     (lambda e, cb=cb, ps=ps: e.activation(out=sho[:, cb * 512:(cb + 1) * 512], in_=ps[:, :], func=AF.Copy)) if cb % 2 else
                     (lambda e, cb=cb, ps=ps: e.tensor_copy(out=sho[:, cb * 512:(cb + 1) * 512], in_=ps[:, :])),
                     reads=[ps], writes=[(sho.name, cb)])
            p.dma("sp", k.shscr[t0 + ts * 128:t0 + (ts + 1) * 128, :], sho[:], reads=[sho], writes=[])
            route_tile(tg * 4 + ts, ts)

    def route_tile(ti, ts):
        t0 = ti * 128
        ps = psb[4]
        for kc in range(16):
            p.op("pe", lambda e, kc=kc: e.matmul(ps[:, 0:64], lhsT=h2Tg[:, kc, ts * 128:(ts + 1) * 128], rhs=routw[:, kc, :],
                                                start=(kc == 0), stop=(kc == 15)), reads=[h2Tg, routw], writes=[ps])
        sc, sel, tmp, msel, emask, wd, key, oh, rk = (R[n] for n in ("sc", "sel", "tmp", "msel", "emask", "wd", "key", "oh", "rk"))
        m1, m2, gs, g8, gm, pen, e8, k8, den = (R[n] for n in ("m1", "m2", "gs", "g8", "gm", "pen", "e8", "k8", "den"))
        v3 = lambda t: t[:].rearrange("p (g e) -> p g e", e=8)
        b3 = lambda t: t[:].unsqueeze(2).to_broadcast([128, 8, 8])
        p.op("act", lambda e: e.activation(out=sc[:], in_=ps[:, 0:64], func=AF.Sigmoid), reads=[ps], writes=[sc])
        p.op("dve", lambda e: e.tensor_tensor(out=sel[:], in0=sc[:], in1=routb[:, 0:64], op=ALU.add), reads=[sc, routb], writes=[sel])
        p.op("dve", lambda e: e.tensor_reduce(out=m1[:], in_=v3(sel), axis=AX.X, op=ALU.max), reads=[sel], writes=[m1])
        p.op("dve", lambda e: e.tensor_tensor(out=v3(tmp), in0=v3(sel), in1=b3(m1), op=ALU.is_equal), reads=[sel, m1], writes=[tmp])
        p.op("dve", lambda e: e.scalar_tensor_tensor(out=tmp[:], in0=tmp[:], scalar=-1e9, in1=sel[:], op0=ALU.mult, op1=ALU.add),
             reads=[tmp, sel], writes=[tmp])
        p.op("dve", lambda e: e.tensor_reduce(out=m2[:], in_=v3(tmp), axis=AX.X, op=ALU.max), reads=[tmp], writes=[m2])
        p.op("dve", lambda e: e.tensor_tensor(out=gs[:], in0=m1[:], in1=m2[:], op=ALU.add), reads=[m1, m2], writes=[gs])
        p.op("dve", lambda e: e.max(out=g8[:], in_=gs[:]), reads=[gs], writes=[g8])
        p.op("dve", lambda e: e.tensor_scalar(out=gm[:], in0=gs[:], scalar1=g8[:, 3:4], scalar2=None, op0=ALU.is_ge), reads=[gs, g8], writes=[gm])
        p.op("dve", lambda e: e.tensor_scalar(out=pen[:], in0=gm[:], scalar1=1e9, scalar2=-1e9, op0=ALU.mult, op1=ALU.add), reads=[gm], writes=[pen])
        p.op("dve", lambda e: e.tensor_tensor(out=v3(msel), in0=v3(sel), in1=b3(gm), op=ALU.mult), reads=[sel, gm], writes=[msel])
        p.op("dve", lambda e: e.tensor_tensor(out=v3(msel), in0=v3(msel), in1=b3(pen), op=ALU.add), reads=[msel, pen], writes=[msel])
        p.op("dve", lambda e: e.max(out=e8[:], in_=msel[:]), reads=[msel], writes=[e8])
        p.op("dve", lambda e: e.tensor_scalar(out=emask[:], in0=msel[:], scalar1=e8[:, 7:8], scalar2=None, op0=ALU.is_ge), reads=[msel, e8], writes=[emask])
        p.op("dve", lambda e: e.tensor_tensor(out=wd[:], in0=sc[:], in1=emask[:], op=ALU.mult), reads=[sc, emask], writes=[wd])
        p.op("dve", lambda e: e.tensor_reduce(out=den[:, 0:1], in_=wd[:], axis=AX.X, op=ALU.add), reads=[wd], writes=[den])
        p.op("dve", lambda e: e.reciprocal(out=den[:, 1:2], in_=den[:, 0:1]), reads=[den], writes=[den])
        p.op("dve", lambda e: e.tensor_scalar(out=wd[:], in0=wd[:], scalar1=den[:, 1:2], scalar2=2.5, op0=ALU.mult, op1=ALU.mult), reads=[wd, den], writes=[wd])
        p.op("act", lambda e: e.activation(out=emb[:], in_=emask[:], func=AF.Copy), reads=[emask], writes=[emb])
        ps2 = psb[5]
        p.op("pe", lambda e: e.matmul(ps2[:, 0:64], lhsT=tri[:], rhs=emb[:], start=True, stop=True), reads=[tri, emb], writes=[ps2])
        p.op("dve", lambda e: e.tensor_tensor(out=rk[:], in0=ps2[:, 0:64], in1=base[:], op=ALU.add), reads=[ps2, base], writes=[rk])
        p.op("dve", lambda e: e.tensor_scalar(out=oh[:], in0=rk[:], scalar1=float(CAP) - 0.5, scalar2=None, op0=ALU.is_lt), reads=[rk], writes=[oh])
        p.op("dve", lambda e: e.tensor_tensor(out=oh[:], in0=oh[:], in1=emask[:], op=ALU.mult), reads=[oh, emask], writes=[oh])
        p.op("dve", lambda e: e.tensor_tensor(out=key[:], in0=rk[:], in1=routb[:, 64:128], op=ALU.add), reads=[rk, routb], writes=[key])
        p.op("dve", lambda e: e.scalar_tensor_tensor(out=key[:], in0=key[:], scalar=1.0, in1=oh[:], op0=ALU.add, op1=ALU.mult), reads=[key, oh], writes=[key])
        p.op("dve", lambda e: e.tensor_scalar(out=key[:], in0=key[:], scalar1=-1.0, scalar2=None, op0=ALU.add), reads=[key], writes=[key])
        p.op("dve", lambda e: e.max(out=k8[:], in_=key[:]), reads=[key], writes=[k8])
        p.op("dve", lambda e: e.tensor_copy(out=slots_all[:, ti * 8:(ti + 1) * 8], in_=k8[:]), reads=[k8], writes=[(slots_all.name, ti)])
        for j in range(8):
            p.op("dve", lambda e, j=j: e.tensor_scalar(out=oh[:], in0=key[:], scalar1=k8[:, j:j + 1], scalar2=None, op0=ALU.is_equal),
                 reads=[key, k8], writes=[oh])
            p.op("dve", lambda e: e.tensor_tensor(out=oh[:], in0=oh[:], in1=wd[:], op=ALU.mult), reads=[oh, wd], writes=[oh])
            p.op("dve", lambda e, j=j: e.tensor_reduce(out=wk_all[:, ti, j:j + 1], in_=oh[:], axis=AX.X, op=ALU.add), reads=[oh],
                 writes=[(wk_all.name, ti)])
        p.op("pe", lambda e: e.matmul(ps2[:, 64:128], lhsT=k.ones_bf[:], rhs=emb[:], start=True, stop=True), reads=[k.ones_bf, emb], writes=[ps2])
        p.op("dve", lambda e: e.tensor_tensor(out=base[:], in0=base[:], in1=ps2[:, 64:128], op=ALU.add), reads=[base, ps2], writes=[base])
        p.dma("sp", h2r[:], k.h2tok[t0:t0 + 128, :], reads=[], writes=[h2r])
        for j in range(8):
            p.dma_fn("pool", lambda e, j=j: e.indirect_dma_start(
                out=k.Xg, out_offset=bass.IndirectOffsetOnAxis(ap=slots_all[:, ti * 8 + j:ti * 8 + j + 1], axis=0), in_=h2r[:], in_offset=None,
                bounds_check=p.getreg(e, NSLOT - 1), oob_is_err=False), reads=[h2r, (slots_all.name, ti)], writes=[])
        if "routdbg" in k.dbg:
            p.dma("sp", k.routdbg[t0:t0 + 128, 0:8], k8[:], reads=[k8], writes=[])
            p.dma("sp", k.routdbg[t0:t0 + 128, 8:16], wk_all[:, ti, :], reads=[(wk_all.name, ti)], writes=[])

    ngE = getattr(k, "ngroupsE", None) or 4
    for tg in range(ngE):
        shared_group(tg)
    p.barrier()
    p.release(mE)
    if getattr(k, "ecut", None) == 1:
        return

    NEXP = getattr(k, "nexp", None) or 64
    Wg = [p.sb("Wg%d" % i, [128, 16, 512], BF16) for i in range(2)]
    Wu = [p.sb("Wu%d" % i, [128, 16, 512], BF16) for i in range(2)]
    Wd = [p.sb("Wd%d" % i, [128, 4, 2048], BF16) for i in range(2)]
    Xs = p.sb("Xs", [128, 4, 2048], BF16)
    XT = p.sb("XT", [128, 16, 512], BF16)
    sg2 = p.sb("sg2", [128, 512], F32)
    act2 = p.sb("act2", [128, 4, 512], BF16)
    yst = [p.sb("yst%d" % i, [128, 2048], BF16) for i in range(2)]
    exg_v = k.exg_d.rearrange("e (kc q) n -> e q kc n", q=128)
    exu_v = k.exu_d.rearrange("e (kc q) n -> e q kc n", q=128)
    exd_v = k.exd_d.rearrange("e (kc q) n -> e q kc n", q=128)
    cnt = {"y": 0}

    def load_w(e_):
        b = e_ % 2
        for h in range(2):
            p.dma("pool", Wg[b][:, h * 8:(h + 1) * 8, :], exg_v[e_][:, h * 8:(h + 1) * 8, :], reads=[], writes=[(Wg[b].name, h)])
            p.dma("pool", Wu[b][:, h * 8:(h + 1) * 8, :], exu_v[e_][:, h * 8:(h + 1) * 8, :], reads=[], writes=[(Wu[b].name, h)])
        for h in range(2):
            p.dma("pool", Wd[b][:, h * 2:(h + 1) * 2, :], exd_v[e_][:, h * 2:(h + 1) * 2, :], reads=[], writes=[(Wd[b].name, h)])

    def expert(e_):
        for off, n in SUBS:
            expert_sub(e_, off, n)

    def expert_sub(e_, off, n):
        b = e_ % 2
        nst = n // 128
        r0 = e_ * CAP + off
        p.dma("sp", Xs[:, 0:nst, :], k.Xg[r0:r0 + n, :].rearrange("(st q) f -> q st f", q=128), reads=[], writes=[Xs])
        for st in range(nst):
            for half in range(2):
                pt = pbt[half]
                for j in range(8):
                    kc = half * 8 + j
                    p.op("pe", lambda e, st=st, kc=kc, j=j, pt=pt: e.transpose(pt[:, j * 128:(j + 1) * 128], Xs[:, st, kc * 128:(kc + 1) * 128], ident[:]),
                         reads=[Xs, ident], writes=[pt])
                if half:
                    p.op("act", lambda e, st=st, pt=pt: e.activation(out=XT[:, 8:16, st * 128:(st + 1) * 128],
                                                                    in_=pt[:].rearrange("p (a b) -> p a b", b=128), func=AF.Copy),
                         reads=[pt], writes=[(XT.name, st)])
                else:
                    p.op("dve", lambda e, st=st, pt=pt: e.tensor_copy(out=XT[:, 0:8, st * 128:(st + 1) * 128],
                                                                     in_=pt[:].rearrange("p (a b) -> p a b", b=128)),
                         reads=[pt], writes=[(XT.name, st)])
        for dc in range(4):
            for kc in range(16):
                p.op("pe", lambda e, dc=dc, kc=kc: e.matmul(psb[0][:, 0:n], lhsT=Wg[b][:, kc, dc * 128:(dc + 1) * 128], rhs=XT[:, kc, 0:n],
                                                           start=(kc == 0), stop=(kc == 15)), reads=[(Wg[b].name, kc // 8), XT], writes=[psb[0]])
            for kc in range(16):
                p.op("pe", lambda e, dc=dc, kc=kc: e.matmul(psb[1][:, 0:n], lhsT=Wu[b][:, kc, dc * 128:(dc + 1) * 128], rhs=XT[:, kc, 0:n],
                                                           start=(kc == 0), stop=(kc == 15)), reads=[(Wu[b].name, kc // 8), XT], writes=[psb[1]])
            p.op("act", lambda e: e.activation(out=sg2[:, 0:n], in_=psb[0][:, 0:n], func=AF.Silu), reads=[psb[0]], writes=[sg2])
            p.op("dve", lambda e, dc=dc: e.tensor_tensor(out=act2[:, dc, 0:n], in0=sg2[:, 0:n], in1=psb[1][:, 0:n], op=ALU.mult), reads=[sg2, psb[1]],
                 writes=[(act2.name, dc)])
        for st in range(nst):
            ys = yst[cnt["y"] % 2]
            cnt["y"] += 1
            for cb in range(4):
                ps = psb[2 + cb]
                for dc in range(4):
                    p.op("pe", lambda e, st=st, cb=cb, dc=dc, ps=ps: e.matmul(ps[:, :], lhsT=act2[:, dc, st * 128:(st + 1) * 128],
                                                                            rhs=Wd[b][:, dc, cb * 512:(cb + 1) * 512], start=(dc == 0), stop=(dc == 3)),
                         reads=[act2, (Wd[b].name, dc // 2)], writes=[ps])
                if cb % 2:
                    p.op("act", lambda e, cb=cb, ps=ps, ys=ys: e.activation(out=ys[:, cb * 512:(cb + 1) * 512], in_=ps[:, :], func=AF.Copy),
                         reads=[ps], writes=[(ys.name, cb)])
                else:
                    p.op("dve", lambda e, cb=cb, ps=ps, ys=ys: e.tensor_copy(out=ys[:, cb * 512:(cb + 1) * 512], in_=ps[:, :]),
                         reads=[ps], writes=[(ys.name, cb)])
            p.dma("sp", k.Yg[r0 + st * 128:r0 + (st + 1) * 128, :], ys[:], reads=[ys], writes=[])

    load_w(0)
    for e_ in range(NEXP):
        if e_ + 1 < NEXP:
            load_w(e_ + 1)
        expert(e_)
    p.barrier()
    p.release(mE)
    if getattr(k, "ecut", None) == 2:
        return

    rows = {}
    for i, nm in ((3, "gt2"),):
        rows[nm] = p.sb("rowE_" + nm, [128, 2048], F32)
        p.dma("sp", rows[nm][:], k.rowbuf[i:i + 1, :].partition_broadcast(128), reads=[], writes=[rows[nm]])
    rows["fing"] = p.sb("rowE_fing", [128, 2048], F32)
    p.dma("sp", rows["fing"][:], k.rows_d[2:3, :].partition_broadcast(128), reads=[], writes=[rows["fing"]])
    acc = p.sb("accE", [128, 2048], F32)
    yk = [p.sb("yk%d" % i, [128, 2048], BF16) for i in range(2)]
    x1t = p.sb("x1E", [128, 2048], F32)
    sqE = p.sb("sqE", [128, 2048], F32)
    ssE = p.sb("ssE", [128, 2], F32)

    def combine_tile(ti):
        t0 = ti * 128
        p.dma("sp", acc[:], k.shscr[t0:t0 + 128, :], reads=[], writes=[acc])
        p.dma("sp", x1t[:], k.x1scr[t0:t0 + 128, :], reads=[], writes=[x1t])
        for j in range(8):
            y = yk[j % 2]
            p.op("pool", lambda e, y=y: e.memset(y[:], 0.0), writes=[y])
            p.dma_fn("pool", lambda e, j=j, y=y: e.indirect_dma_start(
                out=y[:], out_offset=None, in_=k.Yg, in_offset=bass.IndirectOffsetOnAxis(ap=slots_all[:, ti * 8 + j:ti * 8 + j + 1], axis=0),
                bounds_check=p.getreg(e, NSLOT - 1), oob_is_err=False), reads=[y, slots_all], writes=[y])
            p.op("dve", lambda e, j=j, y=y: e.scalar_tensor_tensor(out=acc[:], in0=y[:], scalar=wk_all[:, ti, j:j + 1], in1=acc[:],
                                                                    op0=ALU.mult, op1=ALU.add), reads=[y, wk_all, acc], writes=[acc])
        p.op("dve", lambda e: e.tensor_tensor(out=acc[:], in0=acc[:], in1=rows["gt2"][:], op=ALU.mult), reads=[acc, rows["gt2"]], writes=[acc])
        p.op("dve", lambda e: e.tensor_tensor(out=x1t[:], in0=x1t[:], in1=acc[:], op=ALU.add), reads=[x1t, acc], writes=[x1t])
        p.op("act", lambda e: e.activation(out=sqE[:], in_=x1t[:], func=AF.Square), reads=[x1t], writes=[sqE])
        p.op("dve", lambda e: e.tensor_reduce(out=ssE[:, 0:1], in_=sqE[:], axis=AX.X, op=ALU.add), reads=[sqE], writes=[ssE])
        p.op("act", lambda e: e.activation(out=ssE[:, 1:2], in_=ssE[:, 0:1], func=AF.Sqrt, scale=1.0 / D, bias=k.eps_t[:, 0:1]),
             reads=[ssE, k.eps_t], writes=[ssE])
        p.op("dve", lambda e: e.reciprocal(out=ssE[:, 1:2], in_=ssE[:, 1:2]), reads=[ssE], writes=[ssE])
        p.op("dve", lambda e: e.scalar_tensor_tensor(out=sqE[:], in0=x1t[:], scalar=ssE[:, 1:2], in1=rows["fing"][:], op0=ALU.mult, op1=ALU.mult),
             reads=[x1t, ssE, rows["fing"]], writes=[sqE])
        p.dma("sp", k.out_d[t0:t0 + 128, :], sqE[:], reads=[sqE], writes=[], is_out=True)

    for ti in range(4 * ngE):
        combine_tile(ti)
    p.barrier()


import numpy as np

D = 2048


def fm(v, kc=16):
    return np.ascontiguousarray(v.reshape(kc, 128).T)


def prep_shared(inp):
    out = {}
    w_in = inp["w_in"][0]
    for half in (0, 1):
        u = w_in[:, 0:1024]
        slab = w_in[:, 1024:1024 + 6784]
        gates = w_in[:, 1024 + 6784:]
        r, kk_, v = slab[:, 0:2048], slab[:, 2048:4096], slab[:, 4096:6144]
        wd = [slab[:, 6144:6240], slab[:, 6240:6336]]
        ad = [slab[:, 6336:6432], slab[:, 6432:6528]]
        gd = slab[:, 6528:6784]
        dA, dB = (0, 1) if half == 0 else (1, 0)
        z32 = np.zeros((D, 32), np.float32)
        win = np.concatenate([u, r, kk_, v, wd[dA], z32, wd[dB], z32, ad[dA], z32, ad[dB], z32, gd, gates], axis=1)
        out[half] = {"win": np.ascontiguousarray(win)}
    return out


def prep_core(inp, shared, b, half):
    x = inp["x"][b]
    ctx = inp["ctx"][b]
    if half == 1:
        x = x[::-1]
        ctx = ctx[::-1]
    m = {}
    m["xT"] = np.ascontiguousarray(x.T)
    m["cxT"] = np.ascontiguousarray(ctx.T)
    c2 = np.stack([inp["c"][b], inp["c_ctx"]], axis=1)
    m["cT2"] = np.ascontiguousarray(c2.reshape(16, 128, 2).transpose(1, 0, 2))
    m["adaw"] = inp["ada_w"][0]
    m["adab"] = fm(inp["ada_b"][0], 96)
    m["g1"] = fm(inp["norm1_g"][0])
    m["win"] = shared[half]["win"]
    return m


def prep_core_b(inp, b, half, m):
    dA, dB = (0, 1) if half == 0 else (1, 0)
    mu = inp["shift_mu"][0]
    z32 = np.zeros(32, np.float32)
    secs = [mu[6144:6240], mu[6240:6336]]
    seca = [mu[6336:6432], mu[6432:6528]]
    mup = np.concatenate([mu[0:6144], secs[dA], z32, secs[dB], z32, seca[dA], z32, seca[dB], z32, mu[6528:6784]])
    cls = np.arange(6912) % 4
    valid = np.ones(6912, bool)
    for s0 in (6144, 6272, 6400, 6528):
        valid[s0 + 96:s0 + 128] = False
    def sel(mask):
        return np.where(mask & valid, mup, np.float32(0)).astype(np.float32)
    if half == 0:
        cm1, cp1, cm64, cp64 = sel(cls == 0), sel(cls == 1), sel(cls == 2), sel(cls == 3)
        ccm1, ccp1 = sel(cls % 2 == 0), sel(cls % 2 == 1)
    else:
        cm1, cp1, cm64, cp64 = sel(cls == 1), sel(cls == 0), sel(cls == 3), sel(cls == 2)
        ccm1, ccp1 = sel(cls % 2 == 1), sel(cls % 2 == 0)
    mupv = np.where(valid, mup, np.float32(0)).astype(np.float32)
    arrs = [mupv, cm1, cp1, cm64, cp64, ccm1, ccp1]
    m["mixc"] = np.ascontiguousarray(np.stack([a.reshape(54, 128).T for a in arrs], axis=1))
    w0, a0 = inp["decay_w0"][0], inp["iclr_a0"][0]
    vs = [w0[dA], w0[dB], a0[dA], a0[dB], inp["k_k"][0], inp["k_a"][0], inp["r_k"][0].reshape(-1)]
    m["vecs"] = np.ascontiguousarray(np.stack([fm(v) for v in vs], axis=1))
    w2, a2 = inp["decay_w2"][0], inp["iclr_a2"][0]
    m["lw2"] = np.ascontiguousarray(np.stack([w2[dA], w2[dB], a2[dA], a2[dB]], axis=1))
    pidx = np.arange(128)
    m["bones"] = (pidx[:, None] // 64 == pidx[None, :] // 64).astype(np.float32)
    m["hsel"] = (pidx[:, None] // 64 == np.arange(2)[None, :]).astype(np.float32)
    return m


def prep_core_c(m):
    t = np.arange(64)
    lt = (t[:, None] < t[None, :]).astype(np.float32)
    le = (t[:, None] <= t[None, :]).astype(np.float32)
    gt = (t[:, None] > t[None, :]).astype(np.float32)
    ge = (t[:, None] >= t[None, :]).astype(np.float32)
    mA = np.block([[lt, le], [lt, le]])
    mB = np.block([[gt, ge], [gt, ge]])
    m["cmask"] = np.ascontiguousarray(np.stack([mA, mB], axis=1).astype(np.float32))
    nA = lt.T.copy()
    nB = gt.T.copy()
    m["nmask"] = np.ascontiguousarray(np.stack([nA, nB, np.eye(64, dtype=np.float32)], axis=1).astype(np.float32))
    return m


def prep_core_de(inp, b, half, m, with_experts=True):
    x = inp["x"][b]
    if half == 1:
        x = x[::-1]
    m["xown"] = np.ascontiguousarray(x[:2048])
    m["rows"] = np.ascontiguousarray(np.stack([inp["lnx_w"][0], inp["lnx_b"][0], inp["final_g"]], axis=0))
    m["g2n"] = fm(inp["norm2_g"][0])
    m["g2w"] = inp["gate_g2"][0]
    m["poolw"] = inp["pool_w"][0]
    m["pools"] = fm(inp["pool_scale"][0], 8)
    m["wpo"] = inp["w_pool_out"][0]
    m["wro"] = inp["w_rwkv_out"][0]
    m["wout"] = inp["w_out"][0]
    t = np.arange(64)
    cnts = []
    for w in (2, 4, 8, 16):
        lo = np.clip(t - w // 2, 0, 64)
        hi = np.clip(t + (w - w // 2), 0, 64)
        c = (hi - lo).astype(np.float32)
        if half == 1:
            c = c[::-1]
        cnts.append(np.float32(1.0) / c)
    flags = np.array([1.0, 0.0] if half == 0 else [0.0, 1.0], np.float32)
    m["poolc"] = np.ascontiguousarray(np.concatenate(cnts + [flags]).astype(np.float32)[None, :])
    m["routw"] = inp["router_w"][0]
    ecap = (np.arange(64) * 768).astype(np.float32)
    m["routb"] = np.ascontiguousarray(np.concatenate([inp["router_bias"][0], ecap]).astype(np.float32)[None, :])
    tt = np.arange(128)
    m["tri"] = (tt[:, None] < tt[None, :]).astype(np.float32)
    if with_experts:
        m["exg"] = inp["exp_w_gate"][0]
        m["exu"] = inp["exp_w_up"][0]
        m["exd"] = inp["exp_w_down"][0]
    m["shg"] = inp["shared_w_gate"][0]
    m["shu"] = inp["shared_w_up"][0]
    m["shd"] = inp["shared_w_down"][0]
    return m


from concourse.bass_utils import run_bass_kernel_spmd


def kernel(**inputs):
    inp = {k_: np.asarray(v) for k_, v in inputs.items()}
    shared = prep_shared(inp)
    maps = []
    for core in range(8):
        b, half = core // 2, core % 2
        m = prep_core(inp, shared, b, half)
        m = prep_core_b(inp, b, half, m)
        m = prep_core_c(m)
        m = prep_core_de(inp, b, half, m)
        maps.append(m)
    nc, k = build(stage=5)
    res = run_bass_kernel_spmd(nc, maps, core_ids=list(range(8)))
    out = np.empty((4, 4096, 2048), np.float32)
    for core in range(8):
        b, half = core // 2, core % 2
        o = np.asarray(res.results[core]["out"])
        if half == 0:
            out[b, :2048] = o
        else:
            out[b, 2048:] = o[::-1]
    return out
```

```python
import numpy as np
import concourse.bass as bass
import concourse.mybir as mybir
from contextlib import ExitStack

F32 = mybir.dt.float32
BF16 = mybir.dt.bfloat16
I32 = mybir.dt.int32
U32 = mybir.dt.uint32
ALU = mybir.AluOpType
AF = mybir.ActivationFunctionType
AX = mybir.AxisListType

ENGS = ("pe", "act", "dve", "pool", "sp")
ARENA_BASE = 16640
ARENA_TOP = 229344
DTSIZE = {F32: 4, BF16: 2, I32: 4, U32: 4}
NDSEM = 6


class _Op:
    __slots__ = ("eng", "fn", "deps", "signal", "sigval", "is_dma", "dsem", "dval", "dprev", "idx")

    def __init__(self, eng, fn, is_dma):
        self.eng = eng
        self.fn = fn
        self.deps = []
        self.signal = False
        self.sigval = 0
        self.is_dma = is_dma
        self.dsem = None
        self.dval = 0
        self.dprev = None


class Prog:
    def __init__(self, nc):
        self.nc = nc
        self.ops = []
        self.track = {}
        self.ndma = {e: 0 for e in ENGS}
        self.dma_ops = {e: [] for e in ENGS}
        self.es = ExitStack()
        self.out_dmas = []
        self.sp = ARENA_BASE
        self.sp_max = ARENA_BASE

    def sb(self, name, shape, dtype):
        nbytes = int(np.prod(shape[1:])) * DTSIZE[dtype]
        nbytes = (nbytes + 31) // 32 * 32
        off = self.sp
        self.sp += nbytes
        self.sp_max = max(self.sp_max, self.sp)
        assert self.sp <= ARENA_TOP, "SBUF arena overflow: %s needs %d at %d" % (name, nbytes, off)
        return self.nc.alloc_sbuf_tensor_at(name, list(shape), dtype, offset=off)

    def getreg(self, e, val):
        if not hasattr(self, "_regs"):
            self._regs = {}
        key = (id(e), val)
        if key not in self._regs:
            r = e.alloc_register("creg%d" % len(self._regs))
            e.reg_mov(r, val)
            self._regs[key] = r
        return self._regs[key]

    def mark(self):
        return self.sp

    def release(self, m):
        self.sp = m

    def ps(self, name, shape, dtype=F32):
        return self.es.enter_context(self.nc.psum_tensor(name, list(shape), dtype))

    @staticmethod
    def _key(k):
        if isinstance(k, tuple):
            return k[0], k[1]
        if isinstance(k, str):
            return k, None
        t = getattr(k, "tensor", k)
        return t.name, None

    def _conf(self, name, sub):
        d = self.track.setdefault(name, {})
        if sub is None:
            return list(d.keys())
        return [s for s in (sub, None) if s in d]

    def _record(self, op, reads, writes):
        r2, w2 = [], list(writes)
        for k_ in reads:
            nm = self._key(k_)[0]
            if nm.startswith("psb") or nm.startswith("pbt"):
                w2.append(k_)
            else:
                r2.append(k_)
        reads, writes = r2, w2
        deps = set()
        for k in reads:
            name, sub = self._key(k)
            d = self.track.setdefault(name, {})
            for s in self._conf(name, sub):
                w = d[s][0]
                if w is not None:
                    deps.add(w)
        for k in writes:
            name, sub = self._key(k)
            d = self.track.setdefault(name, {})
            for s in self._conf(name, sub):
                w, rs = d[s]
                if w is not None:
                    deps.add(w)
                for r in rs:
                    deps.add(r)
        for k in reads:
            name, sub = self._key(k)
            d = self.track[name]
            d.setdefault(sub, [None, []])[1].append(op)
        for k in writes:
            name, sub = self._key(k)
            d = self.track[name]
            if sub is None:
                d.clear()
            d[sub] = [op, []]
        deps.discard(op)
        for y in deps:
            if y.eng == "pe" and op.eng == "pe" and not y.is_dma:
                continue
            op.deps.append(y)
            if not y.is_dma:
                y.signal = True

    def op(self, eng, fn, reads=(), writes=(), after=()):
        o = _Op(eng, fn, False)
        o.idx = len(self.ops)
        self.ops.append(o)
        self._record(o, reads, writes)
        for y in after:
            o.deps.append(y)
            if not y.is_dma:
                y.signal = True
        return o

    def dma(self, eng, out, in_, reads=None, writes=None, is_out=False, **kw):
        if reads is None:
            reads = [in_]
        if writes is None:
            writes = [out]

        def fn(e, out=out, in_=in_, kw=kw):
            return e.dma_start(out=out, in_=in_, **kw)
        o = _Op(eng, fn, True)
        o.idx = len(self.ops)
        self.ops.append(o)
        self._record(o, reads, writes)
        n = self.ndma[eng]
        self.ndma[eng] += 1
        o.dsem = (eng, n % NDSEM)
        o.dval = 16 * (n // NDSEM + 1)
        if n >= NDSEM:
            o.dprev = self.dma_ops[eng][n - NDSEM]
        self.dma_ops[eng].append(o)
        if is_out:
            self.out_dmas.append(o)
        return o

    def dma_fn(self, eng, fn, reads, writes, is_out=False):
        o = _Op(eng, fn, True)
        o.idx = len(self.ops)
        self.ops.append(o)
        self._record(o, reads, writes)
        n = self.ndma[eng]
        self.ndma[eng] += 1
        o.dsem = (eng, n % NDSEM)
        o.dval = 16 * (n // NDSEM + 1)
        if n >= NDSEM:
            o.dprev = self.dma_ops[eng][n - NDSEM]
        self.dma_ops[eng].append(o)
        if is_out:
            self.out_dmas.append(o)
        return o

    def barrier(self):
        last = {}
        for o in self.ops:
            if not o.is_dma:
                last[o.eng] = o
        pend = [o for e in ENGS for o in self.dma_ops[e][-NDSEM:]]
        bops = []
        for e in ENGS:
            after = [o for o in last.values()] + pend
            bops.append((e, after))
        res = []
        for e, after in bops:
            res.append(self.op(e, lambda eng: eng.nop(), after=after))
        for o in res:
            o.signal = True
        self._barrier_ops = res
        self.track.clear()
        return res

    def emit(self):
        nc = self.nc
        if self.out_dmas:
            self.op("sp", lambda eng: eng.nop(), after=list(self.out_dmas))
        cnt = {e: 0 for e in ENGS}
        for o in self.ops:
            if o.is_dma:
                continue
            if o.signal:
                cnt[o.eng] += 1
                o.sigval = cnt[o.eng]
        es = self.es
        csem = {e: es.enter_context(nc.semaphore("c_" + e)) for e in ENGS}
        dsem = {}
        for e in ENGS:
            if self.ndma[e]:
                for i in range(min(NDSEM, self.ndma[e])):
                    dsem[(e, i)] = es.enter_context(nc.semaphore("d_%s%d" % (e, i)))
        per = {e: [o for o in self.ops if o.eng == e] for e in ENGS}
        block = es.enter_context(nc.Block())

        def run(eng_name, eng):
            waited = {}

            def wait(sem_key, sem, val):
                if waited.get(sem_key, 0) >= val:
                    return
                waited[sem_key] = val
                eng.wait_ge(sem, val)

            for o in per[eng_name]:
                for y in o.deps:
                    if y.is_dma:
                        wait(y.dsem, dsem[y.dsem], y.dval)
                    else:
                        wait(("c", y.eng), csem[y.eng], y.sigval)
                if o.is_dma:
                    if o.dprev is not None:
                        wait(o.dsem, dsem[o.dsem], o.dprev.dval)
                    ins = o.fn(eng)
                    ins.then_inc(dsem[o.dsem], 16)
                else:
                    ins = o.fn(eng)
                    if o.signal:
                        ins.then_inc(csem[eng_name], 1)

        @block.tensor
        def _(e):
            run("pe", e)

        @block.scalar
        def _(e):
            run("act", e)

        @block.vector
        def _(e):
            run("dve", e)

        @block.gpsimd
        def _(e):
            run("pool", e)

        @block.sync
        def _(e):
            run("sp", e)

    def close(self):
        self.es.close()


import numpy as np

D = 2048
NT = 4096
NOWN = 2048
NCTX = 256
KC = 16
NCH = 94
SLAB0 = 8
NSL = 54
DINP = NCH * 128
EPS = 1e-6
SQD = float(np.sqrt(2048.0))


class K:
    pass


def build(stage=99, dbg=(), ntiles=None, cut=None, skipA=False, nchunks=None, ccut=None, ecut=None, nexp=None):
    nc = bass.Bass("TRN2", target_bir_lowering=False)
    p = Prog(nc)
    k = K()
    k.nc, k.p = nc, p
    k.dbg = {}
    k.ntiles = ntiles
    k.cut = cut
    k.nchunks = nchunks
    k.ccut = ccut
    k.ecut = ecut
    k.nexp = nexp

    def din(name, shape, dt=F32):
        return nc.dram_tensor(name, list(shape), dt, kind="ExternalInput").ap()

    def dscr(name, shape, dt):
        if skipA and name in ("slabL", "slabC", "uT", "sgT"):
            return nc.dram_tensor(name, list(shape), dt, kind="ExternalInput").ap()
        if name in dbg:
            a = nc.dram_tensor(name, list(shape), dt, kind="ExternalOutput").ap()
            k.dbg[name] = a
            return a
        return nc.dram_tensor(name, list(shape), dt).ap()

    k.din, k.dscr = din, dscr
    if not skipA:
        k.xT = din("xT", [D, NT])
        k.cxT = din("cxT", [D, NCTX])
        k.cT2 = din("cT2", [128, KC, 2])
        k.adaw = din("adaw", [D, 6 * D])
        k.adab = din("adab", [128, 96])
        k.g1 = din("g1", [128, KC])
        k.win = din("win", [D, DINP])
    k.slabL = dscr("slabL", [NSL * 128, NT], BF16)
    k.slabC = dscr("slabC", [NSL * 128, NCTX], BF16)
    k.uT = dscr("uT", [1024, NOWN], BF16)
    k.sgT = dscr("sgT", [4096, NOWN], BF16)
    k.modd = dscr("modd", [128, 96, 2], F32)

    k.ones_bf = p.sb("ones_bf", [128, 128], BF16)
    p.op("pool", lambda e: e.memset(k.ones_bf[:], 1.0), writes=[k.ones_bf])
    k.eps_t = p.sb("eps_t", [128, 2], F32)
    p.op("pool", lambda e: e.memset(k.eps_t[:, 0:1], EPS), writes=[k.eps_t])
    p.op("pool", lambda e: e.memset(k.eps_t[:, 1:2], 1e-12), writes=[k.eps_t])
    k.mod = p.sb("mod", [128, 96, 2], F32)
    k.A1 = p.sb("A1", [128, KC, 2], F32)
    k.g1s = p.sb("g1s", [128, KC], F32)
    k.ident = p.sb("ident_bf", [128, 128], BF16)
    k.psb = [p.ps("psb%d" % i, [128, 512], F32) for i in range(6)]
    k.pbt = [p.ps("pbt%d" % i, [128, 1024], BF16) for i in range(2)]

    m0 = p.mark()
    if skipA:
        modin = din("modin", [128, 96, 2])
        p.dma("sp", k.mod[:], modin)
    if not skipA:
        phase0(k)
        p.barrier()
        p.release(m0)
        if stage >= 1:
            phaseA(k)
            p.release(m0)
    mB = p.mark()
    if stage >= 2:
        phaseB(k)
    if stage >= 3:
        mC = p.mark()
        phaseC(k)
        p.release(mC)
    if stage >= 4:
        phaseD(k)
    if stage >= 5:
        phaseE(k)
    p.emit()
    p.close()
    return nc, k


def phase0(k):
    p, nc = k.p, k.nc
    c32 = p.sb("c32", [128, KC, 2], F32)
    cs = p.sb("c_silu", [128, KC, 2], BF16)
    adab = p.sb("adab_sb", [128, 96], F32)
    p.dma("sp", c32[:], k.cT2)
    p.dma("sp", adab[:], k.adab)
    p.dma("sp", k.g1s[:], k.g1)
    p.op("act", lambda e: e.activation(out=cs[:], in_=c32[:], func=AF.Silu), reads=[c32], writes=[cs])
    NB = 16
    CW = 768
    wb = [p.sb("adaw_bf%d" % i, [128, KC, CW], BF16) for i in range(2)]
    src = k.adaw.rearrange("(kc p) n -> p kc n", p=128)
    ps = k.psb[0]
    for blk in range(NB):
        w = wb[blk % 2]
        for h in range(2):
            p.dma("pool", w[:, h * 8:(h + 1) * 8, :], src[:, h * 8:(h + 1) * 8, blk * CW:(blk + 1) * CW],
                  reads=[], writes=[(w.name, h)])
        for oc in range(6):
            g = blk * 6 + oc
            for kc in range(KC):
                p.op("pe", lambda e, w=w, oc=oc, kc=kc, g=g: e.matmul(
                    ps[:, 2 * g:2 * g + 2], lhsT=w[:, kc, oc * 128:(oc + 1) * 128], rhs=cs[:, kc, :],
                    start=(kc == 0), stop=(kc == KC - 1)),
                    reads=[(w.name, kc // 8), cs], writes=[ps])
    p.op("dve", lambda e: e.tensor_tensor(
        out=k.mod[:], in0=ps[:, 0:192].rearrange("p (g t) -> p g t", t=2),
        in1=adab[:].unsqueeze(2).to_broadcast([128, 96, 2]), op=ALU.add),
        reads=[ps, adab], writes=[k.mod])
    p.op("dve", lambda e: e.tensor_scalar(out=k.A1[:], in0=k.mod[:, 16:32, :], scalar1=1.0, scalar2=None,
                                          op0=ALU.add), reads=[k.mod], writes=[k.A1])
    p.op("dve", lambda e: e.tensor_tensor(out=k.A1[:], in0=k.A1[:],
                                          in1=k.g1s[:].unsqueeze(2).to_broadcast([128, KC, 2]), op=ALU.mult),
         reads=[k.A1, k.g1s], writes=[k.A1])
    if "modd" in k.dbg:
        p.dma("sp", k.modd, k.mod[:], is_out=True)


def phaseA(k):
    p, nc = k.p, k.nc
    hT = p.sb("hT", [128, KC, NOWN], BF16)
    xin = [p.sb("xin%d" % i, [128, KC, 256], F32) for i in range(2)]
    sq = p.sb("sq", [128, KC, 256], BF16)
    rstd = p.sb("rstd", [128, 256], F32)
    rms = p.sb("rms", [128, 256], F32)
    xn = p.sb("xn", [128, KC, 256], F32)
    wblk = [p.sb("wblk%d" % i, [128, KC, 256], BF16) for i in range(3)]
    stg = [p.sb("stgA%d" % i, [128, 512], BF16) for i in range(4)]
    winv = k.win.rearrange("(kc p) n -> p kc n", p=128)
    st = {"x": 0, "w": 0, "s": 0, "ps": 0}

    def norm_group(srcT, t0, ntok, which):
        v = srcT.rearrange("(kc p) t -> p kc t", p=128)
        for s in range(ntok // 256):
            xb = xin[st["x"] % 2]
            st["x"] += 1
            for h in range(2):
                p.dma("sp", xb[:, h * 8:(h + 1) * 8, :], v[:, h * 8:(h + 1) * 8, t0 + s * 256:t0 + (s + 1) * 256],
                      reads=[], writes=[(xb.name, h)])
            p.op("act", lambda e, xb=xb: e.activation(out=sq[:], in_=xb[:], func=AF.Square), reads=[xb], writes=[sq])
            ps = k.psb[5]
            for kc in range(KC):
                p.op("pe", lambda e, kc=kc: e.matmul(ps[:, 0:256], lhsT=k.ones_bf[:], rhs=sq[:, kc, :],
                                                      start=(kc == 0), stop=(kc == KC - 1)),
                     reads=[k.ones_bf, sq], writes=[ps])
            p.op("act", lambda e: e.activation(out=rms[:], in_=ps[:, 0:256], func=AF.Sqrt, scale=1.0 / D, bias=k.eps_t[:, 0:1]),
                 reads=[ps, k.eps_t], writes=[rms])
            p.op("dve", lambda e: e.reciprocal(out=rstd[:], in_=rms[:]), reads=[rms], writes=[rstd])
            p.op("dve", lambda e, xb=xb: e.tensor_tensor(out=xn[:], in0=xb[:],
                                                         in1=rstd[:].unsqueeze(1).to_broadcast([128, KC, 256]), op=ALU.mult),
                 reads=[xb, rstd], writes=[xn])
            for kc in range(KC):
                p.op("act", lambda e, kc=kc, s=s: e.activation(
                    out=hT[:, kc, s * 256:(s + 1) * 256], in_=xn[:, kc, :], func=AF.Identity,
                    scale=k.A1[:, kc, which:which + 1], bias=k.mod[:, kc, which:which + 1]),
                    reads=[xn, k.A1, k.mod], writes=[(hT.name, s)])

    def proj_group(ntok, chunks, sink):
        nsub = max(1, ntok // 512)
        w = min(512, ntok)
        for b0 in range(0, len(chunks), 2):
            wb = wblk[st["w"] % 3]
            st["w"] += 1
            c0 = chunks[b0]
            for h in range(2):
                p.dma("pool", wb[:, h * 8:(h + 1) * 8, :], winv[:, h * 8:(h + 1) * 8, c0 * 128:(c0 + 2) * 128],
                      reads=[], writes=[(wb.name, h)])
            for s in range(nsub):
                for j in range(2):
                    ch = chunks[b0 + j]
                    ps = k.psb[st["ps"] % 5]
                    st["ps"] += 1
                    for kc in range(KC):
                        p.op("pe", lambda e, wb=wb, j=j, kc=kc, s=s, ps=ps: e.matmul(
                            ps[:, 0:w], lhsT=wb[:, kc, j * 128:(j + 1) * 128], rhs=hT[:, kc, s * 512:s * 512 + w],
                            start=(kc == 0), stop=(kc == KC - 1)),
                            reads=[(wb.name, kc // 8), (hT.name, (s * 512) // 256), (hT.name, (s * 512 + w - 1) // 256)],
                            writes=[ps])
                    sink(ch, s, w, ps)

    def mk_sink(slab_dst, tok0):
        def sink(ch, s, w, ps):
            sg = stg[st["s"] % 4]
            st["s"] += 1
            eng = "act" if (st["s"] % 2) else "dve"
            if ch >= 62:
                p.op("act", lambda e, sg=sg, ps=ps: e.activation(out=sg[:, 0:w], in_=ps[:, 0:w], func=AF.Sigmoid),
                     reads=[ps], writes=[sg])
                dst = k.sgT[(ch - 62) * 128:(ch - 61) * 128, s * 512:s * 512 + w]
            else:
                if eng == "act":
                    p.op("act", lambda e, sg=sg, ps=ps: e.activation(out=sg[:, 0:w], in_=ps[:, 0:w], func=AF.Copy),
                         reads=[ps], writes=[sg])
                else:
                    p.op("dve", lambda e, sg=sg, ps=ps: e.tensor_copy(out=sg[:, 0:w], in_=ps[:, 0:w]),
                         reads=[ps], writes=[sg])
                if ch < 8:
                    dst = k.uT[ch * 128:(ch + 1) * 128, s * 512:s * 512 + w]
                else:
                    cc = ch - SLAB0
                    dst = slab_dst[cc * 128:(cc + 1) * 128, tok0 + s * 512:tok0 + s * 512 + w]
            p.dma("sp", dst, sg[:, 0:w], reads=[sg], writes=[])
        return sink

    slab_chunks = list(range(SLAB0, SLAB0 + NSL))
    norm_group(k.xT, 0, NOWN, 0)
    proj_group(NOWN, list(range(NCH)), mk_sink(k.slabL, 0))
    norm_group(k.xT, NOWN, NOWN, 0)
    proj_group(NOWN, slab_chunks, mk_sink(k.slabL, NOWN))
    norm_group(k.cxT, 0, NCTX, 1)
    proj_group(NCTX, slab_chunks, mk_sink(k.slabC, 0))
    p.barrier()
    for name in ("slabL", "slabC", "uT", "sgT"):
        if name in k.dbg:
            pass


import numpy as np

C0 = float(np.exp(-0.5))
NCHK = 68
TT = 256


class Cut(Exception):
    pass


def phaseB(k):
    try:
        _phaseB(k)
    except Cut:
        k.p.barrier()


def _phaseB(k):
    def cut(n):
        if getattr(k, "cut", None) == n:
            raise Cut()
    p, nc = k.p, k.nc
    din, dscr = k.din, k.dscr
    k.mixc_d = din("mixc", [128, 7, 54])
    k.vecs_d = din("vecs", [128, 7, 16])
    k.lw2_d = din("lw2", [96, 4, 2048])
    k.bones_d = din("bones", [128, 128])
    k.hsel_d = din("hsel", [128, 2])
    k.Q = [dscr("QA", [2048, NCHK * 256], BF16), dscr("QB", [2048, NCHK * 256], BF16)]
    k.Vtok = dscr("Vtok", [NCHK * 64, 2048], BF16)
    k.KpT = [dscr("KpTA", [NCHK * 64, 2048], BF16), dscr("KpTB", [NCHK * 64, 2048], BF16)]
    k.BpT = [dscr("BpTA", [NCHK * 64, 2048], BF16), dscr("BpTB", [NCHK * 64, 2048], BF16)]
    k.bcoef = dscr("bcoef", [2048, 32], F32)
    k.sgdT = dscr("sgdT", [256, 2048], BF16)
    k.wtot = [p.sb("wtotA", [128, 16, NCHK], F32), p.sb("wtotB", [128, 16, NCHK], F32)]

    markB = p.mark()
    mixc = p.sb("mixc_sb", [128, 7, 54], F32)
    omu = p.sb("omu", [128, 54], F32)
    vecs = p.sb("vecs_sb", [128, 7, 16], F32)
    oka = p.sb("oka", [128, 16], F32)
    lw2 = p.sb("lw2_sb", [96, 4, 2048], BF16)
    bones = p.sb("bones_sb", [128, 128], BF16)
    hsel = p.sb("hsel_sb", [128, 2], BF16)
    rmask = p.sb("rmask", [128, TT], F32)
    ident = k.ident
    p.dma("sp", mixc[:], k.mixc_d)
    p.dma("sp", vecs[:], k.vecs_d)
    for i in range(4):
        p.dma("pool", lw2[:, i, :], k.lw2_d[:, i, :], writes=[(lw2.name, i)])
    p.dma("pool", bones[:], k.bones_d)
    p.dma("pool", hsel[:], k.hsel_d)
    p.op("dve", lambda e: e.tensor_scalar(out=omu[:], in0=mixc[:, 0, :], scalar1=-1.0, scalar2=1.0, op0=ALU.mult, op1=ALU.add),
         reads=[mixc], writes=[omu])
    p.op("dve", lambda e: e.tensor_scalar(out=oka[:], in0=vecs[:, 5, :], scalar1=-1.0, scalar2=1.0, op0=ALU.mult, op1=ALU.add),
         reads=[vecs], writes=[oka])
    p.op("dve", lambda e: e.memset(rmask[:], 1.0), writes=[rmask])
    p.op("dve", lambda e: e.memset(rmask[:].rearrange("p (r c) -> p r c", c=64)[:, :, 0:1], 0.0), reads=[rmask], writes=[rmask])
    p.op("pool", lambda e: e.memset(ident[:], 1.0), writes=[ident])
    p.op("pool", lambda e: e.affine_select(out=ident[:], in_=ident[:], pattern=[[-1, 128]], compare_op=ALU.is_equal,
                                           fill=0.0, base=0, channel_multiplier=1), reads=[ident], writes=[ident])

    cut(1)
    NB = 2
    raw3 = [p.sb("raw3_%d" % i, [128, 3, 384], BF16) for i in range(NB)]
    rawl = [p.sb("rawl_%d" % i, [128, 384], BF16) for i in range(NB)]
    colsv = p.sb("colsv", [128, 4], BF16)
    M3 = [p.sb("M3_%d" % i, [128, 3, TT], F32) for i in range(NB)]
    ML = p.sb("ML", [128, TT], F32)
    tw = p.sb("tw", [128, 4, TT], BF16)
    sgd = p.sb("sgd", [128, 2, TT], BF16)
    f = {}
    for nm in ("lw", "aa", "cL", "X2", "X3", "X4", "e1", "e2", "e3", "e4", "kd", "bd", "ck"):
        f[nm] = [p.sb("f_%s%d" % (nm, i), [128, TT], F32) for i in range(2)]
    kkr = p.sb("kkr", [128, TT], F32)
    sqk = p.sb("sqk", [128, TT], BF16)
    rn = p.sb("rn", [128, TT], F32)
    kk = p.sb("kk", [128, TT], F32)
    vbf = p.sb("vbf", [128, TT], BF16)
    kpb = [p.sb("kpb%d" % i, [128, TT], BF16) for i in range(2)]
    bpb = [p.sb("bpb%d" % i, [128, TT], BF16) for i in range(2)]
    ksum = p.sb("ksum", [128, TT], F32)
    prod = p.sb("prodb", [128, TT], BF16)
    Qst = [[p.sb("Qst%d_%d" % (d, i), [128, 4, 4, 64], BF16) for i in range(2)] for d in range(2)]
    TM = {nm: p.sb("TM_" + nm, [128, 2, 2048], BF16) for nm in ("v", "kA", "bA", "kB", "bB")}
    bco = p.sb("bco", [128, 2, 32], F32)
    psL = [[k.psb[0], k.psb[1]], [k.psb[0], k.psb[1]]]
    psK = k.psb[2]
    psT = [(k.pbt[0], k.pbt[1]), (k.pbt[0], k.pbt[1])]
    psBon = k.psb[3]
    cnt = {"e": 0}

    def ew():
        cnt["e"] += 1
        return "pool" if cnt["e"] % 2 == 0 else "dve"

    colsv3 = p.sb("colsv3", [128, 3, 4], BF16)

    def mix_steps(eng, out, buf, cc, latent, key, okey, cvi):
        cv = colsv3[:, cvi, :]
        cvk = (colsv3.name, cvi)
        if latent:
            ctr = buf[:, 64:320]
            views = [(buf[:, 0:256], 3), (buf[:, 128:384], 4)]
        else:
            ctr = buf[:, 1:257]
            views = [(buf[:, 0:256], 5), (buf[:, 2:258], 6)]
        st = []
        st.append((lambda e: e.tensor_scalar(out=out, in0=ctr, scalar1=omu[:, cc:cc + 1], scalar2=None, op0=ALU.mult),
                   [key, omu], [okey]))
        for v, ci in views:
            st.append((lambda e, v=v, ci=ci: e.scalar_tensor_tensor(out=out, in0=v, scalar=mixc[:, ci, cc:cc + 1], in1=out,
                                                                    op0=ALU.mult, op1=ALU.add), [key, mixc, okey], [okey]))
        if latent:
            c63 = buf[:, 63:256:64]
            c0 = buf[:, 128:321:64]
            st.append((lambda e: e.tensor_copy(out=cv, in_=c63), [key], [cvk]))
            st.append((lambda e: e.memset(c63, 0.0), [key], [key]))
            st.append((lambda e: e.scalar_tensor_tensor(out=out, in0=buf[:, 63:319], scalar=mixc[:, 1, cc:cc + 1], in1=out,
                                                        op0=ALU.mult, op1=ALU.add), [key, mixc, okey], [okey]))
            st.append((lambda e: e.tensor_copy(out=c63, in_=cv), [cvk, key], [key]))
            st.append((lambda e: e.memset(c0, 0.0), [key], [key]))
            st.append((lambda e: e.scalar_tensor_tensor(out=out, in0=buf[:, 65:321], scalar=mixc[:, 2, cc:cc + 1], in1=out,
                                                        op0=ALU.mult, op1=ALU.add), [key, mixc, okey], [okey]))
        return st

    def mix_lockstep(eng, specs):
        allst = [mix_steps(eng, *sp, cvi=i) for i, sp in enumerate(specs)]
        for i in range(max(len(a) for a in allst)):
            for a in allst:
                if i < len(a):
                    fn, rd, wr = a[i]
                    p.op(eng, fn, reads=rd, writes=wr)

    def mix(eng, out, buf, cc, latent, key, okey):
        mix_lockstep(eng, [(out, buf, cc, latent, key, okey)])

    def load_raw(dst, src_rows, ti, latent, name):
        if latent:
            r0 = 4 * ti - 1
            lo, hi = max(r0, 0), min(r0 + 6, 64)
            if r0 < 0:
                p.op("pool", lambda e: e.memset(dst[:, :, 0:64], 0.0), writes=[name])
            if r0 + 6 > 64:
                p.op("pool", lambda e: e.memset(dst[:, :, 320:384], 0.0), writes=[name])
            p.dma("sp", dst[:, :, (lo - r0) * 64:(hi - r0) * 64], src_rows[:, :, lo * 64:hi * 64], reads=[], writes=[name])
        else:
            p.op("pool", lambda e: e.memset(dst[:, :, 0:1], 0.0), writes=[name])
            p.op("pool", lambda e: e.memset(dst[:, :, 257:258], 0.0), writes=[name])
            p.dma("sp", dst[:, :, 1:257], src_rows, reads=[], writes=[name])

    slabLv = k.slabL.rearrange("(cc p) t -> p cc t", p=128)
    slabCv = k.slabC.rearrange("(cc p) t -> p cc t", p=128)
    tiles = [("c", 0)] + [("l", ti) for ti in range(16)]
    if getattr(k, "ntiles", None):
        tiles = tiles[:k.ntiles]
    def do_tile(kind, ti, itbase):
        latent = kind == "l"
        own = latent and ti < 8
        src = slabLv if latent else slabCv
        chunk0 = 4 + 4 * ti if latent else 0
        tok0 = chunk0 * 64
        for j, cc in enumerate(range(48, 54)):
            if cc >= 52 and not own:
                continue
            rb = rawl[j % NB]
            load_raw(rb[:].unsqueeze(1), src[:, cc:cc + 1, :], ti, latent, rb.name)
            mix("dve", ML[:], rb[:], cc, latent, rb.name, ML.name)
            if j < 2:
                p.op("act", lambda e, j=j: e.activation(out=tw[:, j, :], in_=ML[:], func=AF.Tanh), reads=[ML], writes=[(tw.name, j)])
            elif j < 4:
                p.op("act", lambda e, j=j: e.activation(out=tw[:, j, :], in_=ML[:], func=AF.Copy), reads=[ML], writes=[(tw.name, j)])
            else:
                p.op("act", lambda e, j=j: e.activation(out=sgd[:, j - 4, :], in_=ML[:], func=AF.Sigmoid), reads=[ML], writes=[sgd])
                p.dma("sp", k.sgdT[(j - 4) * 128:(j - 3) * 128, ti * TT:(ti + 1) * TT], sgd[:, j - 4, :], reads=[sgd], writes=[])
        cut(2)
        def hp_body(hp, it):
            rb = raw3[it % NB]
            m3 = M3[it % NB]
            load_raw(rb[:], src[:, hp:hp + 33:16, :], ti, latent, rb.name)
            mix_lockstep("dve", [(m3[:, j, :], rb[:, j, :], hp + 16 * j, latent, (rb.name, j), (m3.name, j)) for j in range(3)])
            cut(3)
            rM, kM, vM = m3[:, 0, :], m3[:, 1, :], m3[:, 2, :]
            ch = slice(hp * 128, (hp + 1) * 128)
            pl = psL[it % 2]
            for d in range(2):
                p.op("pe", lambda e, d=d, pl=pl: e.matmul(pl[0][:, d * TT:(d + 1) * TT], lhsT=lw2[:, d, ch], rhs=tw[0:96, d, :],
                                                          start=True, stop=True),
                     reads=[(lw2.name, d), (tw.name, d)], writes=[pl[0]])
            for d in range(2):
                p.op("pe", lambda e, d=d, pl=pl: e.matmul(pl[1][:, d * TT:(d + 1) * TT], lhsT=lw2[:, 2 + d, ch], rhs=tw[0:96, 2 + d, :],
                                                          start=True, stop=True),
                     reads=[(lw2.name, 2 + d), (tw.name, 2 + d)], writes=[pl[1]])
            for d in range(2):
                p.op("act", lambda e, d=d, pl=pl: e.activation(out=f["lw"][d][:], in_=pl[0][:, d * TT:(d + 1) * TT], func=AF.Sigmoid,
                                                               bias=vecs[:, d, hp:hp + 1]), reads=[pl[0], vecs], writes=[f["lw"][d]])
                p.op("act", lambda e, d=d, pl=pl: e.activation(out=f["aa"][d][:], in_=pl[1][:, d * TT:(d + 1) * TT], func=AF.Sigmoid,
                                                               bias=vecs[:, 2 + d, hp:hp + 1]), reads=[pl[1], vecs], writes=[f["aa"][d]])
            p.op("dve", lambda e: e.tensor_scalar(out=kkr[:], in0=kM, scalar1=vecs[:, 4, hp:hp + 1], scalar2=None, op0=ALU.mult),
                 reads=[(m3.name, 1), vecs], writes=[kkr])
            p.op("act", lambda e: e.activation(out=sqk[:], in_=kkr[:], func=AF.Square), reads=[kkr], writes=[sqk])
            p.op("pe", lambda e: e.matmul(psK[:, 0:TT], lhsT=bones[:], rhs=sqk[:], start=True, stop=True), reads=[bones, sqk], writes=[psK])
            p.op("act", lambda e: e.activation(out=rn[:], in_=psK[:, 0:TT], func=AF.Sqrt, bias=k.eps_t[:, 1:2]), reads=[psK, k.eps_t], writes=[rn])
            p.op("dve", lambda e: e.reciprocal(out=rn[:], in_=rn[:]), reads=[rn], writes=[rn])
            p.op(ew(), lambda e: e.tensor_tensor(out=kk[:], in0=kkr[:], in1=rn[:], op=ALU.mult), reads=[kkr, rn], writes=[kk])
            cut(4)
            p.op("act", lambda e: e.activation(out=vbf[:], in_=vM, func=AF.Copy), reads=[(m3.name, 2)], writes=[vbf])
            pt, pt2 = psT[it % 2]
            ptb = pt[:]
            ptb2 = pt2[:]
            for s in range(2):
                p.op("pe", lambda e, s=s, ptb=ptb: e.transpose(ptb[:, s * 128:(s + 1) * 128], vbf[:, s * 128:(s + 1) * 128], ident[:]),
                     reads=[vbf, ident], writes=[pt])
            cut(5)
            def d_body(d):
                lw, aa = f["lw"][d], f["aa"][d]
                cL, X2, X3, X4 = f["cL"][d], f["X2"][d], f["X3"][d], f["X4"][d]
                e1, e2, e3, e4 = f["e1"][d], f["e2"][d], f["e3"][d], f["e4"][d]
                kd, bd, ck = f["kd"][d], f["bd"][d], f["ck"][d]
                Q = Qst[d][it % 2]
                p.op("dve", lambda e, cL=cL, lw=lw: e.tensor_tensor_scan(out=cL[:], data0=rmask[:], data1=lw[:], initial=0.0,
                                                                          op0=ALU.mult, op1=ALU.add), reads=[rmask, lw], writes=[cL])
                tot = cL[:].rearrange("p (r c) -> p r c", c=64)[:, :, 63:64]
                p.op(ew(), lambda e, X2=X2, cL=cL, lw=lw: e.tensor_tensor(out=X2[:], in0=cL[:], in1=lw[:], op=ALU.subtract),
                     reads=[cL, lw], writes=[X2])
                p.op(ew(), lambda e, X3=X3, cL=cL, tot=tot: e.tensor_tensor(
                    out=X3[:].rearrange("p (r c) -> p r c", c=64), in0=tot.to_broadcast([128, 4, 64]),
                    in1=cL[:].rearrange("p (r c) -> p r c", c=64), op=ALU.subtract), reads=[cL], writes=[X3])
                p.op("act", lambda e, d=d, tot=tot: e.activation(out=k.wtot[d][:, hp, chunk0:chunk0 + 4].unsqueeze(2), in_=tot,
                                                                func=AF.Exp, scale=-C0), reads=[cL], writes=[(k.wtot[d].name, it)])
                if d == 0:
                    srcs = [(cL, -C0), (X2, -C0), (cL, C0), (X3, -C0)]
                else:
                    p.op(ew(), lambda e, X4=X4, X3=X3, lw=lw: e.tensor_tensor(out=X4[:], in0=X3[:], in1=lw[:], op=ALU.add),
                         reads=[X3, lw], writes=[X4])
                    srcs = [(X4, -C0), (X3, -C0), (X4, C0), (X2, -C0)]
                for (sx, sc), eo in zip(srcs, (e1, e2, e3, e4)):
                    p.op("act", lambda e, sx=sx, sc=sc, eo=eo: e.activation(out=eo[:], in_=sx[:], func=AF.Exp, scale=sc),
                         reads=[sx], writes=[eo])
                p.op("dve", lambda e, ck=ck, aa=aa: e.tensor_scalar(out=ck[:], in0=aa[:], scalar1=vecs[:, 5, hp:hp + 1], scalar2=oka[:, hp:hp + 1],
                                                                   op0=ALU.mult, op1=ALU.add), reads=[aa, vecs, oka], writes=[ck])
                p.op(ew(), lambda e, kd=kd, ck=ck: e.tensor_tensor(out=kd[:], in0=kM, in1=ck[:], op=ALU.mult), reads=[(m3.name, 1), ck], writes=[kd])
                p.op(ew(), lambda e, bd=bd, aa=aa: e.tensor_tensor(out=bd[:], in0=kk[:], in1=aa[:], op=ALU.mult), reads=[kk, aa], writes=[bd])
                Qv = lambda a: Q[:, :, a, :]
                r3 = lambda t: t.rearrange("p (r c) -> p r c", c=64)
                p.op(ew(), lambda e, e1=e1: e.tensor_tensor(out=Qv(3), in0=r3(rM), in1=r3(e1[:]), op=ALU.mult), reads=[(m3.name, 0), e1], writes=[Q])
                p.op(ew(), lambda e, e2=e2: e.tensor_tensor(out=Qv(2), in0=r3(kk[:]), in1=r3(e2[:]), op=ALU.mult), reads=[kk, e2], writes=[Q])
                p.op(ew(), lambda e, e3=e3, kd=kd: e.tensor_tensor(out=Qv(1), in0=r3(kd[:]), in1=r3(e3[:]), op=ALU.mult), reads=[kd, e3], writes=[Q])
                p.op(ew(), lambda e, e3=e3, bd=bd: e.tensor_tensor(out=Qv(0), in0=r3(bd[:]), in1=r3(e3[:]), op=ALU.mult), reads=[bd, e3], writes=[Q])
                p.dma("sp", k.Q[d][ch, chunk0 * 256:(chunk0 + 4) * 256], Q[:].rearrange("p a b c -> p (a b c)"), reads=[Q], writes=[])
                p.op(ew(), lambda e, e4=e4, kd=kd, d=d: e.tensor_tensor(out=kpb[d][:], in0=kd[:], in1=e4[:], op=ALU.mult), reads=[kd, e4], writes=[kpb[d]])
                p.op(ew(), lambda e, e4=e4, bd=bd, d=d: e.tensor_tensor(out=bpb[d][:], in0=bd[:], in1=e4[:], op=ALU.mult), reads=[bd, e4], writes=[bpb[d]])
                for s in range(2):
                    p.op("pe", lambda e, s=s, d=d, ptb=ptb: e.transpose(ptb[:, (2 + 4 * d + s) * 128:(3 + 4 * d + s) * 128],
                                                                        kpb[d][:, s * 128:(s + 1) * 128], ident[:]),
                         reads=[kpb[d], ident], writes=[pt])
                    if d == 0:
                        p.op("pe", lambda e, s=s, d=d, ptb=ptb: e.transpose(ptb[:, (4 + s) * 128:(5 + s) * 128],
                                                                            bpb[d][:, s * 128:(s + 1) * 128], ident[:]),
                             reads=[bpb[d], ident], writes=[pt])
                    else:
                        p.op("pe", lambda e, s=s, d=d, ptb2=ptb2: e.transpose(ptb2[:, s * 128:(s + 1) * 128],
                                                                              bpb[d][:, s * 128:(s + 1) * 128], ident[:]),
                             reads=[bpb[d], ident], writes=[pt2])
            for d_ in range(2):
                d_body(d_)
            cut(6)
            for bi, nm in enumerate(("v", "kA", "bA", "kB", "bB")):
                for s2 in range(2):
                    if bi < 4:
                        src_ps = ptb[:, (bi * 2 + s2) * 128:(bi * 2 + s2 + 1) * 128]
                        pkey = pt
                    else:
                        src_ps = ptb2[:, s2 * 128:(s2 + 1) * 128]
                        pkey = pt2
                    eng = "act" if (bi + s2) % 2 else "dve"
                    if getattr(k, "evac_dve", False):
                        eng = "dve"
                    if eng == "act":
                        p.op("act", lambda e, nm=nm, src_ps=src_ps, s2=s2: e.activation(out=TM[nm][:, s2, ch], in_=src_ps, func=AF.Copy),
                             reads=[pkey], writes=[(TM[nm].name, hp)])
                    else:
                        p.op("dve", lambda e, nm=nm, src_ps=src_ps, s2=s2: e.tensor_copy(out=TM[nm][:, s2, ch], in_=src_ps),
                             reads=[pkey], writes=[(TM[nm].name, hp)])
            cut(7)
            if own:
                p.op(ew(), lambda e: e.tensor_tensor(out=ksum[:], in0=f["kd"][0][:], in1=f["kd"][1][:], op=ALU.add),
                     reads=[f["kd"][0], f["kd"][1]], writes=[ksum])
                p.op("dve", lambda e: e.scalar_tensor_tensor(out=prod[:], in0=ksum[:], scalar=vecs[:, 6, hp:hp + 1], in1=rM,
                                                            op0=ALU.mult, op1=ALU.mult), reads=[ksum, vecs, (m3.name, 0)], writes=[prod])
                for s in range(2):
                    p.op("pe", lambda e, s=s: e.matmul(psBon[:, s * 32 + 2 * hp:s * 32 + 2 * hp + 2], lhsT=prod[:, s * 128:(s + 1) * 128],
                                                       rhs=hsel[:], start=True, stop=True), reads=[prod, hsel], writes=[psBon])
        for hp_ in range(16):
            hp_body(hp_, itbase + hp_ + 1)
        for nm, dst in (("v", k.Vtok), ("kA", k.KpT[0]), ("bA", k.BpT[0]), ("kB", k.KpT[1]), ("bB", k.BpT[1])):
            for s in range(2):
                p.dma("sp", dst[tok0 + s * 128:tok0 + (s + 1) * 128, :], TM[nm][:, s, :], reads=[TM[nm]], writes=[])
        if own:
            p.op("dve", lambda e: e.tensor_copy(out=bco[:], in_=psBon[:, 0:64].rearrange("p (s h) -> p s h", h=32)), reads=[psBon], writes=[bco])
            for s in range(2):
                p.dma("sp", k.bcoef[ti * TT + s * 128:ti * TT + (s + 1) * 128, :], bco[:, s, :], reads=[bco], writes=[])
    for tix, (kind_, ti_) in enumerate(tiles):
        do_tile(kind_, ti_, tix * 16)
    p.barrier()
    p.release(markB)


import numpy as np

NCHK = 68


def phaseC(k):
    p, nc = k.p, k.nc
    din, dscr = k.din, k.dscr
    k.cmask_d = din("cmask", [128, 2, 128])
    k.nmask_d = din("nmask", [64, 3, 64])
    k.Yscr = [dscr("YA", [2048, 2048], F32), dscr("YB", [2048, 2048], F32)]
    k.Sdbg = dscr("Sdbg", [2, 128, 16 * 64], F32)

    cmask = p.sb("cmask_sb", [128, 2, 128], F32)
    nmask = p.sb("nmask_sb", [64, 3, 64], F32)
    p.dma("sp", cmask[:], k.cmask_d)
    p.dma("sp", nmask[:], k.nmask_d)
    FM = [p.sb("FM%d" % i, [128, 16, 256], BF16) for i in range(2)]
    UV = [p.sb("UV%d" % i, [128, 2048], BF16) for i in range(2)]
    VZ = [p.sb("VZ%d" % i, [128, 2048], BF16) for i in range(2)]
    FMm = [p.sb("FMm%d" % i, [128, 16, 2, 128], BF16) for i in range(2)]
    for i in range(2):
        p.op("pool", lambda e, i=i: e.memset(VZ[i][:], 0.0), writes=[VZ[i]])
        p.op("pool", lambda e, i=i: e.memset(FMm[i][:], 0.0), writes=[FMm[i]])
    KB = [p.sb("KB%d" % i, [128, 2048], BF16) for i in range(2)]
    AT = p.sb("AT_sb", [128, 32, 128], BF16)
    Pm = [p.sb("Pm%d" % g, [128, 8, 64], BF16) for g in range(4)]
    PT = [p.sb("PTm%d" % g, [128, 8, 64], BF16) for g in range(4)]
    Gm = [p.sb("Gm%d" % g, [128, 8, 64], BF16) for g in range(4)]
    Qm = [p.sb("Qm%d" % g, [128, 8, 64], BF16) for g in range(4)]
    nrhs = [p.sb("nrhs%d" % g, [128, 8, 64], BF16) for g in range(4)]
    for g in range(4):
        for t_ in (Pm, PT, Gm, Qm, nrhs):
            p.op("pool", lambda e, t_=t_, g=g: e.memset(t_[g][:], 0.0), writes=[t_[g]])
    Ysb = [p.sb("Ysb%d" % i, [64, 2048], F32) for i in range(2)]
    S32 = p.sb("S32", [128, 16, 64], F32)
    Sbf = p.sb("Sbf", [128, 16, 64], BF16)
    Stmp = p.sb("Stmp", [128, 4, 64], F32)
    psAT = [k.psb[0], k.psb[1]]
    psP, psPT, psQ, psY = k.psb[2], k.psb[3], k.psb[4], k.psb[5]
    psP0, psPT0, psQ0 = psP, psPT, psQ
    ident = nmask[:, 2, :]
    Qv = [k.Q[d].rearrange("(hp p) x -> p hp x", p=128) for d in range(2)]
    cnt = {"e": 0, "ld": 0}

    def evac_eng():
        cnt["e"] += 1
        return "act" if cnt["e"] % 2 else "dve"

    def copy_evac(out, in_, reads, writes, scale=None):
        eng = evac_eng()
        if eng == "act":
            if scale is None:
                p.op("act", lambda e: e.activation(out=out, in_=in_, func=AF.Copy), reads=reads, writes=writes)
            else:
                p.op("act", lambda e: e.activation(out=out, in_=in_, func=AF.Copy, scale=scale), reads=reads, writes=writes)
        else:
            if scale is None:
                p.op("dve", lambda e: e.tensor_copy(out=out, in_=in_), reads=reads, writes=writes)
            else:
                p.op("dve", lambda e: e.tensor_scalar(out=out, in0=in_, scalar1=scale, scalar2=None, op0=ALU.mult), reads=reads, writes=writes)

    def load_chunk(d, c, buf):
        fm, uv, kb = FM[buf], UV[buf], KB[buf]
        vz, fmm = VZ[buf], FMm[buf]
        p.dma("sp", fm[:], Qv[d][:, :, c * 256:(c + 1) * 256], reads=[], writes=[fm])
        p.dma("sp", fmm[0:64, :, 0, :], Qv[d][0:64, :, c * 256 + 128:(c + 1) * 256], reads=[], writes=[(fmm.name, 0)])
        p.dma("sp", fmm[64:128, :, 1, :], Qv[d][64:128, :, c * 256 + 128:(c + 1) * 256], reads=[], writes=[(fmm.name, 1)])
        p.dma("sp", uv[64:128, :], k.Vtok[c * 64:(c + 1) * 64, :], reads=[], writes=[(uv.name, "V")])
        p.dma("sp", vz[64:128, :], k.Vtok[c * 64:(c + 1) * 64, :], reads=[], writes=[(vz.name, "V")])
        p.dma("sp", kb[0:64, :], k.BpT[d][c * 64:(c + 1) * 64, :], reads=[], writes=[(kb.name, 0)])
        p.dma("sp", kb[64:128, :], k.KpT[d][c * 64:(c + 1) * 64, :], reads=[], writes=[(kb.name, 1)])

    def do_chunk(d, c, buf, emit, ytok0):
        fm, uv, kb = FM[buf], UV[buf], KB[buf]
        vz, fmm = VZ[buf], FMm[buf]
        ysb = Ysb[buf]

        def fmv(h, a0, a1):
            return fm[:, h // 2, a0 * 64:a1 * 64]

        def fmk(h, a0, a1):
            return fmm[:, h // 2, h % 2, a0 * 64:a1 * 64]

        def sv(h):
            return Sbf[:, h // 2, :]

        def stage1(g):
            for hl in range(8):
                h = g * 8 + hl
                pa = psAT[hl // 4]
                p.op("pe", lambda e, h=h, hl=hl, pa=pa: e.matmul(pa[:, (hl % 4) * 128:(hl % 4 + 1) * 128], lhsT=fmv(h, 0, 2), rhs=fmk(h, 0, 2),
                                                               start=True, stop=True), reads=[fm, fmm], writes=[pa])
            for hl in range(8):
                h = g * 8 + hl
                p.op("pe", lambda e, h=h, hl=hl: e.matmul(psP[0:64, hl * 64:(hl + 1) * 64], lhsT=fmk(h, 0, 1), rhs=fmv(h, 0, 1),
                                                         start=True, stop=True), reads=[fm, fmm], writes=[psP])
            for half in range(2):
                pa = psAT[half]
                p.op("dve", lambda e, half=half, pa=pa: e.tensor_tensor(
                    out=AT[:, g * 8 + half * 4:g * 8 + half * 4 + 4, :], in0=pa[:].rearrange("p (h c) -> p h c", c=128),
                    in1=cmask[:, d, :].unsqueeze(1).to_broadcast([128, 4, 128]), op=ALU.mult), reads=[pa, cmask], writes=[(AT.name, g)])
            p.op("dve", lambda e: e.tensor_tensor(out=Pm[g][0:64, :, :], in0=psP[0:64, :].rearrange("p (h c) -> p h c", c=64),
                                                  in1=nmask[:, d, :].unsqueeze(1).to_broadcast([64, 8, 64]), op=ALU.mult),
                 reads=[psP, nmask], writes=[Pm[g]])
            p.op("dve", lambda e: e.tensor_tensor(out=Qm[g][0:64, :, :], in0=ident.unsqueeze(1).to_broadcast([64, 8, 64]),
                                                  in1=AT[0:64, g * 8:(g + 1) * 8, 0:64], op=ALU.subtract),
                 reads=[(AT.name, g), nmask], writes=[Qm[g]])

        def ptv(g, kk_, hl):
            if kk_ == 0:
                return AT[:, g * 8 + hl, 0:64]
            return PT[g][:, hl, :]

        def stage2(g, kk_):
            psP, psPT, psQ = (psP0, psPT0, psQ0) if g % 2 == 0 else (psAT[0], psAT[1], psY)
            for hl in range(8):
                p.op("pe", lambda e, hl=hl: e.matmul(psP[0:64, hl * 64:(hl + 1) * 64], lhsT=ptv(g, kk_ - 1, hl), rhs=Pm[g][:, hl, :],
                                                    start=True, stop=True),
                     reads=[Pm[g], PT[g], (AT.name, g)], writes=[psP])
            if kk_ < 5:
                for hl in range(8):
                    p.op("pe", lambda e, hl=hl: e.matmul(psPT[0:64, hl * 64:(hl + 1) * 64], lhsT=Pm[g][:, hl, :], rhs=ptv(g, kk_ - 1, hl),
                                                        start=True, stop=True),
                         reads=[Pm[g], PT[g], (AT.name, g)], writes=[psPT])
            p3 = psP[0:64, :].rearrange("p (h c) -> p h c", c=64)
            p.op("dve", lambda e: e.tensor_tensor(out=Gm[g][0:64, :, :], in0=p3, in1=ident.unsqueeze(1).to_broadcast([64, 8, 64]), op=ALU.add),
                 reads=[psP, nmask], writes=[Gm[g]])
            if kk_ < 5:
                p.op("act", lambda e: e.activation(out=Pm[g][0:64, :, :].rearrange("p h c -> p (h c)"), in_=psP[0:64, :], func=AF.Copy), reads=[psP], writes=[Pm[g]])
                copy_evac(PT[g][0:64, :, :].rearrange("p h c -> p (h c)"), psPT[0:64, :], [psPT], [PT[g]])
            for hl in range(8):
                p.op("pe", lambda e, hl=hl: e.matmul(psQ[0:64, hl * 64:(hl + 1) * 64], lhsT=Gm[g][:, hl, :], rhs=Qm[g][:, hl, :],
                                                    start=True, stop=True), reads=[Gm[g], Qm[g]], writes=[psQ])
            copy_evac(Qm[g][0:64, :, :].rearrange("p h c -> p (h c)"), psQ[0:64, :], [psQ], [Qm[g]])

        def stage3(g):
            for hl in range(8):
                h = g * 8 + hl
                p.op("pe", lambda e, h=h, hl=hl: e.matmul(psQ[0:64, hl * 64:(hl + 1) * 64], lhsT=fmk(h, 0, 1), rhs=sv(h),
                                                         start=True, stop=False), reads=[fmm, (Sbf.name, h // 8)], writes=[psQ])
                p.op("pe", lambda e, h=h, hl=hl: e.matmul(psQ[0:64, hl * 64:(hl + 1) * 64], lhsT=AT[:, h, 0:64],
                                                         rhs=vz[:, h * 64:(h + 1) * 64], start=False, stop=True),
                     reads=[(AT.name, g), vz], writes=[psQ])
            copy_evac(nrhs[g][0:64, :, :].rearrange("p h c -> p (h c)"), psQ[0:64, :], [psQ], [nrhs[g]], scale=-1.0)
            for hl in range(8):
                p.op("pe", lambda e, hl=hl: e.matmul(psPT[0:64, hl * 64:(hl + 1) * 64], lhsT=Qm[g][:, hl, :], rhs=nrhs[g][:, hl, :],
                                                    start=True, stop=True), reads=[Qm[g], nrhs[g]], writes=[psPT])
            copy_evac(uv[0:64, g * 512:(g + 1) * 512], psPT[0:64, :], [psPT], [(uv.name, ("U", g))])

        def stage5(g):
            for hl in range(8):
                h = g * 8 + hl
                p.op("pe", lambda e, h=h, hl=hl: e.matmul(psY[0:64, hl * 64:(hl + 1) * 64], lhsT=fmk(h, 1, 2), rhs=sv(h),
                                                         start=True, stop=False), reads=[fmm, (Sbf.name, h // 8)], writes=[psY])
                p.op("pe", lambda e, h=h, hl=hl: e.matmul(psY[0:64, hl * 64:(hl + 1) * 64], lhsT=AT[:, h, 64:128],
                                                         rhs=uv[:, h * 64:(h + 1) * 64], start=False, stop=True),
                     reads=[(AT.name, g), (uv.name, "V"), (uv.name, ("U", g))], writes=[psY])
            copy_evac(ysb[:, g * 512:(g + 1) * 512], psY[0:64, :], [psY], [(ysb.name, g)])

        def stage6(g):
            ps4 = psY[:].rearrange("p (a b c) -> p a b c", a=4, b=2)
            for hpl in range(4):
                hp = g * 4 + hpl
                h0, h1 = 2 * hp, 2 * hp + 1
                p.op("pe", lambda e, hpl=hpl, h0=h0: e.matmul(ps4[0:64, hpl, 0, :], lhsT=kb[:, h0 * 64:(h0 + 1) * 64], rhs=uv[:, h0 * 64:(h0 + 1) * 64],
                                                             start=True, stop=True),
                     reads=[kb, (uv.name, "V"), (uv.name, ("U", g))], writes=[psY])
                p.op("pe", lambda e, hpl=hpl, h0=h0, h1=h1: e.matmul(ps4[:, hpl, 1, :], lhsT=kb[:, h0 * 64:(h1 + 1) * 64], rhs=uv[:, h1 * 64:(h1 + 1) * 64],
                                                                    start=True, stop=True),
                     reads=[kb, (uv.name, "V"), (uv.name, ("U", g))], writes=[psY])
            for hh in range(2):
                rows = slice(hh * 64, (hh + 1) * 64)
                p.op("dve", lambda e, rows=rows: e.tensor_tensor(
                    out=Stmp[rows, :, :], in0=S32[rows, g * 4:(g + 1) * 4, :],
                    in1=k.wtot[d][rows, g * 4:(g + 1) * 4, c:c + 1].to_broadcast([64, 4, 64]), op=ALU.mult),
                    reads=[(S32.name, g), k.wtot[d]], writes=[(Stmp.name, hh)])
                p.op("dve", lambda e, rows=rows, hh=hh: e.tensor_tensor(
                    out=S32[rows, g * 4:(g + 1) * 4, :], in0=Stmp[rows, :, :], in1=ps4[rows, :, hh, :], op=ALU.add),
                    reads=[(Stmp.name, hh), psY], writes=[(S32.name, g)])
            p.op("act", lambda e: e.activation(out=Sbf[:].rearrange("p a b -> p (a b)")[:, g * 256:(g + 1) * 256],
                                               in_=S32[:].rearrange("p a b -> p (a b)")[:, g * 256:(g + 1) * 256], func=AF.Copy),
                 reads=[(S32.name, g)], writes=[(Sbf.name, g)])

        cc_ = getattr(k, "ccut", None) or 99
        for g in range(4):
            stage1(g)
        if cc_ <= 1:
            return
        for kk_ in range(1, 6):
            for g in range(4):
                stage2(g, kk_)
        if cc_ <= 2:
            return
        for g in range(4):
            stage3(g)
        if cc_ <= 3:
            return
        if emit:
            for g in range(4):
                stage5(g)
            p.dma("sp", k.Yscr[d][ytok0:ytok0 + 64, :], ysb[:], reads=[ysb], writes=[])
        if cc_ <= 5:
            return
        for g in range(4):
            stage6(g)

    ncap = getattr(k, "nchunks", None)
    for d in range(2):
        if d == 0:
            seq = [(c, False) for c in range(4)] + [(c, True) for c in range(4, 36)]
        else:
            seq = [(c, False) for c in range(3, -1, -1)] + [(c, False) for c in range(67, 35, -1)] + [(c, True) for c in range(35, 3, -1)]
        if ncap:
            seq = seq[:ncap]
        p.op("dve", lambda e: e.memset(S32[:], 0.0), writes=[S32])
        p.op("pool", lambda e: e.memset(Sbf[:], 0.0), writes=[Sbf])
        load_chunk(d, seq[0][0], cnt["ld"] % 2)
        for i, (c, emit) in enumerate(seq):
            buf = cnt["ld"] % 2
            cnt["ld"] += 1
            if i + 1 < len(seq):
                load_chunk(d, seq[i + 1][0], cnt["ld"] % 2)
            do_chunk(d, c, buf, emit, (c - 4) * 64)
        if "Sdbg" in k.dbg:
            p.dma("sp", k.Sdbg[d], S32[:].rearrange("p a b -> p (a b)"), reads=[S32], writes=[])
    p.barrier()


import numpy as np

GT = 256
NTS = 2
NR = 4
GN_EPS = 64e-5
NORM_EPS = 1e-6
D = 2048


def phaseD(k):
    p, nc = k.p, k.nc
    din, dscr = k.din, k.dscr
    k.xown_d = din("xown", [2048, 2048])
    k.rows_d = din("rows", [3, 2048])
    k.g2n_d = din("g2n", [128, 16])
    k.g2w_d = din("g2w", [256, 2048])
    k.poolw_d = din("poolw", [4, 256, 256])
    k.pools_d = din("pools", [128, 8])
    k.wpo_d = din("wpo", [1024, 2048])
    k.wro_d = din("wro", [2048, 2048])
    k.wout_d = din("wout", [2048, 2048])
    k.poolc_d = din("poolc", [1, 4 * 64 + 2])
    k.rowbuf = dscr("rowbuf", [4, 2048], F32)
    k.x1scr = dscr("x1scr", [2048, 2048], F32)
    k.h2T = dscr("h2Tscr", [2048, 2048], BF16)
    k.h2tok = dscr("h2tok", [2048, 2048], BF16)

    k.rowt = {}
    mD = p.mark()
    for i, nm in enumerate(("lnxw", "lnxb")):
        k.rowt[nm] = p.sb("row_" + nm, [128, 2048], F32)
        p.dma("sp", k.rowt[nm][:], k.rows_d[i:i + 1, :].partition_broadcast(128))
    g2n = p.sb("g2n_sb", [128, 16], F32)
    A2 = p.sb("A2_fm", [128, 16], F32)
    p.dma("sp", g2n[:], k.g2n_d)
    p.op("dve", lambda e: e.tensor_scalar(out=A2[:], in0=k.mod[:, 64:80, 0], scalar1=1.0, scalar2=None, op0=ALU.add), reads=[k.mod], writes=[A2])
    p.op("dve", lambda e: e.tensor_tensor(out=A2[:], in0=A2[:], in1=g2n[:], op=ALU.mult), reads=[A2, g2n], writes=[A2])
    rb = k.rowbuf.rearrange("r (oc q) -> r q oc", q=128)
    o1 = p.dma("sp", rb[0], k.mod[:, 32:48, 0], reads=[k.mod], writes=["rowbuf"], allow_slow_non_contiguous=True)
    o2 = p.dma("sp", rb[1], A2[:], reads=[A2], writes=["rowbuf"], allow_slow_non_contiguous=True)
    o3 = p.dma("sp", rb[2], k.mod[:, 48:64, 0], reads=[k.mod], writes=["rowbuf"], allow_slow_non_contiguous=True)
    o4 = p.dma("sp", rb[3], k.mod[:, 80:96, 0], reads=[k.mod], writes=["rowbuf"], allow_slow_non_contiguous=True)
    for i, nm in enumerate(("gt1", "A2", "sh2")):
        k.rowt[nm] = p.sb("row_" + nm, [128, 2048], F32)
        p.dma("sp", k.rowt[nm][:], k.rowbuf[i:i + 1, :].partition_broadcast(128), reads=["rowbuf"], writes=[k.rowt[nm]])

    g2w = p.sb("g2w_sb", [128, 2, 2048], BF16)
    poolw = p.sb("poolw_sb", [128, 4, 2, 256], BF16)
    pools = p.sb("pools_sb", [128, 8], F32)
    poolc = p.sb("poolc_sb", [128, 258], F32)
    gneps = p.sb("gneps", [128, 1], F32)
    p.dma("pool", g2w[:], k.g2w_d.rearrange("(kc q) n -> q kc n", q=128))
    p.dma("pool", poolw[:], k.poolw_d.rearrange("g (kc q) d -> q g kc d", q=128))
    p.dma("sp", pools[:], k.pools_d)
    p.dma("sp", poolc[:], k.poolc_d.partition_broadcast(128))
    p.op("pool", lambda e: e.memset(gneps[:], GN_EPS), writes=[gneps])
    ident = k.ident

    ya = p.sb("ya", [128, 2048], F32)
    yb = p.sb("yb", [128, 2048], F32)
    sq = p.sb("ysq", [128, 2048], F32)
    vt = p.sb("vt", [128, 2048], BF16)
    st1 = p.sb("st1", [128, 32], F32)
    st2 = p.sb("st2", [128, 32], F32)
    st3 = p.sb("st3", [128, 32], F32)
    bc = p.sb("bc", [128, 32], F32)
    sgdt = p.sb("sgdt", [128, 2, 128], BF16)
    prebf = p.sb("prebf", [128, 2048], BF16)
    preT = p.sb("preT", [128, 16, GT], BF16)
    Upad = p.sb("Upad", [128, NR, 96], F32)
    Sa = p.sb("Sa", [128, NR, 96], F32)
    Sb_ = p.sb("Sb_", [128, NR, 96], F32)
    dtmp = p.sb("dtmp", [128, NR, 64], F32)
    ubf = p.sb("ubf", [128, GT], BF16)
    dT = p.sb("dT", [128, 8, GT], BF16)
    zT = p.sb("zT", [128, 8, GT], BF16)
    sgt = [p.sb("sgt%d" % i, [128, GT], BF16) for i in range(2)]
    mrg = p.sb("mrg", [128, GT], F32)
    mT = p.sb("mT", [128, 16, GT], BF16)
    wblk = [p.sb("wblkD%d" % i, [128, 16, 512], BF16) for i in range(2)]
    x1g = p.sb("x1g", [128, NTS, 2048], BF16)
    x1 = p.sb("x1t", [128, 2048], F32)
    h2 = p.sb("h2t", [128, 2048], BF16)
    h2Ts = p.sb("h2Ts", [128, 16, 128], BF16)
    ss = p.sb("ssD", [128, 2], F32)
    psb = k.psb
    pbt = k.pbt
    cnt = {"w": 0}
    p.op("dve", lambda e: e.memset(Upad[:], 0.0), writes=[Upad])

    def wload(src_ap, nk):
        w = wblk[cnt["w"] % 2]
        cnt["w"] += 1
        for h in range(0, nk, 8):
            p.dma("pool", w[:, h:h + 8, :], src_ap[:, h:h + 8, :], reads=[], writes=[(w.name, h // 8)])
        return w

    wpo_v = k.wpo_d.rearrange("(kc q) n -> q kc n", q=128)
    wro_v = k.wro_d.rearrange("(kc q) n -> q kc n", q=128)
    wout_v = k.wout_d.rearrange("(kc q) n -> q kc n", q=128)

    def readout_tile(tg, ts):
        ti = tg * NTS + ts
        t0 = ti * 128
        p.dma("sp", ya[:], k.Yscr[0][t0:t0 + 128, :], reads=[], writes=[ya])
        p.dma("sp", yb[:], k.Yscr[1][t0:t0 + 128, :], reads=[], writes=[yb])
        p.dma("sp", vt[:], k.Vtok[256 + t0:256 + t0 + 128, :], reads=[], writes=[vt])
        p.dma("sp", bc[:], k.bcoef[t0:t0 + 128, :], reads=[], writes=[bc])
        p.dma("sp", sgdt[:], k.sgdT.rearrange("(kc q) t -> q kc t", q=128)[:, :, t0:t0 + 128], reads=[], writes=[sgdt])
        y3 = ya[:].rearrange("p (h c) -> p h c", c=64)
        p.op("dve", lambda e: e.tensor_tensor(out=ya[:], in0=ya[:], in1=yb[:], op=ALU.add), reads=[ya, yb], writes=[ya])
        p.op("dve", lambda e: e.tensor_reduce(out=st1[:], in_=y3, axis=AX.X, op=ALU.add), reads=[ya], writes=[st1])
        p.op("act", lambda e: e.activation(out=sq[:], in_=ya[:], func=AF.Square), reads=[ya], writes=[sq])
        p.op("dve", lambda e: e.tensor_reduce(out=st2[:], in_=sq[:].rearrange("p (h c) -> p h c", c=64), axis=AX.X, op=ALU.add),
             reads=[sq], writes=[st2])
        p.op("dve", lambda e: e.tensor_scalar(out=st1[:], in0=st1[:], scalar1=1.0 / 64, scalar2=None, op0=ALU.mult), reads=[st1], writes=[st1])
        p.op("dve", lambda e: e.tensor_tensor(out=st3[:], in0=st1[:], in1=st1[:], op=ALU.mult), reads=[st1], writes=[st3])
        p.op("dve", lambda e: e.scalar_tensor_tensor(out=st2[:], in0=st2[:], scalar=1.0 / 64, in1=st3[:], op0=ALU.mult, op1=ALU.subtract),
             reads=[st2, st3], writes=[st2])
        p.op("act", lambda e: e.activation(out=st2[:], in_=st2[:], func=AF.Sqrt, bias=gneps[:, 0:1]), reads=[st2, gneps], writes=[st2])
        p.op("dve", lambda e: e.reciprocal(out=st2[:], in_=st2[:]), reads=[st2], writes=[st2])
        p.op("dve", lambda e: e.tensor_tensor(out=y3, in0=y3, in1=st1[:].unsqueeze(2).to_broadcast([128, 32, 64]), op=ALU.subtract),
             reads=[ya, st1], writes=[ya])
        p.op("dve", lambda e: e.tensor_tensor(out=y3, in0=y3, in1=st2[:].unsqueeze(2).to_broadcast([128, 32, 64]), op=ALU.mult),
             reads=[ya, st2], writes=[ya])
        p.op("dve", lambda e: e.tensor_tensor(out=ya[:], in0=ya[:], in1=k.rowt["lnxw"][:], op=ALU.mult), reads=[ya, k.rowt["lnxw"]], writes=[ya])
        p.op("dve", lambda e: e.tensor_tensor(out=ya[:], in0=ya[:], in1=k.rowt["lnxb"][:], op=ALU.add), reads=[ya, k.rowt["lnxb"]], writes=[ya])
        p.op("dve", lambda e: e.tensor_tensor(out=yb[:].rearrange("p (h c) -> p h c", c=64), in0=vt[:].rearrange("p (h c) -> p h c", c=64),
                                              in1=bc[:].unsqueeze(2).to_broadcast([128, 32, 64]), op=ALU.mult), reads=[vt, bc], writes=[yb])
        p.op("dve", lambda e: e.tensor_tensor(out=ya[:], in0=ya[:], in1=yb[:], op=ALU.add), reads=[ya, yb], writes=[ya])
        for cb in range(4):
            ps = psb[cb % 2]
            for kc in range(2):
                p.op("pe", lambda e, cb=cb, kc=kc, ps=ps: e.matmul(ps[:, :], lhsT=sgdt[:, kc, :], rhs=g2w[:, kc, cb * 512:(cb + 1) * 512],
                                                                  start=(kc == 0), stop=(kc == 1)), reads=[sgdt, g2w], writes=[ps])
            p.op("dve", lambda e, cb=cb, ps=ps: e.tensor_tensor(out=prebf[:, cb * 512:(cb + 1) * 512], in0=ya[:, cb * 512:(cb + 1) * 512],
                                                                 in1=ps[:, :], op=ALU.mult), reads=[ya, ps], writes=[(prebf.name, cb)])
        for half in range(2):
            pt = pbt[half]
            for j in range(8):
                kc = half * 8 + j
                p.op("pe", lambda e, kc=kc, j=j, pt=pt: e.transpose(pt[:, j * 128:(j + 1) * 128], prebf[:, kc * 128:(kc + 1) * 128], ident[:]),
                     reads=[prebf, ident], writes=[pt])
            if half:
                p.op("act", lambda e, half=half, pt=pt: e.activation(out=preT[:, half * 8:(half + 1) * 8, ts * 128:(ts + 1) * 128],
                                                                    in_=pt[:].rearrange("p (a b) -> p a b", b=128), func=AF.Copy),
                     reads=[pt], writes=[(preT.name, ts)])
            else:
                p.op("dve", lambda e, half=half, pt=pt: e.tensor_copy(out=preT[:, half * 8:(half + 1) * 8, ts * 128:(ts + 1) * 128],
                                                                     in_=pt[:].rearrange("p (a b) -> p a b", b=128)),
                     reads=[pt], writes=[(preT.name, ts)])

    def pool_group(tg):
        t0 = tg * GT
        for cc in range(8):
            gi = cc // 2
            p.dma("sp", ubf[:], k.uT[cc * 128:(cc + 1) * 128, t0:t0 + GT], reads=[], writes=[ubf])
            p.op("act", lambda e: e.activation(out=Upad[:, :, 16:80], in_=ubf[:].rearrange("p (r c) -> p r c", c=64), func=AF.Copy),
                 reads=[ubf], writes=[Upad])
            src, lo, hi = Upad, 2, 94
            p.op("dve", lambda e: e.tensor_tensor(out=Sa[:, :, 2:94], in0=Upad[:, :, 1:93], in1=Upad[:, :, 2:94], op=ALU.add),
                 reads=[Upad], writes=[Sa])
            cur, oth = Sa, Sb_
            sh = 1
            lo, hi = 2, 94
            for step in range(gi):
                nlo, nhi = lo + sh, hi - sh
                p.op("dve", lambda e, cur=cur, oth=oth, nlo=nlo, nhi=nhi, sh=sh: e.tensor_tensor(
                    out=oth[:, :, nlo:nhi], in0=cur[:, :, nlo - sh:nhi - sh], in1=cur[:, :, nlo + sh:nhi + sh], op=ALU.add),
                    reads=[cur], writes=[oth])
                cur, oth = oth, cur
                lo, hi = nlo, nhi
                sh *= 2
            p.op("dve", lambda e, cur=cur: e.tensor_scalar(out=dtmp[:], in0=cur[:, :, 16:80], scalar1=poolc[:, 256:257], scalar2=None, op0=ALU.mult),
                 reads=[cur, poolc], writes=[dtmp])
            p.op("dve", lambda e, cur=cur: e.scalar_tensor_tensor(out=dtmp[:], in0=cur[:, :, 17:81], scalar=poolc[:, 257:258], in1=dtmp[:],
                                                                  op0=ALU.mult, op1=ALU.add), reads=[cur, poolc, dtmp], writes=[dtmp])
            p.op("dve", lambda e, gi=gi: e.tensor_tensor(out=dtmp[:], in0=dtmp[:],
                                                        in1=poolc[:, gi * 64:(gi + 1) * 64].unsqueeze(1).to_broadcast([128, NR, 64]), op=ALU.mult),
                 reads=[dtmp, poolc], writes=[dtmp])
            p.op("dve", lambda e, cc=cc: e.tensor_tensor(out=dT[:, cc, :].rearrange("p (r c) -> p r c", c=64), in0=dtmp[:], in1=Upad[:, :, 16:80],
                                                        op=ALU.subtract), reads=[dtmp, Upad], writes=[(dT.name, cc)])
        for gi in range(4):
            for dc in range(2):
                ps = psb[(gi * 2 + dc) % 2]
                for kc in range(2):
                    p.op("pe", lambda e, gi=gi, dc=dc, kc=kc, ps=ps: e.matmul(ps[:, 0:GT], lhsT=poolw[:, gi, kc, dc * 128:(dc + 1) * 128],
                                                                            rhs=dT[:, gi * 2 + kc, :], start=(kc == 0), stop=(kc == 1)),
                         reads=[poolw, (dT.name, gi * 2 + kc)], writes=[ps])
                oc = gi * 2 + dc
                p.op("act", lambda e, oc=oc, ps=ps: e.activation(out=zT[:, oc, :], in_=ps[:, 0:GT], func=AF.Copy, scale=pools[:, oc:oc + 1]),
                     reads=[ps, pools], writes=[(zT.name, oc)])

    def merge_group(tg):
        t0 = tg * GT
        for blk in range(4):
            wp = wload(wpo_v[:, :, blk * 512:(blk + 1) * 512], 8)
            wr = wload(wro_v[:, :, blk * 512:(blk + 1) * 512], 16)
            for j in range(4):
                fc = blk * 4 + j
                p.dma("sp", sgt[0][:], k.sgT[fc * 128:(fc + 1) * 128, t0:t0 + GT], reads=[], writes=[sgt[0]])
                p.dma("sp", sgt[1][:], k.sgT[2048 + fc * 128:2048 + (fc + 1) * 128, t0:t0 + GT], reads=[], writes=[sgt[1]])
                ps = psb[2]
                for kc in range(8):
                    p.op("pe", lambda e, kc=kc, j=j, wp=wp, ps=ps: e.matmul(ps[:, 0:GT], lhsT=wp[:, kc, j * 128:(j + 1) * 128], rhs=zT[:, kc, :],
                                                                          start=(kc == 0), stop=(kc == 7)), reads=[(wp.name, 0), zT], writes=[ps])
                p.op("dve", lambda e, ps=ps: e.tensor_tensor(out=mrg[:], in0=sgt[0][:], in1=ps[:, 0:GT], op=ALU.mult), reads=[sgt[0], ps], writes=[mrg])
                ps2 = psb[3]
                for kc in range(16):
                    p.op("pe", lambda e, kc=kc, j=j, wr=wr, ps2=ps2: e.matmul(ps2[:, 0:GT], lhsT=wr[:, kc, j * 128:(j + 1) * 128], rhs=preT[:, kc, :],
                                                                            start=(kc == 0), stop=(kc == 15)),
                         reads=[(wr.name, kc // 8), preT], writes=[ps2])
                p.op("dve", lambda e, ps2=ps2: e.tensor_tensor(out=sgt[1][:], in0=sgt[1][:], in1=ps2[:, 0:GT], op=ALU.mult), reads=[sgt[1], ps2], writes=[sgt[1]])
                p.op("dve", lambda e, fc=fc: e.tensor_tensor(out=mT[:, fc, :], in0=mrg[:], in1=sgt[1][:], op=ALU.add), reads=[mrg, sgt[1]],
                     writes=[(mT.name, fc)])

    def outproj_group(tg):
        for cb in range(4):
            w = wload(wout_v[:, :, cb * 512:(cb + 1) * 512], 16)
            cs = slice(cb * 512, (cb + 1) * 512)
            for ts in range(NTS):
                ps = psb[4 + ts % 2]
                for kc in range(16):
                    p.op("pe", lambda e, kc=kc, w=w, ps=ps, ts=ts: e.matmul(ps[:, :], lhsT=mT[:, kc, ts * 128:(ts + 1) * 128], rhs=w[:, kc, :],
                                                                          start=(kc == 0), stop=(kc == 15)), reads=[(w.name, kc // 8), mT], writes=[ps])
                p.op("dve", lambda e, cs=cs, ps=ps, ts=ts: e.tensor_tensor(out=x1g[:, ts, cs], in0=ps[:, :], in1=k.rowt["gt1"][:, cs], op=ALU.mult),
                     reads=[ps, k.rowt["gt1"]], writes=[(x1g.name, ts)])
        for ts in range(NTS):
            outproj_tile(tg, ts)

    def outproj_tile(tg, ts):
            ti = tg * NTS + ts
            t0 = ti * 128
            p.dma("sp", yb[:], k.xown_d[t0:t0 + 128, :], reads=[], writes=[yb])
            p.op("dve", lambda e: e.tensor_tensor(out=x1[:], in0=x1g[:, ts, :], in1=yb[:], op=ALU.add), reads=[(x1g.name, ts), yb], writes=[x1])
            p.dma("sp", k.x1scr[t0:t0 + 128, :], x1[:], reads=[x1], writes=[])
            p.op("act", lambda e: e.activation(out=sq[:], in_=x1[:], func=AF.Square), reads=[x1], writes=[sq])
            p.op("dve", lambda e: e.tensor_reduce(out=ss[:, 0:1], in_=sq[:], axis=AX.X, op=ALU.add), reads=[sq], writes=[ss])
            p.op("act", lambda e: e.activation(out=ss[:, 1:2], in_=ss[:, 0:1], func=AF.Sqrt, scale=1.0 / D, bias=k.eps_t[:, 0:1]),
                 reads=[ss, k.eps_t], writes=[ss])
            p.op("dve", lambda e: e.reciprocal(out=ss[:, 1:2], in_=ss[:, 1:2]), reads=[ss], writes=[ss])
            p.op("dve", lambda e: e.scalar_tensor_tensor(out=sq[:], in0=x1[:], scalar=ss[:, 1:2], in1=k.rowt["A2"][:], op0=ALU.mult, op1=ALU.mult),
                 reads=[x1, ss, k.rowt["A2"]], writes=[sq])
            p.op("dve", lambda e: e.tensor_tensor(out=h2[:], in0=sq[:], in1=k.rowt["sh2"][:], op=ALU.add), reads=[sq, k.rowt["sh2"]], writes=[h2])
            p.dma("sp", k.h2tok[t0:t0 + 128, :], h2[:], reads=[h2], writes=[])
            for half in range(2):
                pt = pbt[half]
                for j in range(8):
                    kc = half * 8 + j
                    p.op("pe", lambda e, kc=kc, j=j, pt=pt: e.transpose(pt[:, j * 128:(j + 1) * 128], h2[:, kc * 128:(kc + 1) * 128], ident[:]),
                         reads=[h2, ident], writes=[pt])
                if half:
                    p.op("act", lambda e, pt=pt: e.activation(out=h2Ts[:, 8:16, :].rearrange("p a b -> p (a b)"), in_=pt[:], func=AF.Copy),
                         reads=[pt], writes=[(h2Ts.name, 1)])
                else:
                    p.op("dve", lambda e, pt=pt: e.tensor_copy(out=h2Ts[:, 0:8, :].rearrange("p a b -> p (a b)"), in_=pt[:]),
                         reads=[pt], writes=[(h2Ts.name, 0)])
            p.dma("sp", k.h2T.rearrange("(kc q) t -> q kc t", q=128)[:, :, t0:t0 + 128], h2Ts[:], reads=[h2Ts], writes=[])

    ng = getattr(k, "ngroupsD", None) or (2048 // GT)
    for tg in range(ng):
        for ts in range(NTS):
            readout_tile(tg, ts)
        pool_group(tg)
        merge_group(tg)
        outproj_group(tg)
    p.barrier()
    p.release(mD)


import numpy as np

CAP = 768
SUBS = ((0, 512), (512, 256))
NSLOT = 64 * CAP
D = 2048


def phaseE(k):
    p, nc = k.p, k.nc
    din, dscr = k.din, k.dscr
    k.routw_d = din("routw", [2048, 64])
    k.routb_d = din("routb", [1, 128])
    k.tri_d = din("tri", [128, 128])
    k.exg_d = din("exg", [64, 2048, 512])
    k.exu_d = din("exu", [64, 2048, 512])
    k.exd_d = din("exd", [64, 512, 2048])
    k.shg_d = din("shg", [2048, 512])
    k.shu_d = din("shu", [2048, 512])
    k.shd_d = din("shd", [512, 2048])
    k.out_d = nc.dram_tensor("out", [2048, 2048], F32, kind="ExternalOutput").ap()
    k.Xg = dscr("Xg", [NSLOT, 2048], BF16)
    k.Yg = dscr("Yg", [NSLOT, 2048], BF16)
    k.shscr = dscr("shscr", [2048, 2048], F32)
    k.routdbg = dscr("routdbg", [2048, 16], F32)
    psb, pbt = k.psb, k.pbt
    ident = k.ident

    slots_all = p.sb("slots_all", [128, 128], I32)
    wk_all = p.sb("wk_all", [128, 16, 8], F32)
    mE = p.mark()

    routw = p.sb("routw_sb", [128, 16, 64], BF16)
    routb = p.sb("routb_sb", [128, 128], F32)
    tri = p.sb("tri_sb", [128, 128], BF16)
    base = p.sb("base_cnt", [128, 64], F32)
    shg = p.sb("shg_sb", [128, 16, 512], BF16)
    shu = p.sb("shu_sb", [128, 16, 512], BF16)
    shd = p.sb("shd_sb", [128, 4, 2048], BF16)
    p.dma("pool", routw[:], k.routw_d.rearrange("(kc q) n -> q kc n", q=128))
    p.dma("sp", routb[:], k.routb_d.partition_broadcast(128))
    p.dma("pool", tri[:], k.tri_d)
    for h in range(2):
        p.dma("pool", shg[:, h * 8:(h + 1) * 8, :], k.shg_d.rearrange("(kc q) n -> q kc n", q=128)[:, h * 8:(h + 1) * 8, :], writes=[(shg.name, h)])
        p.dma("pool", shu[:, h * 8:(h + 1) * 8, :], k.shu_d.rearrange("(kc q) n -> q kc n", q=128)[:, h * 8:(h + 1) * 8, :], writes=[(shu.name, h)])
    p.dma("pool", shd[:], k.shd_d.rearrange("(kc q) n -> q kc n", q=128))
    p.op("dve", lambda e: e.memset(base[:], 0.0), writes=[base])
    h2Tg = p.sb("h2Tg", [128, 16, 512], BF16)
    h2r = p.sb("h2r", [128, 2048], BF16)
    sg = p.sb("sgE", [128, 512], F32)
    actT = p.sb("actT", [128, 4, 512], BF16)
    sho = p.sb("sho", [128, 2048], F32)
    R = {}
    for nm, w in (("sc", 64), ("sel", 64), ("tmp", 64), ("msel", 64), ("emask", 64), ("wd", 64), ("key", 64), ("oh", 64), ("rk", 64),
                  ("m1", 8), ("m2", 8), ("gs", 8), ("g8", 8), ("gm", 8), ("pen", 8), ("e8", 8), ("k8", 8), ("den", 2)):
        R[nm] = p.sb("R_" + nm, [128, w], F32)
    emb = p.sb("emask_bf", [128, 64], BF16)
    h2Tv = k.h2T.rearrange("(kc q) t -> q kc t", q=128)

    def shared_group(tg):
        t0 = tg * 512
        for h in range(2):
            p.dma("sp", h2Tg[:, h * 8:(h + 1) * 8, :], h2Tv[:, h * 8:(h + 1) * 8, t0:t0 + 512], reads=[], writes=[(h2Tg.name, h)])
        for dc in range(4):
            for kc in range(16):
                p.op("pe", lambda e, dc=dc, kc=kc: e.matmul(psb[0][:, :], lhsT=shg[:, kc, dc * 128:(dc + 1) * 128], rhs=h2Tg[:, kc, :],
                                                           start=(kc == 0), stop=(kc == 15)), reads=[shg, h2Tg], writes=[psb[0]])
            for kc in range(16):
                p.op("pe", lambda e, dc=dc, kc=kc: e.matmul(psb[1][:, :], lhsT=shu[:, kc, dc * 128:(dc + 1) * 128], rhs=h2Tg[:, kc, :],
                                                           start=(kc == 0), stop=(kc == 15)), reads=[shu, h2Tg], writes=[psb[1]])
            p.op("act", lambda e: e.activation(out=sg[:], in_=psb[0][:, :], func=AF.Silu), reads=[psb[0]], writes=[sg])
            p.op("dve", lambda e, dc=dc: e.tensor_tensor(out=actT[:, dc, :], in0=sg[:], in1=psb[1][:, :], op=ALU.mult), reads=[sg, psb[1]],
                 writes=[(actT.name, dc)])
        for ts in range(4):
            for cb in range(4):
                ps = psb[2 + cb % 2]
                for dc in range(4):
                    p.op("pe", lambda e, ts=ts, cb=cb, dc=dc, ps=ps: e.matmul(ps[:, :], lhsT=actT[:, dc, ts * 128:(ts + 1) * 128],
                                                                            rhs=shd[:, dc, cb * 512:(cb + 1) * 512], start=(dc == 0), stop=(dc == 3)),
                         reads=[actT, shd], writes=[ps])
                p.op("act" if cb % 2 else "dve",
                     (lambda e, cb=cb, ps=ps: e.activation(out=sho[:, cb * 512:(cb + 1) * 512], in_=ps[:, :], func=AF.Copy)) if cb % 2 else
                     (lambda e, cb=cb, ps=ps: e.tensor_copy(out=sho[:, cb * 512:(cb + 1) * 512], in_=ps[:, :])),
                     reads=[ps], writes=[(sho.name, cb)])
            p.dma("sp", k.shscr[t0 + ts * 128:t0 + (ts + 1) * 128, :], sho[:], reads=[sho], writes=[])
            route_tile(tg * 4 + ts, ts)

    def route_tile(ti, ts):
        t0 = ti * 128
        ps = psb[4]
        for kc in range(16):
            p.op("pe", lambda e, kc=kc: e.matmul(ps[:, 0:64], lhsT=h2Tg[:, kc, ts * 128:(ts + 1) * 128], rhs=routw[:, kc, :],
                                                start=(kc == 0), stop=(kc == 15)), reads=[h2Tg, routw], writes=[ps])
        sc, sel, tmp, msel, emask, wd, key, oh, rk = (R[n] for n in ("sc", "sel", "tmp", "msel", "emask", "wd", "key", "oh", "rk"))
        m1, m2, gs, g8, gm, pen, e8, k8, den = (R[n] for n in ("m1", "m2", "gs", "g8", "gm", "pen", "e8", "k8", "den"))
        v3 = lambda t: t[:].rearrange("p (g e) -> p g e", e=8)
        b3 = lambda t: t[:].unsqueeze(2).to_broadcast([128, 8, 8])
        p.op("act", lambda e: e.activation(out=sc[:], in_=ps[:, 0:64], func=AF.Sigmoid), reads=[ps], writes=[sc])
        p.op("dve", lambda e: e.tensor_tensor(out=sel[:], in0=sc[:], in1=routb[:, 0:64], op=ALU.add), reads=[sc, routb], writes=[sel])
        p.op("dve", lambda e: e.tensor_reduce(out=m1[:], in_=v3(sel), axis=AX.X, op=ALU.max), reads=[sel], writes=[m1])
        p.op("dve", lambda e: e.tensor_tensor(out=v3(tmp), in0=v3(sel), in1=b3(m1), op=ALU.is_equal), reads=[sel, m1], writes=[tmp])
        p.op("dve", lambda e: e.scalar_tensor_tensor(out=tmp[:], in0=tmp[:], scalar=-1e9, in1=sel[:], op0=ALU.mult, op1=ALU.add),
             reads=[tmp, sel], writes=[tmp])
        p.op("dve", lambda e: e.tensor_reduce(out=m2[:], in_=v3(tmp), axis=AX.X, op=ALU.max), reads=[tmp], writes=[m2])
        p.op("dve", lambda e: e.tensor_tensor(out=gs[:], in0=m1[:], in1=m2[:], op=ALU.add), reads=[m1, m2], writes=[gs])
        p.op("dve", lambda e: e.max(out=g8[:], in_=gs[:]), reads=[gs], writes=[g8])
        p.op("dve", lambda e: e.tensor_scalar(out=gm[:], in0=gs[:], scalar1=g8[:, 3:4], scalar2=None, op0=ALU.is_ge), reads=[gs, g8], writes=[gm])
        p.op("dve", lambda e: e.tensor_scalar(out=pen[:], in0=gm[:], scalar1=1e9, scalar2=-1e9, op0=ALU.mult, op1=ALU.add), reads=[gm], writes=[pen])
        p.op("dve", lambda e: e.tensor_tensor(out=v3(msel), in0=v3(sel), in1=b3(gm), op=ALU.mult), reads=[sel, gm], writes=[msel])
        p.op("dve", lambda e: e.tensor_tensor(out=v3(msel), in0=v3(msel), in1=b3(pen), op=ALU.add), reads=[msel, pen], writes=[msel])
        p.op("dve", lambda e: e.max(out=e8[:], in_=msel[:]), reads=[msel], writes=[e8])
        p.op("dve", lambda e: e.tensor_scalar(out=emask[:], in0=msel[:], scalar1=e8[:, 7:8], scalar2=None, op0=ALU.is_ge), reads=[msel, e8], writes=[emask])
        p.op("dve", lambda e: e.tensor_tensor(out=wd[:], in0=sc[:], in1=emask[:], op=ALU.mult), reads=[sc, emask], writes=[wd])
        p.op("dve", lambda e: e.tensor_reduce(out=den[:, 0:1], in_=wd[:], axis=AX.X, op=ALU.add), reads=[wd], writes=[den])
        p.op("dve", lambda e: e.reciprocal(out=den[:, 1:2], in_=den[:, 0:1]), reads=[den], writes=[den])
        p.op("dve", lambda e: e.tensor_scalar(out=wd[:], in0=wd[:], scalar1=den[:, 1:2], scalar2=2.5, op0=ALU.mult, op1=ALU.mult), reads=[wd, den], writes=[wd])
        p.op("act", lambda e: e.activation(out=emb[:], in_=emask[:], func=AF.Copy), reads=[emask], writes=[emb])
        ps2 = psb[5]
        p.op("pe", lambda e: e.matmul(ps2[:, 0:64], lhsT=tri[:], rhs=emb[:], start=True, stop=True), reads=[tri, emb], writes=[ps2])
        p.op("dve", lambda e: e.tensor_tensor(out=rk[:], in0=ps2[:, 0:64], in1=base[:], op=ALU.add), reads=[ps2, base], writes=[rk])
        p.op("dve", lambda e: e.tensor_scalar(out=oh[:], in0=rk[:], scalar1=float(CAP) - 0.5, scalar2=None, op0=ALU.is_lt), reads=[rk], writes=[oh])
        p.op("dve", lambda e: e.tensor_tensor(out=oh[:], in0=oh[:], in1=emask[:], op=ALU.mult), reads=[oh, emask], writes=[oh])
        p.op("dve", lambda e: e.tensor_tensor(out=key[:], in0=rk[:], in1=routb[:, 64:128], op=ALU.add), reads=[rk, routb], writes=[key])
        p.op("dve", lambda e: e.scalar_tensor_tensor(out=key[:], in0=key[:], scalar=1.0, in1=oh[:], op0=ALU.add, op1=ALU.mult), reads=[key, oh], writes=[key])
        p.op("dve", lambda e: e.tensor_scalar(out=key[:], in0=key[:], scalar1=-1.0, scalar2=None, op0=ALU.add), reads=[key], writes=[key])
        p.op("dve", lambda e: e.max(out=k8[:], in_=key[:]), reads=[key], writes=[k8])
        p.op("dve", lambda e: e.tensor_copy(out=slots_all[:, ti * 8:(ti + 1) * 8], in_=k8[:]), reads=[k8], writes=[(slots_all.name, ti)])
        for j in range(8):
            p.op("dve", lambda e, j=j: e.tensor_scalar(out=oh[:], in0=key[:], scalar1=k8[:, j:j + 1], scalar2=None, op0=ALU.is_equal),
                 reads=[key, k8], writes=[oh])
            p.op("dve", lambda e: e.tensor_tensor(out=oh[:], in0=oh[:], in1=wd[:], op=ALU.mult), reads=[oh, wd], writes=[oh])
            p.op("dve", lambda e, j=j: e.tensor_reduce(out=wk_all[:, ti, j:j + 1], in_=oh[:], axis=AX.X, op=ALU.add), reads=[oh],
                 writes=[(wk_all.name, ti)])
        p.op("pe", lambda e: e.matmul(ps2[:, 64:128], lhsT=k.ones_bf[:], rhs=emb[:], start=True, stop=True), reads=[k.ones_bf, emb], writes=[ps2])
        p.op("dve", lambda e: e.tensor_tensor(out=base[:], in0=base[:], in1=ps2[:, 64:128], op=ALU.add), reads=[base, ps2], writes=[base])
        p.dma("sp", h2r[:], k.h2tok[t0:t0 + 128, :], reads=[], writes=[h2r])
        for j in range(8):
            p.dma_fn("pool", lambda e, j=j: e.indirect_dma_start(
                out=k.Xg, out_offset=bass.IndirectOffsetOnAxis(ap=slots_all[:, ti * 8 + j:ti * 8 + j + 1], axis=0), in_=h2r[:], in_offset=None,
                bounds_check=p.getreg(e, NSLOT - 1), oob_is_err=False), reads=[h2r, (slots_all.name, ti)], writes=[])
        if "routdbg" in k.dbg:
            p.dma("sp", k.routdbg[t0:t0 + 128, 0:8], k8[:], reads=[k8], writes=[])
            p.dma("sp", k.routdbg[t0:t0 + 128, 8:16], wk_all[:, ti, :], reads=[(wk_all.name, ti)], writes=[])

    ngE = getattr(k, "ngroupsE", None) or 4
    for tg in range(ngE):
        shared_group(tg)
    p.barrier()
    p.release(mE)
    if getattr(k, "ecut", None) == 1:
        return

    NEXP = getattr(k, "nexp", None) or 64
    Wg = [p.sb("Wg%d" % i, [128, 16, 512], BF16) for i in range(2)]
    Wu = [p.sb("Wu%d" % i, [128, 16, 512], BF16) for i in range(2)]
    Wd = [p.sb("Wd%d" % i, [128, 4, 2048], BF16) for i in range(2)]
    Xs = p.sb("Xs", [128, 4, 2048], BF16)
    XT = p.sb("XT", [128, 16, 512], BF16)
    sg2 = p.sb("sg2", [128, 512], F32)
    act2 = p.sb("act2", [128, 4, 512], BF16)
    yst = [p.sb("yst%d" % i, [128, 2048], BF16) for i in range(2)]
    exg_v = k.exg_d.rearrange("e (kc q) n -> e q kc n", q=128)
    exu_v = k.exu_d.rearrange("e (kc q) n -> e q kc n", q=128)
    exd_v = k.exd_d.rearrange("e (kc q) n -> e q kc n", q=128)
    cnt = {"y": 0}

    def load_w(e_):
        b = e_ % 2
        for h in range(2):
            p.dma("pool", Wg[b][:, h * 8:(h + 1) * 8, :], exg_v[e_][:, h * 8:(h + 1) * 8, :], reads=[], writes=[(Wg[b].name, h)])
            p.dma("pool", Wu[b][:, h * 8:(h + 1) * 8, :], exu_v[e_][:, h * 8:(h + 1) * 8, :], reads=[], writes=[(Wu[b].name, h)])
        for h in range(2):
            p.dma("pool", Wd[b][:, h * 2:(h + 1) * 2, :], exd_v[e_][:, h * 2:(h + 1) * 2, :], reads=[], writes=[(Wd[b].name, h)])

    def expert(e_):
        for off, n in SUBS:
            expert_sub(e_, off, n)

    def expert_sub(e_, off, n):
        b = e_ % 2
        nst = n // 128
        r0 = e_ * CAP + off
        p.dma("sp", Xs[:, 0:nst, :], k.Xg[r0:r0 + n, :].rearrange("(st q) f -> q st f", q=128), reads=[], writes=[Xs])
        for st in range(nst):
            for half in range(2):
                pt = pbt[half]
                for j in range(8):
                    kc = half * 8 + j
                    p.op("pe", lambda e, st=st, kc=kc, j=j, pt=pt: e.transpose(pt[:, j * 128:(j + 1) * 128], Xs[:, st, kc * 128:(kc + 1) * 128], ident[:]),
                         reads=[Xs, ident], writes=[pt])
                if half:
                    p.op("act", lambda e, st=st, pt=pt: e.activation(out=XT[:, 8:16, st * 128:(st + 1) * 128],
                                                                    in_=pt[:].rearrange("p (a b) -> p a b", b=128), func=AF.Copy),
                         reads=[pt], writes=[(XT.name, st)])
                else:
                    p.op("dve", lambda e, st=st, pt=pt: e.tensor_copy(out=XT[:, 0:8, st * 128:(st + 1) * 128],
                                                                     in_=pt[:].rearrange("p (a b) -> p a b", b=128)),
                         reads=[pt], writes=[(XT.name, st)])
        for dc in range(4):
            for kc in range(16):
                p.op("pe", lambda e, dc=dc, kc=kc: e.matmul(psb[0][:, 0:n], lhsT=Wg[b][:, kc, dc * 128:(dc + 1) * 128], rhs=XT[:, kc, 0:n],
                                                           start=(kc == 0), stop=(kc == 15)), reads=[(Wg[b].name, kc // 8), XT], writes=[psb[0]])
            for kc in range(16):
                p.op("pe", lambda e, dc=dc, kc=kc: e.matmul(psb[1][:, 0:n], lhsT=Wu[b][:, kc, dc * 128:(dc + 1) * 128], rhs=XT[:, kc, 0:n],
                                                           start=(kc == 0), stop=(kc == 15)), reads=[(Wu[b].name, kc // 8), XT], writes=[psb[1]])
            p.op("act", lambda e: e.activation(out=sg2[:, 0:n], in_=psb[0][:, 0:n], func=AF.Silu), reads=[psb[0]], writes=[sg2])
            p.op("dve", lambda e, dc=dc: e.tensor_tensor(out=act2[:, dc, 0:n], in0=sg2[:, 0:n], in1=psb[1][:, 0:n], op=ALU.mult), reads=[sg2, psb[1]],
                 writes=[(act2.name, dc)])
        for st in range(nst):
            ys = yst[cnt["y"] % 2]
            cnt["y"] += 1
            for cb in range(4):
                ps = psb[2 + cb]
                for dc in range(4):
                    p.op("pe", lambda e, st=st, cb=cb, dc=dc, ps=ps: e.matmul(ps[:, :], lhsT=act2[:, dc, st * 128:(st + 1) * 128],
                                                                            rhs=Wd[b][:, dc, cb * 512:(cb + 1) * 512], start=(dc == 0), stop=(dc == 3)),
                         reads=[act2, (Wd[b].name, dc // 2)], writes=[ps])
                if cb % 2:
                    p.op("act", lambda e, cb=cb, ps=ps, ys=ys: e.activation(out=ys[:, cb * 512:(cb + 1) * 512], in_=ps[:, :], func=AF.Copy),
                         reads=[ps], writes=[(ys.name, cb)])
                else:
                    p.op("dve", lambda e, cb=cb, ps=ps, ys=ys: e.tensor_copy(out=ys[:, cb * 512:(cb + 1) * 512], in_=ps[:, :]),
                         reads=[ps], writes=[(ys.name, cb)])
            p.dma("sp", k.Yg[r0 + st * 128:r0 + (st + 1) * 128, :], ys[:], reads=[ys], writes=[])

    load_w(0)
    for e_ in range(NEXP):
        if e_ + 1 < NEXP:
            load_w(e_ + 1)
        expert(e_)
    p.barrier()
    p.release(mE)
    if getattr(k, "ecut", None) == 2:
        return

    rows = {}
    for i, nm in ((3, "gt2"),):
        rows[nm] = p.sb("rowE_" + nm, [128, 2048], F32)
        p.dma("sp", rows[nm][:], k.rowbuf[i:i + 1, :].partition_broadcast(128), reads=[], writes=[rows[nm]])
    rows["fing"] = p.sb("rowE_fing", [128, 2048], F32)
    p.dma("sp", rows["fing"][:], k.rows_d[2:3, :].partition_broadcast(128), reads=[], writes=[rows["fing"]])
    acc = p.sb("accE", [128, 2048], F32)
    yk = [p.sb("yk%d" % i, [128, 2048], BF16) for i in range(2)]
    x1t = p.sb("x1E", [128, 2048], F32)
    sqE = p.sb("sqE", [128, 2048], F32)
    ssE = p.sb("ssE", [128, 2], F32)

    def combine_tile(ti):
        t0 = ti * 128
        p.dma("sp", acc[:], k.shscr[t0:t0 + 128, :], reads=[], writes=[acc])
        p.dma("sp", x1t[:], k.x1scr[t0:t0 + 128, :], reads=[], writes=[x1t])
        for j in range(8):
            y = yk[j % 2]
            p.op("pool", lambda e, y=y: e.memset(y[:], 0.0), writes=[y])
            p.dma_fn("pool", lambda e, j=j, y=y: e.indirect_dma_start(
                out=y[:], out_offset=None, in_=k.Yg, in_offset=bass.IndirectOffsetOnAxis(ap=slots_all[:, ti * 8 + j:ti * 8 + j + 1], axis=0),
                bounds_check=p.getreg(e, NSLOT - 1), oob_is_err=False), reads=[y, slots_all], writes=[y])
            p.op("dve", lambda e, j=j, y=y: e.scalar_tensor_tensor(out=acc[:], in0=y[:], scalar=wk_all[:, ti, j:j + 1], in1=acc[:],
                                                                    op0=ALU.mult, op1=ALU.add), reads=[y, wk_all, acc], writes=[acc])
        p.op("dve", lambda e: e.tensor_tensor(out=acc[:], in0=acc[:], in1=rows["gt2"][:], op=ALU.mult), reads=[acc, rows["gt2"]], writes=[acc])
        p.op("dve", lambda e: e.tensor_tensor(out=x1t[:], in0=x1t[:], in1=acc[:], op=ALU.add), reads=[x1t, acc], writes=[x1t])
        p.op("act", lambda e: e.activation(out=sqE[:], in_=x1t[:], func=AF.Square), reads=[x1t], writes=[sqE])
        p.op("dve", lambda e: e.tensor_reduce(out=ssE[:, 0:1], in_=sqE[:], axis=AX.X, op=ALU.add), reads=[sqE], writes=[ssE])
        p.op("act", lambda e: e.activation(out=ssE[:, 1:2], in_=ssE[:, 0:1], func=AF.Sqrt, scale=1.0 / D, bias=k.eps_t[:, 0:1]),
             reads=[ssE, k.eps_t], writes=[ssE])
        p.op("dve", lambda e: e.reciprocal(out=ssE[:, 1:2], in_=ssE[:, 1:2]), reads=[ssE], writes=[ssE])
        p.op("dve", lambda e: e.scalar_tensor_tensor(out=sqE[:], in0=x1t[:], scalar=ssE[:, 1:2], in1=rows["fing"][:], op0=ALU.mult, op1=ALU.mult),
             reads=[x1t, ssE, rows["fing"]], writes=[sqE])
        p.dma("sp", k.out_d[t0:t0 + 128, :], sqE[:], reads=[sqE], writes=[], is_out=True)

    for ti in range(4 * ngE):
        combine_tile(ti)
    p.barrier()


import numpy as np

D = 2048


def fm(v, kc=16):
    return np.ascontiguousarray(v.reshape(kc, 128).T)


def prep_shared(inp):
    out = {}
    w_in = inp["w_in"][0]
    for half in (0, 1):
        u = w_in[:, 0:1024]
        slab = w_in[:, 1024:1024 + 6784]
        gates = w_in[:, 1024 + 6784:]
        r, kk_, v = slab[:, 0:2048], slab[:, 2048:4096], slab[:, 4096:6144]
        wd = [slab[:, 6144:6240], slab[:, 6240:6336]]
        ad = [slab[:, 6336:6432], slab[:, 6432:6528]]
        gd = slab[:, 6528:6784]
        dA, dB = (0, 1) if half == 0 else (1, 0)
        z32 = np.zeros((D, 32), np.float32)
        win = np.concatenate([u, r, kk_, v, wd[dA], z32, wd[dB], z32, ad[dA], z32, ad[dB], z32, gd, gates], axis=1)
        out[half] = {"win": np.ascontiguousarray(win)}
    return out


def prep_core(inp, shared, b, half):
    x = inp["x"][b]
    ctx = inp["ctx"][b]
    if half == 1:
        x = x[::-1]
        ctx = ctx[::-1]
    m = {}
    m["xT"] = np.ascontiguousarray(x.T)
    m["cxT"] = np.ascontiguousarray(ctx.T)
    c2 = np.stack([inp["c"][b], inp["c_ctx"]], axis=1)
    m["cT2"] = np.ascontiguousarray(c2.reshape(16, 128, 2).transpose(1, 0, 2))
    m["adaw"] = inp["ada_w"][0]
    m["adab"] = fm(inp["ada_b"][0], 96)
    m["g1"] = fm(inp["norm1_g"][0])
    m["win"] = shared[half]["win"]
    return m


def prep_core_b(inp, b, half, m):
    dA, dB = (0, 1) if half == 0 else (1, 0)
    mu = inp["shift_mu"][0]
    z32 = np.zeros(32, np.float32)
    secs = [mu[6144:6240], mu[6240:6336]]
    seca = [mu[6336:6432], mu[6432:6528]]
    mup = np.concatenate([mu[0:6144], secs[dA], z32, secs[dB], z32, seca[dA], z32, seca[dB], z32, mu[6528:6784]])
    cls = np.arange(6912) % 4
    valid = np.ones(6912, bool)
    for s0 in (6144, 6272, 6400, 6528):
        valid[s0 + 96:s0 + 128] = False
    def sel(mask):
        return np.where(mask & valid, mup, np.float32(0)).astype(np.float32)
    if half == 0:
        cm1, cp1, cm64, cp64 = sel(cls == 0), sel(cls == 1), sel(cls == 2), sel(cls == 3)
        ccm1, ccp1 = sel(cls % 2 == 0), sel(cls % 2 == 1)
    else:
        cm1, cp1, cm64, cp64 = sel(cls == 1), sel(cls == 0), sel(cls == 3), sel(cls == 2)
        ccm1, ccp1 = sel(cls % 2 == 1), sel(cls % 2 == 0)
    mupv = np.where(valid, mup, np.float32(0)).astype(np.float32)
    arrs = [mupv, cm1, cp1, cm64, cp64, ccm1, ccp1]
    m["mixc"] = np.ascontiguousarray(np.stack([a.reshape(54, 128).T for a in arrs], axis=1))
    w0, a0 = inp["decay_w0"][0], inp["iclr_a0"][0]
    vs = [w0[dA], w0[dB], a0[dA], a0[dB], inp["k_k"][0], inp["k_a"][0], inp["r_k"][0].reshape(-1)]
    m["vecs"] = np.ascontiguousarray(np.stack([fm(v) for v in vs], axis=1))
    w2, a2 = inp["decay_w2"][0], inp["iclr_a2"][0]
    m["lw2"] = np.ascontiguousarray(np.stack([w2[dA], w2[dB], a2[dA], a2[dB]], axis=1))
    pidx = np.arange(128)
    m["bones"] = (pidx[:, None] // 64 == pidx[None, :] // 64).astype(np.float32)
    m["hsel"] = (pidx[:, None] // 64 == np.arange(2)[None, :]).astype(np.float32)
    return m


def prep_core_c(m):
    t = np.arange(64)
    lt = (t[:, None] < t[None, :]).astype(np.float32)
    le = (t[:, None] <= t[None, :]).astype(np.float32)
    gt = (t[:, None] > t[None, :]).astype(np.float32)
    ge = (t[:, None] >= t[None, :]).astype(np.float32)
    mA = np.block([[lt, le], [lt, le]])
    mB = np.block([[gt, ge], [gt, ge]])
    m["cmask"] = np.ascontiguousarray(np.stack([mA, mB], axis=1).astype(np.float32))
    nA = lt.T.copy()
    nB = gt.T.copy()
    m["nmask"] = np.ascontiguousarray(np.stack([nA, nB, np.eye(64, dtype=np.float32)], axis=1).astype(np.float32))
    return m


def prep_core_de(inp, b, half, m, with_experts=True):
    x = inp["x"][b]
    if half == 1:
        x = x[::-1]
    m["xown"] = np.ascontiguousarray(x[:2048])
    m["rows"] = np.ascontiguousarray(np.stack([inp["lnx_w"][0], inp["lnx_b"][0], inp["final_g"]], axis=0))
    m["g2n"] = fm(inp["norm2_g"][0])
    m["g2w"] = inp["gate_g2"][0]
    m["poolw"] = inp["pool_w"][0]
    m["pools"] = fm(inp["pool_scale"][0], 8)
    m["wpo"] = inp["w_pool_out"][0]
    m["wro"] = inp["w_rwkv_out"][0]
    m["wout"] = inp["w_out"][0]
    t = np.arange(64)
    cnts = []
    for w in (2, 4, 8, 16):
        lo = np.clip(t - w // 2, 0, 64)
        hi = np.clip(t + (w - w // 2), 0, 64)
        c = (hi - lo).astype(np.float32)
        if half == 1:
            c = c[::-1]
        cnts.append(np.float32(1.0) / c)
    flags = np.array([1.0, 0.0] if half == 0 else [0.0, 1.0], np.float32)
    m["poolc"] = np.ascontiguousarray(np.concatenate(cnts + [flags]).astype(np.float32)[None, :])
    m["routw"] = inp["router_w"][0]
    ecap = (np.arange(64) * 768).astype(np.float32)
    m["routb"] = np.ascontiguousarray(np.concatenate([inp["router_bias"][0], ecap]).astype(np.float32)[None, :])
    tt = np.arange(128)
    m["tri"] = (tt[:, None] < tt[None, :]).astype(np.float32)
    if with_experts:
        m["exg"] = inp["exp_w_gate"][0]
        m["exu"] = inp["exp_w_up"][0]
        m["exd"] = inp["exp_w_down"][0]
    m["shg"] = inp["shared_w_gate"][0]
    m["shu"] = inp["shared_w_up"][0]
    m["shd"] = inp["shared_w_down"][0]
    return m


from concourse.bass_utils import run_bass_kernel_spmd


def kernel(**inputs):
    inp = {k_: np.asarray(v) for k_, v in inputs.items()}
    shared = prep_shared(inp)
    maps = []
    for core in range(8):
        b, half = core // 2, core % 2
        m = prep_core(inp, shared, b, half)
        m = prep_core_b(inp, b, half, m)
        m = prep_core_c(m)
        m = prep_core_de(inp, b, half, m)
        maps.append(m)
    nc, k = build(stage=5)
    res = run_bass_kernel_spmd(nc, maps, core_ids=list(range(8)))
    out = np.empty((4, 4096, 2048), np.float32)
    for core in range(8):
        b, half = core // 2, core % 2
        o = np.asarray(res.results[core]["out"])
        if half == 0:
            out[b, :2048] = o
        else:
            out[b, 2048:] = o[::-1]
    return out
```

```python
import numpy as np
import concourse.bass as bass
import concourse.mybir as mybir
from contextlib import ExitStack

F32 = mybir.dt.float32
BF16 = mybir.dt.bfloat16
I32 = mybir.dt.int32
U32 = mybir.dt.uint32
ALU = mybir.AluOpType
AF = mybir.ActivationFunctionType
AX = mybir.AxisListType

ENGS = ("pe", "act", "dve", "pool", "sp")
ARENA_BASE = 16640
ARENA_TOP = 229344
DTSIZE = {F32: 4, BF16: 2, I32: 4, U32: 4}
NDSEM = 6


class _Op:
    __slots__ = ("eng", "fn", "deps", "signal", "sigval", "is_dma", "dsem", "dval", "dprev", "idx")

    def __init__(self, eng, fn, is_dma):
        self.eng = eng
        self.fn = fn
        self.deps = []
        self.signal = False
        self.sigval = 0
        self.is_dma = is_dma
        self.dsem = None
        self.dval = 0
        self.dprev = None


class Prog:
    def __init__(self, nc):
        self.nc = nc
        self.ops = []
        self.track = {}
        self.ndma = {e: 0 for e in ENGS}
        self.dma_ops = {e: [] for e in ENGS}
        self.es = ExitStack()
        self.out_dmas = []
        self.sp = ARENA_BASE
        self.sp_max = ARENA_BASE

    def sb(self, name, shape, dtype):
        nbytes = int(np.prod(shape[1:])) * DTSIZE[dtype]
        nbytes = (nbytes + 31) // 32 * 32
        off = self.sp
        self.sp += nbytes
        self.sp_max = max(self.sp_max, self.sp)
        assert self.sp <= ARENA_TOP, "SBUF arena overflow: %s needs %d at %d" % (name, nbytes, off)
        return self.nc.alloc_sbuf_tensor_at(name, list(shape), dtype, offset=off)

    def getreg(self, e, val):
        if not hasattr(self, "_regs"):
            self._regs = {}
        key = (id(e), val)
        if key not in self._regs:
            r = e.alloc_register("creg%d" % len(self._regs))
            e.reg_mov(r, val)
            self._regs[key] = r
        return self._regs[key]

    def mark(self):
        return self.sp

    def release(self, m):
        self.sp = m

    def ps(self, name, shape, dtype=F32):
        return self.es.enter_context(self.nc.psum_tensor(name, list(shape), dtype))

    @staticmethod
    def _key(k):
        if isinstance(k, tuple):
            return k[0], k[1]
        if isinstance(k, str):
            return k, None
        t = getattr(k, "tensor", k)
        return t.name, None

    def _conf(self, name, sub):
        d = self.track.setdefault(name, {})
        if sub is None:
            return list(d.keys())
        return [s for s in (sub, None) if s in d]

    def _record(self, op, reads, writes):
        r2, w2 = [], list(writes)
        for k_ in reads:
            nm = self._key(k_)[0]
            if nm.startswith("psb") or nm.startswith("pbt"):
                w2.append(k_)
            else:
                r2.append(k_)
        reads, writes = r2, w2
        deps = set()
        for k in reads:
            name, sub = self._key(k)
            d = self.track.setdefault(name, {})
            for s in self._conf(name, sub):
                w = d[s][0]
                if w is not None:
                    deps.add(w)
        for k in writes:
            name, sub = self._key(k)
            d = self.track.setdefault(name, {})
            for s in self._conf(name, sub):
                w, rs = d[s]
                if w is not None:
                    deps.add(w)
                for r in rs:
                    deps.add(r)
        for k in reads:
            name, sub = self._key(k)
            d = self.track[name]
            d.setdefault(sub, [None, []])[1].append(op)
        for k in writes:
            name, sub = self._key(k)
            d = self.track[name]
            if sub is None:
                d.clear()
            d[sub] = [op, []]
        deps.discard(op)
        for y in deps:
            if y.eng == "pe" and op.eng == "pe" and not y.is_dma:
                continue
            op.deps.append(y)
            if not y.is_dma:
                y.signal = True

    def op(self, eng, fn, reads=(), writes=(), after=()):
        o = _Op(eng, fn, False)
        o.idx = len(self.ops)
        self.ops.append(o)
        self._record(o, reads, writes)
        for y in after:
            o.deps.append(y)
            if not y.is_dma:
                y.signal = True
        return o

    def dma(self, eng, out, in_, reads=None, writes=None, is_out=False, **kw):
        if reads is None:
            reads = [in_]
        if writes is None:
            writes = [out]

        def fn(e, out=out, in_=in_, kw=kw):
            return e.dma_start(out=out, in_=in_, **kw)
        o = _Op(eng, fn, True)
        o.idx = len(self.ops)
        self.ops.append(o)
        self._record(o, reads, writes)
        n = self.ndma[eng]
        self.ndma[eng] += 1
        o.dsem = (eng, n % NDSEM)
        o.dval = 16 * (n // NDSEM + 1)
        if n >= NDSEM:
            o.dprev = self.dma_ops[eng][n - NDSEM]
        self.dma_ops[eng].append(o)
        if is_out:
            self.out_dmas.append(o)
        return o

    def dma_fn(self, eng, fn, reads, writes, is_out=False):
        o = _Op(eng, fn, True)
        o.idx = len(self.ops)
        self.ops.append(o)
        self._record(o, reads, writes)
        n = self.ndma[eng]
        self.ndma[eng] += 1
        o.dsem = (eng, n % NDSEM)
        o.dval = 16 * (n // NDSEM + 1)
        if n >= NDSEM:
            o.dprev = self.dma_ops[eng][n - NDSEM]
        self.dma_ops[eng].append(o)
        if is_out:
            self.out_dmas.append(o)
        return o

    def capture(self, fn):
        rec = []
        orig_op, orig_dma, orig_dfn = self.op, self.dma, self.dma_fn
        self.op = lambda *a, **k: rec.append((orig_op, a, k))
        self.dma = lambda *a, **k: rec.append((orig_dma, a, k))
        self.dma_fn = lambda *a, **k: rec.append((orig_dfn, a, k))
        try:
            fn()
        finally:
            self.op, self.dma, self.dma_fn = orig_op, orig_dma, orig_dfn
        return rec

    def interleave(self, fns):
        recs = [self.capture(f) for f in fns]
        for i in range(max(len(r) for r in recs)):
            for r in recs:
                if i < len(r):
                    f, a, k = r[i]
                    f(*a, **k)

    def barrier(self):
        last = {}
        for o in self.ops:
            if not o.is_dma:
                last[o.eng] = o
        pend = [o for e in ENGS for o in self.dma_ops[e][-NDSEM:]]
        bops = []
        for e in ENGS:
            after = [o for o in last.values()] + pend
            bops.append((e, after))
        res = []
        for e, after in bops:
            res.append(self.op(e, lambda eng: eng.nop(), after=after))
        for o in res:
            o.signal = True
        self._barrier_ops = res
        self.track.clear()
        return res

    def emit(self):
        nc = self.nc
        if self.out_dmas:
            self.op("sp", lambda eng: eng.nop(), after=list(self.out_dmas))
        cnt = {e: 0 for e in ENGS}
        for o in self.ops:
            if o.is_dma:
                continue
            if o.signal:
                cnt[o.eng] += 1
                o.sigval = cnt[o.eng]
        es = self.es
        csem = {e: es.enter_context(nc.semaphore("c_" + e)) for e in ENGS}
        dsem = {}
        for e in ENGS:
            if self.ndma[e]:
                for i in range(min(NDSEM, self.ndma[e])):
                    dsem[(e, i)] = es.enter_context(nc.semaphore("d_%s%d" % (e, i)))
        per = {e: [o for o in self.ops if o.eng == e] for e in ENGS}
        block = es.enter_context(nc.Block())

        def run(eng_name, eng):
            waited = {}

            def wait(sem_key, sem, val):
                if waited.get(sem_key, 0) >= val:
                    return
                waited[sem_key] = val
                eng.wait_ge(sem, val)

            for o in per[eng_name]:
                for y in o.deps:
                    if y.is_dma:
                        wait(y.dsem, dsem[y.dsem], y.dval)
                    else:
                        wait(("c", y.eng), csem[y.eng], y.sigval)
                if o.is_dma:
                    if o.dprev is not None:
                        wait(o.dsem, dsem[o.dsem], o.dprev.dval)
                    ins = o.fn(eng)
                    ins.then_inc(dsem[o.dsem], 16)
                else:
                    ins = o.fn(eng)
                    if o.signal:
                        ins.then_inc(csem[eng_name], 1)

        @block.tensor
        def _(e):
            run("pe", e)

        @block.scalar
        def _(e):
            run("act", e)

        @block.vector
        def _(e):
            run("dve", e)

        @block.gpsimd
        def _(e):
            run("pool", e)

        @block.sync
        def _(e):
            run("sp", e)

    def close(self):
        self.es.close()


import numpy as np

D = 2048
NT = 4096
NOWN = 2048
NCTX = 256
KC = 16
NCH = 94
SLAB0 = 8
NSL = 54
DINP = NCH * 128
EPS = 1e-6
SQD = float(np.sqrt(2048.0))


class K:
    pass


def build(stage=99, dbg=(), ntiles=None, cut=None, skipA=False, nchunks=None, ccut=None, ecut=None, nexp=None):
    nc = bass.Bass("TRN2", target_bir_lowering=False)
    p = Prog(nc)
    k = K()
    k.nc, k.p = nc, p
    k.dbg = {}
    k.ntiles = ntiles
    k.cut = cut
    k.nchunks = nchunks
    k.ccut = ccut
    k.ecut = ecut
    k.nexp = nexp

    def din(name, shape, dt=F32):
        return nc.dram_tensor(name, list(shape), dt, kind="ExternalInput").ap()

    def dscr(name, shape, dt):
        if skipA and name in ("slabL", "slabC", "uT", "sgT"):
            return nc.dram_tensor(name, list(shape), dt, kind="ExternalInput").ap()
        if name in dbg:
            a = nc.dram_tensor(name, list(shape), dt, kind="ExternalOutput").ap()
            k.dbg[name] = a
            return a
        return nc.dram_tensor(name, list(shape), dt).ap()

    k.din, k.dscr = din, dscr
    if not skipA:
        k.xT = din("xT", [D, NT])
        k.cxT = din("cxT", [D, NCTX])
        k.cT2 = din("cT2", [128, KC, 2])
        k.adaw = din("adaw", [D, 6 * D])
        k.adab = din("adab", [128, 96])
        k.g1 = din("g1", [128, KC])
        k.win = din("win", [D, DINP])
    k.slabL = dscr("slabL", [NSL * 128, NT], BF16)
    k.slabC = dscr("slabC", [NSL * 128, NCTX], BF16)
    k.uT = dscr("uT", [1024, NOWN], BF16)
    k.sgT = dscr("sgT", [4096, NOWN], BF16)
    k.modd = dscr("modd", [128, 96, 2], F32)

    k.ones_bf = p.sb("ones_bf", [128, 128], BF16)
    p.op("pool", lambda e: e.memset(k.ones_bf[:], 1.0), writes=[k.ones_bf])
    k.eps_t = p.sb("eps_t", [128, 2], F32)
    p.op("pool", lambda e: e.memset(k.eps_t[:, 0:1], EPS), writes=[k.eps_t])
    p.op("pool", lambda e: e.memset(k.eps_t[:, 1:2], 1e-12), writes=[k.eps_t])
    k.mod = p.sb("mod", [128, 96, 2], F32)
    k.A1 = p.sb("A1", [128, KC, 2], F32)
    k.g1s = p.sb("g1s", [128, KC], F32)
    k.ident = p.sb("ident_bf", [128, 128], BF16)
    k.psb = [p.ps("psb%d" % i, [128, 512], F32) for i in range(6)]
    k.pbt = [p.ps("pbt%d" % i, [128, 1024], BF16) for i in range(2)]

    m0 = p.mark()
    if skipA:
        modin = din("modin", [128, 96, 2])
        p.dma("sp", k.mod[:], modin)
    if not skipA:
        phase0(k)
        p.barrier()
        p.release(m0)
        if stage >= 1:
            phaseA(k)
            p.release(m0)
    mB = p.mark()
    if stage >= 2:
        phaseB(k)
    if stage >= 3:
        mC = p.mark()
        phaseC(k)
        p.release(mC)
    if stage >= 4:
        phaseD(k)
    if stage >= 5:
        phaseE(k)
    p.emit()
    p.close()
    return nc, k


def phase0(k):
    p, nc = k.p, k.nc
    c32 = p.sb("c32", [128, KC, 2], F32)
    cs = p.sb("c_silu", [128, KC, 2], BF16)
    adab = p.sb("adab_sb", [128, 96], F32)
    p.dma("sp", c32[:], k.cT2)
    p.dma("sp", adab[:], k.adab)
    p.dma("sp", k.g1s[:], k.g1)
    p.op("act", lambda e: e.activation(out=cs[:], in_=c32[:], func=AF.Silu), reads=[c32], writes=[cs])
    NB = 16
    CW = 768
    wb = [p.sb("adaw_bf%d" % i, [128, KC, CW], BF16) for i in range(2)]
    src = k.adaw.rearrange("(kc p) n -> p kc n", p=128)
    ps = k.psb[0]
    for blk in range(NB):
        w = wb[blk % 2]
        for h in range(2):
            p.dma("pool", w[:, h * 8:(h + 1) * 8, :], src[:, h * 8:(h + 1) * 8, blk * CW:(blk + 1) * CW],
                  reads=[], writes=[(w.name, h)])
        for oc in range(6):
            g = blk * 6 + oc
            for kc in range(KC):
                p.op("pe", lambda e, w=w, oc=oc, kc=kc, g=g: e.matmul(
                    ps[:, 2 * g:2 * g + 2], lhsT=w[:, kc, oc * 128:(oc + 1) * 128], rhs=cs[:, kc, :],
                    start=(kc == 0), stop=(kc == KC - 1)),
                    reads=[(w.name, kc // 8), cs], writes=[ps])
    p.op("dve", lambda e: e.tensor_tensor(
        out=k.mod[:], in0=ps[:, 0:192].rearrange("p (g t) -> p g t", t=2),
        in1=adab[:].unsqueeze(2).to_broadcast([128, 96, 2]), op=ALU.add),
        reads=[ps, adab], writes=[k.mod])
    p.op("dve", lambda e: e.tensor_scalar(out=k.A1[:], in0=k.mod[:, 16:32, :], scalar1=1.0, scalar2=None,
                                          op0=ALU.add), reads=[k.mod], writes=[k.A1])
    p.op("dve", lambda e: e.tensor_tensor(out=k.A1[:], in0=k.A1[:],
                                          in1=k.g1s[:].unsqueeze(2).to_broadcast([128, KC, 2]), op=ALU.mult),
         reads=[k.A1, k.g1s], writes=[k.A1])
    if "modd" in k.dbg:
        p.dma("sp", k.modd, k.mod[:], is_out=True)


def phaseA(k):
    p, nc = k.p, k.nc
    hT = p.sb("hT", [128, KC, NOWN], BF16)
    xin = [p.sb("xin%d" % i, [128, KC, 256], F32) for i in range(2)]
    sq = p.sb("sq", [128, KC, 256], BF16)
    rstd = p.sb("rstd", [128, 256], F32)
    rms = p.sb("rms", [128, 256], F32)
    xn = p.sb("xn", [128, KC, 256], F32)
    wblk = [p.sb("wblk%d" % i, [128, KC, 256], BF16) for i in range(3)]
    stg = [p.sb("stgA%d" % i, [128, 512], BF16) for i in range(4)]
    winv = k.win.rearrange("(kc p) n -> p kc n", p=128)
    st = {"x": 0, "w": 0, "s": 0, "ps": 0}

    def norm_group(srcT, t0, ntok, which):
        v = srcT.rearrange("(kc p) t -> p kc t", p=128)
        for s in range(ntok // 256):
            xb = xin[st["x"] % 2]
            st["x"] += 1
            for h in range(2):
                p.dma("sp", xb[:, h * 8:(h + 1) * 8, :], v[:, h * 8:(h + 1) * 8, t0 + s * 256:t0 + (s + 1) * 256],
                      reads=[], writes=[(xb.name, h)])
            p.op("act", lambda e, xb=xb: e.activation(out=sq[:], in_=xb[:], func=AF.Square), reads=[xb], writes=[sq])
            ps = k.psb[5]
            for kc in range(KC):
                p.op("pe", lambda e, kc=kc: e.matmul(ps[:, 0:256], lhsT=k.ones_bf[:], rhs=sq[:, kc, :],
                                                      start=(kc == 0), stop=(kc == KC - 1)),
                     reads=[k.ones_bf, sq], writes=[ps])
            p.op("act", lambda e: e.activation(out=rms[:], in_=ps[:, 0:256], func=AF.Sqrt, scale=1.0 / D, bias=k.eps_t[:, 0:1]),
                 reads=[ps, k.eps_t], writes=[rms])
            p.op("dve", lambda e: e.reciprocal(out=rstd[:], in_=rms[:]), reads=[rms], writes=[rstd])
            p.op("dve", lambda e, xb=xb: e.tensor_tensor(out=xn[:], in0=xb[:],
                                                         in1=rstd[:].unsqueeze(1).to_broadcast([128, KC, 256]), op=ALU.mult),
                 reads=[xb, rstd], writes=[xn])
            for kc in range(KC):
                p.op("act", lambda e, kc=kc, s=s: e.activation(
                    out=hT[:, kc, s * 256:(s + 1) * 256], in_=xn[:, kc, :], func=AF.Identity,
                    scale=k.A1[:, kc, which:which + 1], bias=k.mod[:, kc, which:which + 1]),
                    reads=[xn, k.A1, k.mod], writes=[(hT.name, s)])

    def proj_group(ntok, chunks, sink):
        nsub = max(1, ntok // 512)
        w = min(512, ntok)
        for b0 in range(0, len(chunks), 2):
            wb = wblk[st["w"] % 3]
            st["w"] += 1
            c0 = chunks[b0]
            for h in range(2):
                p.dma("pool", wb[:, h * 8:(h + 1) * 8, :], winv[:, h * 8:(h + 1) * 8, c0 * 128:(c0 + 2) * 128],
                      reads=[], writes=[(wb.name, h)])
            for s in range(nsub):
                for j in range(2):
                    ch = chunks[b0 + j]
                    ps = k.psb[st["ps"] % 5]
                    st["ps"] += 1
                    for kc in range(KC):
                        p.op("pe", lambda e, wb=wb, j=j, kc=kc, s=s, ps=ps: e.matmul(
                            ps[:, 0:w], lhsT=wb[:, kc, j * 128:(j + 1) * 128], rhs=hT[:, kc, s * 512:s * 512 + w],
                            start=(kc == 0), stop=(kc == KC - 1)),
                            reads=[(wb.name, kc // 8), (hT.name, (s * 512) // 256), (hT.name, (s * 512 + w - 1) // 256)],
                            writes=[ps])
                    sink(ch, s, w, ps)

    def mk_sink(slab_dst, tok0):
        def sink(ch, s, w, ps):
            sg = stg[st["s"] % 4]
            st["s"] += 1
            eng = "act" if (st["s"] % 2) else "dve"
            if ch >= 62:
                p.op("act", lambda e, sg=sg, ps=ps: e.activation(out=sg[:, 0:w], in_=ps[:, 0:w], func=AF.Sigmoid),
                     reads=[ps], writes=[sg])
                dst = k.sgT[(ch - 62) * 128:(ch - 61) * 128, s * 512:s * 512 + w]
            else:
                if eng == "act":
                    p.op("act", lambda e, sg=sg, ps=ps: e.activation(out=sg[:, 0:w], in_=ps[:, 0:w], func=AF.Copy),
                         reads=[ps], writes=[sg])
                else:
                    p.op("dve", lambda e, sg=sg, ps=ps: e.tensor_copy(out=sg[:, 0:w], in_=ps[:, 0:w]),
                         reads=[ps], writes=[sg])
                if ch < 8:
                    dst = k.uT[ch * 128:(ch + 1) * 128, s * 512:s * 512 + w]
                else:
                    cc = ch - SLAB0
                    dst = slab_dst[cc * 128:(cc + 1) * 128, tok0 + s * 512:tok0 + s * 512 + w]
            p.dma("sp", dst, sg[:, 0:w], reads=[sg], writes=[])
        return sink

    slab_chunks = list(range(SLAB0, SLAB0 + NSL))
    norm_group(k.xT, 0, NOWN, 0)
    proj_group(NOWN, list(range(NCH)), mk_sink(k.slabL, 0))
    norm_group(k.xT, NOWN, NOWN, 0)
    proj_group(NOWN, slab_chunks, mk_sink(k.slabL, NOWN))
    norm_group(k.cxT, 0, NCTX, 1)
    proj_group(NCTX, slab_chunks, mk_sink(k.slabC, 0))
    p.barrier()
    for name in ("slabL", "slabC", "uT", "sgT"):
        if name in k.dbg:
            pass


import numpy as np

C0 = float(np.exp(-0.5))
NCHK = 68
TT = 256


class Cut(Exception):
    pass


def phaseB(k):
    try:
        _phaseB(k)
    except Cut:
        k.p.barrier()


def _phaseB(k):
    def cut(n):
        if getattr(k, "cut", None) == n:
            raise Cut()
    p, nc = k.p, k.nc
    din, dscr = k.din, k.dscr
    k.mixc_d = din("mixc", [128, 7, 54])
    k.vecs_d = din("vecs", [128, 7, 16])
    k.lw2_d = din("lw2", [96, 4, 2048])
    k.bones_d = din("bones", [128, 128])
    k.hsel_d = din("hsel", [128, 2])
    k.Q = [dscr("QA", [2048, NCHK * 256], BF16), dscr("QB", [2048, NCHK * 256], BF16)]
    k.Vtok = dscr("Vtok", [NCHK * 64, 2048], BF16)
    k.KpT = [dscr("KpTA", [NCHK * 64, 2048], BF16), dscr("KpTB", [NCHK * 64, 2048], BF16)]
    k.BpT = [dscr("BpTA", [NCHK * 64, 2048], BF16), dscr("BpTB", [NCHK * 64, 2048], BF16)]
    k.bcoef = dscr("bcoef", [2048, 32], F32)
    k.sgdT = dscr("sgdT", [256, 2048], BF16)
    k.wtot = [p.sb("wtotA", [128, 16, NCHK], F32), p.sb("wtotB", [128, 16, NCHK], F32)]

    markB = p.mark()
    mixc = p.sb("mixc_sb", [128, 7, 54], F32)
    omu = p.sb("omu", [128, 54], F32)
    vecs = p.sb("vecs_sb", [128, 7, 16], F32)
    oka = p.sb("oka", [128, 16], F32)
    lw2 = p.sb("lw2_sb", [96, 4, 2048], BF16)
    bones = p.sb("bones_sb", [128, 128], BF16)
    hsel = p.sb("hsel_sb", [128, 2], BF16)
    rmask = p.sb("rmask", [128, TT], F32)
    ident = k.ident
    p.dma("sp", mixc[:], k.mixc_d)
    p.dma("sp", vecs[:], k.vecs_d)
    for i in range(4):
        p.dma("pool", lw2[:, i, :], k.lw2_d[:, i, :], writes=[(lw2.name, i)])
    p.dma("pool", bones[:], k.bones_d)
    p.dma("pool", hsel[:], k.hsel_d)
    p.op("dve", lambda e: e.tensor_scalar(out=omu[:], in0=mixc[:, 0, :], scalar1=-1.0, scalar2=1.0, op0=ALU.mult, op1=ALU.add),
         reads=[mixc], writes=[omu])
    p.op("dve", lambda e: e.tensor_scalar(out=oka[:], in0=vecs[:, 5, :], scalar1=-1.0, scalar2=1.0, op0=ALU.mult, op1=ALU.add),
         reads=[vecs], writes=[oka])
    p.op("dve", lambda e: e.memset(rmask[:], 1.0), writes=[rmask])
    p.op("dve", lambda e: e.memset(rmask[:].rearrange("p (r c) -> p r c", c=64)[:, :, 0:1], 0.0), reads=[rmask], writes=[rmask])
    p.op("pool", lambda e: e.memset(ident[:], 1.0), writes=[ident])
    p.op("pool", lambda e: e.affine_select(out=ident[:], in_=ident[:], pattern=[[-1, 128]], compare_op=ALU.is_equal,
                                           fill=0.0, base=0, channel_multiplier=1), reads=[ident], writes=[ident])

    cut(1)
    NB = 2
    raw3 = [p.sb("raw3_%d" % i, [128, 3, 384], BF16) for i in range(NB)]
    rawl = [p.sb("rawl_%d" % i, [128, 384], BF16) for i in range(NB)]
    colsv = p.sb("colsv", [128, 4], BF16)
    M3 = [p.sb("M3_%d" % i, [128, 3, TT], F32) for i in range(NB)]
    ML = p.sb("ML", [128, TT], F32)
    tw = p.sb("tw", [128, 4, TT], BF16)
    sgd = p.sb("sgd", [128, 2, TT], BF16)
    f = {}
    for nm in ("lw", "aa", "cL", "X2", "X3", "X4", "e1", "e2", "e3", "e4", "kd", "bd", "ck"):
        f[nm] = [p.sb("f_%s%d" % (nm, i), [128, TT], F32) for i in range(2)]
    kkr = p.sb("kkr", [128, TT], F32)
    sqk = p.sb("sqk", [128, TT], BF16)
    rn = p.sb("rn", [128, TT], F32)
    kk = p.sb("kk", [128, TT], F32)
    vbf = p.sb("vbf", [128, TT], BF16)
    kpb = [p.sb("kpb%d" % i, [128, TT], BF16) for i in range(2)]
    bpb = [p.sb("bpb%d" % i, [128, TT], BF16) for i in range(2)]
    ksum = p.sb("ksum", [128, TT], F32)
    prod = p.sb("prodb", [128, TT], BF16)
    Qst = [[p.sb("Qst%d_%d" % (d, i), [128, 4, 4, 64], BF16) for i in range(2)] for d in range(2)]
    TM = {nm: p.sb("TM_" + nm, [128, 2, 2048], BF16) for nm in ("v", "kA", "bA", "kB", "bB")}
    bco = p.sb("bco", [128, 2, 32], F32)
    psL = [[k.psb[0], k.psb[1]], [k.psb[0], k.psb[1]]]
    psK = k.psb[2]
    psT = [(k.pbt[0], k.pbt[1]), (k.pbt[0], k.pbt[1])]
    psBon = k.psb[3]
    cnt = {"e": 0}

    def ew():
        cnt["e"] += 1
        return "pool" if cnt["e"] % 2 == 0 else "dve"

    colsv3 = p.sb("colsv3", [128, 3, 4], BF16)

    def mix_steps(eng, out, buf, cc, latent, key, okey, cvi):
        cv = colsv3[:, cvi, :]
        cvk = (colsv3.name, cvi)
        if latent:
            ctr = buf[:, 64:320]
            views = [(buf[:, 0:256], 3), (buf[:, 128:384], 4)]
        else:
            ctr = buf[:, 1:257]
            views = [(buf[:, 0:256], 5), (buf[:, 2:258], 6)]
        st = []
        st.append((lambda e: e.tensor_scalar(out=out, in0=ctr, scalar1=omu[:, cc:cc + 1], scalar2=None, op0=ALU.mult),
                   [key, omu], [okey]))
        for v, ci in views:
            st.append((lambda e, v=v, ci=ci: e.scalar_tensor_tensor(out=out, in0=v, scalar=mixc[:, ci, cc:cc + 1], in1=out,
                                                                    op0=ALU.mult, op1=ALU.add), [key, mixc, okey], [okey]))
        if latent:
            c63 = buf[:, 63:256:64]
            c0 = buf[:, 128:321:64]
            st.append((lambda e: e.tensor_copy(out=cv, in_=c63), [key], [cvk]))
            st.append((lambda e: e.memset(c63, 0.0), [key], [key]))
            st.append((lambda e: e.scalar_tensor_tensor(out=out, in0=buf[:, 63:319], scalar=mixc[:, 1, cc:cc + 1], in1=out,
                                                        op0=ALU.mult, op1=ALU.add), [key, mixc, okey], [okey]))
            st.append((lambda e: e.tensor_copy(out=c63, in_=cv), [cvk, key], [key]))
            st.append((lambda e: e.memset(c0, 0.0), [key], [key]))
            st.append((lambda e: e.scalar_tensor_tensor(out=out, in0=buf[:, 65:321], scalar=mixc[:, 2, cc:cc + 1], in1=out,
                                                        op0=ALU.mult, op1=ALU.add), [key, mixc, okey], [okey]))
        return st

    def mix_lockstep(eng, specs):
        allst = [mix_steps(eng, *sp, cvi=i) for i, sp in enumerate(specs)]
        for i in range(max(len(a) for a in allst)):
            for a in allst:
                if i < len(a):
                    fn, rd, wr = a[i]
                    p.op(eng, fn, reads=rd, writes=wr)

    def mix(eng, out, buf, cc, latent, key, okey):
        mix_lockstep(eng, [(out, buf, cc, latent, key, okey)])

    def load_raw(dst, src_rows, ti, latent, name):
        if latent:
            r0 = 4 * ti - 1
            lo, hi = max(r0, 0), min(r0 + 6, 64)
            if r0 < 0:
                p.op("pool", lambda e: e.memset(dst[:, :, 0:64], 0.0), writes=[name])
            if r0 + 6 > 64:
                p.op("pool", lambda e: e.memset(dst[:, :, 320:384], 0.0), writes=[name])
            p.dma("sp", dst[:, :, (lo - r0) * 64:(hi - r0) * 64], src_rows[:, :, lo * 64:hi * 64], reads=[], writes=[name])
        else:
            p.op("pool", lambda e: e.memset(dst[:, :, 0:1], 0.0), writes=[name])
            p.op("pool", lambda e: e.memset(dst[:, :, 257:258], 0.0), writes=[name])
            p.dma("sp", dst[:, :, 1:257], src_rows, reads=[], writes=[name])

    slabLv = k.slabL.rearrange("(cc p) t -> p cc t", p=128)
    slabCv = k.slabC.rearrange("(cc p) t -> p cc t", p=128)
    tiles = [("c", 0)] + [("l", ti) for ti in range(16)]
    if getattr(k, "ntiles", None):
        tiles = tiles[:k.ntiles]
    def do_tile(kind, ti, itbase):
        latent = kind == "l"
        own = latent and ti < 8
        src = slabLv if latent else slabCv
        chunk0 = 4 + 4 * ti if latent else 0
        tok0 = chunk0 * 64
        for j, cc in enumerate(range(48, 54)):
            if cc >= 52 and not own:
                continue
            rb = rawl[j % NB]
            load_raw(rb[:].unsqueeze(1), src[:, cc:cc + 1, :], ti, latent, rb.name)
            mix("dve", ML[:], rb[:], cc, latent, rb.name, ML.name)
            if j < 2:
                p.op("act", lambda e, j=j: e.activation(out=tw[:, j, :], in_=ML[:], func=AF.Tanh), reads=[ML], writes=[(tw.name, j)])
            elif j < 4:
                p.op("act", lambda e, j=j: e.activation(out=tw[:, j, :], in_=ML[:], func=AF.Copy), reads=[ML], writes=[(tw.name, j)])
            else:
                p.op("act", lambda e, j=j: e.activation(out=sgd[:, j - 4, :], in_=ML[:], func=AF.Sigmoid), reads=[ML], writes=[sgd])
                p.dma("sp", k.sgdT[(j - 4) * 128:(j - 3) * 128, ti * TT:(ti + 1) * TT], sgd[:, j - 4, :], reads=[sgd], writes=[])
        cut(2)
        def hp_body(hp, it):
            rb = raw3[it % NB]
            m3 = M3[it % NB]
            load_raw(rb[:], src[:, hp:hp + 33:16, :], ti, latent, rb.name)
            mix_lockstep("dve", [(m3[:, j, :], rb[:, j, :], hp + 16 * j, latent, (rb.name, j), (m3.name, j)) for j in range(3)])
            cut(3)
            rM, kM, vM = m3[:, 0, :], m3[:, 1, :], m3[:, 2, :]
            ch = slice(hp * 128, (hp + 1) * 128)
            pl = psL[it % 2]
            for d in range(2):
                p.op("pe", lambda e, d=d, pl=pl: e.matmul(pl[0][:, d * TT:(d + 1) * TT], lhsT=lw2[:, d, ch], rhs=tw[0:96, d, :],
                                                          start=True, stop=True),
                     reads=[(lw2.name, d), (tw.name, d)], writes=[pl[0]])
            for d in range(2):
                p.op("pe", lambda e, d=d, pl=pl: e.matmul(pl[1][:, d * TT:(d + 1) * TT], lhsT=lw2[:, 2 + d, ch], rhs=tw[0:96, 2 + d, :],
                                                          start=True, stop=True),
                     reads=[(lw2.name, 2 + d), (tw.name, 2 + d)], writes=[pl[1]])
            for d in range(2):
                p.op("act", lambda e, d=d, pl=pl: e.activation(out=f["lw"][d][:], in_=pl[0][:, d * TT:(d + 1) * TT], func=AF.Sigmoid,
                                                               bias=vecs[:, d, hp:hp + 1]), reads=[pl[0], vecs], writes=[f["lw"][d]])
                p.op("act", lambda e, d=d, pl=pl: e.activation(out=f["aa"][d][:], in_=pl[1][:, d * TT:(d + 1) * TT], func=AF.Sigmoid,
                                                               bias=vecs[:, 2 + d, hp:hp + 1]), reads=[pl[1], vecs], writes=[f["aa"][d]])
            p.op("dve", lambda e: e.tensor_scalar(out=kkr[:], in0=kM, scalar1=vecs[:, 4, hp:hp + 1], scalar2=None, op0=ALU.mult),
                 reads=[(m3.name, 1), vecs], writes=[kkr])
            p.op("act", lambda e: e.activation(out=sqk[:], in_=kkr[:], func=AF.Square), reads=[kkr], writes=[sqk])
            p.op("pe", lambda e: e.matmul(psK[:, 0:TT], lhsT=bones[:], rhs=sqk[:], start=True, stop=True), reads=[bones, sqk], writes=[psK])
            p.op("act", lambda e: e.activation(out=rn[:], in_=psK[:, 0:TT], func=AF.Sqrt, bias=k.eps_t[:, 1:2]), reads=[psK, k.eps_t], writes=[rn])
            p.op("dve", lambda e: e.reciprocal(out=rn[:], in_=rn[:]), reads=[rn], writes=[rn])
            p.op(ew(), lambda e: e.tensor_tensor(out=kk[:], in0=kkr[:], in1=rn[:], op=ALU.mult), reads=[kkr, rn], writes=[kk])
            cut(4)
            p.op("act", lambda e: e.activation(out=vbf[:], in_=vM, func=AF.Copy), reads=[(m3.name, 2)], writes=[vbf])
            pt, pt2 = psT[it % 2]
            ptb = pt[:]
            ptb2 = pt2[:]
            for s in range(2):
                p.op("pe", lambda e, s=s, ptb=ptb: e.transpose(ptb[:, s * 128:(s + 1) * 128], vbf[:, s * 128:(s + 1) * 128], ident[:]),
                     reads=[vbf, ident], writes=[pt])
            cut(5)
            def d_body(d):
                lw, aa = f["lw"][d], f["aa"][d]
                cL, X2, X3, X4 = f["cL"][d], f["X2"][d], f["X3"][d], f["X4"][d]
                e1, e2, e3, e4 = f["e1"][d], f["e2"][d], f["e3"][d], f["e4"][d]
                kd, bd, ck = f["kd"][d], f["bd"][d], f["ck"][d]
                Q = Qst[d][it % 2]
                p.op("dve", lambda e, cL=cL, lw=lw: e.tensor_tensor_scan(out=cL[:], data0=rmask[:], data1=lw[:], initial=0.0,
                                                                          op0=ALU.mult, op1=ALU.add), reads=[rmask, lw], writes=[cL])
                tot = cL[:].rearrange("p (r c) -> p r c", c=64)[:, :, 63:64]
                p.op(ew(), lambda e, X2=X2, cL=cL, lw=lw: e.tensor_tensor(out=X2[:], in0=cL[:], in1=lw[:], op=ALU.subtract),
                     reads=[cL, lw], writes=[X2])
                p.op(ew(), lambda e, X3=X3, cL=cL, tot=tot: e.tensor_tensor(
                    out=X3[:].rearrange("p (r c) -> p r c", c=64), in0=tot.to_broadcast([128, 4, 64]),
                    in1=cL[:].rearrange("p (r c) -> p r c", c=64), op=ALU.subtract), reads=[cL], writes=[X3])
                p.op("act", lambda e, d=d, tot=tot: e.activation(out=k.wtot[d][:, hp, chunk0:chunk0 + 4].unsqueeze(2), in_=tot,
                                                                func=AF.Exp, scale=-C0), reads=[cL], writes=[(k.wtot[d].name, it)])
                if d == 0:
                    srcs = [(cL, -C0), (X2, -C0), (cL, C0), (X3, -C0)]
                else:
                    p.op(ew(), lambda e, X4=X4, X3=X3, lw=lw: e.tensor_tensor(out=X4[:], in0=X3[:], in1=lw[:], op=ALU.add),
                         reads=[X3, lw], writes=[X4])
                    srcs = [(X4, -C0), (X3, -C0), (X4, C0), (X2, -C0)]
                for (sx, sc), eo in zip(srcs, (e1, e2, e3, e4)):
                    p.op("act", lambda e, sx=sx, sc=sc, eo=eo: e.activation(out=eo[:], in_=sx[:], func=AF.Exp, scale=sc),
                         reads=[sx], writes=[eo])
                p.op("dve", lambda e, ck=ck, aa=aa: e.tensor_scalar(out=ck[:], in0=aa[:], scalar1=vecs[:, 5, hp:hp + 1], scalar2=oka[:, hp:hp + 1],
                                                                   op0=ALU.mult, op1=ALU.add), reads=[aa, vecs, oka], writes=[ck])
                p.op(ew(), lambda e, kd=kd, ck=ck: e.tensor_tensor(out=kd[:], in0=kM, in1=ck[:], op=ALU.mult), reads=[(m3.name, 1), ck], writes=[kd])
                p.op(ew(), lambda e, bd=bd, aa=aa: e.tensor_tensor(out=bd[:], in0=kk[:], in1=aa[:], op=ALU.mult), reads=[kk, aa], writes=[bd])
                Qv = lambda a: Q[:, :, a, :]
                r3 = lambda t: t.rearrange("p (r c) -> p r c", c=64)
                p.op(ew(), lambda e, e1=e1: e.tensor_tensor(out=Qv(3), in0=r3(rM), in1=r3(e1[:]), op=ALU.mult), reads=[(m3.name, 0), e1], writes=[Q])
                p.op(ew(), lambda e, e2=e2: e.tensor_tensor(out=Qv(2), in0=r3(kk[:]), in1=r3(e2[:]), op=ALU.mult), reads=[kk, e2], writes=[Q])
                p.op(ew(), lambda e, e3=e3, kd=kd: e.tensor_tensor(out=Qv(1), in0=r3(kd[:]), in1=r3(e3[:]), op=ALU.mult), reads=[kd, e3], writes=[Q])
                p.op(ew(), lambda e, e3=e3, bd=bd: e.tensor_tensor(out=Qv(0), in0=r3(bd[:]), in1=r3(e3[:]), op=ALU.mult), reads=[bd, e3], writes=[Q])
                p.dma("sp", k.Q[d][ch, chunk0 * 256:(chunk0 + 4) * 256], Q[:].rearrange("p a b c -> p (a b c)"), reads=[Q], writes=[])
                p.op(ew(), lambda e, e4=e4, kd=kd, d=d: e.tensor_tensor(out=kpb[d][:], in0=kd[:], in1=e4[:], op=ALU.mult), reads=[kd, e4], writes=[kpb[d]])
                p.op(ew(), lambda e, e4=e4, bd=bd, d=d: e.tensor_tensor(out=bpb[d][:], in0=bd[:], in1=e4[:], op=ALU.mult), reads=[bd, e4], writes=[bpb[d]])
                for s in range(2):
                    p.op("pe", lambda e, s=s, d=d, ptb=ptb: e.transpose(ptb[:, (2 + 4 * d + s) * 128:(3 + 4 * d + s) * 128],
                                                                        kpb[d][:, s * 128:(s + 1) * 128], ident[:]),
                         reads=[kpb[d], ident], writes=[pt])
                    if d == 0:
                        p.op("pe", lambda e, s=s, d=d, ptb=ptb: e.transpose(ptb[:, (4 + s) * 128:(5 + s) * 128],
                                                                            bpb[d][:, s * 128:(s + 1) * 128], ident[:]),
                             reads=[bpb[d], ident], writes=[pt])
                    else:
                        p.op("pe", lambda e, s=s, d=d, ptb2=ptb2: e.transpose(ptb2[:, s * 128:(s + 1) * 128],
                                                                              bpb[d][:, s * 128:(s + 1) * 128], ident[:]),
                             reads=[bpb[d], ident], writes=[pt2])
            p.interleave([lambda: d_body(0), lambda: d_body(1)])
            cut(6)
            for bi, nm in enumerate(("v", "kA", "bA", "kB", "bB")):
                for s2 in range(2):
                    if bi < 4:
                        src_ps = ptb[:, (bi * 2 + s2) * 128:(bi * 2 + s2 + 1) * 128]
                        pkey = pt
                    else:
                        src_ps = ptb2[:, s2 * 128:(s2 + 1) * 128]
                        pkey = pt2
                    eng = "act" if (bi + s2) % 2 else "dve"
                    if getattr(k, "evac_dve", False):
                        eng = "dve"
                    if eng == "act":
                        p.op("act", lambda e, nm=nm, src_ps=src_ps, s2=s2: e.activation(out=TM[nm][:, s2, ch], in_=src_ps, func=AF.Copy),
                             reads=[pkey], writes=[(TM[nm].name, hp)])
                    else:
                        p.op("dve", lambda e, nm=nm, src_ps=src_ps, s2=s2: e.tensor_copy(out=TM[nm][:, s2, ch], in_=src_ps),
                             reads=[pkey], writes=[(TM[nm].name, hp)])
            cut(7)
            if own:
                p.op(ew(), lambda e: e.tensor_tensor(out=ksum[:], in0=f["kd"][0][:], in1=f["kd"][1][:], op=ALU.add),
                     reads=[f["kd"][0], f["kd"][1]], writes=[ksum])
                p.op("dve", lambda e: e.scalar_tensor_tensor(out=prod[:], in0=ksum[:], scalar=vecs[:, 6, hp:hp + 1], in1=rM,
                                                            op0=ALU.mult, op1=ALU.mult), reads=[ksum, vecs, (m3.name, 0)], writes=[prod])
                for s in range(2):
                    p.op("pe", lambda e, s=s: e.matmul(psBon[:, s * 32 + 2 * hp:s * 32 + 2 * hp + 2], lhsT=prod[:, s * 128:(s + 1) * 128],
                                                       rhs=hsel[:], start=True, stop=True), reads=[prod, hsel], writes=[psBon])
        for hp_ in range(16):
            hp_body(hp_, itbase + hp_ + 1)
        for nm, dst in (("v", k.Vtok), ("kA", k.KpT[0]), ("bA", k.BpT[0]), ("kB", k.KpT[1]), ("bB", k.BpT[1])):
            for s in range(2):
                p.dma("sp", dst[tok0 + s * 128:tok0 + (s + 1) * 128, :], TM[nm][:, s, :], reads=[TM[nm]], writes=[])
        if own:
            p.op("dve", lambda e: e.tensor_copy(out=bco[:], in_=psBon[:, 0:64].rearrange("p (s h) -> p s h", h=32)), reads=[psBon], writes=[bco])
            for s in range(2):
                p.dma("sp", k.bcoef[ti * TT + s * 128:ti * TT + (s + 1) * 128, :], bco[:, s, :], reads=[bco], writes=[])
    for tix, (kind_, ti_) in enumerate(tiles):
        do_tile(kind_, ti_, tix * 16)
    p.barrier()
    p.release(markB)


import numpy as np

NCHK = 68


def phaseC(k):
    p, nc = k.p, k.nc
    din, dscr = k.din, k.dscr
    k.cmask_d = din("cmask", [128, 2, 128])
    k.nmask_d = din("nmask", [64, 3, 64])
    k.Yscr = [dscr("YA", [2048, 2048], F32), dscr("YB", [2048, 2048], F32)]
    k.Sdbg = dscr("Sdbg", [2, 128, 16 * 64], F32)

    cmask = p.sb("cmask_sb", [128, 2, 128], F32)
    nmask = p.sb("nmask_sb", [64, 3, 64], F32)
    p.dma("sp", cmask[:], k.cmask_d)
    p.dma("sp", nmask[:], k.nmask_d)
    FM = [p.sb("FM%d" % i, [128, 16, 256], BF16) for i in range(2)]
    UV = [p.sb("UV%d" % i, [128, 2048], BF16) for i in range(2)]
    VZ = [p.sb("VZ%d" % i, [128, 2048], BF16) for i in range(2)]
    FMm = [p.sb("FMm%d" % i, [128, 16, 2, 128], BF16) for i in range(2)]
    for i in range(2):
        p.op("pool", lambda e, i=i: e.memset(VZ[i][:], 0.0), writes=[VZ[i]])
        p.op("pool", lambda e, i=i: e.memset(FMm[i][:], 0.0), writes=[FMm[i]])
    KB = [p.sb("KB%d" % i, [128, 2048], BF16) for i in range(2)]
    AT = p.sb("AT_sb", [128, 32, 128], BF16)
    Pm = [p.sb("Pm%d" % g, [128, 8, 64], BF16) for g in range(4)]
    PT = [p.sb("PTm%d" % g, [128, 8, 64], BF16) for g in range(4)]
    Gm = [p.sb("Gm%d" % g, [128, 8, 64], BF16) for g in range(4)]
    Qm = [p.sb("Qm%d" % g, [128, 8, 64], BF16) for g in range(4)]
    nrhs = [p.sb("nrhs%d" % g, [128, 8, 64], BF16) for g in range(4)]
    for g in range(4):
        for t_ in (Pm, PT, Gm, Qm, nrhs):
            p.op("pool", lambda e, t_=t_, g=g: e.memset(t_[g][:], 0.0), writes=[t_[g]])
    Ysb = [p.sb("Ysb%d" % i, [64, 2048], F32) for i in range(2)]
    S32 = p.sb("S32", [128, 16, 64], F32)
    Sbf = p.sb("Sbf", [128, 16, 64], BF16)
    Stmp = p.sb("Stmp", [128, 4, 64], F32)
    psAT = [k.psb[0], k.psb[1]]
    psP, psPT, psQ, psY = k.psb[2], k.psb[3], k.psb[4], k.psb[5]
    psP0, psPT0, psQ0 = psP, psPT, psQ
    ident = nmask[:, 2, :]
    Qv = [k.Q[d].rearrange("(hp p) x -> p hp x", p=128) for d in range(2)]
    cnt = {"e": 0, "ld": 0}

    def evac_eng():
        cnt["e"] += 1
        return "act" if cnt["e"] % 2 else "dve"

    def copy_evac(out, in_, reads, writes, scale=None):
        eng = evac_eng()
        if eng == "act":
            if scale is None:
                p.op("act", lambda e: e.activation(out=out, in_=in_, func=AF.Copy), reads=reads, writes=writes)
            else:
                p.op("act", lambda e: e.activation(out=out, in_=in_, func=AF.Copy, scale=scale), reads=reads, writes=writes)
        else:
            if scale is None:
                p.op("dve", lambda e: e.tensor_copy(out=out, in_=in_), reads=reads, writes=writes)
            else:
                p.op("dve", lambda e: e.tensor_scalar(out=out, in0=in_, scalar1=scale, scalar2=None, op0=ALU.mult), reads=reads, writes=writes)

    def load_chunk(d, c, buf):
        fm, uv, kb = FM[buf], UV[buf], KB[buf]
        vz, fmm = VZ[buf], FMm[buf]
        p.dma("sp", fm[:], Qv[d][:, :, c * 256:(c + 1) * 256], reads=[], writes=[fm])
        p.dma("sp", fmm[0:64, :, 0, :], Qv[d][0:64, :, c * 256 + 128:(c + 1) * 256], reads=[], writes=[(fmm.name, 0)])
        p.dma("sp", fmm[64:128, :, 1, :], Qv[d][64:128, :, c * 256 + 128:(c + 1) * 256], reads=[], writes=[(fmm.name, 1)])
        p.dma("sp", uv[64:128, :], k.Vtok[c * 64:(c + 1) * 64, :], reads=[], writes=[(uv.name, "V")])
        p.dma("sp", vz[64:128, :], k.Vtok[c * 64:(c + 1) * 64, :], reads=[], writes=[(vz.name, "V")])
        p.dma("sp", kb[0:64, :], k.BpT[d][c * 64:(c + 1) * 64, :], reads=[], writes=[(kb.name, 0)])
        p.dma("sp", kb[64:128, :], k.KpT[d][c * 64:(c + 1) * 64, :], reads=[], writes=[(kb.name, 1)])

    def do_chunk(d, c, buf, emit, ytok0):
        fm, uv, kb = FM[buf], UV[buf], KB[buf]
        vz, fmm = VZ[buf], FMm[buf]
        ysb = Ysb[buf]

        def fmv(h, a0, a1):
            return fm[:, h // 2, a0 * 64:a1 * 64]

        def fmk(h, a0, a1):
            return fmm[:, h // 2, h % 2, a0 * 64:a1 * 64]

        def sv(h):
            return Sbf[:, h // 2, :]

        def stage1(g):
            for hl in range(8):
                h = g * 8 + hl
                pa = psAT[hl // 4]
                p.op("pe", lambda e, h=h, hl=hl, pa=pa: e.matmul(pa[:, (hl % 4) * 128:(hl % 4 + 1) * 128], lhsT=fmv(h, 0, 2), rhs=fmk(h, 0, 2),
                                                               start=True, stop=True), reads=[fm, fmm], writes=[pa])
            for hl in range(8):
                h = g * 8 + hl
                p.op("pe", lambda e, h=h, hl=hl: e.matmul(psP[0:64, hl * 64:(hl + 1) * 64], lhsT=fmk(h, 0, 1), rhs=fmv(h, 0, 1),
                                                         start=True, stop=True), reads=[fm, fmm], writes=[psP])
            for half in range(2):
                pa = psAT[half]
                p.op("dve", lambda e, half=half, pa=pa: e.tensor_tensor(
                    out=AT[:, g * 8 + half * 4:g * 8 + half * 4 + 4, :], in0=pa[:].rearrange("p (h c) -> p h c", c=128),
                    in1=cmask[:, d, :].unsqueeze(1).to_broadcast([128, 4, 128]), op=ALU.mult), reads=[pa, cmask], writes=[(AT.name, g)])
            p.op("dve", lambda e: e.tensor_tensor(out=Pm[g][0:64, :, :], in0=psP[0:64, :].rearrange("p (h c) -> p h c", c=64),
                                                  in1=nmask[:, d, :].unsqueeze(1).to_broadcast([64, 8, 64]), op=ALU.mult),
                 reads=[psP, nmask], writes=[Pm[g]])
            p.op("dve", lambda e: e.tensor_tensor(out=Qm[g][0:64, :, :], in0=ident.unsqueeze(1).to_broadcast([64, 8, 64]),
                                                  in1=AT[0:64, g * 8:(g + 1) * 8, 0:64], op=ALU.subtract),
                 reads=[(AT.name, g), nmask], writes=[Qm[g]])

        def ptv(g, kk_, hl):
            if kk_ == 0:
                return AT[:, g * 8 + hl, 0:64]
            return PT[g][:, hl, :]

        def stage2(g, kk_):
            psP, psPT, psQ = (psP0, psPT0, psQ0) if g % 2 == 0 else (psAT[0], psAT[1], psY)
            for hl in range(8):
                p.op("pe", lambda e, hl=hl: e.matmul(psP[0:64, hl * 64:(hl + 1) * 64], lhsT=ptv(g, kk_ - 1, hl), rhs=Pm[g][:, hl, :],
                                                    start=True, stop=True),
                     reads=[Pm[g], PT[g], (AT.name, g)], writes=[psP])
            if kk_ < 5:
                for hl in range(8):
                    p.op("pe", lambda e, hl=hl: e.matmul(psPT[0:64, hl * 64:(hl + 1) * 64], lhsT=Pm[g][:, hl, :], rhs=ptv(g, kk_ - 1, hl),
                                                        start=True, stop=True),
                         reads=[Pm[g], PT[g], (AT.name, g)], writes=[psPT])
            p3 = psP[0:64, :].rearrange("p (h c) -> p h c", c=64)
            p.op("dve", lambda e: e.tensor_tensor(out=Gm[g][0:64, :, :], in0=p3, in1=ident.unsqueeze(1).to_broadcast([64, 8, 64]), op=ALU.add),
                 reads=[psP, nmask], writes=[Gm[g]])
            if kk_ < 5:
                p.op("act", lambda e: e.activation(out=Pm[g][0:64, :, :].rearrange("p h c -> p (h c)"), in_=psP[0:64, :], func=AF.Copy), reads=[psP], writes=[Pm[g]])
                copy_evac(PT[g][0:64, :, :].rearrange("p h c -> p (h c)"), psPT[0:64, :], [psPT], [PT[g]])
            for hl in range(8):
                p.op("pe", lambda e, hl=hl: e.matmul(psQ[0:64, hl * 64:(hl + 1) * 64], lhsT=Gm[g][:, hl, :], rhs=Qm[g][:, hl, :],
                                                    start=True, stop=True), reads=[Gm[g], Qm[g]], writes=[psQ])
            copy_evac(Qm[g][0:64, :, :].rearrange("p h c -> p (h c)"), psQ[0:64, :], [psQ], [Qm[g]])

        def stage3_all():
            bq = lambda g: psQ if g % 2 == 0 else psAT[0]
            bu = lambda g: psPT if g % 2 == 0 else psAT[1]
            for g in range(4):
                pq = bq(g)
                for hl in range(8):
                    h = g * 8 + hl
                    p.op("pe", lambda e, h=h, hl=hl, pq=pq: e.matmul(pq[0:64, hl * 64:(hl + 1) * 64], lhsT=fmk(h, 0, 1), rhs=sv(h),
                                                                    start=True, stop=False), reads=[fmm, (Sbf.name, h // 8)], writes=[pq])
                    p.op("pe", lambda e, h=h, hl=hl, pq=pq: e.matmul(pq[0:64, hl * 64:(hl + 1) * 64], lhsT=AT[:, h, 0:64],
                                                                    rhs=vz[:, h * 64:(h + 1) * 64], start=False, stop=True),
                         reads=[(AT.name, g), vz], writes=[pq])
                if g % 2 == 1:
                    for gg in (g - 1, g):
                        copy_evac(nrhs[gg][0:64, :, :].rearrange("p h c -> p (h c)"), bq(gg)[0:64, :], [bq(gg)], [nrhs[gg]], scale=-1.0)
                    for gg in (g - 1, g):
                        pu = bu(gg)
                        for hl in range(8):
                            p.op("pe", lambda e, hl=hl, gg=gg, pu=pu: e.matmul(pu[0:64, hl * 64:(hl + 1) * 64], lhsT=Qm[gg][:, hl, :],
                                                                              rhs=nrhs[gg][:, hl, :], start=True, stop=True),
                                 reads=[Qm[gg], nrhs[gg]], writes=[pu])
                    for gg in (g - 1, g):
                        copy_evac(uv[0:64, gg * 512:(gg + 1) * 512], bu(gg)[0:64, :], [bu(gg)], [(uv.name, ("U", gg))])

        def stage5_all():
            by = lambda g: psY if g % 2 == 0 else psP
            for g in range(4):
                py = by(g)
                for hl in range(8):
                    h = g * 8 + hl
                    p.op("pe", lambda e, h=h, hl=hl, py=py: e.matmul(py[0:64, hl * 64:(hl + 1) * 64], lhsT=fmk(h, 1, 2), rhs=sv(h),
                                                                    start=True, stop=False), reads=[fmm, (Sbf.name, h // 8)], writes=[py])
                    p.op("pe", lambda e, h=h, hl=hl, py=py: e.matmul(py[0:64, hl * 64:(hl + 1) * 64], lhsT=AT[:, h, 64:128],
                                                                    rhs=uv[:, h * 64:(h + 1) * 64], start=False, stop=True),
                         reads=[(AT.name, g), (uv.name, "V"), (uv.name, ("U", g))], writes=[py])
                if g % 2 == 1:
                    for gg in (g - 1, g):
                        copy_evac(ysb[:, gg * 512:(gg + 1) * 512], by(gg)[0:64, :], [by(gg)], [(ysb.name, gg)])

        def stage6(g):
            ps4 = psY[:].rearrange("p (a b c) -> p a b c", a=4, b=2)
            for hpl in range(4):
                hp = g * 4 + hpl
                h0, h1 = 2 * hp, 2 * hp + 1
                p.op("pe", lambda e, hpl=hpl, h0=h0: e.matmul(ps4[0:64, hpl, 0, :], lhsT=kb[:, h0 * 64:(h0 + 1) * 64], rhs=uv[:, h0 * 64:(h0 + 1) * 64],
                                                             start=True, stop=True),
                     reads=[kb, (uv.name, "V"), (uv.name, ("U", g))], writes=[psY])
                p.op("pe", lambda e, hpl=hpl, h0=h0, h1=h1: e.matmul(ps4[:, hpl, 1, :], lhsT=kb[:, h0 * 64:(h1 + 1) * 64], rhs=uv[:, h1 * 64:(h1 + 1) * 64],
                                                                    start=True, stop=True),
                     reads=[kb, (uv.name, "V"), (uv.name, ("U", g))], writes=[psY])
            for hh in range(2):
                rows = slice(hh * 64, (hh + 1) * 64)
                p.op("dve", lambda e, rows=rows: e.tensor_tensor(
                    out=Stmp[rows, :, :], in0=S32[rows, g * 4:(g + 1) * 4, :],
                    in1=k.wtot[d][rows, g * 4:(g + 1) * 4, c:c + 1].to_broadcast([64, 4, 64]), op=ALU.mult),
                    reads=[(S32.name, g), k.wtot[d]], writes=[(Stmp.name, hh)])
                p.op("dve", lambda e, rows=rows, hh=hh: e.tensor_tensor(
                    out=S32[rows, g * 4:(g + 1) * 4, :], in0=Stmp[rows, :, :], in1=ps4[rows, :, hh, :], op=ALU.add),
                    reads=[(Stmp.name, hh), psY], writes=[(S32.name, g)])
            p.op("act", lambda e: e.activation(out=Sbf[:].rearrange("p a b -> p (a b)")[:, g * 256:(g + 1) * 256],
                                               in_=S32[:].rearrange("p a b -> p (a b)")[:, g * 256:(g + 1) * 256], func=AF.Copy),
                 reads=[(S32.name, g)], writes=[(Sbf.name, g)])

        cc_ = getattr(k, "ccut", None) or 99
        for g in range(4):
            stage1(g)
        if cc_ <= 1:
            return
        for kk_ in range(1, 6):
            for g in range(4):
                stage2(g, kk_)
        if cc_ <= 2:
            return
        stage3_all()
        if cc_ <= 3:
            return
        if emit:
            stage5_all()
            p.dma("sp", k.Yscr[d][ytok0:ytok0 + 64, :], ysb[:], reads=[ysb], writes=[])
        if cc_ <= 5:
            return
        for g in range(4):
            stage6(g)

    ncap = getattr(k, "nchunks", None)
    for d in range(2):
        if d == 0:
            seq = [(c, False) for c in range(4)] + [(c, True) for c in range(4, 36)]
        else:
            seq = [(c, False) for c in range(3, -1, -1)] + [(c, False) for c in range(67, 35, -1)] + [(c, True) for c in range(35, 3, -1)]
        if ncap:
            seq = seq[:ncap]
        p.op("dve", lambda e: e.memset(S32[:], 0.0), writes=[S32])
        p.op("pool", lambda e: e.memset(Sbf[:], 0.0), writes=[Sbf])
        load_chunk(d, seq[0][0], cnt["ld"] % 2)
        for i, (c, emit) in enumerate(seq):
            buf = cnt["ld"] % 2
            cnt["ld"] += 1
            if i + 1 < len(seq):
                load_chunk(d, seq[i + 1][0], cnt["ld"] % 2)
            do_chunk(d, c, buf, emit, (c - 4) * 64)
        if "Sdbg" in k.dbg:
            p.dma("sp", k.Sdbg[d], S32[:].rearrange("p a b -> p (a b)"), reads=[S32], writes=[])
    p.barrier()


import numpy as np

GT = 256
NTS = 2
NR = 4
GN_EPS = 64e-5
NORM_EPS = 1e-6
D = 2048


def phaseD(k):
    p, nc = k.p, k.nc
    din, dscr = k.din, k.dscr
    k.xown_d = din("xown", [2048, 2048])
    k.rows_d = din("rows", [3, 2048])
    k.g2n_d = din("g2n", [128, 16])
    k.g2w_d = din("g2w", [256, 2048])
    k.poolw_d = din("poolw", [4, 256, 256])
    k.pools_d = din("pools", [128, 8])
    k.wpo_d = din("wpo", [1024, 2048])
    k.wro_d = din("wro", [2048, 2048])
    k.wout_d = din("wout", [2048, 2048])
    k.poolc_d = din("poolc", [1, 4 * 64 + 2])
    k.rowbuf = dscr("rowbuf", [4, 2048], F32)
    k.x1scr = dscr("x1scr", [2048, 2048], F32)
    k.h2T = dscr("h2Tscr", [2048, 2048], BF16)
    k.h2tok = dscr("h2tok", [2048, 2048], BF16)

    k.rowt = {}
    mD = p.mark()
    for i, nm in enumerate(("lnxw", "lnxb")):
        k.rowt[nm] = p.sb("row_" + nm, [128, 2048], F32)
        p.dma("sp", k.rowt[nm][:], k.rows_d[i:i + 1, :].partition_broadcast(128))
    g2n = p.sb("g2n_sb", [128, 16], F32)
    A2 = p.sb("A2_fm", [128, 16], F32)
    p.dma("sp", g2n[:], k.g2n_d)
    p.op("dve", lambda e: e.tensor_scalar(out=A2[:], in0=k.mod[:, 64:80, 0], scalar1=1.0, scalar2=None, op0=ALU.add), reads=[k.mod], writes=[A2])
    p.op("dve", lambda e: e.tensor_tensor(out=A2[:], in0=A2[:], in1=g2n[:], op=ALU.mult), reads=[A2, g2n], writes=[A2])
    rb = k.rowbuf.rearrange("r (oc q) -> r q oc", q=128)
    o1 = p.dma("sp", rb[0], k.mod[:, 32:48, 0], reads=[k.mod], writes=["rowbuf"], allow_slow_non_contiguous=True)
    o2 = p.dma("sp", rb[1], A2[:], reads=[A2], writes=["rowbuf"], allow_slow_non_contiguous=True)
    o3 = p.dma("sp", rb[2], k.mod[:, 48:64, 0], reads=[k.mod], writes=["rowbuf"], allow_slow_non_contiguous=True)
    o4 = p.dma("sp", rb[3], k.mod[:, 80:96, 0], reads=[k.mod], writes=["rowbuf"], allow_slow_non_contiguous=True)
    for i, nm in enumerate(("gt1", "A2", "sh2")):
        k.rowt[nm] = p.sb("row_" + nm, [128, 2048], F32)
        p.dma("sp", k.rowt[nm][:], k.rowbuf[i:i + 1, :].partition_broadcast(128), reads=["rowbuf"], writes=[k.rowt[nm]])

    g2w = p.sb("g2w_sb", [128, 2, 2048], BF16)
    poolw = p.sb("poolw_sb", [128, 4, 2, 256], BF16)
    pools = p.sb("pools_sb", [128, 8], F32)
    poolc = p.sb("poolc_sb", [128, 258], F32)
    gneps = p.sb("gneps", [128, 1], F32)
    p.dma("pool", g2w[:], k.g2w_d.rearrange("(kc q) n -> q kc n", q=128))
    p.dma("pool", poolw[:], k.poolw_d.rearrange("g (kc q) d -> q g kc d", q=128))
    p.dma("sp", pools[:], k.pools_d)
    p.dma("sp", poolc[:], k.poolc_d.partition_broadcast(128))
    p.op("pool", lambda e: e.memset(gneps[:], GN_EPS), writes=[gneps])
    ident = k.ident

    ya = p.sb("ya", [128, 2048], F32)
    yb = p.sb("yb", [128, 2048], F32)
    sq = p.sb("ysq", [128, 2048], F32)
    vt = p.sb("vt", [128, 2048], BF16)
    st1 = p.sb("st1", [128, 32], F32)
    st2 = p.sb("st2", [128, 32], F32)
    st3 = p.sb("st3", [128, 32], F32)
    bc = p.sb("bc", [128, 32], F32)
    sgdt = p.sb("sgdt", [128, 2, 128], BF16)
    prebf = p.sb("prebf", [128, 2048], BF16)
    preT = p.sb("preT", [128, 16, GT], BF16)
    Upad = p.sb("Upad", [128, NR, 96], F32)
    Sa = p.sb("Sa", [128, NR, 96], F32)
    Sb_ = p.sb("Sb_", [128, NR, 96], F32)
    dtmp = p.sb("dtmp", [128, NR, 64], F32)
    ubf = p.sb("ubf", [128, GT], BF16)
    dT = p.sb("dT", [128, 8, GT], BF16)
    zT = p.sb("zT", [128, 8, GT], BF16)
    sgt = [p.sb("sgt%d" % i, [128, GT], BF16) for i in range(2)]
    mrg = p.sb("mrg", [128, GT], F32)
    mT = p.sb("mT", [128, 16, GT], BF16)
    wblk = [p.sb("wblkD%d" % i, [128, 16, 512], BF16) for i in range(2)]
    x1g = p.sb("x1g", [128, NTS, 2048], BF16)
    x1 = p.sb("x1t", [128, 2048], F32)
    h2 = p.sb("h2t", [128, 2048], BF16)
    h2Ts = p.sb("h2Ts", [128, 16, 128], BF16)
    ss = p.sb("ssD", [128, 2], F32)
    psb = k.psb
    pbt = k.pbt
    cnt = {"w": 0}
    p.op("dve", lambda e: e.memset(Upad[:], 0.0), writes=[Upad])

    def wload(src_ap, nk):
        w = wblk[cnt["w"] % 2]
        cnt["w"] += 1
        for h in range(0, nk, 8):
            p.dma("pool", w[:, h:h + 8, :], src_ap[:, h:h + 8, :], reads=[], writes=[(w.name, h // 8)])
        return w

    wpo_v = k.wpo_d.rearrange("(kc q) n -> q kc n", q=128)
    wro_v = k.wro_d.rearrange("(kc q) n -> q kc n", q=128)
    wout_v = k.wout_d.rearrange("(kc q) n -> q kc n", q=128)

    def readout_tile(tg, ts):
        ti = tg * NTS + ts
        t0 = ti * 128
        p.dma("sp", ya[:], k.Yscr[0][t0:t0 + 128, :], reads=[], writes=[ya])
        p.dma("sp", yb[:], k.Yscr[1][t0:t0 + 128, :], reads=[], writes=[yb])
        p.dma("sp", vt[:], k.Vtok[256 + t0:256 + t0 + 128, :], reads=[], writes=[vt])
        p.dma("sp", bc[:], k.bcoef[t0:t0 + 128, :], reads=[], writes=[bc])
        p.dma("sp", sgdt[:], k.sgdT.rearrange("(kc q) t -> q kc t", q=128)[:, :, t0:t0 + 128], reads=[], writes=[sgdt])
        y3 = ya[:].rearrange("p (h c) -> p h c", c=64)
        p.op("dve", lambda e: e.tensor_tensor(out=ya[:], in0=ya[:], in1=yb[:], op=ALU.add), reads=[ya, yb], writes=[ya])
        p.op("dve", lambda e: e.tensor_reduce(out=st1[:], in_=y3, axis=AX.X, op=ALU.add), reads=[ya], writes=[st1])
        p.op("act", lambda e: e.activation(out=sq[:], in_=ya[:], func=AF.Square), reads=[ya], writes=[sq])
        p.op("dve", lambda e: e.tensor_reduce(out=st2[:], in_=sq[:].rearrange("p (h c) -> p h c", c=64), axis=AX.X, op=ALU.add),
             reads=[sq], writes=[st2])
        p.op("dve", lambda e: e.tensor_scalar(out=st1[:], in0=st1[:], scalar1=1.0 / 64, scalar2=None, op0=ALU.mult), reads=[st1], writes=[st1])
        p.op("dve", lambda e: e.tensor_tensor(out=st3[:], in0=st1[:], in1=st1[:], op=ALU.mult), reads=[st1], writes=[st3])
        p.op("dve", lambda e: e.scalar_tensor_tensor(out=st2[:], in0=st2[:], scalar=1.0 / 64, in1=st3[:], op0=ALU.mult, op1=ALU.subtract),
             reads=[st2, st3], writes=[st2])
        p.op("act", lambda e: e.activation(out=st2[:], in_=st2[:], func=AF.Sqrt, bias=gneps[:, 0:1]), reads=[st2, gneps], writes=[st2])
        p.op("dve", lambda e: e.reciprocal(out=st2[:], in_=st2[:]), reads=[st2], writes=[st2])
        p.op("dve", lambda e: e.tensor_tensor(out=y3, in0=y3, in1=st1[:].unsqueeze(2).to_broadcast([128, 32, 64]), op=ALU.subtract),
             reads=[ya, st1], writes=[ya])
        p.op("dve", lambda e: e.tensor_tensor(out=y3, in0=y3, in1=st2[:].unsqueeze(2).to_broadcast([128, 32, 64]), op=ALU.mult),
             reads=[ya, st2], writes=[ya])
        p.op("dve", lambda e: e.tensor_tensor(out=ya[:], in0=ya[:], in1=k.rowt["lnxw"][:], op=ALU.mult), reads=[ya, k.rowt["lnxw"]], writes=[ya])
        p.op("dve", lambda e: e.tensor_tensor(out=ya[:], in0=ya[:], in1=k.rowt["lnxb"][:], op=ALU.add), reads=[ya, k.rowt["lnxb"]], writes=[ya])
        p.op("dve", lambda e: e.tensor_tensor(out=yb[:].rearrange("p (h c) -> p h c", c=64), in0=vt[:].rearrange("p (h c) -> p h c", c=64),
                                              in1=bc[:].unsqueeze(2).to_broadcast([128, 32, 64]), op=ALU.mult), reads=[vt, bc], writes=[yb])
        p.op("dve", lambda e: e.tensor_tensor(out=ya[:], in0=ya[:], in1=yb[:], op=ALU.add), reads=[ya, yb], writes=[ya])
        for cb in range(4):
            ps = psb[cb % 2]
            for kc in range(2):
                p.op("pe", lambda e, cb=cb, kc=kc, ps=ps: e.matmul(ps[:, :], lhsT=sgdt[:, kc, :], rhs=g2w[:, kc, cb * 512:(cb + 1) * 512],
                                                                  start=(kc == 0), stop=(kc == 1)), reads=[sgdt, g2w], writes=[ps])
            p.op("dve", lambda e, cb=cb, ps=ps: e.tensor_tensor(out=prebf[:, cb * 512:(cb + 1) * 512], in0=ya[:, cb * 512:(cb + 1) * 512],
                                                                 in1=ps[:, :], op=ALU.mult), reads=[ya, ps], writes=[(prebf.name, cb)])
        for half in range(2):
            pt = pbt[half]
            for j in range(8):
                kc = half * 8 + j
                p.op("pe", lambda e, kc=kc, j=j, pt=pt: e.transpose(pt[:, j * 128:(j + 1) * 128], prebf[:, kc * 128:(kc + 1) * 128], ident[:]),
                     reads=[prebf, ident], writes=[pt])
            if half:
                p.op("act", lambda e, half=half, pt=pt: e.activation(out=preT[:, half * 8:(half + 1) * 8, ts * 128:(ts + 1) * 128],
                                                                    in_=pt[:].rearrange("p (a b) -> p a b", b=128), func=AF.Copy),
                     reads=[pt], writes=[(preT.name, ts)])
            else:
                p.op("dve", lambda e, half=half, pt=pt: e.tensor_copy(out=preT[:, half * 8:(half + 1) * 8, ts * 128:(ts + 1) * 128],
                                                                     in_=pt[:].rearrange("p (a b) -> p a b", b=128)),
                     reads=[pt], writes=[(preT.name, ts)])

    def pool_group(tg):
        t0 = tg * GT
        for cc in range(8):
            gi = cc // 2
            p.dma("sp", ubf[:], k.uT[cc * 128:(cc + 1) * 128, t0:t0 + GT], reads=[], writes=[ubf])
            p.op("act", lambda e: e.activation(out=Upad[:, :, 16:80], in_=ubf[:].rearrange("p (r c) -> p r c", c=64), func=AF.Copy),
                 reads=[ubf], writes=[Upad])
            src, lo, hi = Upad, 2, 94
            p.op("dve", lambda e: e.tensor_tensor(out=Sa[:, :, 2:94], in0=Upad[:, :, 1:93], in1=Upad[:, :, 2:94], op=ALU.add),
                 reads=[Upad], writes=[Sa])
            cur, oth = Sa, Sb_
            sh = 1
            lo, hi = 2, 94
            for step in range(gi):
                nlo, nhi = lo + sh, hi - sh
                p.op("dve", lambda e, cur=cur, oth=oth, nlo=nlo, nhi=nhi, sh=sh: e.tensor_tensor(
                    out=oth[:, :, nlo:nhi], in0=cur[:, :, nlo - sh:nhi - sh], in1=cur[:, :, nlo + sh:nhi + sh], op=ALU.add),
                    reads=[cur], writes=[oth])
                cur, oth = oth, cur
                lo, hi = nlo, nhi
                sh *= 2
            p.op("dve", lambda e, cur=cur: e.tensor_scalar(out=dtmp[:], in0=cur[:, :, 16:80], scalar1=poolc[:, 256:257], scalar2=None, op0=ALU.mult),
                 reads=[cur, poolc], writes=[dtmp])
            p.op("dve", lambda e, cur=cur: e.scalar_tensor_tensor(out=dtmp[:], in0=cur[:, :, 17:81], scalar=poolc[:, 257:258], in1=dtmp[:],
                                                                  op0=ALU.mult, op1=ALU.add), reads=[cur, poolc, dtmp], writes=[dtmp])
            p.op("dve", lambda e, gi=gi: e.tensor_tensor(out=dtmp[:], in0=dtmp[:],
                                                        in1=poolc[:, gi * 64:(gi + 1) * 64].unsqueeze(1).to_broadcast([128, NR, 64]), op=ALU.mult),
                 reads=[dtmp, poolc], writes=[dtmp])
            p.op("dve", lambda e, cc=cc: e.tensor_tensor(out=dT[:, cc, :].rearrange("p (r c) -> p r c", c=64), in0=dtmp[:], in1=Upad[:, :, 16:80],
                                                        op=ALU.subtract), reads=[dtmp, Upad], writes=[(dT.name, cc)])
        for gi in range(4):
            for dc in range(2):
                ps = psb[(gi * 2 + dc) % 2]
                for kc in range(2):
                    p.op("pe", lambda e, gi=gi, dc=dc, kc=kc, ps=ps: e.matmul(ps[:, 0:GT], lhsT=poolw[:, gi, kc, dc * 128:(dc + 1) * 128],
                                                                            rhs=dT[:, gi * 2 + kc, :], start=(kc == 0), stop=(kc == 1)),
                         reads=[poolw, (dT.name, gi * 2 + kc)], writes=[ps])
                oc = gi * 2 + dc
                p.op("act", lambda e, oc=oc, ps=ps: e.activation(out=zT[:, oc, :], in_=ps[:, 0:GT], func=AF.Copy, scale=pools[:, oc:oc + 1]),
                     reads=[ps, pools], writes=[(zT.name, oc)])

    def merge_group(tg):
        t0 = tg * GT
        for blk in range(4):
            wp = wload(wpo_v[:, :, blk * 512:(blk + 1) * 512], 8)
            wr = wload(wro_v[:, :, blk * 512:(blk + 1) * 512], 16)
            for j in range(4):
                fc = blk * 4 + j
                p.dma("sp", sgt[0][:], k.sgT[fc * 128:(fc + 1) * 128, t0:t0 + GT], reads=[], writes=[sgt[0]])
                p.dma("sp", sgt[1][:], k.sgT[2048 + fc * 128:2048 + (fc + 1) * 128, t0:t0 + GT], reads=[], writes=[sgt[1]])
                ps = psb[2]
                for kc in range(8):
                    p.op("pe", lambda e, kc=kc, j=j, wp=wp, ps=ps: e.matmul(ps[:, 0:GT], lhsT=wp[:, kc, j * 128:(j + 1) * 128], rhs=zT[:, kc, :],
                                                                          start=(kc == 0), stop=(kc == 7)), reads=[(wp.name, 0), zT], writes=[ps])
                p.op("dve", lambda e, ps=ps: e.tensor_tensor(out=mrg[:], in0=sgt[0][:], in1=ps[:, 0:GT], op=ALU.mult), reads=[sgt[0], ps], writes=[mrg])
                ps2 = psb[3]
                for kc in range(16):
                    p.op("pe", lambda e, kc=kc, j=j, wr=wr, ps2=ps2: e.matmul(ps2[:, 0:GT], lhsT=wr[:, kc, j * 128:(j + 1) * 128], rhs=preT[:, kc, :],
                                                                            start=(kc == 0), stop=(kc == 15)),
                         reads=[(wr.name, kc // 8), preT], writes=[ps2])
                p.op("dve", lambda e, ps2=ps2: e.tensor_tensor(out=sgt[1][:], in0=sgt[1][:], in1=ps2[:, 0:GT], op=ALU.mult), reads=[sgt[1], ps2], writes=[sgt[1]])
                p.op("dve", lambda e, fc=fc: e.tensor_tensor(out=mT[:, fc, :], in0=mrg[:], in1=sgt[1][:], op=ALU.add), reads=[mrg, sgt[1]],
                     writes=[(mT.name, fc)])

    def outproj_group(tg):
        for cb in range(4):
            w = wload(wout_v[:, :, cb * 512:(cb + 1) * 512], 16)
            cs = slice(cb * 512, (cb + 1) * 512)
            for ts in range(NTS):
                ps = psb[4 + ts % 2]
                for kc in range(16):
                    p.op("pe", lambda e, kc=kc, w=w, ps=ps, ts=ts: e.matmul(ps[:, :], lhsT=mT[:, kc, ts * 128:(ts + 1) * 128], rhs=w[:, kc, :],
                                                                          start=(kc == 0), stop=(kc == 15)), reads=[(w.name, kc // 8), mT], writes=[ps])
                p.op("dve", lambda e, cs=cs, ps=ps, ts=ts: e.tensor_tensor(out=x1g[:, ts, cs], in0=ps[:, :], in1=k.rowt["gt1"][:, cs], op=ALU.mult),
                     reads=[ps, k.rowt["gt1"]], writes=[(x1g.name, ts)])
        for ts in range(NTS):
            outproj_tile(tg, ts)

    def outproj_tile(tg, ts):
            ti = tg * NTS + ts
            t0 = ti * 128
            p.dma("sp", yb[:], k.xown_d[t0:t0 + 128, :], reads=[], writes=[yb])
            p.op("dve", lambda e: e.tensor_tensor(out=x1[:], in0=x1g[:, ts, :], in1=yb[:], op=ALU.add), reads=[(x1g.name, ts), yb], writes=[x1])
            p.dma("sp", k.x1scr[t0:t0 + 128, :], x1[:], reads=[x1], writes=[])
            p.op("act", lambda e: e.activation(out=sq[:], in_=x1[:], func=AF.Square), reads=[x1], writes=[sq])
            p.op("dve", lambda e: e.tensor_reduce(out=ss[:, 0:1], in_=sq[:], axis=AX.X, op=ALU.add), reads=[sq], writes=[ss])
            p.op("act", lambda e: e.activation(out=ss[:, 1:2], in_=ss[:, 0:1], func=AF.Sqrt, scale=1.0 / D, bias=k.eps_t[:, 0:1]),
                 reads=[ss, k.eps_t], writes=[ss])
            p.op("dve", lambda e: e.reciprocal(out=ss[:, 1:2], in_=ss[:, 1:2]), reads=[ss], writes=[ss])
            p.op("dve", lambda e: e.scalar_tensor_tensor(out=sq[:], in0=x1[:], scalar=ss[:, 1:2], in1=k.rowt["A2"][:], op0=ALU.mult, op1=ALU.mult),
                 reads=[x1, ss, k.rowt["A2"]], writes=[sq])
            p.op("dve", lambda e: e.tensor_tensor(out=h2[:], in0=sq[:], in1=k.rowt["sh2"][:], op=ALU.add), reads=[sq, k.rowt["sh2"]], writes=[h2])
            p.dma("sp", k.h2tok[t0:t0 + 128, :], h2[:], reads=[h2], writes=[])
            for half in range(2):
                pt = pbt[half]
                for j in range(8):
                    kc = half * 8 + j
                    p.op("pe", lambda e, kc=kc, j=j, pt=pt: e.transpose(pt[:, j * 128:(j + 1) * 128], h2[:, kc * 128:(kc + 1) * 128], ident[:]),
                         reads=[h2, ident], writes=[pt])
                if half:
                    p.op("act", lambda e, pt=pt: e.activation(out=h2Ts[:, 8:16, :].rearrange("p a b -> p (a b)"), in_=pt[:], func=AF.Copy),
                         reads=[pt], writes=[(h2Ts.name, 1)])
                else:
                    p.op("dve", lambda e, pt=pt: e.tensor_copy(out=h2Ts[:, 0:8, :].rearrange("p a b -> p (a b)"), in_=pt[:]),
                         reads=[pt], writes=[(h2Ts.name, 0)])
            p.dma("sp", k.h2T.rearrange("(kc q) t -> q kc t", q=128)[:, :, t0:t0 + 128], h2Ts[:], reads=[h2Ts], writes=[])

    ng = getattr(k, "ngroupsD", None) or (2048 // GT)
    for tg in range(ng):
        for ts in range(NTS):
            readout_tile(tg, ts)
        pool_group(tg)
        merge_group(tg)
        outproj_group(tg)
    p.barrier()
    p.release(mD)


import numpy as np

CAP = 768
SUBS = ((0, 512), (512, 256))
NSLOT = 64 * CAP
D = 2048


def phaseE(k):
    p, nc = k.p, k.nc
    din, dscr = k.din, k.dscr
    k.routw_d = din("routw", [2048, 64])
    k.routb_d = din("routb", [1, 128])
    k.tri_d = din("tri", [128, 128])
    k.exg_d = din("exg", [64, 2048, 512])
    k.exu_d = din("exu", [64, 2048, 512])
    k.exd_d = din("exd", [64, 512, 2048])
    k.shg_d = din("shg", [2048, 512])
    k.shu_d = din("shu", [2048, 512])
    k.shd_d = din("shd", [512, 2048])
    k.out_d = nc.dram_tensor("out", [2048, 2048], F32, kind="ExternalOutput").ap()
    k.Xg = dscr("Xg", [NSLOT, 2048], BF16)
    k.Yg = dscr("Yg", [NSLOT, 2048], BF16)
    k.shscr = dscr("shscr", [2048, 2048], F32)
    k.routdbg = dscr("routdbg", [2048, 16], F32)
    psb, pbt = k.psb, k.pbt
    ident = k.ident

    slots_all = p.sb("slots_all", [128, 128], I32)
    wk_all = p.sb("wk_all", [128, 16, 8], F32)
    mE = p.mark()

    routw = p.sb("routw_sb", [128, 16, 64], BF16)
    routb = p.sb("routb_sb", [128, 128], F32)
    tri = p.sb("tri_sb", [128, 128], BF16)
    base = p.sb("base_cnt", [128, 64], F32)
    shg = p.sb("shg_sb", [128, 16, 512], BF16)
    shu = p.sb("shu_sb", [128, 16, 512], BF16)
    shd = p.sb("shd_sb", [128, 4, 2048], BF16)
    p.dma("pool", routw[:], k.routw_d.rearrange("(kc q) n -> q kc n", q=128))
    p.dma("sp", routb[:], k.routb_d.partition_broadcast(128))
    p.dma("pool", tri[:], k.tri_d)
    for h in range(2):
        p.dma("pool", shg[:, h * 8:(h + 1) * 8, :], k.shg_d.rearrange("(kc q) n -> q kc n", q=128)[:, h * 8:(h + 1) * 8, :], writes=[(shg.name, h)])
        p.dma("pool", shu[:, h * 8:(h + 1) * 8, :], k.shu_d.rearrange("(kc q) n -> q kc n", q=128)[:, h * 8:(h + 1) * 8, :], writes=[(shu.name, h)])
    p.dma("pool", shd[:], k.shd_d.rearrange("(kc q) n -> q kc n", q=128))
    p.op("dve", lambda e: e.memset(base[:], 0.0), writes=[base])
    h2Tg = p.sb("h2Tg", [128, 16, 512], BF16)
    h2r = p.sb("h2r", [128, 2048], BF16)
    sg = p.sb("sgE", [128, 512], F32)
    actT = p.sb("actT", [128, 4, 512], BF16)
    sho = p.sb("sho", [128, 2048], F32)
    R = {}
    for nm, w in (("sc", 64), ("sel", 64), ("tmp", 64), ("msel", 64), ("emask", 64), ("wd", 64), ("key", 64), ("oh", 64), ("rk", 64),
                  ("m1", 8), ("m2", 8), ("gs", 8), ("g8", 8), ("gm", 8), ("pen", 8), ("e8", 8), ("k8", 8), ("den", 2)):
        R[nm] = p.sb("R_" + nm, [128, w], F32)
    emb = p.sb("emask_bf", [128, 64], BF16)
    h2Tv = k.h2T.rearrange("(kc q) t -> q kc t", q=128)

    def shared_group(tg):
        t0 = tg * 512
        for h in range(2):
            p.dma("sp", h2Tg[:, h * 8:(h + 1) * 8, :], h2Tv[:, h * 8:(h + 1) * 8, t0:t0 + 512], reads=[], writes=[(h2Tg.name, h)])
        for dc in range(4):
            for kc in range(16):
                p.op("pe", lambda e, dc=dc, kc=kc: e.matmul(psb[0][:, :], lhsT=shg[:, kc, dc * 128:(dc + 1) * 128], rhs=h2Tg[:, kc, :],
                                                           start=(kc == 0), stop=(kc == 15)), reads=[shg, h2Tg], writes=[psb[0]])
            for kc in range(16):
                p.op("pe", lambda e, dc=dc, kc=kc: e.matmul(psb[1][:, :], lhsT=shu[:, kc, dc * 128:(dc + 1) * 128], rhs=h2Tg[:, kc, :],
                                                           start=(kc == 0), stop=(kc == 15)), reads=[shu, h2Tg], writes=[psb[1]])
            p.op("act", lambda e: e.activation(out=sg[:], in_=psb[0][:, :], func=AF.Silu), reads=[psb[0]], writes=[sg])
            p.op("dve", lambda e, dc=dc: e.tensor_tensor(out=actT[:, dc, :], in0=sg[:], in1=psb[1][:, :], op=ALU.mult), reads=[sg, psb[1]],
                 writes=[(actT.name, dc)])
        for ts in range(4):
            for cb in range(4):
                ps = psb[2 + cb % 2]
                for dc in range(4):
                    p.op("pe", lambda e, ts=ts, cb=cb, dc=dc, ps=ps: e.matmul(ps[:, :], lhsT=actT[:, dc, ts * 128:(ts + 1) * 128],
                                                                            rhs=shd[:, dc, cb * 512:(cb + 1) * 512], start=(dc == 0), stop=(dc == 3)),
                         reads=[actT, shd], writes=[ps])
                p.op("act" if cb % 2 else "dve",
                     (lambda e, cb=cb, ps=ps: e.activation(out=sho[:, cb * 512:(cb + 1) * 512], in_=ps[:, :], func=AF.Copy)) if cb % 2 else
                     (lambda e, cb=cb, ps=ps: e.tensor_copy(out=sho[:, cb * 512:(cb + 1) * 512], in_=ps[:, :])),
                     reads=[ps], writes=[(sho.name, cb)])
            p.dma("sp", k.shscr[t0 + ts * 128:t0 + (ts + 1) * 128, :], sho[:], reads=[sho], writes=[])
            route_tile(tg * 4 + ts, ts)

    def route_tile(ti, ts):
        t0 = ti * 128
        ps = psb[4]
        for kc in range(16):
            p.op("pe", lambda e, kc=kc: e.matmul(ps[:, 0:64], lhsT=h2Tg[:, kc, ts * 128:(ts + 1) * 128], rhs=routw[:, kc, :],
                                                start=(kc == 0), stop=(kc == 15)), reads=[h2Tg, routw], writes=[ps])
        sc, sel, tmp, msel, emask, wd, key, oh, rk = (R[n] for n in ("sc", "sel", "tmp", "msel", "emask", "wd", "key", "oh", "rk"))
        m1, m2, gs, g8, gm, pen, e8, k8, den = (R[n] for n in ("m1", "m2", "gs", "g8", "gm", "pen", "e8", "k8", "den"))
        v3 = lambda t: t[:].rearrange("p (g e) -> p g e", e=8)
        b3 = lambda t: t[:].unsqueeze(2).to_broadcast([128, 8, 8])
        p.op("act", lambda e: e.activation(out=sc[:], in_=ps[:, 0:64], func=AF.Sigmoid), reads=[ps], writes=[sc])
        p.op("dve", lambda e: e.tensor_tensor(out=sel[:], in0=sc[:], in1=routb[:, 0:64], op=ALU.add), reads=[sc, routb], writes=[sel])
        p.op("dve", lambda e: e.tensor_reduce(out=m1[:], in_=v3(sel), axis=AX.X, op=ALU.max), reads=[sel], writes=[m1])
        p.op("dve", lambda e: e.tensor_tensor(out=v3(tmp), in0=v3(sel), in1=b3(m1), op=ALU.is_equal), reads=[sel, m1], writes=[tmp])
        p.op("dve", lambda e: e.scalar_tensor_tensor(out=tmp[:], in0=tmp[:], scalar=-1e9, in1=sel[:], op0=ALU.mult, op1=ALU.add),
             reads=[tmp, sel], writes=[tmp])
        p.op("dve", lambda e: e.tensor_reduce(out=m2[:], in_=v3(tmp), axis=AX.X, op=ALU.max), reads=[tmp], writes=[m2])
        p.op("dve", lambda e: e.tensor_tensor(out=gs[:], in0=m1[:], in1=m2[:], op=ALU.add), reads=[m1, m2], writes=[gs])
        p.op("dve", lambda e: e.max(out=g8[:], in_=gs[:]), reads=[gs], writes=[g8])
        p.op("dve", lambda e: e.tensor_scalar(out=gm[:], in0=gs[:], scalar1=g8[:, 3:4], scalar2=None, op0=ALU.is_ge), reads=[gs, g8], writes=[gm])
        p.op("dve", lambda e: e.tensor_scalar(out=pen[:], in0=gm[:], scalar1=1e9, scalar2=-1e9, op0=ALU.mult, op1=ALU.add), reads=[gm], writes=[pen])
        p.op("dve", lambda e: e.tensor_tensor(out=v3(msel), in0=v3(sel), in1=b3(gm), op=ALU.mult), reads=[sel, gm], writes=[msel])
        p.op("dve", lambda e: e.tensor_tensor(out=v3(msel), in0=v3(msel), in1=b3(pen), op=ALU.add), reads=[msel, pen], writes=[msel])
        p.op("dve", lambda e: e.max(out=e8[:], in_=msel[:]), reads=[msel], writes=[e8])
        p.op("dve", lambda e: e.tensor_scalar(out=emask[:], in0=msel[:], scalar1=e8[:, 7:8], scalar2=None, op0=ALU.is_ge), reads=[msel, e8], writes=[emask])
        p.op("dve", lambda e: e.tensor_tensor(out=wd[:], in0=sc[:], in1=emask[:], op=ALU.mult), reads=[sc, emask], writes=[wd])
        p.op("dve", lambda e: e.tensor_reduce(out=den[:, 0:1], in_=wd[:], axis=AX.X, op=ALU.add), reads=[wd], writes=[den])
        p.op("dve", lambda e: e.reciprocal(out=den[:, 1:2], in_=den[:, 0:1]), reads=[den], writes=[den])
        p.op("dve", lambda e: e.tensor_scalar(out=wd[:], in0=wd[:], scalar1=den[:, 1:2], scalar2=2.5, op0=ALU.mult, op1=ALU.mult), reads=[wd, den], writes=[wd])
        p.op("act", lambda e: e.activation(out=emb[:], in_=emask[:], func=AF.Copy), reads=[emask], writes=[emb])
        ps2 = psb[5]
        p.op("pe", lambda e: e.matmul(ps2[:, 0:64], lhsT=tri[:], rhs=emb[:], start=True, stop=True), reads=[tri, emb], writes=[ps2])
        p.op("dve", lambda e: e.tensor_tensor(out=rk[:], in0=ps2[:, 0:64], in1=base[:], op=ALU.add), reads=[ps2, base], writes=[rk])
        p.op("dve", lambda e: e.tensor_scalar(out=oh[:], in0=rk[:], scalar1=float(CAP) - 0.5, scalar2=None, op0=ALU.is_lt), reads=[rk], writes=[oh])
        p.op("dve", lambda e: e.tensor_tensor(out=oh[:], in0=oh[:], in1=emask[:], op=ALU.mult), reads=[oh, emask], writes=[oh])
        p.op("dve", lambda e: e.tensor_tensor(out=key[:], in0=rk[:], in1=routb[:, 64:128], op=ALU.add), reads=[rk, routb], writes=[key])
        p.op("dve", lambda e: e.scalar_tensor_tensor(out=key[:], in0=key[:], scalar=1.0, in1=oh[:], op0=ALU.add, op1=ALU.mult), reads=[key, oh], writes=[key])
        p.op("dve", lambda e: e.tensor_scalar(out=key[:], in0=key[:], scalar1=-1.0, scalar2=None, op0=ALU.add), reads=[key], writes=[key])
        p.op("dve", lambda e: e.max(out=k8[:], in_=key[:]), reads=[key], writes=[k8])
        p.op("dve", lambda e: e.tensor_copy(out=slots_all[:, ti * 8:(ti + 1) * 8], in_=k8[:]), reads=[k8], writes=[(slots_all.name, ti)])
        for j in range(8):
            p.op("dve", lambda e, j=j: e.tensor_scalar(out=oh[:], in0=key[:], scalar1=k8[:, j:j + 1], scalar2=None, op0=ALU.is_equal),
                 reads=[key, k8], writes=[oh])
            p.op("dve", lambda e: e.tensor_tensor(out=oh[:], in0=oh[:], in1=wd[:], op=ALU.mult), reads=[oh, wd], writes=[oh])
            p.op("dve", lambda e, j=j: e.tensor_reduce(out=wk_all[:, ti, j:j + 1], in_=oh[:], axis=AX.X, op=ALU.add), reads=[oh],
                 writes=[(wk_all.name, ti)])
        p.op("pe", lambda e: e.matmul(ps2[:, 64:128], lhsT=k.ones_bf[:], rhs=emb[:], start=True, stop=True), reads=[k.ones_bf, emb], writes=[ps2])
        p.op("dve", lambda e: e.tensor_tensor(out=base[:], in0=base[:], in1=ps2[:, 64:128], op=ALU.add), reads=[base, ps2], writes=[base])
        p.dma("sp", h2r[:], k.h2tok[t0:t0 + 128, :], reads=[], writes=[h2r])
        for j in range(8):
            p.dma_fn("pool", lambda e, j=j: e.indirect_dma_start(
                out=k.Xg, out_offset=bass.IndirectOffsetOnAxis(ap=slots_all[:, ti * 8 + j:ti * 8 + j + 1], axis=0), in_=h2r[:], in_offset=None,
                bounds_check=p.getreg(e, NSLOT - 1), oob_is_err=False), reads=[h2r, (slots_all.name, ti)], writes=[])
        if "routdbg" in k.dbg:
            p.dma("sp", k.routdbg[t0:t0 + 128, 0:8], k8[:], reads=[k8], writes=[])
            p.dma("sp", k.routdbg[t0:t0 + 128, 8:16], wk_all[:, ti, :], reads=[(wk_all.name, ti)], writes=[])

    ngE = getattr(k, "ngroupsE", None) or 4
    for tg in range(ngE):
        shared_group(tg)
    p.barrier()
    p.release(mE)
    if getattr(k, "ecut", None) == 1:
        return

    NEXP = getattr(k, "nexp", None) or 64
    Wg = [p.sb("Wg%d" % i, [128, 16, 512], BF16) for i in range(2)]
    Wu = [p.sb("Wu%d" % i, [128, 16, 512], BF16) for i in range(2)]
    Wd = [p.sb("Wd%d" % i, [128, 4, 2048], BF16) for i in range(2)]
    Xs = p.sb("Xs", [128, 4, 2048], BF16)
    XT = p.sb("XT", [128, 16, 512], BF16)
    sg2 = p.sb("sg2", [128, 512], F32)
    act2 = p.sb("act2", [128, 4, 512], BF16)
    yst = [p.sb("yst%d" % i, [128, 2048], BF16) for i in range(2)]
    exg_v = k.exg_d.rearrange("e (kc q) n -> e q kc n", q=128)
    exu_v = k.exu_d.rearrange("e (kc q) n -> e q kc n", q=128)
    exd_v = k.exd_d.rearrange("e (kc q) n -> e q kc n", q=128)
    cnt = {"y": 0}

    def load_w(e_):
        b = e_ % 2
        for h in range(2):
            p.dma("pool", Wg[b][:, h * 8:(h + 1) * 8, :], exg_v[e_][:, h * 8:(h + 1) * 8, :], reads=[], writes=[(Wg[b].name, h)])
            p.dma("pool", Wu[b][:, h * 8:(h + 1) * 8, :], exu_v[e_][:, h * 8:(h + 1) * 8, :], reads=[], writes=[(Wu[b].name, h)])
        for h in range(2):
            p.dma("pool", Wd[b][:, h * 2:(h + 1) * 2, :], exd_v[e_][:, h * 2:(h + 1) * 2, :], reads=[], writes=[(Wd[b].name, h)])

    def expert(e_):
        for off, n in SUBS:
            expert_sub(e_, off, n)

    def expert_sub(e_, off, n):
        b = e_ % 2
        nst = n // 128
        r0 = e_ * CAP + off
        p.dma("sp", Xs[:, 0:nst, :], k.Xg[r0:r0 + n, :].rearrange("(st q) f -> q st f", q=128), reads=[], writes=[Xs])
        for st in range(nst):
            for half in range(2):
                pt = pbt[half]
                for j in range(8):
                    kc = half * 8 + j
                    p.op("pe", lambda e, st=st, kc=kc, j=j, pt=pt: e.transpose(pt[:, j * 128:(j + 1) * 128], Xs[:, st, kc * 128:(kc + 1) * 128], ident[:]),
                         reads=[Xs, ident], writes=[pt])
                if half:
                    p.op("act", lambda e, st=st, pt=pt: e.activation(out=XT[:, 8:16, st * 128:(st + 1) * 128],
                                                                    in_=pt[:].rearrange("p (a b) -> p a b", b=128), func=AF.Copy),
                         reads=[pt], writes=[(XT.name, st)])
                else:
                    p.op("dve", lambda e, st=st, pt=pt: e.tensor_copy(out=XT[:, 0:8, st * 128:(st + 1) * 128],
                                                                     in_=pt[:].rearrange("p (a b) -> p a b", b=128)),
                         reads=[pt], writes=[(XT.name, st)])
        for dc in range(4):
            for kc in range(16):
                p.op("pe", lambda e, dc=dc, kc=kc: e.matmul(psb[0][:, 0:n], lhsT=Wg[b][:, kc, dc * 128:(dc + 1) * 128], rhs=XT[:, kc, 0:n],
                                                           start=(kc == 0), stop=(kc == 15)), reads=[(Wg[b].name, kc // 8), XT], writes=[psb[0]])
            for kc in range(16):
                p.op("pe", lambda e, dc=dc, kc=kc: e.matmul(psb[1][:, 0:n], lhsT=Wu[b][:, kc, dc * 128:(dc + 1) * 128], rhs=XT[:, kc, 0:n],
                                                           start=(kc == 0), stop=(kc == 15)), reads=[(Wu[b].name, kc // 8), XT], writes=[psb[1]])
            p.op("act", lambda e: e.activation(out=sg2[:, 0:n], in_=psb[0][:, 0:n], func=AF.Silu), reads=[psb[0]], writes=[sg2])
            p.op("dve", lambda e, dc=dc: e.tensor_tensor(out=act2[:, dc, 0:n], in0=sg2[:, 0:n], in1=psb[1][:, 0:n], op=ALU.mult), reads=[sg2, psb[1]],
                 writes=[(act2.name, dc)])
        for st in range(nst):
            ys = yst[cnt["y"] % 2]
            cnt["y"] += 1
            for cb in range(4):
                ps = psb[2 + cb]
                for dc in range(4):
                    p.op("pe", lambda e, st=st, cb=cb, dc=dc, ps=ps: e.matmul(ps[:, :], lhsT=act2[:, dc, st * 128:(st + 1) * 128],
                                                                            rhs=Wd[b][:, dc, cb * 512:(cb + 1) * 512], start=(dc == 0), stop=(dc == 3)),
                         reads=[act2, (Wd[b].name, dc // 2)], writes=[ps])
                if cb % 2:
                    p.op("act", lambda e, cb=cb, ps=ps, ys=ys: e.activation(out=ys[:, cb * 512:(cb + 1) * 512], in_=ps[:, :], func=AF.Copy),
                         reads=[ps], writes=[(ys.name, cb)])
                else:
                    p.op("dve", lambda e, cb=cb, ps=ps, ys=ys: e.tensor_copy(out=ys[:, cb * 512:(cb + 1) * 512], in_=ps[:, :]),
                         reads=[ps], writes=[(ys.name, cb)])
            p.dma("sp", k.Yg[r0 + st * 128:r0 + (st + 1) * 128, :], ys[:], reads=[ys], writes=[])

    load_w(0)
    for e_ in range(NEXP):
        if e_ + 1 < NEXP:
            load_w(e_ + 1)
        expert(e_)
    p.barrier()
    p.release(mE)
    if getattr(k, "ecut", None) == 2:
        return

    rows = {}
    for i, nm in ((3, "gt2"),):
        rows[nm] = p.sb("rowE_" + nm, [128, 2048], F32)
        p.dma("sp", rows[nm][:], k.rowbuf[i:i + 1, :].partition_broadcast(128), reads=[], writes=[rows[nm]])
    rows["fing"] = p.sb("rowE_fing", [128, 2048], F32)
    p.dma("sp", rows["fing"][:], k.rows_d[2:3, :].partition_broadcast(128), reads=[], writes=[rows["fing"]])
    acc = p.sb("accE", [128, 2048], F32)
    yk = [p.sb("yk%d" % i, [128, 2048], BF16) for i in range(2)]
    x1t = p.sb("x1E", [128, 2048], F32)
    sqE = p.sb("sqE", [128, 2048], F32)
    ssE = p.sb("ssE", [128, 2], F32)

    def combine_tile(ti):
        t0 = ti * 128
        p.dma("sp", acc[:], k.shscr[t0:t0 + 128, :], reads=[], writes=[acc])
        p.dma("sp", x1t[:], k.x1scr[t0:t0 + 128, :], reads=[], writes=[x1t])
        for j in range(8):
            y = yk[j % 2]
            p.op("pool", lambda e, y=y: e.memset(y[:], 0.0), writes=[y])
            p.dma_fn("pool", lambda e, j=j, y=y: e.indirect_dma_start(
                out=y[:], out_offset=None, in_=k.Yg, in_offset=bass.IndirectOffsetOnAxis(ap=slots_all[:, ti * 8 + j:ti * 8 + j + 1], axis=0),
                bounds_check=p.getreg(e, NSLOT - 1), oob_is_err=False), reads=[y, slots_all], writes=[y])
            p.op("dve", lambda e, j=j, y=y: e.scalar_tensor_tensor(out=acc[:], in0=y[:], scalar=wk_all[:, ti, j:j + 1], in1=acc[:],
                                                                    op0=ALU.mult, op1=ALU.add), reads=[y, wk_all, acc], writes=[acc])
        p.op("dve", lambda e: e.tensor_tensor(out=acc[:], in0=acc[:], in1=rows["gt2"][:], op=ALU.mult), reads=[acc, rows["gt2"]], writes=[acc])
        p.op("dve", lambda e: e.tensor_tensor(out=x1t[:], in0=x1t[:], in1=acc[:], op=ALU.add), reads=[x1t, acc], writes=[x1t])
        p.op("act", lambda e: e.activation(out=sqE[:], in_=x1t[:], func=AF.Square), reads=[x1t], writes=[sqE])
        p.op("dve", lambda e: e.tensor_reduce(out=ssE[:, 0:1], in_=sqE[:], axis=AX.X, op=ALU.add), reads=[sqE], writes=[ssE])
        p.op("act", lambda e: e.activation(out=ssE[:, 1:2], in_=ssE[:, 0:1], func=AF.Sqrt, scale=1.0 / D, bias=k.eps_t[:, 0:1]),
             reads=[ssE, k.eps_t], writes=[ssE])
        p.op("dve", lambda e: e.reciprocal(out=ssE[:, 1:2], in_=ssE[:, 1:2]), reads=[ssE], writes=[ssE])
        p.op("dve", lambda e: e.scalar_tensor_tensor(out=sqE[:], in0=x1t[:], scalar=ssE[:, 1:2], in1=rows["fing"][:], op0=ALU.mult, op1=ALU.mult),
             reads=[x1t, ssE, rows["fing"]], writes=[sqE])
        p.dma("sp", k.out_d[t0:t0 + 128, :], sqE[:], reads=[sqE], writes=[], is_out=True)

    for ti in range(4 * ngE):
        combine_tile(ti)
    p.barrier()


import numpy as np

D = 2048


def fm(v, kc=16):
    return np.ascontiguousarray(v.reshape(kc, 128).T)


def prep_shared(inp):
    out = {}
    w_in = inp["w_in"][0]
    for half in (0, 1):
        u = w_in[:, 0:1024]
        slab = w_in[:, 1024:1024 + 6784]
        gates = w_in[:, 1024 + 6784:]
        r, kk_, v = slab[:, 0:2048], slab[:, 2048:4096], slab[:, 4096:6144]
        wd = [slab[:, 6144:6240], slab[:, 6240:6336]]
        ad = [slab[:, 6336:6432], slab[:, 6432:6528]]
        gd = slab[:, 6528:6784]
        dA, dB = (0, 1) if half == 0 else (1, 0)
        z32 = np.zeros((D, 32), np.float32)
        win = np.concatenate([u, r, kk_, v, wd[dA], z32, wd[dB], z32, ad[dA], z32, ad[dB], z32, gd, gates], axis=1)
        out[half] = {"win": np.ascontiguousarray(win)}
    return out


def prep_core(inp, shared, b, half):
    x = inp["x"][b]
    ctx = inp["ctx"][b]
    if half == 1:
        x = x[::-1]
        ctx = ctx[::-1]
    m = {}
    m["xT"] = np.ascontiguousarray(x.T)
    m["cxT"] = np.ascontiguousarray(ctx.T)
    c2 = np.stack([inp["c"][b], inp["c_ctx"]], axis=1)
    m["cT2"] = np.ascontiguousarray(c2.reshape(16, 128, 2).transpose(1, 0, 2))
    m["adaw"] = inp["ada_w"][0]
    m["adab"] = fm(inp["ada_b"][0], 96)
    m["g1"] = fm(inp["norm1_g"][0])
    m["win"] = shared[half]["win"]
    return m


def prep_core_b(inp, b, half, m):
    dA, dB = (0, 1) if half == 0 else (1, 0)
    mu = inp["shift_mu"][0]
    z32 = np.zeros(32, np.float32)
    secs = [mu[6144:6240], mu[6240:6336]]
    seca = [mu[6336:6432], mu[6432:6528]]
    mup = np.concatenate([mu[0:6144], secs[dA], z32, secs[dB], z32, seca[dA], z32, seca[dB], z32, mu[6528:6784]])
    cls = np.arange(6912) % 4
    valid = np.ones(6912, bool)
    for s0 in (6144, 6272, 6400, 6528):
        valid[s0 + 96:s0 + 128] = False
    def sel(mask):
        return np.where(mask & valid, mup, np.float32(0)).astype(np.float32)
    if half == 0:
        cm1, cp1, cm64, cp64 = sel(cls == 0), sel(cls == 1), sel(cls == 2), sel(cls == 3)
        ccm1, ccp1 = sel(cls % 2 == 0), sel(cls % 2 == 1)
    else:
        cm1, cp1, cm64, cp64 = sel(cls == 1), sel(cls == 0), sel(cls == 3), sel(cls == 2)
        ccm1, ccp1 = sel(cls % 2 == 1), sel(cls % 2 == 0)
    mupv = np.where(valid, mup, np.float32(0)).astype(np.float32)
    arrs = [mupv, cm1, cp1, cm64, cp64, ccm1, ccp1]
    m["mixc"] = np.ascontiguousarray(np.stack([a.reshape(54, 128).T for a in arrs], axis=1))
    w0, a0 = inp["decay_w0"][0], inp["iclr_a0"][0]
    vs = [w0[dA], w0[dB], a0[dA], a0[dB], inp["k_k"][0], inp["k_a"][0], inp["r_k"][0].reshape(-1)]
    m["vecs"] = np.ascontiguousarray(np.stack([fm(v) for v in vs], axis=1))
    w2, a2 = inp["decay_w2"][0], inp["iclr_a2"][0]
    m["lw2"] = np.ascontiguousarray(np.stack([w2[dA], w2[dB], a2[dA], a2[dB]], axis=1))
    pidx = np.arange(128)
    m["bones"] = (pidx[:, None] // 64 == pidx[None, :] // 64).astype(np.float32)
    m["hsel"] = (pidx[:, None] // 64 == np.arange(2)[None, :]).astype(np.float32)
    return m


def prep_core_c(m):
    t = np.arange(64)
    lt = (t[:, None] < t[None, :]).astype(np.float32)
    le = (t[:, None] <= t[None, :]).astype(np.float32)
    gt = (t[:, None] > t[None, :]).astype(np.float32)
    ge = (t[:, None] >= t[None, :]).astype(np.float32)
    mA = np.block([[lt, le], [lt, le]])
    mB = np.block([[gt, ge], [gt, ge]])
    m["cmask"] = np.ascontiguousarray(np.stack([mA, mB], axis=1).astype(np.float32))
    nA = lt.T.copy()
    nB = gt.T.copy()
    m["nmask"] = np.ascontiguousarray(np.stack([nA, nB, np.eye(64, dtype=np.float32)], axis=1).astype(np.float32))
    return m


def prep_core_de(inp, b, half, m, with_experts=True):
    x = inp["x"][b]
    if half == 1:
        x = x[::-1]
    m["xown"] = np.ascontiguousarray(x[:2048])
    m["rows"] = np.ascontiguousarray(np.stack([inp["lnx_w"][0], inp["lnx_b"][0], inp["final_g"]], axis=0))
    m["g2n"] = fm(inp["norm2_g"][0])
    m["g2w"] = inp["gate_g2"][0]
    m["poolw"] = inp["pool_w"][0]
    m["pools"] = fm(inp["pool_scale"][0], 8)
    m["wpo"] = inp["w_pool_out"][0]
    m["wro"] = inp["w_rwkv_out"][0]
    m["wout"] = inp["w_out"][0]
    t = np.arange(64)
    cnts = []
    for w in (2, 4, 8, 16):
        lo = np.clip(t - w // 2, 0, 64)
        hi = np.clip(t + (w - w // 2), 0, 64)
        c = (hi - lo).astype(np.float32)
        if half == 1:
            c = c[::-1]
        cnts.append(np.float32(1.0) / c)
    flags = np.array([1.0, 0.0] if half == 0 else [0.0, 1.0], np.float32)
    m["poolc"] = np.ascontiguousarray(np.concatenate(cnts + [flags]).astype(np.float32)[None, :])
    m["routw"] = inp["router_w"][0]
    ecap = (np.arange(64) * 768).astype(np.float32)
    m["routb"] = np.ascontiguousarray(np.concatenate([inp["router_bias"][0], ecap]).astype(np.float32)[None, :])
    tt = np.arange(128)
    m["tri"] = (tt[:, None] < tt[None, :]).astype(np.float32)
    if with_experts:
        m["exg"] = inp["exp_w_gate"][0]
        m["exu"] = inp["exp_w_up"][0]
        m["exd"] = inp["exp_w_down"][0]
    m["shg"] = inp["shared_w_gate"][0]
    m["shu"] = inp["shared_w_up"][0]
    m["shd"] = inp["shared_w_down"][0]
    return m


from concourse.bass_utils import run_bass_kernel_spmd


def kernel(**inputs):
    inp = {k_: np.asarray(v) for k_, v in inputs.items()}
    shared = prep_shared(inp)
    maps = []
    for core in range(8):
        b, half = core // 2, core % 2
        m = prep_core(inp, shared, b, half)
        m = prep_core_b(inp, b, half, m)
        m = prep_core_c(m)
        m = prep_core_de(inp, b, half, m)
        maps.append(m)
    nc, k = build(stage=5)
    res = run_bass_kernel_spmd(nc, maps, core_ids=list(range(8)))
    out = np.empty((4, 4096, 2048), np.float32)
    for core in range(8):
        b, half = core // 2, core % 2
        o = np.asarray(res.results[core]["out"])
        if half == 0:
            out[b, :2048] = o
        else:
            out[b, 2048:] = o[::-1]
    return out
```

```python
import numpy as np
import concourse.bass as bass
import concourse.mybir as mybir
from contextlib import ExitStack

F32 = mybir.dt.float32
BF16 = mybir.dt.bfloat16
I32 = mybir.dt.int32
U32 = mybir.dt.uint32
ALU = mybir.AluOpType
AF = mybir.ActivationFunctionType
AX = mybir.AxisListType

ENGS = ("pe", "act", "dve", "pool", "sp")
ARENA_BASE = 16640
ARENA_TOP = 229344
DTSIZE = {F32: 4, BF16: 2, I32: 4, U32: 4}
NDSEM = 8


class _Op:
    __slots__ = ("eng", "fn", "deps", "signal", "sigval", "is_dma", "dsem", "dval", "dprev", "idx")

    def __init__(self, eng, fn, is_dma):
        self.eng = eng
        self.fn = fn
        self.deps = []
        self.signal = False
        self.sigval = 0
        self.is_dma = is_dma
        self.dsem = None
        self.dval = 0
        self.dprev = None


class Prog:
    def __init__(self, nc):
        self.nc = nc
        self.ops = []
        self.track = {}
        self.ndma = {e: 0 for e in ENGS}
        self.dma_ops = {e: [] for e in ENGS}
        self.es = ExitStack()
        self.out_dmas = []
        self.sp = ARENA_BASE
        self.sp_max = ARENA_BASE

    def sb(self, name, shape, dtype):
        nbytes = int(np.prod(shape[1:])) * DTSIZE[dtype]
        nbytes = (nbytes + 31) // 32 * 32
        off = self.sp
        self.sp += nbytes
        self.sp_max = max(self.sp_max, self.sp)
        assert self.sp <= ARENA_TOP, "SBUF arena overflow: %s needs %d at %d" % (name, nbytes, off)
        return self.nc.alloc_sbuf_tensor_at(name, list(shape), dtype, offset=off)

    def getreg(self, e, val):
        if not hasattr(self, "_regs"):
            self._regs = {}
        key = (id(e), val)
        if key not in self._regs:
            r = e.alloc_register("creg%d" % len(self._regs))
            e.reg_mov(r, val)
            self._regs[key] = r
        return self._regs[key]

    def mark(self):
        return self.sp

    def release(self, m):
        self.sp = m

    def ps(self, name, shape, dtype=F32):
        return self.es.enter_context(self.nc.psum_tensor(name, list(shape), dtype))

    @staticmethod
    def _key(k):
        if isinstance(k, tuple):
            return k[0], k[1]
        if isinstance(k, str):
            return k, None
        t = getattr(k, "tensor", k)
        return t.name, None

    def _conf(self, name, sub):
        d = self.track.setdefault(name, {})
        if sub is None:
            return list(d.keys())
        return [s for s in (sub, None) if s in d]

    def _record(self, op, reads, writes):
        r2, w2 = [], list(writes)
        for k_ in reads:
            nm = self._key(k_)[0]
            if nm.startswith("psb") or nm.startswith("pbt"):
                w2.append(k_)
            else:
                r2.append(k_)
        reads, writes = r2, w2
        deps = set()
        for k in reads:
            name, sub = self._key(k)
            d = self.track.setdefault(name, {})
            for s in self._conf(name, sub):
                w = d[s][0]
                if w is not None:
                    deps.add(w)
        for k in writes:
            name, sub = self._key(k)
            d = self.track.setdefault(name, {})
            for s in self._conf(name, sub):
                w, rs = d[s]
                if w is not None:
                    deps.add(w)
                for r in rs:
                    deps.add(r)
        for k in reads:
            name, sub = self._key(k)
            d = self.track[name]
            d.setdefault(sub, [None, []])[1].append(op)
        for k in writes:
            name, sub = self._key(k)
            d = self.track[name]
            if sub is None:
                d.clear()
            d[sub] = [op, []]
        deps.discard(op)
        for y in deps:
            if y.eng == "pe" and op.eng == "pe" and not y.is_dma:
                continue
            op.deps.append(y)
            if not y.is_dma:
                y.signal = True

    def op(self, eng, fn, reads=(), writes=(), after=()):
        o = _Op(eng, fn, False)
        o.idx = len(self.ops)
        self.ops.append(o)
        self._record(o, reads, writes)
        for y in after:
            o.deps.append(y)
            if not y.is_dma:
                y.signal = True
        return o

    def dma(self, eng, out, in_, reads=None, writes=None, is_out=False, **kw):
        if reads is None:
            reads = [in_]
        if writes is None:
            writes = [out]

        def fn(e, out=out, in_=in_, kw=kw):
            return e.dma_start(out=out, in_=in_, **kw)
        o = _Op(eng, fn, True)
        o.idx = len(self.ops)
        self.ops.append(o)
        self._record(o, reads, writes)
        n = self.ndma[eng]
        self.ndma[eng] += 1
        o.dsem = (eng, n % NDSEM)
        o.dval = 16 * (n // NDSEM + 1)
        if n >= NDSEM:
            o.dprev = self.dma_ops[eng][n - NDSEM]
        self.dma_ops[eng].append(o)
        if is_out:
            self.out_dmas.append(o)
        return o

    def dma_fn(self, eng, fn, reads, writes, is_out=False):
        o = _Op(eng, fn, True)
        o.idx = len(self.ops)
        self.ops.append(o)
        self._record(o, reads, writes)
        n = self.ndma[eng]
        self.ndma[eng] += 1
        o.dsem = (eng, n % NDSEM)
        o.dval = 16 * (n // NDSEM + 1)
        if n >= NDSEM:
            o.dprev = self.dma_ops[eng][n - NDSEM]
        self.dma_ops[eng].append(o)
        if is_out:
            self.out_dmas.append(o)
        return o

    def capture(self, fn):
        rec = []
        orig_op, orig_dma, orig_dfn = self.op, self.dma, self.dma_fn
        self.op = lambda *a, **k: rec.append((orig_op, a, k))
        self.dma = lambda *a, **k: rec.append((orig_dma, a, k))
        self.dma_fn = lambda *a, **k: rec.append((orig_dfn, a, k))
        try:
            fn()
        finally:
            self.op, self.dma, self.dma_fn = orig_op, orig_dma, orig_dfn
        return rec

    def interleave(self, fns):
        recs = [self.capture(f) for f in fns]
        for i in range(max(len(r) for r in recs)):
            for r in recs:
                if i < len(r):
                    f, a, k = r[i]
                    f(*a, **k)

    def barrier(self):
        last = {}
        for o in self.ops:
            if not o.is_dma:
                last[o.eng] = o
        pend = [o for e in ENGS for o in self.dma_ops[e][-NDSEM:]]
        bops = []
        for e in ENGS:
            after = [o for o in last.values()] + pend
            bops.append((e, after))
        res = []
        for e, after in bops:
            res.append(self.op(e, lambda eng: eng.nop(), after=after))
        for o in res:
            o.signal = True
        self._barrier_ops = res
        self.track.clear()
        return res

    def emit(self):
        nc = self.nc
        if self.out_dmas:
            self.op("sp", lambda eng: eng.nop(), after=list(self.out_dmas))
        cnt = {e: 0 for e in ENGS}
        for o in self.ops:
            if o.is_dma:
                continue
            if o.signal:
                cnt[o.eng] += 1
                o.sigval = cnt[o.eng]
        es = self.es
        csem = {e: es.enter_context(nc.semaphore("c_" + e)) for e in ENGS}
        dsem = {}
        for e in ENGS:
            if self.ndma[e]:
                for i in range(min(NDSEM, self.ndma[e])):
                    dsem[(e, i)] = es.enter_context(nc.semaphore("d_%s%d" % (e, i)))
        per = {e: [o for o in self.ops if o.eng == e] for e in ENGS}
        block = es.enter_context(nc.Block())

        def run(eng_name, eng):
            waited = {}

            def wait(sem_key, sem, val):
                if waited.get(sem_key, 0) >= val:
                    return
                waited[sem_key] = val
                eng.wait_ge(sem, val)

            for o in per[eng_name]:
                for y in o.deps:
                    if y.is_dma:
                        wait(y.dsem, dsem[y.dsem], y.dval)
                    else:
                        wait(("c", y.eng), csem[y.eng], y.sigval)
                if o.is_dma:
                    if o.dprev is not None:
                        wait(o.dsem, dsem[o.dsem], o.dprev.dval)
                    ins = o.fn(eng)
                    ins.then_inc(dsem[o.dsem], 16)
                else:
                    ins = o.fn(eng)
                    if o.signal:
                        ins.then_inc(csem[eng_name], 1)

        @block.tensor
        def _(e):
            run("pe", e)

        @block.scalar
        def _(e):
            run("act", e)

        @block.vector
        def _(e):
            run("dve", e)

        @block.gpsimd
        def _(e):
            run("pool", e)

        @block.sync
        def _(e):
            run("sp", e)

    def close(self):
        self.es.close()


import numpy as np

D = 2048
NT = 4096
NOWN = 2048
NCTX = 256
KC = 16
NCH = 94
SLAB0 = 8
NSL = 54
DINP = NCH * 128
EPS = 1e-6
SQD = float(np.sqrt(2048.0))


class K:
    pass


def build(stage=99, dbg=(), ntiles=None, cut=None, skipA=False, nchunks=None, ccut=None, ecut=None, nexp=None):
    nc = bass.Bass("TRN2", target_bir_lowering=False)
    p = Prog(nc)
    k = K()
    k.nc, k.p = nc, p
    k.dbg = {}
    k.ntiles = ntiles
    k.cut = cut
    k.nchunks = nchunks
    k.ccut = ccut
    k.ecut = ecut
    k.nexp = nexp

    def din(name, shape, dt=F32):
        return nc.dram_tensor(name, list(shape), dt, kind="ExternalInput").ap()

    def dscr(name, shape, dt):
        if skipA and name in ("slabL", "slabC", "uT", "sgT"):
            return nc.dram_tensor(name, list(shape), dt, kind="ExternalInput").ap()
        if name in dbg:
            a = nc.dram_tensor(name, list(shape), dt, kind="ExternalOutput").ap()
            k.dbg[name] = a
            return a
        return nc.dram_tensor(name, list(shape), dt).ap()

    k.din, k.dscr = din, dscr
    if not skipA:
        k.xT = din("xT", [D, NT])
        k.cxT = din("cxT", [D, NCTX])
        k.cT2 = din("cT2", [128, KC, 2])
        k.adaw = din("adaw", [D, 6 * D])
        k.adab = din("adab", [128, 96])
        k.g1 = din("g1", [128, KC])
        k.win = din("win", [D, DINP])
    k.slabL = dscr("slabL", [NSL * 128, NT], BF16)
    k.slabC = dscr("slabC", [NSL * 128, NCTX], BF16)
    k.uT = dscr("uT", [1024, NOWN], BF16)
    k.sgT = dscr("sgT", [4096, NOWN], BF16)
    k.modd = dscr("modd", [128, 96, 2], F32)

    k.ones_bf = p.sb("ones_bf", [128, 128], BF16)
    p.op("pool", lambda e: e.memset(k.ones_bf[:], 1.0), writes=[k.ones_bf])
    k.eps_t = p.sb("eps_t", [128, 2], F32)
    p.op("pool", lambda e: e.memset(k.eps_t[:, 0:1], EPS), writes=[k.eps_t])
    p.op("pool", lambda e: e.memset(k.eps_t[:, 1:2], 1e-12), writes=[k.eps_t])
    k.mod = p.sb("mod", [128, 96, 2], F32)
    k.A1 = p.sb("A1", [128, KC, 2], F32)
    k.g1s = p.sb("g1s", [128, KC], F32)
    k.ident = p.sb("ident_bf", [128, 128], BF16)
    k.psb = [p.ps("psb%d" % i, [128, 512], F32) for i in range(6)]
    k.pbt = [p.ps("pbt%d" % i, [128, 1024], BF16) for i in range(2)]

    m0 = p.mark()
    if skipA:
        modin = din("modin", [128, 96, 2])
        p.dma("sp", k.mod[:], modin)
    if not skipA:
        phase0(k)
        p.barrier()
        p.release(m0)
        if stage >= 1:
            phaseA(k)
            p.release(m0)
    mB = p.mark()
    if stage >= 2:
        phaseB(k)
    if stage >= 3:
        mC = p.mark()
        phaseC(k)
        p.release(mC)
    if stage >= 4:
        phaseD(k)
    if stage >= 5:
        phaseE(k)
    p.emit()
    p.close()
    return nc, k


def phase0(k):
    p, nc = k.p, k.nc
    c32 = p.sb("c32", [128, KC, 2], F32)
    cs = p.sb("c_silu", [128, KC, 2], BF16)
    adab = p.sb("adab_sb", [128, 96], F32)
    p.dma("sp", c32[:], k.cT2)
    p.dma("sp", adab[:], k.adab)
    p.dma("sp", k.g1s[:], k.g1)
    p.op("act", lambda e: e.activation(out=cs[:], in_=c32[:], func=AF.Silu), reads=[c32], writes=[cs])
    NB = 16
    CW = 768
    wb = [p.sb("adaw_bf%d" % i, [128, KC, CW], BF16) for i in range(2)]
    src = k.adaw.rearrange("(kc p) n -> p kc n", p=128)
    ps = k.psb[0]
    for blk in range(NB):
        w = wb[blk % 2]
        for h in range(2):
            p.dma("pool", w[:, h * 8:(h + 1) * 8, :], src[:, h * 8:(h + 1) * 8, blk * CW:(blk + 1) * CW],
                  reads=[], writes=[(w.name, h)])
        for oc in range(6):
            g = blk * 6 + oc
            for kc in range(KC):
                p.op("pe", lambda e, w=w, oc=oc, kc=kc, g=g: e.matmul(
                    ps[:, 2 * g:2 * g + 2], lhsT=w[:, kc, oc * 128:(oc + 1) * 128], rhs=cs[:, kc, :],
                    start=(kc == 0), stop=(kc == KC - 1)),
                    reads=[(w.name, kc // 8), cs], writes=[ps])
    p.op("dve", lambda e: e.tensor_tensor(
        out=k.mod[:], in0=ps[:, 0:192].rearrange("p (g t) -> p g t", t=2),
        in1=adab[:].unsqueeze(2).to_broadcast([128, 96, 2]), op=ALU.add),
        reads=[ps, adab], writes=[k.mod])
    p.op("dve", lambda e: e.tensor_scalar(out=k.A1[:], in0=k.mod[:, 16:32, :], scalar1=1.0, scalar2=None,
                                          op0=ALU.add), reads=[k.mod], writes=[k.A1])
    p.op("dve", lambda e: e.tensor_tensor(out=k.A1[:], in0=k.A1[:],
                                          in1=k.g1s[:].unsqueeze(2).to_broadcast([128, KC, 2]), op=ALU.mult),
         reads=[k.A1, k.g1s], writes=[k.A1])
    if "modd" in k.dbg:
        p.dma("sp", k.modd, k.mod[:], is_out=True)


def phaseA(k):
    p, nc = k.p, k.nc
    hT = p.sb("hT", [128, KC, NOWN], BF16)
    xin = [p.sb("xin%d" % i, [128, KC, 256], F32) for i in range(2)]
    sq = p.sb("sq", [128, KC, 256], BF16)
    rstd = p.sb("rstd", [128, 256], F32)
    rms = p.sb("rms", [128, 256], F32)
    xn = p.sb("xn", [128, KC, 256], F32)
    wblk = [p.sb("wblk%d" % i, [128, KC, 256], BF16) for i in range(3)]
    stg = [p.sb("stgA%d" % i, [128, 512], BF16) for i in range(4)]
    winv = k.win.rearrange("(kc p) n -> p kc n", p=128)
    st = {"x": 0, "w": 0, "s": 0, "ps": 0}

    def norm_group(srcT, t0, ntok, which):
        v = srcT.rearrange("(kc p) t -> p kc t", p=128)
        for s in range(ntok // 256):
            xb = xin[st["x"] % 2]
            st["x"] += 1
            for h in range(2):
                p.dma("sp", xb[:, h * 8:(h + 1) * 8, :], v[:, h * 8:(h + 1) * 8, t0 + s * 256:t0 + (s + 1) * 256],
                      reads=[], writes=[(xb.name, h)])
            p.op("act", lambda e, xb=xb: e.activation(out=sq[:], in_=xb[:], func=AF.Square), reads=[xb], writes=[sq])
            ps = k.psb[5]
            for kc in range(KC):
                p.op("pe", lambda e, kc=kc: e.matmul(ps[:, 0:256], lhsT=k.ones_bf[:], rhs=sq[:, kc, :],
                                                      start=(kc == 0), stop=(kc == KC - 1)),
                     reads=[k.ones_bf, sq], writes=[ps])
            p.op("act", lambda e: e.activation(out=rms[:], in_=ps[:, 0:256], func=AF.Sqrt, scale=1.0 / D, bias=k.eps_t[:, 0:1]),
                 reads=[ps, k.eps_t], writes=[rms])
            p.op("dve", lambda e: e.reciprocal(out=rstd[:], in_=rms[:]), reads=[rms], writes=[rstd])
            p.op("dve", lambda e, xb=xb: e.tensor_tensor(out=xn[:], in0=xb[:],
                                                         in1=rstd[:].unsqueeze(1).to_broadcast([128, KC, 256]), op=ALU.mult),
                 reads=[xb, rstd], writes=[xn])
            for kc in range(KC):
                p.op("act", lambda e, kc=kc, s=s: e.activation(
                    out=hT[:, kc, s * 256:(s + 1) * 256], in_=xn[:, kc, :], func=AF.Identity,
                    scale=k.A1[:, kc, which:which + 1], bias=k.mod[:, kc, which:which + 1]),
                    reads=[xn, k.A1, k.mod], writes=[(hT.name, s)])

    def proj_group(ntok, chunks, sink):
        nsub = max(1, ntok // 512)
        w = min(512, ntok)
        for b0 in range(0, len(chunks), 2):
            wb = wblk[st["w"] % 3]
            st["w"] += 1
            c0 = chunks[b0]
            for h in range(2):
                p.dma("pool", wb[:, h * 8:(h + 1) * 8, :], winv[:, h * 8:(h + 1) * 8, c0 * 128:(c0 + 2) * 128],
                      reads=[], writes=[(wb.name, h)])
            for s in range(nsub):
                for j in range(2):
                    ch = chunks[b0 + j]
                    ps = k.psb[st["ps"] % 5]
                    st["ps"] += 1
                    for kc in range(KC):
                        p.op("pe", lambda e, wb=wb, j=j, kc=kc, s=s, ps=ps: e.matmul(
                            ps[:, 0:w], lhsT=wb[:, kc, j * 128:(j + 1) * 128], rhs=hT[:, kc, s * 512:s * 512 + w],
                            start=(kc == 0), stop=(kc == KC - 1)),
                            reads=[(wb.name, kc // 8), (hT.name, (s * 512) // 256), (hT.name, (s * 512 + w - 1) // 256)],
                            writes=[ps])
                    sink(ch, s, w, ps)

    def mk_sink(slab_dst, tok0):
        def sink(ch, s, w, ps):
            sg = stg[st["s"] % 4]
            st["s"] += 1
            eng = "act" if (st["s"] % 2) else "dve"
            if ch >= 62:
                p.op("act", lambda e, sg=sg, ps=ps: e.activation(out=sg[:, 0:w], in_=ps[:, 0:w], func=AF.Sigmoid),
                     reads=[ps], writes=[sg])
                dst = k.sgT[(ch - 62) * 128:(ch - 61) * 128, s * 512:s * 512 + w]
            else:
                if eng == "act":
                    p.op("act", lambda e, sg=sg, ps=ps: e.activation(out=sg[:, 0:w], in_=ps[:, 0:w], func=AF.Copy),
                         reads=[ps], writes=[sg])
                else:
                    p.op("dve", lambda e, sg=sg, ps=ps: e.tensor_copy(out=sg[:, 0:w], in_=ps[:, 0:w]),
                         reads=[ps], writes=[sg])
                if ch < 8:
                    dst = k.uT[ch * 128:(ch + 1) * 128, s * 512:s * 512 + w]
                else:
                    cc = ch - SLAB0
                    dst = slab_dst[cc * 128:(cc + 1) * 128, tok0 + s * 512:tok0 + s * 512 + w]
            p.dma("sp", dst, sg[:, 0:w], reads=[sg], writes=[])
        return sink

    slab_chunks = list(range(SLAB0, SLAB0 + NSL))
    norm_group(k.xT, 0, NOWN, 0)
    proj_group(NOWN, list(range(NCH)), mk_sink(k.slabL, 0))
    norm_group(k.xT, NOWN, NOWN, 0)
    proj_group(NOWN, slab_chunks, mk_sink(k.slabL, NOWN))
    norm_group(k.cxT, 0, NCTX, 1)
    proj_group(NCTX, slab_chunks, mk_sink(k.slabC, 0))
    p.barrier()
    for name in ("slabL", "slabC", "uT", "sgT"):
        if name in k.dbg:
            pass


import numpy as np

C0 = float(np.exp(-0.5))
NCHK = 68
TT = 256


class Cut(Exception):
    pass


def phaseB(k):
    try:
        _phaseB(k)
    except Cut:
        k.p.barrier()


def _phaseB(k):
    def cut(n):
        if getattr(k, "cut", None) == n:
            raise Cut()
    p, nc = k.p, k.nc
    din, dscr = k.din, k.dscr
    k.mixc_d = din("mixc", [128, 7, 54])
    k.vecs_d = din("vecs", [128, 7, 16])
    k.lw2_d = din("lw2", [96, 4, 2048])
    k.bones_d = din("bones", [128, 128])
    k.hsel_d = din("hsel", [128, 2])
    k.Q = [dscr("QA", [2048, NCHK * 256], BF16), dscr("QB", [2048, NCHK * 256], BF16)]
    k.Vtok = dscr("Vtok", [NCHK * 64, 2048], BF16)
    k.KpT = [dscr("KpTA", [NCHK * 64, 2048], BF16), dscr("KpTB", [NCHK * 64, 2048], BF16)]
    k.BpT = [dscr("BpTA", [NCHK * 64, 2048], BF16), dscr("BpTB", [NCHK * 64, 2048], BF16)]
    k.bcoef = dscr("bcoef", [2048, 32], F32)
    k.sgdT = dscr("sgdT", [256, 2048], BF16)
    k.wtot = [p.sb("wtotA", [128, 16, NCHK], F32), p.sb("wtotB", [128, 16, NCHK], F32)]

    markB = p.mark()
    mixc = p.sb("mixc_sb", [128, 7, 54], F32)
    omu = p.sb("omu", [128, 54], F32)
    vecs = p.sb("vecs_sb", [128, 7, 16], F32)
    oka = p.sb("oka", [128, 16], F32)
    lw2 = p.sb("lw2_sb", [96, 4, 2048], BF16)
    bones = p.sb("bones_sb", [128, 128], BF16)
    hsel = p.sb("hsel_sb", [128, 2], BF16)
    rmask = p.sb("rmask", [128, TT], F32)
    ident = k.ident
    p.dma("sp", mixc[:], k.mixc_d)
    p.dma("sp", vecs[:], k.vecs_d)
    for i in range(4):
        p.dma("pool", lw2[:, i, :], k.lw2_d[:, i, :], writes=[(lw2.name, i)])
    p.dma("pool", bones[:], k.bones_d)
    p.dma("pool", hsel[:], k.hsel_d)
    p.op("dve", lambda e: e.tensor_scalar(out=omu[:], in0=mixc[:, 0, :], scalar1=-1.0, scalar2=1.0, op0=ALU.mult, op1=ALU.add),
         reads=[mixc], writes=[omu])
    p.op("dve", lambda e: e.tensor_scalar(out=oka[:], in0=vecs[:, 5, :], scalar1=-1.0, scalar2=1.0, op0=ALU.mult, op1=ALU.add),
         reads=[vecs], writes=[oka])
    p.op("dve", lambda e: e.memset(rmask[:], 1.0), writes=[rmask])
    p.op("dve", lambda e: e.memset(rmask[:].rearrange("p (r c) -> p r c", c=64)[:, :, 0:1], 0.0), reads=[rmask], writes=[rmask])
    p.op("pool", lambda e: e.memset(ident[:], 1.0), writes=[ident])
    p.op("pool", lambda e: e.affine_select(out=ident[:], in_=ident[:], pattern=[[-1, 128]], compare_op=ALU.is_equal,
                                           fill=0.0, base=0, channel_multiplier=1), reads=[ident], writes=[ident])

    cut(1)
    NB = 2
    raw3 = [p.sb("raw3_%d" % i, [128, 3, 384], BF16) for i in range(NB)]
    rawl = [p.sb("rawl_%d" % i, [128, 384], BF16) for i in range(NB)]
    colsv = p.sb("colsv", [128, 4], BF16)
    M3 = [p.sb("M3_%d" % i, [128, 3, TT], F32) for i in range(NB)]
    ML = p.sb("ML", [128, TT], F32)
    tw = p.sb("tw", [128, 4, TT], BF16)
    sgd = p.sb("sgd", [128, 2, TT], BF16)
    f = {}
    for nm in ("lw", "aa", "cL", "X2", "X3", "X4", "e1", "e2", "e3", "e4", "kd", "bd", "ck"):
        f[nm] = [p.sb("f_%s%d" % (nm, i), [128, TT], F32) for i in range(2)]
    kkr = p.sb("kkr", [128, TT], F32)
    sqk = p.sb("sqk", [128, TT], BF16)
    rn = p.sb("rn", [128, TT], F32)
    kk = p.sb("kk", [128, TT], F32)
    vbf = p.sb("vbf", [128, TT], BF16)
    kpb = [p.sb("kpb%d" % i, [128, TT], BF16) for i in range(2)]
    bpb = [p.sb("bpb%d" % i, [128, TT], BF16) for i in range(2)]
    ksum = p.sb("ksum", [128, TT], F32)
    prod = p.sb("prodb", [128, TT], BF16)
    Qst = [[p.sb("Qst%d_%d" % (d, i), [128, 4, 4, 64], BF16) for i in range(2)] for d in range(2)]
    TM = {nm: p.sb("TM_" + nm, [128, 2, 2048], BF16) for nm in ("v", "kA", "bA", "kB", "bB")}
    bco = p.sb("bco", [128, 2, 32], F32)
    psL = [[k.psb[0], k.psb[1]], [k.psb[0], k.psb[1]]]
    psK = k.psb[2]
    psT = [(k.pbt[0], k.pbt[1]), (k.pbt[0], k.pbt[1])]
    psBon = k.psb[3]
    cnt = {"e": 0}

    def ew():
        cnt["e"] += 1
        return "pool" if cnt["e"] % 2 == 0 else "dve"

    colsv3 = p.sb("colsv3", [128, 3, 4], BF16)

    def mix_steps(eng, out, buf, cc, latent, key, okey, cvi):
        cv = colsv3[:, cvi, :]
        cvk = (colsv3.name, cvi)
        if latent:
            ctr = buf[:, 64:320]
            views = [(buf[:, 0:256], 3), (buf[:, 128:384], 4)]
        else:
            ctr = buf[:, 1:257]
            views = [(buf[:, 0:256], 5), (buf[:, 2:258], 6)]
        st = []
        st.append((lambda e: e.tensor_scalar(out=out, in0=ctr, scalar1=omu[:, cc:cc + 1], scalar2=None, op0=ALU.mult),
                   [key, omu], [okey]))
        for v, ci in views:
            st.append((lambda e, v=v, ci=ci: e.scalar_tensor_tensor(out=out, in0=v, scalar=mixc[:, ci, cc:cc + 1], in1=out,
                                                                    op0=ALU.mult, op1=ALU.add), [key, mixc, okey], [okey]))
        if latent:
            c63 = buf[:, 63:256:64]
            c0 = buf[:, 128:321:64]
            st.append((lambda e: e.tensor_copy(out=cv, in_=c63), [key], [cvk]))
            st.append((lambda e: e.memset(c63, 0.0), [key], [key]))
            st.append((lambda e: e.scalar_tensor_tensor(out=out, in0=buf[:, 63:319], scalar=mixc[:, 1, cc:cc + 1], in1=out,
                                                        op0=ALU.mult, op1=ALU.add), [key, mixc, okey], [okey]))
            st.append((lambda e: e.tensor_copy(out=c63, in_=cv), [cvk, key], [key]))
            st.append((lambda e: e.memset(c0, 0.0), [key], [key]))
            st.append((lambda e: e.scalar_tensor_tensor(out=out, in0=buf[:, 65:321], scalar=mixc[:, 2, cc:cc + 1], in1=out,
                                                        op0=ALU.mult, op1=ALU.add), [key, mixc, okey], [okey]))
        return st

    def mix_lockstep(eng, specs):
        allst = [mix_steps(eng, *sp, cvi=i) for i, sp in enumerate(specs)]
        for i in range(max(len(a) for a in allst)):
            for a in allst:
                if i < len(a):
                    fn, rd, wr = a[i]
                    p.op(eng, fn, reads=rd, writes=wr)

    def mix(eng, out, buf, cc, latent, key, okey):
        mix_lockstep(eng, [(out, buf, cc, latent, key, okey)])

    def load_raw(dst, src_rows, ti, latent, name):
        if latent:
            r0 = 4 * ti - 1
            lo, hi = max(r0, 0), min(r0 + 6, 64)
            if r0 < 0:
                p.op("pool", lambda e: e.memset(dst[:, :, 0:64], 0.0), writes=[name])
            if r0 + 6 > 64:
                p.op("pool", lambda e: e.memset(dst[:, :, 320:384], 0.0), writes=[name])
            p.dma("sp", dst[:, :, (lo - r0) * 64:(hi - r0) * 64], src_rows[:, :, lo * 64:hi * 64], reads=[], writes=[name])
        else:
            p.op("pool", lambda e: e.memset(dst[:, :, 0:1], 0.0), writes=[name])
            p.op("pool", lambda e: e.memset(dst[:, :, 257:258], 0.0), writes=[name])
            p.dma("sp", dst[:, :, 1:257], src_rows, reads=[], writes=[name])

    slabLv = k.slabL.rearrange("(cc p) t -> p cc t", p=128)
    slabCv = k.slabC.rearrange("(cc p) t -> p cc t", p=128)
    tiles = [("c", 0)] + [("l", ti) for ti in range(16)]
    if getattr(k, "ntiles", None):
        tiles = tiles[:k.ntiles]
    def do_tile(kind, ti, itbase):
        latent = kind == "l"
        own = latent and ti < 8
        src = slabLv if latent else slabCv
        chunk0 = 4 + 4 * ti if latent else 0
        tok0 = chunk0 * 64
        for j, cc in enumerate(range(48, 54)):
            if cc >= 52 and not own:
                continue
            rb = rawl[j % NB]
            load_raw(rb[:].unsqueeze(1), src[:, cc:cc + 1, :], ti, latent, rb.name)
            mix("dve", ML[:], rb[:], cc, latent, rb.name, ML.name)
            if j < 2:
                p.op("act", lambda e, j=j: e.activation(out=tw[:, j, :], in_=ML[:], func=AF.Tanh), reads=[ML], writes=[(tw.name, j)])
            elif j < 4:
                p.op("act", lambda e, j=j: e.activation(out=tw[:, j, :], in_=ML[:], func=AF.Copy), reads=[ML], writes=[(tw.name, j)])
            else:
                p.op("act", lambda e, j=j: e.activation(out=sgd[:, j - 4, :], in_=ML[:], func=AF.Sigmoid), reads=[ML], writes=[sgd])
                p.dma("sp", k.sgdT[(j - 4) * 128:(j - 3) * 128, ti * TT:(ti + 1) * TT], sgd[:, j - 4, :], reads=[sgd], writes=[])
        cut(2)
        def hp_body(hp, it):
            rb = raw3[it % NB]
            m3 = M3[it % NB]
            load_raw(rb[:], src[:, hp:hp + 33:16, :], ti, latent, rb.name)
            mix_lockstep("dve", [(m3[:, j, :], rb[:, j, :], hp + 16 * j, latent, (rb.name, j), (m3.name, j)) for j in range(3)])
            cut(3)
            rM, kM, vM = m3[:, 0, :], m3[:, 1, :], m3[:, 2, :]
            ch = slice(hp * 128, (hp + 1) * 128)
            pl = psL[it % 2]
            for d in range(2):
                p.op("pe", lambda e, d=d, pl=pl: e.matmul(pl[0][:, d * TT:(d + 1) * TT], lhsT=lw2[:, d, ch], rhs=tw[0:96, d, :],
                                                          start=True, stop=True),
                     reads=[(lw2.name, d), (tw.name, d)], writes=[pl[0]])
            for d in range(2):
                p.op("pe", lambda e, d=d, pl=pl: e.matmul(pl[1][:, d * TT:(d + 1) * TT], lhsT=lw2[:, 2 + d, ch], rhs=tw[0:96, 2 + d, :],
                                                          start=True, stop=True),
                     reads=[(lw2.name, 2 + d), (tw.name, 2 + d)], writes=[pl[1]])
            for d in range(2):
                p.op("act", lambda e, d=d, pl=pl: e.activation(out=f["lw"][d][:], in_=pl[0][:, d * TT:(d + 1) * TT], func=AF.Sigmoid,
                                                               bias=vecs[:, d, hp:hp + 1]), reads=[pl[0], vecs], writes=[f["lw"][d]])
                p.op("act", lambda e, d=d, pl=pl: e.activation(out=f["aa"][d][:], in_=pl[1][:, d * TT:(d + 1) * TT], func=AF.Sigmoid,
                                                               bias=vecs[:, 2 + d, hp:hp + 1]), reads=[pl[1], vecs], writes=[f["aa"][d]])
            p.op("dve", lambda e: e.tensor_scalar(out=kkr[:], in0=kM, scalar1=vecs[:, 4, hp:hp + 1], scalar2=None, op0=ALU.mult),
                 reads=[(m3.name, 1), vecs], writes=[kkr])
            p.op("act", lambda e: e.activation(out=sqk[:], in_=kkr[:], func=AF.Square), reads=[kkr], writes=[sqk])
            p.op("pe", lambda e: e.matmul(psK[:, 0:TT], lhsT=bones[:], rhs=sqk[:], start=True, stop=True), reads=[bones, sqk], writes=[psK])
            p.op("act", lambda e: e.activation(out=rn[:], in_=psK[:, 0:TT], func=AF.Sqrt, bias=k.eps_t[:, 1:2]), reads=[psK, k.eps_t], writes=[rn])
            p.op("dve", lambda e: e.reciprocal(out=rn[:], in_=rn[:]), reads=[rn], writes=[rn])
            p.op(ew(), lambda e: e.tensor_tensor(out=kk[:], in0=kkr[:], in1=rn[:], op=ALU.mult), reads=[kkr, rn], writes=[kk])
            cut(4)
            p.op("act", lambda e: e.activation(out=vbf[:], in_=vM, func=AF.Copy), reads=[(m3.name, 2)], writes=[vbf])
            pt, pt2 = psT[it % 2]
            ptb = pt[:]
            ptb2 = pt2[:]
            for s in range(2):
                p.op("pe", lambda e, s=s, ptb=ptb: e.transpose(ptb[:, s * 128:(s + 1) * 128], vbf[:, s * 128:(s + 1) * 128], ident[:]),
                     reads=[vbf, ident], writes=[pt])
            cut(5)
            def d_body(d):
                lw, aa = f["lw"][d], f["aa"][d]
                cL, X2, X3, X4 = f["cL"][d], f["X2"][d], f["X3"][d], f["X4"][d]
                e1, e2, e3, e4 = f["e1"][d], f["e2"][d], f["e3"][d], f["e4"][d]
                kd, bd, ck = f["kd"][d], f["bd"][d], f["ck"][d]
                Q = Qst[d][it % 2]
                p.op("dve", lambda e, cL=cL, lw=lw: e.tensor_tensor_scan(out=cL[:], data0=rmask[:], data1=lw[:], initial=0.0,
                                                                          op0=ALU.mult, op1=ALU.add), reads=[rmask, lw], writes=[cL])
                tot = cL[:].rearrange("p (r c) -> p r c", c=64)[:, :, 63:64]
                p.op(ew(), lambda e, X2=X2, cL=cL, lw=lw: e.tensor_tensor(out=X2[:], in0=cL[:], in1=lw[:], op=ALU.subtract),
                     reads=[cL, lw], writes=[X2])
                p.op(ew(), lambda e, X3=X3, cL=cL, tot=tot: e.tensor_tensor(
                    out=X3[:].rearrange("p (r c) -> p r c", c=64), in0=tot.to_broadcast([128, 4, 64]),
                    in1=cL[:].rearrange("p (r c) -> p r c", c=64), op=ALU.subtract), reads=[cL], writes=[X3])
                p.op("act", lambda e, d=d, tot=tot: e.activation(out=k.wtot[d][:, hp, chunk0:chunk0 + 4].unsqueeze(2), in_=tot,
                                                                func=AF.Exp, scale=-C0), reads=[cL], writes=[(k.wtot[d].name, it)])
                if d == 0:
                    srcs = [(cL, -C0), (X2, -C0), (cL, C0), (X3, -C0)]
                else:
                    p.op(ew(), lambda e, X4=X4, X3=X3, lw=lw: e.tensor_tensor(out=X4[:], in0=X3[:], in1=lw[:], op=ALU.add),
                         reads=[X3, lw], writes=[X4])
                    srcs = [(X4, -C0), (X3, -C0), (X4, C0), (X2, -C0)]
                for (sx, sc), eo in zip(srcs, (e1, e2, e3, e4)):
                    p.op("act", lambda e, sx=sx, sc=sc, eo=eo: e.activation(out=eo[:], in_=sx[:], func=AF.Exp, scale=sc),
                         reads=[sx], writes=[eo])
                p.op("dve", lambda e, ck=ck, aa=aa: e.tensor_scalar(out=ck[:], in0=aa[:], scalar1=vecs[:, 5, hp:hp + 1], scalar2=oka[:, hp:hp + 1],
                                                                   op0=ALU.mult, op1=ALU.add), reads=[aa, vecs, oka], writes=[ck])
                p.op(ew(), lambda e, kd=kd, ck=ck: e.tensor_tensor(out=kd[:], in0=kM, in1=ck[:], op=ALU.mult), reads=[(m3.name, 1), ck], writes=[kd])
                p.op(ew(), lambda e, bd=bd, aa=aa: e.tensor_tensor(out=bd[:], in0=kk[:], in1=aa[:], op=ALU.mult), reads=[kk, aa], writes=[bd])
                Qv = lambda a: Q[:, :, a, :]
                r3 = lambda t: t.rearrange("p (r c) -> p r c", c=64)
                p.op(ew(), lambda e, e1=e1: e.tensor_tensor(out=Qv(3), in0=r3(rM), in1=r3(e1[:]), op=ALU.mult), reads=[(m3.name, 0), e1], writes=[Q])
                p.op(ew(), lambda e, e2=e2: e.tensor_tensor(out=Qv(2), in0=r3(kk[:]), in1=r3(e2[:]), op=ALU.mult), reads=[kk, e2], writes=[Q])
                p.op(ew(), lambda e, e3=e3, kd=kd: e.tensor_tensor(out=Qv(1), in0=r3(kd[:]), in1=r3(e3[:]), op=ALU.mult), reads=[kd, e3], writes=[Q])
                p.op(ew(), lambda e, e3=e3, bd=bd: e.tensor_tensor(out=Qv(0), in0=r3(bd[:]), in1=r3(e3[:]), op=ALU.mult), reads=[bd, e3], writes=[Q])
                p.dma("sp", k.Q[d][ch, chunk0 * 256:(chunk0 + 4) * 256], Q[:].rearrange("p a b c -> p (a b c)"), reads=[Q], writes=[])
                p.op(ew(), lambda e, e4=e4, kd=kd, d=d: e.tensor_tensor(out=kpb[d][:], in0=kd[:], in1=e4[:], op=ALU.mult), reads=[kd, e4], writes=[kpb[d]])
                p.op(ew(), lambda e, e4=e4, bd=bd, d=d: e.tensor_tensor(out=bpb[d][:], in0=bd[:], in1=e4[:], op=ALU.mult), reads=[bd, e4], writes=[bpb[d]])
                for s in range(2):
                    p.op("pe", lambda e, s=s, d=d, ptb=ptb: e.transpose(ptb[:, (2 + 4 * d + s) * 128:(3 + 4 * d + s) * 128],
                                                                        kpb[d][:, s * 128:(s + 1) * 128], ident[:]),
                         reads=[kpb[d], ident], writes=[pt])
                    if d == 0:
                        p.op("pe", lambda e, s=s, d=d, ptb=ptb: e.transpose(ptb[:, (4 + s) * 128:(5 + s) * 128],
                                                                            bpb[d][:, s * 128:(s + 1) * 128], ident[:]),
                             reads=[bpb[d], ident], writes=[pt])
                    else:
                        p.op("pe", lambda e, s=s, d=d, ptb2=ptb2: e.transpose(ptb2[:, s * 128:(s + 1) * 128],
                                                                              bpb[d][:, s * 128:(s + 1) * 128], ident[:]),
                             reads=[bpb[d], ident], writes=[pt2])
            p.interleave([lambda: d_body(0), lambda: d_body(1)])
            cut(6)
            for bi, nm in enumerate(("v", "kA", "bA", "kB", "bB")):
                for s2 in range(2):
                    if bi < 4:
                        src_ps = ptb[:, (bi * 2 + s2) * 128:(bi * 2 + s2 + 1) * 128]
                        pkey = pt
                    else:
                        src_ps = ptb2[:, s2 * 128:(s2 + 1) * 128]
                        pkey = pt2
                    eng = "act" if (bi + s2) % 2 else "dve"
                    if getattr(k, "evac_dve", False):
                        eng = "dve"
                    if eng == "act":
                        p.op("act", lambda e, nm=nm, src_ps=src_ps, s2=s2: e.activation(out=TM[nm][:, s2, ch], in_=src_ps, func=AF.Copy),
                             reads=[pkey], writes=[(TM[nm].name, hp)])
                    else:
                        p.op("dve", lambda e, nm=nm, src_ps=src_ps, s2=s2: e.tensor_copy(out=TM[nm][:, s2, ch], in_=src_ps),
                             reads=[pkey], writes=[(TM[nm].name, hp)])
            cut(7)
            if own:
                p.op(ew(), lambda e: e.tensor_tensor(out=ksum[:], in0=f["kd"][0][:], in1=f["kd"][1][:], op=ALU.add),
                     reads=[f["kd"][0], f["kd"][1]], writes=[ksum])
                p.op("dve", lambda e: e.scalar_tensor_tensor(out=prod[:], in0=ksum[:], scalar=vecs[:, 6, hp:hp + 1], in1=rM,
                                                            op0=ALU.mult, op1=ALU.mult), reads=[ksum, vecs, (m3.name, 0)], writes=[prod])
                for s in range(2):
                    p.op("pe", lambda e, s=s: e.matmul(psBon[:, s * 32 + 2 * hp:s * 32 + 2 * hp + 2], lhsT=prod[:, s * 128:(s + 1) * 128],
                                                       rhs=hsel[:], start=True, stop=True), reads=[prod, hsel], writes=[psBon])
        for hp_ in range(16):
            hp_body(hp_, itbase + hp_ + 1)
        for nm, dst in (("v", k.Vtok), ("kA", k.KpT[0]), ("bA", k.BpT[0]), ("kB", k.KpT[1]), ("bB", k.BpT[1])):
            for s in range(2):
                p.dma("sp", dst[tok0 + s * 128:tok0 + (s + 1) * 128, :], TM[nm][:, s, :], reads=[TM[nm]], writes=[])
        if own:
            p.op("dve", lambda e: e.tensor_copy(out=bco[:], in_=psBon[:, 0:64].rearrange("p (s h) -> p s h", h=32)), reads=[psBon], writes=[bco])
            for s in range(2):
                p.dma("sp", k.bcoef[ti * TT + s * 128:ti * TT + (s + 1) * 128, :], bco[:, s, :], reads=[bco], writes=[])
    for tix, (kind_, ti_) in enumerate(tiles):
        do_tile(kind_, ti_, tix * 16)
    p.barrier()
    p.release(markB)


import numpy as np

NCHK = 68


def phaseC(k):
    p, nc = k.p, k.nc
    din, dscr = k.din, k.dscr
    k.cmask_d = din("cmask", [128, 2, 128])
    k.nmask_d = din("nmask", [64, 3, 64])
    k.Yscr = [dscr("YA", [2048, 2048], F32), dscr("YB", [2048, 2048], F32)]
    k.Sdbg = dscr("Sdbg", [2, 128, 16 * 64], F32)

    cmask = p.sb("cmask_sb", [128, 2, 128], F32)
    nmask = p.sb("nmask_sb", [64, 3, 64], F32)
    p.dma("sp", cmask[:], k.cmask_d)
    p.dma("sp", nmask[:], k.nmask_d)
    FM = [p.sb("FM%d" % i, [128, 16, 256], BF16) for i in range(2)]
    UV = [p.sb("UV%d" % i, [128, 2048], BF16) for i in range(2)]
    VZ = [p.sb("VZ%d" % i, [128, 2048], BF16) for i in range(2)]
    FMm = [p.sb("FMm%d" % i, [128, 16, 2, 128], BF16) for i in range(2)]
    for i in range(2):
        p.op("pool", lambda e, i=i: e.memset(VZ[i][:], 0.0), writes=[VZ[i]])
        p.op("pool", lambda e, i=i: e.memset(FMm[i][:], 0.0), writes=[FMm[i]])
    KB = [p.sb("KB%d" % i, [128, 2048], BF16) for i in range(2)]
    AT = p.sb("AT_sb", [128, 32, 128], BF16)
    Pm = [p.sb("Pm%d" % g, [128, 8, 64], BF16) for g in range(4)]
    PT = [p.sb("PTm%d" % g, [128, 8, 64], BF16) for g in range(4)]
    Gm = [p.sb("Gm%d" % g, [128, 8, 64], BF16) for g in range(4)]
    Qm = [p.sb("Qm%d" % g, [128, 8, 64], BF16) for g in range(4)]
    nrhs = [p.sb("nrhs%d" % g, [128, 8, 64], BF16) for g in range(4)]
    for g in range(4):
        for t_ in (Pm, PT, Gm, Qm, nrhs):
            p.op("pool", lambda e, t_=t_, g=g: e.memset(t_[g][:], 0.0), writes=[t_[g]])
    Ysb = [p.sb("Ysb%d" % i, [64, 2048], F32) for i in range(2)]
    S32 = p.sb("S32", [128, 16, 64], F32)
    Sbf = p.sb("Sbf", [128, 16, 64], BF16)
    Stmp = p.sb("Stmp", [128, 4, 64], F32)
    psAT = [k.psb[0], k.psb[1]]
    psP, psPT, psQ, psY = k.psb[2], k.psb[3], k.psb[4], k.psb[5]
    psP0, psPT0, psQ0 = psP, psPT, psQ
    ident = nmask[:, 2, :]
    Qv = [k.Q[d].rearrange("(hp p) x -> p hp x", p=128) for d in range(2)]
    cnt = {"e": 0, "ld": 0}

    def evac_eng():
        cnt["e"] += 1
        return "act" if cnt["e"] % 2 else "dve"

    def copy_evac(out, in_, reads, writes, scale=None):
        eng = evac_eng()
        if eng == "act":
            if scale is None:
                p.op("act", lambda e: e.activation(out=out, in_=in_, func=AF.Copy), reads=reads, writes=writes)
            else:
                p.op("act", lambda e: e.activation(out=out, in_=in_, func=AF.Copy, scale=scale), reads=reads, writes=writes)
        else:
            if scale is None:
                p.op("dve", lambda e: e.tensor_copy(out=out, in_=in_), reads=reads, writes=writes)
            else:
                p.op("dve", lambda e: e.tensor_scalar(out=out, in0=in_, scalar1=scale, scalar2=None, op0=ALU.mult), reads=reads, writes=writes)

    def load_chunk(d, c, buf):
        fm, uv, kb = FM[buf], UV[buf], KB[buf]
        vz, fmm = VZ[buf], FMm[buf]
        p.dma("sp", fm[:], Qv[d][:, :, c * 256:(c + 1) * 256], reads=[], writes=[fm])
        p.dma("sp", fmm[0:64, :, 0, :], Qv[d][0:64, :, c * 256 + 128:(c + 1) * 256], reads=[], writes=[(fmm.name, 0)])
        p.dma("sp", fmm[64:128, :, 1, :], Qv[d][64:128, :, c * 256 + 128:(c + 1) * 256], reads=[], writes=[(fmm.name, 1)])
        p.dma("sp", uv[64:128, :], k.Vtok[c * 64:(c + 1) * 64, :], reads=[], writes=[(uv.name, "V")])
        p.dma("sp", vz[64:128, :], k.Vtok[c * 64:(c + 1) * 64, :], reads=[], writes=[(vz.name, "V")])
        p.dma("sp", kb[0:64, :], k.BpT[d][c * 64:(c + 1) * 64, :], reads=[], writes=[(kb.name, 0)])
        p.dma("sp", kb[64:128, :], k.KpT[d][c * 64:(c + 1) * 64, :], reads=[], writes=[(kb.name, 1)])

    def do_chunk(d, c, buf, emit, ytok0):
        fm, uv, kb = FM[buf], UV[buf], KB[buf]
        vz, fmm = VZ[buf], FMm[buf]
        ysb = Ysb[buf]

        def fmv(h, a0, a1):
            return fm[:, h // 2, a0 * 64:a1 * 64]

        def fmk(h, a0, a1):
            return fmm[:, h // 2, h % 2, a0 * 64:a1 * 64]

        def sv(h):
            return Sbf[:, h // 2, :]

        def stage1(g):
            for hl in range(8):
                h = g * 8 + hl
                pa = psAT[hl // 4]
                p.op("pe", lambda e, h=h, hl=hl, pa=pa: e.matmul(pa[:, (hl % 4) * 128:(hl % 4 + 1) * 128], lhsT=fmv(h, 0, 2), rhs=fmk(h, 0, 2),
                                                               start=True, stop=True), reads=[fm, fmm], writes=[pa])
            for hl in range(8):
                h = g * 8 + hl
                p.op("pe", lambda e, h=h, hl=hl: e.matmul(psP[0:64, hl * 64:(hl + 1) * 64], lhsT=fmk(h, 0, 1), rhs=fmv(h, 0, 1),
                                                         start=True, stop=True), reads=[fm, fmm], writes=[psP])
            for half in range(2):
                pa = psAT[half]
                p.op("dve", lambda e, half=half, pa=pa: e.tensor_tensor(
                    out=AT[:, g * 8 + half * 4:g * 8 + half * 4 + 4, :], in0=pa[:].rearrange("p (h c) -> p h c", c=128),
                    in1=cmask[:, d, :].unsqueeze(1).to_broadcast([128, 4, 128]), op=ALU.mult), reads=[pa, cmask], writes=[(AT.name, g)])
            p.op("dve", lambda e: e.tensor_tensor(out=Pm[g][0:64, :, :], in0=psP[0:64, :].rearrange("p (h c) -> p h c", c=64),
                                                  in1=nmask[:, d, :].unsqueeze(1).to_broadcast([64, 8, 64]), op=ALU.mult),
                 reads=[psP, nmask], writes=[Pm[g]])
            p.op("dve", lambda e: e.tensor_tensor(out=Qm[g][0:64, :, :], in0=ident.unsqueeze(1).to_broadcast([64, 8, 64]),
                                                  in1=AT[0:64, g * 8:(g + 1) * 8, 0:64], op=ALU.subtract),
                 reads=[(AT.name, g), nmask], writes=[Qm[g]])

        def ptv(g, kk_, hl):
            if kk_ == 0:
                return AT[:, g * 8 + hl, 0:64]
            return PT[g][:, hl, :]

        def stage2(g, kk_):
            psP, psPT, psQ = (psP0, psPT0, psQ0) if g % 2 == 0 else (psAT[0], psAT[1], psY)
            for hl in range(8):
                p.op("pe", lambda e, hl=hl: e.matmul(psP[0:64, hl * 64:(hl + 1) * 64], lhsT=ptv(g, kk_ - 1, hl), rhs=Pm[g][:, hl, :],
                                                    start=True, stop=True),
                     reads=[Pm[g], PT[g], (AT.name, g)], writes=[psP])
            if kk_ < 5:
                for hl in range(8):
                    p.op("pe", lambda e, hl=hl: e.matmul(psPT[0:64, hl * 64:(hl + 1) * 64], lhsT=Pm[g][:, hl, :], rhs=ptv(g, kk_ - 1, hl),
                                                        start=True, stop=True),
                         reads=[Pm[g], PT[g], (AT.name, g)], writes=[psPT])
            p3 = psP[0:64, :].rearrange("p (h c) -> p h c", c=64)
            p.op("dve", lambda e: e.tensor_tensor(out=Gm[g][0:64, :, :], in0=p3, in1=ident.unsqueeze(1).to_broadcast([64, 8, 64]), op=ALU.add),
                 reads=[psP, nmask], writes=[Gm[g]])
            if kk_ < 5:
                p.op("act", lambda e: e.activation(out=Pm[g][0:64, :, :].rearrange("p h c -> p (h c)"), in_=psP[0:64, :], func=AF.Copy), reads=[psP], writes=[Pm[g]])
                copy_evac(PT[g][0:64, :, :].rearrange("p h c -> p (h c)"), psPT[0:64, :], [psPT], [PT[g]])
            for hl in range(8):
                p.op("pe", lambda e, hl=hl: e.matmul(psQ[0:64, hl * 64:(hl + 1) * 64], lhsT=Gm[g][:, hl, :], rhs=Qm[g][:, hl, :],
                                                    start=True, stop=True), reads=[Gm[g], Qm[g]], writes=[psQ])
            copy_evac(Qm[g][0:64, :, :].rearrange("p h c -> p (h c)"), psQ[0:64, :], [psQ], [Qm[g]])

        def stage3_all():
            bq = lambda g: psQ if g % 2 == 0 else psAT[0]
            bu = lambda g: psPT if g % 2 == 0 else psAT[1]
            for g in range(4):
                pq = bq(g)
                for hl in range(8):
                    h = g * 8 + hl
                    p.op("pe", lambda e, h=h, hl=hl, pq=pq: e.matmul(pq[0:64, hl * 64:(hl + 1) * 64], lhsT=fmk(h, 0, 1), rhs=sv(h),
                                                                    start=True, stop=False), reads=[fmm, (Sbf.name, h // 8)], writes=[pq])
                    p.op("pe", lambda e, h=h, hl=hl, pq=pq: e.matmul(pq[0:64, hl * 64:(hl + 1) * 64], lhsT=AT[:, h, 0:64],
                                                                    rhs=vz[:, h * 64:(h + 1) * 64], start=False, stop=True),
                         reads=[(AT.name, g), vz], writes=[pq])
                if g % 2 == 1:
                    for gg in (g - 1, g):
                        copy_evac(nrhs[gg][0:64, :, :].rearrange("p h c -> p (h c)"), bq(gg)[0:64, :], [bq(gg)], [nrhs[gg]], scale=-1.0)
                    for gg in (g - 1, g):
                        pu = bu(gg)
                        for hl in range(8):
                            p.op("pe", lambda e, hl=hl, gg=gg, pu=pu: e.matmul(pu[0:64, hl * 64:(hl + 1) * 64], lhsT=Qm[gg][:, hl, :],
                                                                              rhs=nrhs[gg][:, hl, :], start=True, stop=True),
                                 reads=[Qm[gg], nrhs[gg]], writes=[pu])
                    for gg in (g - 1, g):
                        copy_evac(uv[0:64, gg * 512:(gg + 1) * 512], bu(gg)[0:64, :], [bu(gg)], [(uv.name, ("U", gg))])

        def stage5_all():
            by = lambda g: psY if g % 2 == 0 else psP
            for g in range(4):
                py = by(g)
                for hl in range(8):
                    h = g * 8 + hl
                    p.op("pe", lambda e, h=h, hl=hl, py=py: e.matmul(py[0:64, hl * 64:(hl + 1) * 64], lhsT=fmk(h, 1, 2), rhs=sv(h),
                                                                    start=True, stop=False), reads=[fmm, (Sbf.name, h // 8)], writes=[py])
                    p.op("pe", lambda e, h=h, hl=hl, py=py: e.matmul(py[0:64, hl * 64:(hl + 1) * 64], lhsT=AT[:, h, 64:128],
                                                                    rhs=uv[:, h * 64:(h + 1) * 64], start=False, stop=True),
                         reads=[(AT.name, g), (uv.name, "V"), (uv.name, ("U", g))], writes=[py])
                if g % 2 == 1:
                    for gg in (g - 1, g):
                        copy_evac(ysb[:, gg * 512:(gg + 1) * 512], by(gg)[0:64, :], [by(gg)], [(ysb.name, gg)])

        def stage6(g):
            ps4 = psY[:].rearrange("p (a b c) -> p a b c", a=4, b=2)
            for hpl in range(4):
                hp = g * 4 + hpl
                h0, h1 = 2 * hp, 2 * hp + 1
                p.op("pe", lambda e, hpl=hpl, h0=h0: e.matmul(ps4[0:64, hpl, 0, :], lhsT=kb[:, h0 * 64:(h0 + 1) * 64], rhs=uv[:, h0 * 64:(h0 + 1) * 64],
                                                             start=True, stop=True),
                     reads=[kb, (uv.name, "V"), (uv.name, ("U", g))], writes=[psY])
                p.op("pe", lambda e, hpl=hpl, h0=h0, h1=h1: e.matmul(ps4[:, hpl, 1, :], lhsT=kb[:, h0 * 64:(h1 + 1) * 64], rhs=uv[:, h1 * 64:(h1 + 1) * 64],
                                                                    start=True, stop=True),
                     reads=[kb, (uv.name, "V"), (uv.name, ("U", g))], writes=[psY])
            for hh in range(2):
                rows = slice(hh * 64, (hh + 1) * 64)
                p.op("dve", lambda e, rows=rows: e.tensor_tensor(
                    out=Stmp[rows, :, :], in0=S32[rows, g * 4:(g + 1) * 4, :],
                    in1=k.wtot[d][rows, g * 4:(g + 1) * 4, c:c + 1].to_broadcast([64, 4, 64]), op=ALU.mult),
                    reads=[(S32.name, g), k.wtot[d]], writes=[(Stmp.name, hh)])
            for hh in range(2):
                rows = slice(hh * 64, (hh + 1) * 64)
                p.op("dve", lambda e, rows=rows, hh=hh: e.tensor_tensor(
                    out=S32[rows, g * 4:(g + 1) * 4, :], in0=Stmp[rows, :, :], in1=ps4[rows, :, hh, :], op=ALU.add),
                    reads=[(Stmp.name, hh), psY], writes=[(S32.name, g)])
            p.op("act", lambda e: e.activation(out=Sbf[:].rearrange("p a b -> p (a b)")[:, g * 256:(g + 1) * 256],
                                               in_=S32[:].rearrange("p a b -> p (a b)")[:, g * 256:(g + 1) * 256], func=AF.Copy),
                 reads=[(S32.name, g)], writes=[(Sbf.name, g)])

        cc_ = getattr(k, "ccut", None) or 99
        for g in range(4):
            stage1(g)
        if cc_ <= 1:
            return
        for kk_ in range(1, 6):
            for g in range(4):
                stage2(g, kk_)
        if cc_ <= 2:
            return
        stage3_all()
        if cc_ <= 3:
            return
        if emit:
            stage5_all()
            p.dma("sp", k.Yscr[d][ytok0:ytok0 + 64, :], ysb[:], reads=[ysb], writes=[])
        if cc_ <= 5:
            return
        for g in range(4):
            stage6(g)

    ncap = getattr(k, "nchunks", None)
    for d in range(2):
        if d == 0:
            seq = [(c, False) for c in range(4)] + [(c, True) for c in range(4, 36)]
        else:
            seq = [(c, False) for c in range(3, -1, -1)] + [(c, False) for c in range(67, 35, -1)] + [(c, True) for c in range(35, 3, -1)]
        if ncap:
            seq = seq[:ncap]
        p.op("dve", lambda e: e.memset(S32[:], 0.0), writes=[S32])
        p.op("pool", lambda e: e.memset(Sbf[:], 0.0), writes=[Sbf])
        load_chunk(d, seq[0][0], cnt["ld"] % 2)
        for i, (c, emit) in enumerate(seq):
            buf = cnt["ld"] % 2
            cnt["ld"] += 1
            if i + 1 < len(seq):
                load_chunk(d, seq[i + 1][0], cnt["ld"] % 2)
            do_chunk(d, c, buf, emit, (c - 4) * 64)
        if "Sdbg" in k.dbg:
            p.dma("sp", k.Sdbg[d], S32[:].rearrange("p a b -> p (a b)"), reads=[S32], writes=[])
    p.barrier()


import numpy as np

GT = 256
NTS = 2
NR = 4
GN_EPS = 64e-5
NORM_EPS = 1e-6
D = 2048


def phaseD(k):
    p, nc = k.p, k.nc
    din, dscr = k.din, k.dscr
    k.xown_d = din("xown", [2048, 2048])
    k.rows_d = din("rows", [3, 2048])
    k.g2n_d = din("g2n", [128, 16])
    k.g2w_d = din("g2w", [256, 2048])
    k.poolw_d = din("poolw", [4, 256, 256])
    k.pools_d = din("pools", [128, 8])
    k.wpo_d = din("wpo", [1024, 2048])
    k.wro_d = din("wro", [2048, 2048])
    k.wout_d = din("wout", [2048, 2048])
    k.poolc_d = din("poolc", [1, 4 * 64 + 2])
    k.rowbuf = dscr("rowbuf", [4, 2048], F32)
    k.x1scr = dscr("x1scr", [2048, 2048], F32)
    k.h2T = dscr("h2Tscr", [2048, 2048], BF16)
    k.h2tok = dscr("h2tok", [2048, 2048], BF16)

    k.rowt = {}
    mD = p.mark()
    for i, nm in enumerate(("lnxw", "lnxb")):
        k.rowt[nm] = p.sb("row_" + nm, [128, 2048], F32)
        p.dma("sp", k.rowt[nm][:], k.rows_d[i:i + 1, :].partition_broadcast(128))
    g2n = p.sb("g2n_sb", [128, 16], F32)
    A2 = p.sb("A2_fm", [128, 16], F32)
    p.dma("sp", g2n[:], k.g2n_d)
    p.op("dve", lambda e: e.tensor_scalar(out=A2[:], in0=k.mod[:, 64:80, 0], scalar1=1.0, scalar2=None, op0=ALU.add), reads=[k.mod], writes=[A2])
    p.op("dve", lambda e: e.tensor_tensor(out=A2[:], in0=A2[:], in1=g2n[:], op=ALU.mult), reads=[A2, g2n], writes=[A2])
    rb = k.rowbuf.rearrange("r (oc q) -> r q oc", q=128)
    o1 = p.dma("sp", rb[0], k.mod[:, 32:48, 0], reads=[k.mod], writes=["rowbuf"], allow_slow_non_contiguous=True)
    o2 = p.dma("sp", rb[1], A2[:], reads=[A2], writes=["rowbuf"], allow_slow_non_contiguous=True)
    o3 = p.dma("sp", rb[2], k.mod[:, 48:64, 0], reads=[k.mod], writes=["rowbuf"], allow_slow_non_contiguous=True)
    o4 = p.dma("sp", rb[3], k.mod[:, 80:96, 0], reads=[k.mod], writes=["rowbuf"], allow_slow_non_contiguous=True)
    for i, nm in enumerate(("gt1", "A2", "sh2")):
        k.rowt[nm] = p.sb("row_" + nm, [128, 2048], F32)
        p.dma("sp", k.rowt[nm][:], k.rowbuf[i:i + 1, :].partition_broadcast(128), reads=["rowbuf"], writes=[k.rowt[nm]])

    g2w = p.sb("g2w_sb", [128, 2, 2048], BF16)
    poolw = p.sb("poolw_sb", [128, 4, 2, 256], BF16)
    pools = p.sb("pools_sb", [128, 8], F32)
    poolc = p.sb("poolc_sb", [128, 258], F32)
    gneps = p.sb("gneps", [128, 1], F32)
    p.dma("pool", g2w[:], k.g2w_d.rearrange("(kc q) n -> q kc n", q=128))
    p.dma("pool", poolw[:], k.poolw_d.rearrange("g (kc q) d -> q g kc d", q=128))
    p.dma("sp", pools[:], k.pools_d)
    p.dma("sp", poolc[:], k.poolc_d.partition_broadcast(128))
    p.op("pool", lambda e: e.memset(gneps[:], GN_EPS), writes=[gneps])
    ident = k.ident

    ya = p.sb("ya", [128, 2048], F32)
    yb = p.sb("yb", [128, 2048], F32)
    sq = p.sb("ysq", [128, 2048], F32)
    vt = p.sb("vt", [128, 2048], BF16)
    st1 = p.sb("st1", [128, 32], F32)
    st2 = p.sb("st2", [128, 32], F32)
    st3 = p.sb("st3", [128, 32], F32)
    bc = p.sb("bc", [128, 32], F32)
    sgdt = p.sb("sgdt", [128, 2, 128], BF16)
    prebf = p.sb("prebf", [128, 2048], BF16)
    preT = p.sb("preT", [128, 16, GT], BF16)
    Upad = p.sb("Upad", [128, NR, 96], F32)
    Sa = p.sb("Sa", [128, NR, 96], F32)
    Sb_ = p.sb("Sb_", [128, NR, 96], F32)
    dtmp = p.sb("dtmp", [128, NR, 64], F32)
    ubf = p.sb("ubf", [128, GT], BF16)
    dT = p.sb("dT", [128, 8, GT], BF16)
    zT = p.sb("zT", [128, 8, GT], BF16)
    sgt = [p.sb("sgt%d" % i, [128, GT], BF16) for i in range(2)]
    mrg = p.sb("mrg", [128, GT], F32)
    mT = p.sb("mT", [128, 16, GT], BF16)
    wblk = [p.sb("wblkD%d" % i, [128, 16, 512], BF16) for i in range(2)]
    x1g = p.sb("x1g", [128, NTS, 2048], BF16)
    x1 = p.sb("x1t", [128, 2048], F32)
    h2 = p.sb("h2t", [128, 2048], BF16)
    h2Ts = p.sb("h2Ts", [128, 16, 128], BF16)
    ss = p.sb("ssD", [128, 2], F32)
    psb = k.psb
    pbt = k.pbt
    cnt = {"w": 0}
    p.op("dve", lambda e: e.memset(Upad[:], 0.0), writes=[Upad])

    def wload(src_ap, nk):
        w = wblk[cnt["w"] % 2]
        cnt["w"] += 1
        for h in range(0, nk, 8):
            p.dma("pool", w[:, h:h + 8, :], src_ap[:, h:h + 8, :], reads=[], writes=[(w.name, h // 8)])
        return w

    wpo_v = k.wpo_d.rearrange("(kc q) n -> q kc n", q=128)
    wro_v = k.wro_d.rearrange("(kc q) n -> q kc n", q=128)
    wout_v = k.wout_d.rearrange("(kc q) n -> q kc n", q=128)

    def readout_tile(tg, ts):
        ti = tg * NTS + ts
        t0 = ti * 128
        p.dma("sp", ya[:], k.Yscr[0][t0:t0 + 128, :], reads=[], writes=[ya])
        p.dma("sp", yb[:], k.Yscr[1][t0:t0 + 128, :], reads=[], writes=[yb])
        p.dma("sp", vt[:], k.Vtok[256 + t0:256 + t0 + 128, :], reads=[], writes=[vt])
        p.dma("sp", bc[:], k.bcoef[t0:t0 + 128, :], reads=[], writes=[bc])
        p.dma("sp", sgdt[:], k.sgdT.rearrange("(kc q) t -> q kc t", q=128)[:, :, t0:t0 + 128], reads=[], writes=[sgdt])
        y3 = ya[:].rearrange("p (h c) -> p h c", c=64)
        p.op("dve", lambda e: e.tensor_tensor(out=ya[:], in0=ya[:], in1=yb[:], op=ALU.add), reads=[ya, yb], writes=[ya])
        p.op("dve", lambda e: e.tensor_reduce(out=st1[:], in_=y3, axis=AX.X, op=ALU.add), reads=[ya], writes=[st1])
        p.op("act", lambda e: e.activation(out=sq[:], in_=ya[:], func=AF.Square), reads=[ya], writes=[sq])
        p.op("dve", lambda e: e.tensor_reduce(out=st2[:], in_=sq[:].rearrange("p (h c) -> p h c", c=64), axis=AX.X, op=ALU.add),
             reads=[sq], writes=[st2])
        p.op("dve", lambda e: e.tensor_scalar(out=st1[:], in0=st1[:], scalar1=1.0 / 64, scalar2=None, op0=ALU.mult), reads=[st1], writes=[st1])
        p.op("dve", lambda e: e.tensor_tensor(out=st3[:], in0=st1[:], in1=st1[:], op=ALU.mult), reads=[st1], writes=[st3])
        p.op("dve", lambda e: e.scalar_tensor_tensor(out=st2[:], in0=st2[:], scalar=1.0 / 64, in1=st3[:], op0=ALU.mult, op1=ALU.subtract),
             reads=[st2, st3], writes=[st2])
        p.op("act", lambda e: e.activation(out=st2[:], in_=st2[:], func=AF.Sqrt, bias=gneps[:, 0:1]), reads=[st2, gneps], writes=[st2])
        p.op("dve", lambda e: e.reciprocal(out=st2[:], in_=st2[:]), reads=[st2], writes=[st2])
        p.op("dve", lambda e: e.tensor_tensor(out=y3, in0=y3, in1=st1[:].unsqueeze(2).to_broadcast([128, 32, 64]), op=ALU.subtract),
             reads=[ya, st1], writes=[ya])
        p.op("dve", lambda e: e.tensor_tensor(out=y3, in0=y3, in1=st2[:].unsqueeze(2).to_broadcast([128, 32, 64]), op=ALU.mult),
             reads=[ya, st2], writes=[ya])
        p.op("dve", lambda e: e.tensor_tensor(out=ya[:], in0=ya[:], in1=k.rowt["lnxw"][:], op=ALU.mult), reads=[ya, k.rowt["lnxw"]], writes=[ya])
        p.op("dve", lambda e: e.tensor_tensor(out=ya[:], in0=ya[:], in1=k.rowt["lnxb"][:], op=ALU.add), reads=[ya, k.rowt["lnxb"]], writes=[ya])
        p.op("dve", lambda e: e.tensor_tensor(out=yb[:].rearrange("p (h c) -> p h c", c=64), in0=vt[:].rearrange("p (h c) -> p h c", c=64),
                                              in1=bc[:].unsqueeze(2).to_broadcast([128, 32, 64]), op=ALU.mult), reads=[vt, bc], writes=[yb])
        p.op("dve", lambda e: e.tensor_tensor(out=ya[:], in0=ya[:], in1=yb[:], op=ALU.add), reads=[ya, yb], writes=[ya])
        for cb in range(4):
            ps = psb[cb % 2]
            for kc in range(2):
                p.op("pe", lambda e, cb=cb, kc=kc, ps=ps: e.matmul(ps[:, :], lhsT=sgdt[:, kc, :], rhs=g2w[:, kc, cb * 512:(cb + 1) * 512],
                                                                  start=(kc == 0), stop=(kc == 1)), reads=[sgdt, g2w], writes=[ps])
            p.op("dve", lambda e, cb=cb, ps=ps: e.tensor_tensor(out=prebf[:, cb * 512:(cb + 1) * 512], in0=ya[:, cb * 512:(cb + 1) * 512],
                                                                 in1=ps[:, :], op=ALU.mult), reads=[ya, ps], writes=[(prebf.name, cb)])
        for half in range(2):
            pt = pbt[half]
            for j in range(8):
                kc = half * 8 + j
                p.op("pe", lambda e, kc=kc, j=j, pt=pt: e.transpose(pt[:, j * 128:(j + 1) * 128], prebf[:, kc * 128:(kc + 1) * 128], ident[:]),
                     reads=[prebf, ident], writes=[pt])
            if half:
                p.op("act", lambda e, half=half, pt=pt: e.activation(out=preT[:, half * 8:(half + 1) * 8, ts * 128:(ts + 1) * 128],
                                                                    in_=pt[:].rearrange("p (a b) -> p a b", b=128), func=AF.Copy),
                     reads=[pt], writes=[(preT.name, ts)])
            else:
                p.op("dve", lambda e, half=half, pt=pt: e.tensor_copy(out=preT[:, half * 8:(half + 1) * 8, ts * 128:(ts + 1) * 128],
                                                                     in_=pt[:].rearrange("p (a b) -> p a b", b=128)),
                     reads=[pt], writes=[(preT.name, ts)])

    def pool_group(tg):
        t0 = tg * GT
        for cc in range(8):
            gi = cc // 2
            p.dma("sp", ubf[:], k.uT[cc * 128:(cc + 1) * 128, t0:t0 + GT], reads=[], writes=[ubf])
            p.op("act", lambda e: e.activation(out=Upad[:, :, 16:80], in_=ubf[:].rearrange("p (r c) -> p r c", c=64), func=AF.Copy),
                 reads=[ubf], writes=[Upad])
            src, lo, hi = Upad, 2, 94
            p.op("dve", lambda e: e.tensor_tensor(out=Sa[:, :, 2:94], in0=Upad[:, :, 1:93], in1=Upad[:, :, 2:94], op=ALU.add),
                 reads=[Upad], writes=[Sa])
            cur, oth = Sa, Sb_
            sh = 1
            lo, hi = 2, 94
            for step in range(gi):
                nlo, nhi = lo + sh, hi - sh
                p.op("dve", lambda e, cur=cur, oth=oth, nlo=nlo, nhi=nhi, sh=sh: e.tensor_tensor(
                    out=oth[:, :, nlo:nhi], in0=cur[:, :, nlo - sh:nhi - sh], in1=cur[:, :, nlo + sh:nhi + sh], op=ALU.add),
                    reads=[cur], writes=[oth])
                cur, oth = oth, cur
                lo, hi = nlo, nhi
                sh *= 2
            p.op("dve", lambda e, cur=cur: e.tensor_scalar(out=dtmp[:], in0=cur[:, :, 16:80], scalar1=poolc[:, 256:257], scalar2=None, op0=ALU.mult),
                 reads=[cur, poolc], writes=[dtmp])
            p.op("dve", lambda e, cur=cur: e.scalar_tensor_tensor(out=dtmp[:], in0=cur[:, :, 17:81], scalar=poolc[:, 257:258], in1=dtmp[:],
                                                                  op0=ALU.mult, op1=ALU.add), reads=[cur, poolc, dtmp], writes=[dtmp])
            p.op("dve", lambda e, gi=gi: e.tensor_tensor(out=dtmp[:], in0=dtmp[:],
                                                        in1=poolc[:, gi * 64:(gi + 1) * 64].unsqueeze(1).to_broadcast([128, NR, 64]), op=ALU.mult),
                 reads=[dtmp, poolc], writes=[dtmp])
            p.op("dve", lambda e, cc=cc: e.tensor_tensor(out=dT[:, cc, :].rearrange("p (r c) -> p r c", c=64), in0=dtmp[:], in1=Upad[:, :, 16:80],
                                                        op=ALU.subtract), reads=[dtmp, Upad], writes=[(dT.name, cc)])
        for gi in range(4):
            for dc in range(2):
                ps = psb[(gi * 2 + dc) % 2]
                for kc in range(2):
                    p.op("pe", lambda e, gi=gi, dc=dc, kc=kc, ps=ps: e.matmul(ps[:, 0:GT], lhsT=poolw[:, gi, kc, dc * 128:(dc + 1) * 128],
                                                                            rhs=dT[:, gi * 2 + kc, :], start=(kc == 0), stop=(kc == 1)),
                         reads=[poolw, (dT.name, gi * 2 + kc)], writes=[ps])
                oc = gi * 2 + dc
                p.op("act", lambda e, oc=oc, ps=ps: e.activation(out=zT[:, oc, :], in_=ps[:, 0:GT], func=AF.Copy, scale=pools[:, oc:oc + 1]),
                     reads=[ps, pools], writes=[(zT.name, oc)])

    def merge_group(tg):
        t0 = tg * GT
        for blk in range(4):
            wp = wload(wpo_v[:, :, blk * 512:(blk + 1) * 512], 8)
            wr = wload(wro_v[:, :, blk * 512:(blk + 1) * 512], 16)
            for j in range(4):
                fc = blk * 4 + j
                p.dma("sp", sgt[0][:], k.sgT[fc * 128:(fc + 1) * 128, t0:t0 + GT], reads=[], writes=[sgt[0]])
                p.dma("sp", sgt[1][:], k.sgT[2048 + fc * 128:2048 + (fc + 1) * 128, t0:t0 + GT], reads=[], writes=[sgt[1]])
                ps = psb[2]
                for kc in range(8):
                    p.op("pe", lambda e, kc=kc, j=j, wp=wp, ps=ps: e.matmul(ps[:, 0:GT], lhsT=wp[:, kc, j * 128:(j + 1) * 128], rhs=zT[:, kc, :],
                                                                          start=(kc == 0), stop=(kc == 7)), reads=[(wp.name, 0), zT], writes=[ps])
                p.op("dve", lambda e, ps=ps: e.tensor_tensor(out=mrg[:], in0=sgt[0][:], in1=ps[:, 0:GT], op=ALU.mult), reads=[sgt[0], ps], writes=[mrg])
                ps2 = psb[3]
                for kc in range(16):
                    p.op("pe", lambda e, kc=kc, j=j, wr=wr, ps2=ps2: e.matmul(ps2[:, 0:GT], lhsT=wr[:, kc, j * 128:(j + 1) * 128], rhs=preT[:, kc, :],
                                                                            start=(kc == 0), stop=(kc == 15)),
                         reads=[(wr.name, kc // 8), preT], writes=[ps2])
                p.op("dve", lambda e, ps2=ps2: e.tensor_tensor(out=sgt[1][:], in0=sgt[1][:], in1=ps2[:, 0:GT], op=ALU.mult), reads=[sgt[1], ps2], writes=[sgt[1]])
                p.op("dve", lambda e, fc=fc: e.tensor_tensor(out=mT[:, fc, :], in0=mrg[:], in1=sgt[1][:], op=ALU.add), reads=[mrg, sgt[1]],
                     writes=[(mT.name, fc)])

    def outproj_group(tg):
        for cb in range(4):
            w = wload(wout_v[:, :, cb * 512:(cb + 1) * 512], 16)
            cs = slice(cb * 512, (cb + 1) * 512)
            for ts in range(NTS):
                ps = psb[4 + ts % 2]
                for kc in range(16):
                    p.op("pe", lambda e, kc=kc, w=w, ps=ps, ts=ts: e.matmul(ps[:, :], lhsT=mT[:, kc, ts * 128:(ts + 1) * 128], rhs=w[:, kc, :],
                                                                          start=(kc == 0), stop=(kc == 15)), reads=[(w.name, kc // 8), mT], writes=[ps])
                p.op("dve", lambda e, cs=cs, ps=ps, ts=ts: e.tensor_tensor(out=x1g[:, ts, cs], in0=ps[:, :], in1=k.rowt["gt1"][:, cs], op=ALU.mult),
                     reads=[ps, k.rowt["gt1"]], writes=[(x1g.name, ts)])
        for ts in range(NTS):
            outproj_tile(tg, ts)

    def outproj_tile(tg, ts):
            ti = tg * NTS + ts
            t0 = ti * 128
            p.dma("sp", yb[:], k.xown_d[t0:t0 + 128, :], reads=[], writes=[yb])
            p.op("dve", lambda e: e.tensor_tensor(out=x1[:], in0=x1g[:, ts, :], in1=yb[:], op=ALU.add), reads=[(x1g.name, ts), yb], writes=[x1])
            p.dma("sp", k.x1scr[t0:t0 + 128, :], x1[:], reads=[x1], writes=[])
            p.op("act", lambda e: e.activation(out=sq[:], in_=x1[:], func=AF.Square), reads=[x1], writes=[sq])
            p.op("dve", lambda e: e.tensor_reduce(out=ss[:, 0:1], in_=sq[:], axis=AX.X, op=ALU.add), reads=[sq], writes=[ss])
            p.op("act", lambda e: e.activation(out=ss[:, 1:2], in_=ss[:, 0:1], func=AF.Sqrt, scale=1.0 / D, bias=k.eps_t[:, 0:1]),
                 reads=[ss, k.eps_t], writes=[ss])
            p.op("dve", lambda e: e.reciprocal(out=ss[:, 1:2], in_=ss[:, 1:2]), reads=[ss], writes=[ss])
            p.op("dve", lambda e: e.scalar_tensor_tensor(out=sq[:], in0=x1[:], scalar=ss[:, 1:2], in1=k.rowt["A2"][:], op0=ALU.mult, op1=ALU.mult),
                 reads=[x1, ss, k.rowt["A2"]], writes=[sq])
            p.op("dve", lambda e: e.tensor_tensor(out=h2[:], in0=sq[:], in1=k.rowt["sh2"][:], op=ALU.add), reads=[sq, k.rowt["sh2"]], writes=[h2])
            p.dma("sp", k.h2tok[t0:t0 + 128, :], h2[:], reads=[h2], writes=[])
            for half in range(2):
                pt = pbt[half]
                for j in range(8):
                    kc = half * 8 + j
                    p.op("pe", lambda e, kc=kc, j=j, pt=pt: e.transpose(pt[:, j * 128:(j + 1) * 128], h2[:, kc * 128:(kc + 1) * 128], ident[:]),
                         reads=[h2, ident], writes=[pt])
                if half:
                    p.op("act", lambda e, pt=pt: e.activation(out=h2Ts[:, 8:16, :].rearrange("p a b -> p (a b)"), in_=pt[:], func=AF.Copy),
                         reads=[pt], writes=[(h2Ts.name, 1)])
                else:
                    p.op("dve", lambda e, pt=pt: e.tensor_copy(out=h2Ts[:, 0:8, :].rearrange("p a b -> p (a b)"), in_=pt[:]),
                         reads=[pt], writes=[(h2Ts.name, 0)])
            p.dma("sp", k.h2T.rearrange("(kc q) t -> q kc t", q=128)[:, :, t0:t0 + 128], h2Ts[:], reads=[h2Ts], writes=[])

    ng = getattr(k, "ngroupsD", None) or (2048 // GT)
    for tg in range(ng):
        for ts in range(NTS):
            readout_tile(tg, ts)
        pool_group(tg)
        merge_group(tg)
        outproj_group(tg)
    p.barrier()
    p.release(mD)


import numpy as np

CAP = 768
SUBS = ((0, 512), (512, 256))
NSLOT = 64 * CAP
D = 2048


def phaseE(k):
    p, nc = k.p, k.nc
    din, dscr = k.din, k.dscr
    k.routw_d = din("routw", [2048, 64])
    k.routb_d = din("routb", [1, 128])
    k.tri_d = din("tri", [128, 128])
    k.exg_d = din("exg", [64, 2048, 512])
    k.exu_d = din("exu", [64, 2048, 512])
    k.exd_d = din("exd", [64, 512, 2048])
    k.shg_d = din("shg", [2048, 512])
    k.shu_d = din("shu", [2048, 512])
    k.shd_d = din("shd", [512, 2048])
    k.out_d = nc.dram_tensor("out", [2048, 2048], F32, kind="ExternalOutput").ap()
    k.Xg = dscr("Xg", [NSLOT, 2048], BF16)
    k.Yg = dscr("Yg", [NSLOT, 2048], BF16)
    k.shscr = dscr("shscr", [2048, 2048], F32)
    k.routdbg = dscr("routdbg", [2048, 16], F32)
    psb, pbt = k.psb, k.pbt
    ident = k.ident

    slots_all = p.sb("slots_all", [128, 128], I32)
    wk_all = p.sb("wk_all", [128, 16, 8], F32)
    mE = p.mark()

    routw = p.sb("routw_sb", [128, 16, 64], BF16)
    routb = p.sb("routb_sb", [128, 128], F32)
    tri = p.sb("tri_sb", [128, 128], BF16)
    base = p.sb("base_cnt", [128, 64], F32)
    shg = p.sb("shg_sb", [128, 16, 512], BF16)
    shu = p.sb("shu_sb", [128, 16, 512], BF16)
    shd = p.sb("shd_sb", [128, 4, 2048], BF16)
    p.dma("pool", routw[:], k.routw_d.rearrange("(kc q) n -> q kc n", q=128))
    p.dma("sp", routb[:], k.routb_d.partition_broadcast(128))
    p.dma("pool", tri[:], k.tri_d)
    for h in range(2):
        p.dma("pool", shg[:, h * 8:(h + 1) * 8, :], k.shg_d.rearrange("(kc q) n -> q kc n", q=128)[:, h * 8:(h + 1) * 8, :], writes=[(shg.name, h)])
        p.dma("pool", shu[:, h * 8:(h + 1) * 8, :], k.shu_d.rearrange("(kc q) n -> q kc n", q=128)[:, h * 8:(h + 1) * 8, :], writes=[(shu.name, h)])
    p.dma("pool", shd[:], k.shd_d.rearrange("(kc q) n -> q kc n", q=128))
    p.op("dve", lambda e: e.memset(base[:], 0.0), writes=[base])
    h2Tg = p.sb("h2Tg", [128, 16, 512], BF16)
    h2r = p.sb("h2r", [128, 2048], BF16)
    sg = p.sb("sgE", [128, 512], F32)
    actT = p.sb("actT", [128, 4, 512], BF16)
    sho = p.sb("sho", [128, 2048], F32)
    R = {}
    for nm, w in (("sc", 64), ("sel", 64), ("tmp", 64), ("msel", 64), ("emask", 64), ("wd", 64), ("key", 64), ("oh", 64), ("rk", 64),
                  ("m1", 8), ("m2", 8), ("gs", 8), ("g8", 8), ("gm", 8), ("pen", 8), ("e8", 8), ("k8", 8), ("den", 2)):
        R[nm] = p.sb("R_" + nm, [128, w], F32)
    emb = p.sb("emask_bf", [128, 64], BF16)
    h2Tv = k.h2T.rearrange("(kc q) t -> q kc t", q=128)

    def shared_group(tg):
        t0 = tg * 512
        for h in range(2):
            p.dma("sp", h2Tg[:, h * 8:(h + 1) * 8, :], h2Tv[:, h * 8:(h + 1) * 8, t0:t0 + 512], reads=[], writes=[(h2Tg.name, h)])
        for dc in range(4):
            for kc in range(16):
                p.op("pe", lambda e, dc=dc, kc=kc: e.matmul(psb[0][:, :], lhsT=shg[:, kc, dc * 128:(dc + 1) * 128], rhs=h2Tg[:, kc, :],
                                                           start=(kc == 0), stop=(kc == 15)), reads=[shg, h2Tg], writes=[psb[0]])
            for kc in range(16):
                p.op("pe", lambda e, dc=dc, kc=kc: e.matmul(psb[1][:, :], lhsT=shu[:, kc, dc * 128:(dc + 1) * 128], rhs=h2Tg[:, kc, :],
                                                           start=(kc == 0), stop=(kc == 15)), reads=[shu, h2Tg], writes=[psb[1]])
            p.op("act", lambda e: e.activation(out=sg[:], in_=psb[0][:, :], func=AF.Silu), reads=[psb[0]], writes=[sg])
            p.op("dve", lambda e, dc=dc: e.tensor_tensor(out=actT[:, dc, :], in0=sg[:], in1=psb[1][:, :], op=ALU.mult), reads=[sg, psb[1]],
                 writes=[(actT.name, dc)])
        for ts in range(4):
            for cb in range(4):
                ps = psb[2 + cb % 2]
                for dc in range(4):
                    p.op("pe", lambda e, ts=ts, cb=cb, dc=dc, ps=ps: e.matmul(ps[:, :], lhsT=actT[:, dc, ts * 128:(ts + 1) * 128],
                                                                            rhs=shd[:, dc, cb * 512:(cb + 1) * 512], start=(dc == 0), stop=(dc == 3)),
                         reads=[actT, shd], writes=[ps])
                p.op("act" if cb % 2 else "dve",
                     (lambda e, cb=cb, ps=ps: e.activation(out=sho[:, cb * 512:(cb + 1) * 512], in_=ps[:, :], func=AF.Copy)) if cb % 2 else
                     (lambda e, cb=cb, ps=ps: e.tensor_copy(out=sho[:, cb * 512:(cb + 1) * 512], in_=ps[:, :])),
                     reads=[ps], writes=[(sho.name, cb)])
            p.dma("sp", k.shscr[t0 + ts * 128:t0 + (ts + 1) * 128, :], sho[:], reads=[sho], writes=[])
            route_tile(tg * 4 + ts, ts)

    def route_tile(ti, ts):
        t0 = ti * 128
        ps = psb[4]
        for kc in range(16):
            p.op("pe", lambda e, kc=kc: e.matmul(ps[:, 0:64], lhsT=h2Tg[:, kc, ts * 128:(ts + 1) * 128], rhs=routw[:, kc, :],
                                                start=(kc == 0), stop=(kc == 15)), reads=[h2Tg, routw], writes=[ps])
        sc, sel, tmp, msel, emask, wd, key, oh, rk = (R[n] for n in ("sc", "sel", "tmp", "msel", "emask", "wd", "key", "oh", "rk"))
        m1, m2, gs, g8, gm, pen, e8, k8, den = (R[n] for n in ("m1", "m2", "gs", "g8", "gm", "pen", "e8", "k8", "den"))
        v3 = lambda t: t[:].rearrange("p (g e) -> p g e", e=8)
        b3 = lambda t: t[:].unsqueeze(2).to_broadcast([128, 8, 8])
        p.op("act", lambda e: e.activation(out=sc[:], in_=ps[:, 0:64], func=AF.Sigmoid), reads=[ps], writes=[sc])
        p.op("dve", lambda e: e.tensor_tensor(out=sel[:], in0=sc[:], in1=routb[:, 0:64], op=ALU.add), reads=[sc, routb], writes=[sel])
        p.op("dve", lambda e: e.tensor_reduce(out=m1[:], in_=v3(sel), axis=AX.X, op=ALU.max), reads=[sel], writes=[m1])
        p.op("dve", lambda e: e.tensor_tensor(out=v3(tmp), in0=v3(sel), in1=b3(m1), op=ALU.is_equal), reads=[sel, m1], writes=[tmp])
        p.op("dve", lambda e: e.scalar_tensor_tensor(out=tmp[:], in0=tmp[:], scalar=-1e9, in1=sel[:], op0=ALU.mult, op1=ALU.add),
             reads=[tmp, sel], writes=[tmp])
        p.op("dve", lambda e: e.tensor_reduce(out=m2[:], in_=v3(tmp), axis=AX.X, op=ALU.max), reads=[tmp], writes=[m2])
        p.op("dve", lambda e: e.tensor_tensor(out=gs[:], in0=m1[:], in1=m2[:], op=ALU.add), reads=[m1, m2], writes=[gs])
        p.op("dve", lambda e: e.max(out=g8[:], in_=gs[:]), reads=[gs], writes=[g8])
        p.op("dve", lambda e: e.tensor_scalar(out=gm[:], in0=gs[:], scalar1=g8[:, 3:4], scalar2=None, op0=ALU.is_ge), reads=[gs, g8], writes=[gm])
        p.op("dve", lambda e: e.tensor_scalar(out=pen[:], in0=gm[:], scalar1=1e9, scalar2=-1e9, op0=ALU.mult, op1=ALU.add), reads=[gm], writes=[pen])
        p.op("dve", lambda e: e.tensor_tensor(out=v3(msel), in0=v3(sel), in1=b3(gm), op=ALU.mult), reads=[sel, gm], writes=[msel])
        p.op("dve", lambda e: e.tensor_tensor(out=v3(msel), in0=v3(msel), in1=b3(pen), op=ALU.add), reads=[msel, pen], writes=[msel])
        p.op("dve", lambda e: e.max(out=e8[:], in_=msel[:]), reads=[msel], writes=[e8])
        p.op("dve", lambda e: e.tensor_scalar(out=emask[:], in0=msel[:], scalar1=e8[:, 7:8], scalar2=None, op0=ALU.is_ge), reads=[msel, e8], writes=[emask])
        p.op("dve", lambda e: e.tensor_tensor(out=wd[:], in0=sc[:], in1=emask[:], op=ALU.mult), reads=[sc, emask], writes=[wd])
        p.op("dve", lambda e: e.tensor_reduce(out=den[:, 0:1], in_=wd[:], axis=AX.X, op=ALU.add), reads=[wd], writes=[den])
        p.op("dve", lambda e: e.reciprocal(out=den[:, 1:2], in_=den[:, 0:1]), reads=[den], writes=[den])
        p.op("dve", lambda e: e.tensor_scalar(out=wd[:], in0=wd[:], scalar1=den[:, 1:2], scalar2=2.5, op0=ALU.mult, op1=ALU.mult), reads=[wd, den], writes=[wd])
        p.op("act", lambda e: e.activation(out=emb[:], in_=emask[:], func=AF.Copy), reads=[emask], writes=[emb])
        ps2 = psb[5]
        p.op("pe", lambda e: e.matmul(ps2[:, 0:64], lhsT=tri[:], rhs=emb[:], start=True, stop=True), reads=[tri, emb], writes=[ps2])
        p.op("dve", lambda e: e.tensor_tensor(out=rk[:], in0=ps2[:, 0:64], in1=base[:], op=ALU.add), reads=[ps2, base], writes=[rk])
        p.op("dve", lambda e: e.tensor_scalar(out=oh[:], in0=rk[:], scalar1=float(CAP) - 0.5, scalar2=None, op0=ALU.is_lt), reads=[rk], writes=[oh])
        p.op("dve", lambda e: e.tensor_tensor(out=oh[:], in0=oh[:], in1=emask[:], op=ALU.mult), reads=[oh, emask], writes=[oh])
        p.op("dve", lambda e: e.tensor_tensor(out=key[:], in0=rk[:], in1=routb[:, 64:128], op=ALU.add), reads=[rk, routb], writes=[key])
        p.op("dve", lambda e: e.scalar_tensor_tensor(out=key[:], in0=key[:], scalar=1.0, in1=oh[:], op0=ALU.add, op1=ALU.mult), reads=[key, oh], writes=[key])
        p.op("dve", lambda e: e.tensor_scalar(out=key[:], in0=key[:], scalar1=-1.0, scalar2=None, op0=ALU.add), reads=[key], writes=[key])
        p.op("dve", lambda e: e.max(out=k8[:], in_=key[:]), reads=[key], writes=[k8])
        p.op("dve", lambda e: e.tensor_copy(out=slots_all[:, ti * 8:(ti + 1) * 8], in_=k8[:]), reads=[k8], writes=[(slots_all.name, ti)])
        for j in range(8):
            p.op("dve", lambda e, j=j: e.tensor_scalar(out=oh[:], in0=key[:], scalar1=k8[:, j:j + 1], scalar2=None, op0=ALU.is_equal),
                 reads=[key, k8], writes=[oh])
            p.op("dve", lambda e: e.tensor_tensor(out=oh[:], in0=oh[:], in1=wd[:], op=ALU.mult), reads=[oh, wd], writes=[oh])
            p.op("dve", lambda e, j=j: e.tensor_reduce(out=wk_all[:, ti, j:j + 1], in_=oh[:], axis=AX.X, op=ALU.add), reads=[oh],
                 writes=[(wk_all.name, ti)])
        p.op("pe", lambda e: e.matmul(ps2[:, 64:128], lhsT=k.ones_bf[:], rhs=emb[:], start=True, stop=True), reads=[k.ones_bf, emb], writes=[ps2])
        p.op("dve", lambda e: e.tensor_tensor(out=base[:], in0=base[:], in1=ps2[:, 64:128], op=ALU.add), reads=[base, ps2], writes=[base])
        p.dma("sp", h2r[:], k.h2tok[t0:t0 + 128, :], reads=[], writes=[h2r])
        for j in range(8):
            p.dma_fn("pool", lambda e, j=j: e.indirect_dma_start(
                out=k.Xg, out_offset=bass.IndirectOffsetOnAxis(ap=slots_all[:, ti * 8 + j:ti * 8 + j + 1], axis=0), in_=h2r[:], in_offset=None,
                bounds_check=p.getreg(e, NSLOT - 1), oob_is_err=False), reads=[h2r, (slots_all.name, ti)], writes=[])
        if "routdbg" in k.dbg:
            p.dma("sp", k.routdbg[t0:t0 + 128, 0:8], k8[:], reads=[k8], writes=[])
            p.dma("sp", k.routdbg[t0:t0 + 128, 8:16], wk_all[:, ti, :], reads=[(wk_all.name, ti)], writes=[])

    ngE = getattr(k, "ngroupsE", None) or 4
    for tg in range(ngE):
        shared_group(tg)
    p.barrier()
    p.release(mE)
    if getattr(k, "ecut", None) == 1:
        return

    NEXP = getattr(k, "nexp", None) or 64
    Wg = [p.sb("Wg%d" % i, [128, 16, 512], BF16) for i in range(2)]
    Wu = [p.sb("Wu%d" % i, [128, 16, 512], BF16) for i in range(2)]
    Wd = [p.sb("Wd%d" % i, [128, 4, 2048], BF16) for i in range(2)]
    Xs = p.sb("Xs", [128, 4, 2048], BF16)
    XT = p.sb("XT", [128, 16, 512], BF16)
    sg2 = p.sb("sg2", [128, 512], F32)
    act2 = p.sb("act2", [128, 4, 512], BF16)
    yst = [p.sb("yst%d" % i, [128, 2048], BF16) for i in range(2)]
    exg_v = k.exg_d.rearrange("e (kc q) n -> e q kc n", q=128)
    exu_v = k.exu_d.rearrange("e (kc q) n -> e q kc n", q=128)
    exd_v = k.exd_d.rearrange("e (kc q) n -> e q kc n", q=128)
    cnt = {"y": 0}

    def load_w(e_):
        b = e_ % 2
        for h in range(2):
            p.dma("pool", Wg[b][:, h * 8:(h + 1) * 8, :], exg_v[e_][:, h * 8:(h + 1) * 8, :], reads=[], writes=[(Wg[b].name, h)])
            p.dma("pool", Wu[b][:, h * 8:(h + 1) * 8, :], exu_v[e_][:, h * 8:(h + 1) * 8, :], reads=[], writes=[(Wu[b].name, h)])
        for h in range(2):
            p.dma("pool", Wd[b][:, h * 2:(h + 1) * 2, :], exd_v[e_][:, h * 2:(h + 1) * 2, :], reads=[], writes=[(Wd[b].name, h)])

    def expert(e_):
        for off, n in SUBS:
            expert_sub(e_, off, n)

    def expert_sub(e_, off, n):
        b = e_ % 2
        nst = n // 128
        r0 = e_ * CAP + off
        p.dma("sp", Xs[:, 0:nst, :], k.Xg[r0:r0 + n, :].rearrange("(st q) f -> q st f", q=128), reads=[], writes=[Xs])
        for st in range(nst):
            for half in range(2):
                pt = pbt[half]
                for j in range(8):
                    kc = half * 8 + j
                    p.op("pe", lambda e, st=st, kc=kc, j=j, pt=pt: e.transpose(pt[:, j * 128:(j + 1) * 128], Xs[:, st, kc * 128:(kc + 1) * 128], ident[:]),
                         reads=[Xs, ident], writes=[pt])
                if half:
                    p.op("act", lambda e, st=st, pt=pt: e.activation(out=XT[:, 8:16, st * 128:(st + 1) * 128],
                                                                    in_=pt[:].rearrange("p (a b) -> p a b", b=128), func=AF.Copy),
                         reads=[pt], writes=[(XT.name, st)])
                else:
                    p.op("dve", lambda e, st=st, pt=pt: e.tensor_copy(out=XT[:, 0:8, st * 128:(st + 1) * 128],
                                                                     in_=pt[:].rearrange("p (a b) -> p a b", b=128)),
                         reads=[pt], writes=[(XT.name, st)])
        for dc in range(4):
            for kc in range(16):
                p.op("pe", lambda e, dc=dc, kc=kc: e.matmul(psb[0][:, 0:n], lhsT=Wg[b][:, kc, dc * 128:(dc + 1) * 128], rhs=XT[:, kc, 0:n],
                                                           start=(kc == 0), stop=(kc == 15)), reads=[(Wg[b].name, kc // 8), XT], writes=[psb[0]])
            for kc in range(16):
                p.op("pe", lambda e, dc=dc, kc=kc: e.matmul(psb[1][:, 0:n], lhsT=Wu[b][:, kc, dc * 128:(dc + 1) * 128], rhs=XT[:, kc, 0:n],
                                                           start=(kc == 0), stop=(kc == 15)), reads=[(Wu[b].name, kc // 8), XT], writes=[psb[1]])
            p.op("act", lambda e: e.activation(out=sg2[:, 0:n], in_=psb[0][:, 0:n], func=AF.Silu), reads=[psb[0]], writes=[sg2])
            p.op("dve", lambda e, dc=dc: e.tensor_tensor(out=act2[:, dc, 0:n], in0=sg2[:, 0:n], in1=psb[1][:, 0:n], op=ALU.mult), reads=[sg2, psb[1]],
                 writes=[(act2.name, dc)])
        for st in range(nst):
            ys = yst[cnt["y"] % 2]
            cnt["y"] += 1
            for cb in range(4):
                ps = psb[2 + cb]
                for dc in range(4):
                    p.op("pe", lambda e, st=st, cb=cb, dc=dc, ps=ps: e.matmul(ps[:, :], lhsT=act2[:, dc, st * 128:(st + 1) * 128],
                                                                            rhs=Wd[b][:, dc, cb * 512:(cb + 1) * 512], start=(dc == 0), stop=(dc == 3)),
                         reads=[act2, (Wd[b].name, dc // 2)], writes=[ps])
                if cb % 2:
                    p.op("act", lambda e, cb=cb, ps=ps, ys=ys: e.activation(out=ys[:, cb * 512:(cb + 1) * 512], in_=ps[:, :], func=AF.Copy),
                         reads=[ps], writes=[(ys.name, cb)])
                else:
                    p.op("dve", lambda e, cb=cb, ps=ps, ys=ys: e.tensor_copy(out=ys[:, cb * 512:(cb + 1) * 512], in_=ps[:, :]),
                         reads=[ps], writes=[(ys.name, cb)])
            p.dma("sp", k.Yg[r0 + st * 128:r0 + (st + 1) * 128, :], ys[:], reads=[ys], writes=[])

    load_w(0)
    for e_ in range(NEXP):
        if e_ + 1 < NEXP:
            load_w(e_ + 1)
        expert(e_)
    p.barrier()
    p.release(mE)
    if getattr(k, "ecut", None) == 2:
        return

    rows = {}
    for i, nm in ((3, "gt2"),):
        rows[nm] = p.sb("rowE_" + nm, [128, 2048], F32)
        p.dma("sp", rows[nm][:], k.rowbuf[i:i + 1, :].partition_broadcast(128), reads=[], writes=[rows[nm]])
    rows["fing"] = p.sb("rowE_fing", [128, 2048], F32)
    p.dma("sp", rows["fing"][:], k.rows_d[2:3, :].partition_broadcast(128), reads=[], writes=[rows["fing"]])
    acc = p.sb("accE", [128, 2048], F32)
    yk = [p.sb("yk%d" % i, [128, 2048], BF16) for i in range(2)]
    x1t = p.sb("x1E", [128, 2048], F32)
    sqE = p.sb("sqE", [128, 2048], F32)
    ssE = p.sb("ssE", [128, 2], F32)

    def combine_tile(ti):
        t0 = ti * 128
        p.dma("sp", acc[:], k.shscr[t0:t0 + 128, :], reads=[], writes=[acc])
        p.dma("sp", x1t[:], k.x1scr[t0:t0 + 128, :], reads=[], writes=[x1t])
        for j in range(8):
            y = yk[j % 2]
            p.op("pool", lambda e, y=y: e.memset(y[:], 0.0), writes=[y])
            p.dma_fn("pool", lambda e, j=j, y=y: e.indirect_dma_start(
                out=y[:], out_offset=None, in_=k.Yg, in_offset=bass.IndirectOffsetOnAxis(ap=slots_all[:, ti * 8 + j:ti * 8 + j + 1], axis=0),
                bounds_check=p.getreg(e, NSLOT - 1), oob_is_err=False), reads=[y, slots_all], writes=[y])
            p.op("dve", lambda e, j=j, y=y: e.scalar_tensor_tensor(out=acc[:], in0=y[:], scalar=wk_all[:, ti, j:j + 1], in1=acc[:],
                                                                    op0=ALU.mult, op1=ALU.add), reads=[y, wk_all, acc], writes=[acc])
        p.op("dve", lambda e: e.tensor_tensor(out=acc[:], in0=acc[:], in1=rows["gt2"][:], op=ALU.mult), reads=[acc, rows["gt2"]], writes=[acc])
        p.op("dve", lambda e: e.tensor_tensor(out=x1t[:], in0=x1t[:], in1=acc[:], op=ALU.add), reads=[x1t, acc], writes=[x1t])
        p.op("act", lambda e: e.activation(out=sqE[:], in_=x1t[:], func=AF.Square), reads=[x1t], writes=[sqE])
        p.op("dve", lambda e: e.tensor_reduce(out=ssE[:, 0:1], in_=sqE[:], axis=AX.X, op=ALU.add), reads=[sqE], writes=[ssE])
        p.op("act", lambda e: e.activation(out=ssE[:, 1:2], in_=ssE[:, 0:1], func=AF.Sqrt, scale=1.0 / D, bias=k.eps_t[:, 0:1]),
             reads=[ssE, k.eps_t], writes=[ssE])
        p.op("dve", lambda e: e.reciprocal(out=ssE[:, 1:2], in_=ssE[:, 1:2]), reads=[ssE], writes=[ssE])
        p.op("dve", lambda e: e.scalar_tensor_tensor(out=sqE[:], in0=x1t[:], scalar=ssE[:, 1:2], in1=rows["fing"][:], op0=ALU.mult, op1=ALU.mult),
             reads=[x1t, ssE, rows["fing"]], writes=[sqE])
        p.dma("sp", k.out_d[t0:t0 + 128, :], sqE[:], reads=[sqE], writes=[], is_out=True)

    for ti in range(4 * ngE):
        combine_tile(ti)
    p.barrier()


import numpy as np

D = 2048


def fm(v, kc=16):
    return np.ascontiguousarray(v.reshape(kc, 128).T)


def prep_shared(inp):
    out = {}
    w_in = inp["w_in"][0]
    for half in (0, 1):
        u = w_in[:, 0:1024]
        slab = w_in[:, 1024:1024 + 6784]
        gates = w_in[:, 1024 + 6784:]
        r, kk_, v = slab[:, 0:2048], slab[:, 2048:4096], slab[:, 4096:6144]
        wd = [slab[:, 6144:6240], slab[:, 6240:6336]]
        ad = [slab[:, 6336:6432], slab[:, 6432:6528]]
        gd = slab[:, 6528:6784]
        dA, dB = (0, 1) if half == 0 else (1, 0)
        z32 = np.zeros((D, 32), np.float32)
        win = np.concatenate([u, r, kk_, v, wd[dA], z32, wd[dB], z32, ad[dA], z32, ad[dB], z32, gd, gates], axis=1)
        out[half] = {"win": np.ascontiguousarray(win)}
    return out


def prep_core(inp, shared, b, half):
    x = inp["x"][b]
    ctx = inp["ctx"][b]
    if half == 1:
        x = x[::-1]
        ctx = ctx[::-1]
    m = {}
    m["xT"] = np.ascontiguousarray(x.T)
    m["cxT"] = np.ascontiguousarray(ctx.T)
    c2 = np.stack([inp["c"][b], inp["c_ctx"]], axis=1)
    m["cT2"] = np.ascontiguousarray(c2.reshape(16, 128, 2).transpose(1, 0, 2))
    m["adaw"] = inp["ada_w"][0]
    m["adab"] = fm(inp["ada_b"][0], 96)
    m["g1"] = fm(inp["norm1_g"][0])
    m["win"] = shared[half]["win"]
    return m


def prep_core_b(inp, b, half, m):
    dA, dB = (0, 1) if half == 0 else (1, 0)
    mu = inp["shift_mu"][0]
    z32 = np.zeros(32, np.float32)
    secs = [mu[6144:6240], mu[6240:6336]]
    seca = [mu[6336:6432], mu[6432:6528]]
    mup = np.concatenate([mu[0:6144], secs[dA], z32, secs[dB], z32, seca[dA], z32, seca[dB], z32, mu[6528:6784]])
    cls = np.arange(6912) % 4
    valid = np.ones(6912, bool)
    for s0 in (6144, 6272, 6400, 6528):
        valid[s0 + 96:s0 + 128] = False
    def sel(mask):
        return np.where(mask & valid, mup, np.float32(0)).astype(np.float32)
    if half == 0:
        cm1, cp1, cm64, cp64 = sel(cls == 0), sel(cls == 1), sel(cls == 2), sel(cls == 3)
        ccm1, ccp1 = sel(cls % 2 == 0), sel(cls % 2 == 1)
    else:
        cm1, cp1, cm64, cp64 = sel(cls == 1), sel(cls == 0), sel(cls == 3), sel(cls == 2)
        ccm1, ccp1 = sel(cls % 2 == 1), sel(cls % 2 == 0)
    mupv = np.where(valid, mup, np.float32(0)).astype(np.float32)
    arrs = [mupv, cm1, cp1, cm64, cp64, ccm1, ccp1]
    m["mixc"] = np.ascontiguousarray(np.stack([a.reshape(54, 128).T for a in arrs], axis=1))
    w0, a0 = inp["decay_w0"][0], inp["iclr_a0"][0]
    vs = [w0[dA], w0[dB], a0[dA], a0[dB], inp["k_k"][0], inp["k_a"][0], inp["r_k"][0].reshape(-1)]
    m["vecs"] = np.ascontiguousarray(np.stack([fm(v) for v in vs], axis=1))
    w2, a2 = inp["decay_w2"][0], inp["iclr_a2"][0]
    m["lw2"] = np.ascontiguousarray(np.stack([w2[dA], w2[dB], a2[dA], a2[dB]], axis=1))
    pidx = np.arange(128)
    m["bones"] = (pidx[:, None] // 64 == pidx[None, :] // 64).astype(np.float32)
    m["hsel"] = (pidx[:, None] // 64 == np.arange(2)[None, :]).astype(np.float32)
    return m


def prep_core_c(m):
    t = np.arange(64)
    lt = (t[:, None] < t[None, :]).astype(np.float32)
    le = (t[:, None] <= t[None, :]).astype(np.float32)
    gt = (t[:, None] > t[None, :]).astype(np.float32)
    ge = (t[:, None] >= t[None, :]).astype(np.float32)
    mA = np.block([[lt, le], [lt, le]])
    mB = np.block([[gt, ge], [gt, ge]])
    m["cmask"] = np.ascontiguousarray(np.stack([mA, mB], axis=1).astype(np.float32))
    nA = lt.T.copy()
    nB = gt.T.copy()
    m["nmask"] = np.ascontiguousarray(np.stack([nA, nB, np.eye(64, dtype=np.float32)], axis=1).astype(np.float32))
    return m


def prep_core_de(inp, b, half, m, with_experts=True):
    x = inp["x"][b]
    if half == 1:
        x = x[::-1]
    m["xown"] = np.ascontiguousarray(x[:2048])
    m["rows"] = np.ascontiguousarray(np.stack([inp["lnx_w"][0], inp["lnx_b"][0], inp["final_g"]], axis=0))
    m["g2n"] = fm(inp["norm2_g"][0])
    m["g2w"] = inp["gate_g2"][0]
    m["poolw"] = inp["pool_w"][0]
    m["pools"] = fm(inp["pool_scale"][0], 8)
    m["wpo"] = inp["w_pool_out"][0]
    m["wro"] = inp["w_rwkv_out"][0]
    m["wout"] = inp["w_out"][0]
    t = np.arange(64)
    cnts = []
    for w in (2, 4, 8, 16):
        lo = np.clip(t - w // 2, 0, 64)
        hi = np.clip(t + (w - w // 2), 0, 64)
        c = (hi - lo).astype(np.float32)
        if half == 1:
            c = c[::-1]
        cnts.append(np.float32(1.0) / c)
    flags = np.array([1.0, 0.0] if half == 0 else [0.0, 1.0], np.float32)
    m["poolc"] = np.ascontiguousarray(np.concatenate(cnts + [flags]).astype(np.float32)[None, :])
    m["routw"] = inp["router_w"][0]
    ecap = (np.arange(64) * 768).astype(np.float32)
    m["routb"] = np.ascontiguousarray(np.concatenate([inp["router_bias"][0], ecap]).astype(np.float32)[None, :])
    tt = np.arange(128)
    m["tri"] = (tt[:, None] < tt[None, :]).astype(np.float32)
    if with_experts:
        m["exg"] = inp["exp_w_gate"][0]
        m["exu"] = inp["exp_w_up"][0]
        m["exd"] = inp["exp_w_down"][0]
    m["shg"] = inp["shared_w_gate"][0]
    m["shu"] = inp["shared_w_up"][0]
    m["shd"] = inp["shared_w_down"][0]
    return m


from concourse.bass_utils import run_bass_kernel_spmd


def kernel(**inputs):
    inp = {k_: np.asarray(v) for k_, v in inputs.items()}
    shared = prep_shared(inp)
    maps = []
    for core in range(8):
        b, half = core // 2, core % 2
        m = prep_core(inp, shared, b, half)
        m = prep_core_b(inp, b, half, m)
        m = prep_core_c(m)
        m = prep_core_de(inp, b, half, m)
        maps.append(m)
    nc, k = build(stage=5)
    res = run_bass_kernel_spmd(nc, maps, core_ids=list(range(8)))
    out = np.empty((4, 4096, 2048), np.float32)
    for core in range(8):
        b, half = core // 2, core % 2
        o = np.asarray(res.results[core]["out"])
        if half == 0:
            out[b, :2048] = o
        else:
            out[b, 2048:] = o[::-1]
    return out
```
